# Optimizing a Trainium2 kernel written in Bass

```python
import math
import jax, jax.numpy as jnp
from jax import lax
import numpy as np

D_MODEL = 1024
BATCH = 8
SEQ = 4096
DEPTH = 1

N_ATTN_HEADS = 4
ATTN_HEAD_DIM = 64
ATTN_V_DIM = 2 * ATTN_HEAD_DIM
ATTN_WIDTH = N_ATTN_HEADS * ATTN_V_DIM
ROPE_THETA = 500000.0
ROPE_DIM = ATTN_HEAD_DIM // 4
Q_BLOCK = 128
SSM_WIDTH = D_MODEL // 2
SSM_GROUP = 16
SSM_GROUPS = SSM_WIDTH // SSM_GROUP
SSM_STATE = 64
DT_MIN = 1e-3
DT_MAX = 1e-1
N_BRANCH = 2
Q_COLS = N_ATTN_HEADS * 2 * ATTN_HEAD_DIM
K_COLS = N_ATTN_HEADS * 2 * ATTN_HEAD_DIM
V_COLS = ATTN_WIDTH
U_COLS = SSM_WIDTH
G_COLS = N_BRANCH * D_MODEL
IN_COLS = Q_COLS + K_COLS + V_COLS + U_COLS + G_COLS
N_EXPERT_GROUPS = 4
EXPERTS_PER_GROUP = 8
TOP_K = 2
D_EXPERT = D_MODEL // 4
EPS = 1e-6

kernel_name = "hybrid_diffattn_s5_hiermoe"


def rms_norm(x, g):
    xf = x.astype(jnp.float32)
    y = xf * lax.rsqrt(jnp.mean(xf * xf, axis=-1, keepdims=True) + EPS)
    return (y * g.astype(jnp.float32)).astype(x.dtype)


def rope_partial(t, positions):
    half = ROPE_DIM // 2
    inv = ROPE_THETA ** (-jnp.arange(0, ROPE_DIM, 2, dtype=jnp.float32) / ROPE_DIM)
    ang = positions.astype(jnp.float32)[..., None] * inv
    cos = jnp.cos(ang)[:, :, None, None, :]
    sin = jnp.sin(ang)[:, :, None, None, :]
    tf = t.astype(jnp.float32)
    r1, r2, rest = tf[..., :half], tf[..., half:ROPE_DIM], tf[..., ROPE_DIM:]
    out = jnp.concatenate([r1 * cos - r2 * sin, r2 * cos + r1 * sin, rest], axis=-1)
    return out.astype(t.dtype)


def diff_attention(q, k, v, lam):
    b, h, _, s, dh = q.shape
    dv = v.shape[-1]
    nb = s // Q_BLOCK
    qb = q.reshape(b, h, 2, nb, Q_BLOCK, dh).transpose(3, 0, 1, 2, 4, 5)
    kpos = jnp.arange(s)
    scale = dh ** -0.5
    neg = jnp.finfo(jnp.float32).min

    def block(args):
        qi, i = args
        sc = jnp.einsum('bhcqd,bhckd->bhcqk', qi, k).astype(jnp.float32) * scale
        qpos = i * Q_BLOCK + jnp.arange(Q_BLOCK)
        mask = kpos[None, :] <= qpos[:, None]
        p = jax.nn.softmax(jnp.where(mask, sc, neg), axis=-1)
        w = p[:, :, 0] - lam * p[:, :, 1]
        return jnp.einsum('bhqk,bhkd->bhqd', w.astype(v.dtype), v)

    out = lax.map(block, (qb, jnp.arange(nb)))
    return out.transpose(1, 2, 0, 3, 4).reshape(b, h, s, dv)


def s5_grouped(u, lam_re, lam_im, log_dt, b_re, b_im, c_re, c_im, d_skip):
    bsz, s, _ = u.shape
    uf = u.astype(jnp.float32).reshape(bsz, s, SSM_GROUPS, SSM_GROUP)
    lam = lax.complex(lam_re.astype(jnp.float32), lam_im.astype(jnp.float32))
    dt = jnp.exp(log_dt.astype(jnp.float32))[:, None]
    lam_bar = jnp.exp(lam * dt)
    bmat = lax.complex(b_re.astype(jnp.float32), b_im.astype(jnp.float32))
    b_bar = ((lam_bar - 1.0) / lam)[..., None] * bmat
    bu = jnp.einsum('gph,bsgh->bsgp', b_bar, uf.astype(jnp.complex64))
    a = jnp.broadcast_to(lam_bar, (1, s) + lam_bar.shape)

    def combine(e1, e2):
        a1, b1 = e1
        a2, b2 = e2
        return a1 * a2, a2 * b1 + b2

    _, states = lax.associative_scan(combine, (a, bu), axis=1)
    cmat = lax.complex(c_re.astype(jnp.float32), c_im.astype(jnp.float32))
    y = jnp.einsum('ghp,bsgp->bsgh', cmat, states).real
    y = y + d_skip.astype(jnp.float32).reshape(SSM_GROUPS, SSM_GROUP) * uf
    return y.reshape(bsz, s, SSM_WIDTH).astype(u.dtype)


def hier_moe(h, w_rg, b_rg, w_re, b_re, w_gate, w_up, w_down):
    bsz, s, d = h.shape
    t = h.reshape(-1, d)
    g_prob = jax.nn.softmax((t @ w_rg).astype(jnp.float32) + b_rg.astype(jnp.float32), axis=-1)
    grp = jnp.argmax(g_prob, axis=-1)
    p_grp = jnp.max(g_prob, axis=-1)
    grp_oh = jax.nn.one_hot(grp, N_EXPERT_GROUPS, dtype=jnp.float32)
    e_logits = jnp.einsum('td,dge->tge', t, w_re).astype(jnp.float32) + b_re.astype(jnp.float32)
    e_sel = jnp.einsum('tge,tg->te', e_logits, grp_oh)
    top_v, top_i = lax.top_k(e_sel, TOP_K)
    top_w = jax.nn.softmax(top_v, axis=-1) * p_grp[:, None]
    expert_w = jnp.sum(jax.nn.one_hot(top_i, EXPERTS_PER_GROUP, dtype=jnp.float32) * top_w[..., None], axis=1)
    comb = (grp_oh[:, :, None] * expert_w[:, None, :]).astype(h.dtype)
    out = jnp.zeros((t.shape[0], d), jnp.float32)
    for gi in range(N_EXPERT_GROUPS):
        hg = jax.nn.silu(jnp.einsum('td,edf->tef', t, w_gate[gi])) * jnp.einsum('td,edf->tef', t, w_up[gi])
        hg = hg * comb[:, gi, :, None]
        out = out + jnp.einsum('tef,efd->td', hg, w_down[gi]).astype(jnp.float32)
    return out.reshape(bsz, s, d).astype(h.dtype)


def setup_inputs(seed: int = 0) -> dict:
    key = jax.random.key(seed)
    ks = jax.random.split(key, 32)
    f32 = jnp.float32
    L = DEPTH
    nrm = lambda k, shp, sc: jax.random.normal(k, shp, f32) * sc
    x = jax.random.normal(ks[0], (BATCH, SEQ, D_MODEL), f32)
    offset = jax.random.randint(ks[1], (BATCH, 1), 0, 1024, dtype=jnp.int32)
    positions = (offset + jnp.arange(SEQ, dtype=jnp.int32)[None, :]).astype(jnp.int32)
    return {
        "x": x,
        "positions": positions,
        "norm_mix_g": 1.0 + nrm(ks[2], (L, D_MODEL), 0.02),
        "w_in": nrm(ks[3], (L, D_MODEL, IN_COLS), D_MODEL ** -0.5),
        "q_norm_g": 1.0 + nrm(ks[4], (L, ATTN_HEAD_DIM), 0.02),
        "k_norm_g": 1.0 + nrm(ks[5], (L, ATTN_HEAD_DIM), 0.02),
        "lambda_q1": nrm(ks[6], (L, ATTN_HEAD_DIM), 0.1),
        "lambda_k1": nrm(ks[7], (L, ATTN_HEAD_DIM), 0.1),
        "lambda_q2": nrm(ks[8], (L, ATTN_HEAD_DIM), 0.1),
        "lambda_k2": nrm(ks[9], (L, ATTN_HEAD_DIM), 0.1),
        "subln_g": 1.0 + nrm(ks[10], (L, ATTN_V_DIM), 0.02),
        "w_o_attn": nrm(ks[11], (L, ATTN_WIDTH, D_MODEL), ATTN_WIDTH ** -0.5),
        "ssm_lambda_re": -0.5 + nrm(ks[12], (L, SSM_GROUPS, SSM_STATE), 0.01),
        "ssm_lambda_im": math.pi * jnp.arange(SSM_STATE, dtype=f32)[None, None, :] + nrm(ks[13], (L, SSM_GROUPS, SSM_STATE), 0.01),
        "ssm_log_dt": jax.random.uniform(ks[14], (L, SSM_GROUPS), f32, math.log(DT_MIN), math.log(DT_MAX)),
        "ssm_b_re": nrm(ks[15], (L, SSM_GROUPS, SSM_STATE, SSM_GROUP), (2 * SSM_GROUP) ** -0.5),
        "ssm_b_im": nrm(ks[16], (L, SSM_GROUPS, SSM_STATE, SSM_GROUP), (2 * SSM_GROUP) ** -0.5),
        "ssm_c_re": nrm(ks[17], (L, SSM_GROUPS, SSM_GROUP, SSM_STATE), SSM_STATE ** -0.5),
        "ssm_c_im": nrm(ks[18], (L, SSM_GROUPS, SSM_GROUP, SSM_STATE), SSM_STATE ** -0.5),
        "ssm_d": nrm(ks[19], (L, SSM_WIDTH), 1.0),
        "w_glu": nrm(ks[20], (L, SSM_WIDTH, 2 * D_MODEL), SSM_WIDTH ** -0.5),
        "w_out": nrm(ks[21], (L, D_MODEL, D_MODEL), D_MODEL ** -0.5),
        "norm_ffn_g": 1.0 + nrm(ks[22], (L, D_MODEL), 0.02),
        "w_router_group": nrm(ks[23], (L, D_MODEL, N_EXPERT_GROUPS), D_MODEL ** -0.5),
        "b_router_group": nrm(ks[24], (L, N_EXPERT_GROUPS), 0.01),
        "w_router_expert": nrm(ks[25], (L, D_MODEL, N_EXPERT_GROUPS, EXPERTS_PER_GROUP), D_MODEL ** -0.5),
        "b_router_expert": nrm(ks[26], (L, N_EXPERT_GROUPS, EXPERTS_PER_GROUP), 0.01),
        "w_expert_gate": nrm(ks[27], (L, N_EXPERT_GROUPS, EXPERTS_PER_GROUP, D_MODEL, D_EXPERT), D_MODEL ** -0.5),
        "w_expert_up": nrm(ks[28], (L, N_EXPERT_GROUPS, EXPERTS_PER_GROUP, D_MODEL, D_EXPERT), D_MODEL ** -0.5),
        "w_expert_down": nrm(ks[29], (L, N_EXPERT_GROUPS, EXPERTS_PER_GROUP, D_EXPERT, D_MODEL), D_EXPERT ** -0.5),
    }


def reference(x, positions, norm_mix_g, w_in, q_norm_g, k_norm_g, lambda_q1, lambda_k1,
              lambda_q2, lambda_k2, subln_g, w_o_attn, ssm_lambda_re, ssm_lambda_im,
              ssm_log_dt, ssm_b_re, ssm_b_im, ssm_c_re, ssm_c_im, ssm_d, w_glu, w_out,
              norm_ffn_g, w_router_group, b_router_group, w_router_expert, b_router_expert,
              w_expert_gate, w_expert_up, w_expert_down):
    bsz, s, _ = x.shape
    splits = [Q_COLS, Q_COLS + K_COLS, Q_COLS + K_COLS + V_COLS, Q_COLS + K_COLS + V_COLS + U_COLS]
    for l in range(DEPTH):
        lam_init = 0.8 - 0.6 * math.exp(-0.3 * l)
        h = rms_norm(x, norm_mix_g[l])
        proj = h @ w_in[l]
        q, k, v, u, gates = jnp.split(proj, splits, axis=-1)

        q = q.reshape(bsz, s, N_ATTN_HEADS, 2, ATTN_HEAD_DIM)
        k = k.reshape(bsz, s, N_ATTN_HEADS, 2, ATTN_HEAD_DIM)
        q = rope_partial(rms_norm(q, q_norm_g[l]), positions).transpose(0, 2, 3, 1, 4)
        k = rope_partial(rms_norm(k, k_norm_g[l]), positions).transpose(0, 2, 3, 1, 4)
        v = v.reshape(bsz, s, N_ATTN_HEADS, ATTN_V_DIM).transpose(0, 2, 1, 3)
        lam = (jnp.exp(jnp.sum(lambda_q1[l].astype(jnp.float32) * lambda_k1[l].astype(jnp.float32)))
               - jnp.exp(jnp.sum(lambda_q2[l].astype(jnp.float32) * lambda_k2[l].astype(jnp.float32)))
               + lam_init)
        o = diff_attention(q, k, v, lam)
        o = rms_norm(o, subln_g[l]) * (1.0 - lam_init)
        o_a = o.transpose(0, 2, 1, 3).reshape(bsz, s, ATTN_WIDTH) @ w_o_attn[l]

        y = s5_grouped(u, ssm_lambda_re[l], ssm_lambda_im[l], ssm_log_dt[l], ssm_b_re[l],
                       ssm_b_im[l], ssm_c_re[l], ssm_c_im[l], ssm_d[l])
        z = jax.nn.gelu(y) @ w_glu[l]
        o_s = z[..., :D_MODEL] * jax.nn.sigmoid(z[..., D_MODEL:])

        g = jax.nn.sigmoid(gates.astype(jnp.float32)).reshape(bsz, s, N_BRANCH, D_MODEL)
        merged = g[..., 0, :] * o_a.astype(jnp.float32) + g[..., 1, :] * o_s.astype(jnp.float32)
        x = x + merged.astype(x.dtype) @ w_out[l]

        x = x + hier_moe(rms_norm(x, norm_ffn_g[l]), w_router_group[l], b_router_group[l],
                         w_router_expert[l], b_router_expert[l], w_expert_gate[l],
                         w_expert_up[l], w_expert_down[l])
    return x
```

```python
import contextlib
import math
import os
import numpy as np
import concourse.bass as bass
import concourse.mybir as mybir
from concourse.bass_utils import run_bass_kernel_spmd

F32 = mybir.dt.float32
BF16 = mybir.dt.bfloat16
I32 = mybir.dt.int32
AF = mybir.ActivationFunctionType
ALU = mybir.AluOpType
AX = mybir.AxisListType

SEM_LIMIT = 30000
S = 4096
D = 1024
NT = 32
EPS = 1e-6
SB_BASE = 17408
LAM_INIT = 0.8 - 0.6 * math.exp(-0.3 * 0)


class Buf:
    __slots__ = ("name", "w", "r", "pr")

    def __init__(self, name=""):
        self.name = name
        self.w = {}
        self.r = {}
        self.pr = {}


class Eng:
    def __init__(self, name):
        self.name = name
        self.ops = []
        self.cnt = 0
        self.semidx = 0
        self.waited = {}
        self.pending = []
        self.last = None

    @property
    def semkey(self):
        return "%s_%d" % (self.name, self.semidx)


class FW:
    def __init__(self, nc, n_dma_sems=32):
        self.nc = nc
        self.stack = contextlib.ExitStack()
        self.engs = {n: Eng(n) for n in ("pe", "act", "dve", "pool", "sp")}
        self.sems = {}
        names = ["dma%d" % i for i in range(n_dma_sems)]
        self.dma_sem_val = {n: 0 for n in names}
        self.dma_rr = {"sp": 0, "pool": 0}
        k = n_dma_sems // 2
        self.dma_pool_of = {"sp": names[:k], "pool": names[k:]}
        self.out_tokens = []

    def sem(self, key):
        if key not in self.sems:
            self.sems[key] = self.stack.enter_context(self.nc.semaphore(key))
        return self.sems[key]

    def psum(self, name, shape, dt):
        return self.stack.enter_context(self.nc.psum_tensor(name, list(shape), dt))

    def _wait(self, eng, tok):
        key, val = tok[0], tok[1]
        assert val is not None, "wait on unsignalled token"
        if eng.waited.get(key, 0) >= val:
            return
        eng.waited[key] = val
        self.sem(key)
        eng.ops.append(("wait", key, val))

    def _deps(self, eng, reads, writes, add):
        toks = []
        for b in reads:
            toks.extend(b.w.values())
        for b in writes:
            if add:
                toks.extend(b.pr.values())
            else:
                toks.extend(b.w.values())
            toks.extend(b.r.values())
        for t in toks:
            if eng.name == "pe" and t[0].startswith("pe_"):
                continue
            self._wait(eng, t)

    def _update(self, key, tok, reads, writes, add):
        for b in reads:
            b.r[key] = tok
        for b in writes:
            if add:
                for k_, v_ in b.r.items():
                    b.pr["r:" + k_] = v_
                b.w[key] = tok
            else:
                npr = {}
                for k_, v_ in b.w.items():
                    npr["w:" + k_] = v_
                for k_, v_ in b.r.items():
                    npr["r:" + k_] = v_
                b.pr = npr
                b.w = {key: tok}
            b.r = {}

    def op(self, engname, fn, reads=(), writes=(), sig=True, add=False):
        eng = self.engs[engname]
        self._deps(eng, reads, writes, add)
        if sig:
            if eng.cnt >= SEM_LIMIT:
                eng.semidx += 1
                eng.cnt = 0
            eng.cnt += 1
            tok = [eng.semkey, eng.cnt]
            self.sem(tok[0])
            for p in eng.pending:
                p[0], p[1] = tok[0], tok[1]
            eng.pending = []
            eng.last = tok
            key = tok[0]
        else:
            tok = ["pe_pending", None]
            eng.pending.append(tok)
            key = "pe_pend"
        eng.ops.append(("op", fn, tok if sig else None))
        self._update(key, tok, reads, writes, add)
        return tok

    def dma(self, qname, out, in_, reads=(), writes=(), add=False, is_output=False, **kw):
        eng = self.engs[qname]
        self._deps(eng, reads, writes, add)
        pool = self.dma_pool_of[qname]
        name = pool[self.dma_rr[qname] % len(pool)]
        self.dma_rr[qname] += 1
        prev = self.dma_sem_val[name]
        if prev > 0:
            self._wait(eng, [name, prev])
        val = prev + 16
        self.dma_sem_val[name] = val
        tok = [name, val]
        self.sem(name)

        def fn(e, out=out, in_=in_, kw=kw):
            return e.dma_start(out=out, in_=in_, **kw)
        eng.ops.append(("op", fn, tok))
        self._update(name, tok, reads, writes, add)
        if is_output:
            self.out_tokens.append(tok)
        return tok

    def barrier(self):
        toks = [e.last for e in self.engs.values() if e.last is not None]
        for e in self.engs.values():
            assert not e.pending
        toks += [[n, v] for n, v in self.dma_sem_val.items() if v > 0]
        for e in self.engs.values():
            for t in toks:
                if e.name == "pe" and t[0].startswith("pe_"):
                    continue
                self._wait(e, t)

    def finish(self):
        sp = self.engs["sp"]
        for t in self.out_tokens:
            self._wait(sp, t)
        nc = self.nc
        sems = self.sems
        engs = self.engs

        def replay(e, eng):
            for o in eng.ops:
                if o[0] == "wait":
                    e.wait_ge(sems[o[1]], o[2])
                else:
                    inst = o[1](e)
                    if o[2] is not None:
                        key = o[2][0]
                        inst.then_inc(sems[key], 16 if key.startswith("dma") else 1)

        with nc.Block() as block:
            @block.tensor
            def _(e):
                replay(e, engs["pe"])

            @block.scalar
            def _(e):
                replay(e, engs["act"])

            @block.vector
            def _(e):
                replay(e, engs["dve"])

            @block.gpsimd
            def _(e):
                replay(e, engs["pool"])

            @block.sync
            def _(e):
                replay(e, engs["sp"])
        self.stack.close()


def build_program(debug=False, stop=None):
    nc = bass.Bass("TRN2", target_bir_lowering=False)
    fw = FW(nc)

    def din(name, shape, dt=F32):
        return nc.dram_tensor(name, list(shape), dt, kind="ExternalInput")

    x_d = din("x", [S, D]).ap()
    pos_d = din("pos", [128, NT], I32).ap()
    gmix_d = din("norm_mix_g", [D])
    w_in_d = din("w_in", [D, 4096]).ap()
    qg_d = din("q_norm_g", [64])
    kg_d = din("k_norm_g", [64])
    lq1_d = din("lambda_q1", [64]); lk1_d = din("lambda_k1", [64])
    lq2_d = din("lambda_q2", [64]); lk2_d = din("lambda_k2", [64])
    subg_d = din("subln_g", [128])
    wo_d = din("w_o_attn", [512, D]).ap()
    lamre_d = din("lamre_t", [64, 32]).ap(); lamim_d = din("lamim_t", [64, 32]).ap()
    logdt_d = din("ssm_log_dt", [32])
    bre_d = din("bre_t", [64, 512]).ap(); bim_d = din("bim_t", [64, 512]).ap()
    cre_d = din("cre_t", [64, 512]).ap(); cim_d = din("cim_t", [64, 512]).ap()
    dsk_d = din("d_t", [16, 32]).ap()
    wglu_d = din("w_glu", [512, 2048]).ap()
    wout_d = din("w_out", [D, D]).ap()
    gffn_d = din("norm_ffn_g", [D])
    wr_d = din("w_router", [D, 36]).ap()
    br_d = din("b_router", [36])
    weg_d = din("w_expert_gate", [32, D, 256]).ap()
    weu_d = din("w_expert_up", [32, D, 256]).ap()
    wed_d = din("w_expert_down", [32, 256, D]).ap()
    out_d = nc.dram_tensor("out", [S, D], F32, kind="ExternalOutput").ap()
    dbg = {}
    if debug:
        lst = [("dbg_x1", [S, D]), ("dbg_comb", [128, NT * 32]), ("dbg_T", [128, 4096]), ("dbg_ks", [128, 16 * 18]), ("dbg_ug", [128, 32 * 512])]
        for nm_ in ("dbg_gy", "dbg_o", "dbg_q", "dbg_k"):
            lst += [(nm_ + str(q_), [128, S]) for q_ in range(4)]
        for nm, shp in lst:
            dbg[nm] = nc.dram_tensor(nm, shp, F32, kind="ExternalOutput").ap()

    def bc_rows(t, n, reps=1, parts=128):
        if reps == 1:
            return bass.AP(t, 0, [[0, parts], [1, n]])
        return bass.AP(t, 0, [[0, parts], [0, reps], [1, n]])

    KB = 1024

    def A(name, shape, dt, off):
        nbytes = int(np.prod(shape[1:])) * (2 if dt == BF16 else 4)
        assert SB_BASE + off + nbytes <= 229376 - 32, (name, off, nbytes)
        return nc.alloc_sbuf_tensor_at(name, list(shape), dt, offset=SB_BASE + off)

    c_off = [0]

    def CA(name, shape, dt):
        n = int(np.prod(shape[1:])) * (2 if dt == BF16 else 4)
        t = A(name, shape, dt, c_off[0])
        c_off[0] += (n + 31) // 32 * 32
        return t

    ident_f = CA("ident_f", [128, 128], F32)
    ident_b = CA("ident_b", [128, 128], BF16)
    maskf = CA("maskf", [128, 128], F32)
    maskneg_b = CA("maskneg_b", [128, 128], BF16)
    gq_t = CA("gq_t", [128, 512], F32)
    gk_t = CA("gk_t", [128, 512], F32)
    sg08_t = CA("sg08_t", [128, 128], F32)
    gmix_t = CA("gmix_t", [128, D], F32)
    gffn_t = CA("gffn_t", [128, D], F32)
    cos_t = CA("cos_t", [128, NT * 8], F32)
    sin_t = CA("sin_t", [128, NT * 8], F32)
    lamv = CA("lamv", [128, 8], F32)
    comb_all = CA("comb_all", [128, NT * 32], F32)
    wr32 = CA("wr32", [128, 8 * 36], F32)
    br_t = CA("br_t", [128, 36], F32)
    ks_are = CA("ks_are", [128, 16 * 9], F32)
    ks_aim = CA("ks_aim", [128, 16 * 9], F32)
    ks_naim = CA("ks_naim", [128, 16 * 9], F32)
    dvec = CA("dvec", [128, 32], F32)
    stat = CA("stat", [128, 64], F32)
    assert c_off[0] <= 24 * KB, c_off[0]
    M0 = 24 * KB
    R_G, R_O, R_Q, R_K, R_V, R_T = M0, M0 + 32 * KB, M0 + 64 * KB, M0 + 96 * KB, M0 + 128 * KB, M0 + 161 * KB
    R_END = 229376 - SB_BASE - 64

    pp = fw.psum("pp", [128, 8 * 512], F32)
    ppb = pp.bitcast(BF16)
    PB = [Buf("psum%d" % i) for i in range(8)]

    def bank(i, a=0, b=512):
        return pp[:, i * 512 + a:i * 512 + b]

    def bankb(i, a=0, b=1024):
        return ppb[:, i * 1024 + a:i * 1024 + b]

    def MM(out, lhsT, rhs, start, stop, r, w, sig=True, add=False, skip=False):
        if skip:
            return fw.op("pe", lambda e: e.matmul(out, lhsT, rhs, start=start, stop=stop, skip_group_check=True), reads=r, writes=w, sig=sig, add=add)
        return fw.op("pe", lambda e: e.matmul(out, lhsT, rhs, start=start, stop=stop), reads=r, writes=w, sig=sig, add=add)

    def TR(out, in_, ident, r, w, sig=True, add=False):
        return fw.op("pe", lambda e: e.transpose(out, in_, ident), reads=r, writes=w, sig=sig, add=add)

    def ACT(out, in_, func, r, w, add=False, **kw):
        return fw.op("act", lambda e: e.activation(out, in_, func, **kw), reads=r, writes=w, add=add)

    def TT(eng, out, in0, in1, op, r, w, add=False):
        return fw.op(eng, lambda e: e.tensor_tensor(out, in0, in1, op), reads=r, writes=w, add=add)

    def TS(eng, out, in0, s1, s2, op0, op1, r, w, add=False):
        if s2 is None:
            return fw.op(eng, lambda e: e.tensor_scalar(out, in0, s1, None, op0), reads=r, writes=w, add=add)
        return fw.op(eng, lambda e: e.tensor_scalar(out, in0, s1, s2, op0, op1), reads=r, writes=w, add=add)

    def STT(out, in0, sc, in1, op0, op1, r, w, add=False):
        return fw.op("dve", lambda e: e.scalar_tensor_tensor(out, in0, sc, in1, op0, op1), reads=r, writes=w, add=add)

    def CP(eng, out, in_, r, w, add=False):
        if eng == "act":
            return ACT(out, in_, AF.Copy, r, w, add=add)
        return fw.op(eng, lambda e: e.tensor_copy(out, in_), reads=r, writes=w, add=add)

    def RECIP(out, in_, r, w, add=False):
        return fw.op("dve", lambda e: e.reciprocal(out, in_), reads=r, writes=w, add=add)

    def RSUM(out, in_, r, w, add=False):
        return fw.op("dve", lambda e: e.reduce_sum(out, in_, axis=AX.X), reads=r, writes=w, add=add)

    def RMAX(out, in_, r, w, add=False):
        return fw.op("dve", lambda e: e.reduce_max(out, in_, axis=AX.X), reads=r, writes=w, add=add)

    def MEMSET(eng, out, val, r, w, add=False):
        return fw.op(eng, lambda e: e.memset(out, val), reads=r, writes=w, add=add)

    def V(t, off, dims):
        pstride = int(np.prod(t.shape[1:]))
        return bass.AP(t, off, [[pstride, t.shape[0]]] + [list(d) for d in dims])

    def VP(t, p0, pn, off, dims):
        pstride = int(np.prod(t.shape[1:]))
        return bass.AP(t, p0 * pstride + off, [[pstride, pn]] + [list(d) for d in dims])

    Bc = Buf("consts")
    MEMSET("pool", ident_f[:], 1.0, [], [Bc])
    fw.op("pool", lambda e: e.affine_select(ident_f[:], ident_f[:], pattern=[[-1, 128]], compare_op=ALU.is_equal, fill=0.0, base=0, channel_multiplier=1), reads=[Bc], writes=[Bc])
    CP("pool", ident_b[:], ident_f[:], [Bc], [Bc])
    MEMSET("pool", maskf[:], 0.0, [Bc], [Bc])
    fw.op("pool", lambda e: e.affine_select(maskf[:], maskf[:], pattern=[[1, 128]], compare_op=ALU.is_ge, fill=-30000.0, base=0, channel_multiplier=-1), reads=[Bc], writes=[Bc])
    CP("pool", maskneg_b[:], maskf[:], [Bc], [Bc])
    fw.dma("sp", gq_t[:], bc_rows(qg_d, 64, 8), writes=[Bc], add=True)
    fw.dma("sp", gk_t[:], bc_rows(kg_d, 64, 8), writes=[Bc], add=True)
    fw.dma("sp", sg08_t[:], bc_rows(subg_d, 128), writes=[Bc], add=True)
    fw.dma("sp", gmix_t[:], bc_rows(gmix_d, D), writes=[Bc], add=True)
    fw.dma("sp", gffn_t[:], bc_rows(gffn_d, D), writes=[Bc], add=True)
    fw.dma("sp", br_t[:], bc_rows(br_d, 36), writes=[Bc], add=True)
    fw.dma("sp", wr32[:], wr_d.rearrange("(k p) n -> p k n", p=128), writes=[Bc], add=True)
    for hh in range(8):
        fw.dma("sp", dvec[hh * 16:(hh + 1) * 16, :], dsk_d, writes=[Bc], add=True)
    tmp0 = A("c_tmp0", [128, 4 * 64], F32, R_T)
    posi = A("c_posi", [128, NT], I32, R_T + 1 * KB)
    posf = A("c_posf", [128, NT], F32, R_T + 1 * KB + 128)
    ang = A("c_ang", [128, NT * 8], F32, R_T + 2 * KB)
    ang2 = A("c_ang2", [128, NT * 8], F32, R_T + 3 * KB)
    ang3 = A("c_ang3", [128, NT * 8], F32, R_T + 4 * KB)
    Bt = Buf("ctmp")
    for i, dd in enumerate((lq1_d, lk1_d, lq2_d, lk2_d)):
        fw.dma("sp", tmp0[:, i * 64:(i + 1) * 64], bc_rows(dd, 64), writes=[Bt], add=True)
    fw.dma("sp", posi[:], pos_d, writes=[Bt], add=True)
    TS("dve", sg08_t[:], sg08_t[:], 1.0 - LAM_INIT, None, ALU.mult, None, [Bc], [Bc])
    TT("dve", tmp0[:, 0:64], tmp0[:, 0:64], tmp0[:, 64:128], ALU.mult, [Bt], [Bt])
    TT("dve", tmp0[:, 128:192], tmp0[:, 128:192], tmp0[:, 192:256], ALU.mult, [Bt], [Bt])
    RSUM(lamv[:, 0:1], tmp0[:, 0:64], [Bt], [Bc])
    RSUM(lamv[:, 1:2], tmp0[:, 128:192], [Bt], [Bc])
    ACT(lamv[:, 0:2], lamv[:, 0:2], AF.Exp, [Bc], [Bc])
    TT("dve", lamv[:, 2:3], lamv[:, 1:2], lamv[:, 0:1], ALU.subtract, [Bc], [Bc])
    TS("dve", lamv[:, 3:4], lamv[:, 2:3], -LAM_INIT, None, ALU.add, None, [Bc], [Bc])
    CP("dve", posf[:], posi[:], [Bt], [Bt])
    for i in range(8):
        inv = (500000.0 ** (-i / 8.0)) / (2.0 * math.pi)
        TS("dve", V(ang, i, [[8, NT]]), posf[:], inv, None, ALU.mult, None, [Bt], [Bt], add=True)
    MAGIC = 12582912.0
    for (dst, shift) in ((sin_t, 0.0), (cos_t, 0.25)):
        TS("dve", ang2[:], ang[:], shift, None, ALU.add, None, [Bt], [Bt])
        TS("dve", ang3[:], ang2[:], MAGIC, MAGIC, ALU.add, ALU.subtract, [Bt], [Bt])
        TT("dve", ang2[:], ang2[:], ang3[:], ALU.subtract, [Bt], [Bt])
        ACT(dst[:], ang2[:], AF.Sin, [Bt], [Bc], scale=6.283185)

    if stop == 'p0a':
        fw.finish()
        return nc
    T_b = A("T_b", [128, 32 * 128], BF16, R_Q)
    VTre_b = A("VTre_b", [128, 32 * 64], BF16, R_Q + 8 * KB)
    VTim_b = A("VTim_b", [128, 32 * 64], BF16, R_Q + 12 * KB)
    Wre_b = A("Wre_b", [128, 32 * 128], BF16, R_Q + 16 * KB)
    Wimn_b = A("Wimn_b", [128, 32 * 128], BF16, R_Q + 24 * KB)
    Bs5w = Buf("s5w")
    Gre = A("Gre_", [128, 4096], F32, M0 + 0)
    Gim = A("Gim_", [128, 4096], F32, M0 + 16 * KB)
    HHre = A("HHre_", [128, 32 * 144], F32, M0 + 32 * KB)
    HHim = A("HHim_", [128, 32 * 144], F32, M0 + 96 * KB)
    VVre = A("VVre_", [128, 4096], F32, M0 + 114 * KB)
    VVim = A("VVim_", [128, 4096], F32, M0 + 130 * KB)
    GS = A("GS_", [128, 4096], F32, M0 + 146 * KB)
    HS = A("HS_", [128, 4096], F32, M0 + 162 * KB)
    so = [M0 + 50 * KB]

    def SA(name, n):
        t = A(name, [128, n], F32, so[0])
        so[0] += n * 4
        return t
    lre = SA("lre", 32); lim = SA("lim", 32); dtt = SA("dtt", 32); ar = SA("ar", 32); ai = SA("ai", 32)
    mm_ = SA("mm_", 32); minv = SA("minv", 32); kk = SA("kk", 32); rr = SA("rr", 32); x8 = SA("x8", 32); x2 = SA("x2", 32)
    pp_ = SA("pp_", 32); cc = SA("cc", 32); ss_ = SA("ss_", 32); t1 = SA("t1", 32); t2 = SA("t2", 32); t3 = SA("t3", 32); t4 = SA("t4", 32)
    LPre = SA("LPre", 32 * 9); LPim = SA("LPim", 32 * 9); LIre = SA("LIre", 32 * 8); LIim = SA("LIim", 32 * 8)
    Are = SA("Are", 32 * 9); Aim = SA("Aim", 32 * 9)
    fre = SA("fre", 32); fim = SA("fim", 32); ire = SA("ire", 32); iim = SA("iim", 32); den = SA("den", 32)
    assert so[0] <= M0 + 64 * KB
    Bin = A("Bin_re", [128, 512], F32, M0 + 178 * KB)
    Bin_im = A("Bin_im", [128, 512], F32, M0 + 180 * KB)
    Cre = A("Cre_in", [128, 512], F32, M0 + 146 * KB)
    Cim = A("Cim_in", [128, 512], F32, M0 + 148 * KB)
    Bbre = A("Bbre", [128, 512], F32, M0 + 150 * KB)
    Bbim = A("Bbim", [128, 512], F32, M0 + 152 * KB)
    W1 = A("W1", [128, 4608], F32, M0 + 114 * KB)
    W2 = A("W2_", [128, 4608], F32, M0 + 154 * KB)
    Bp = Buf("s5prep")

    for half in range(2):
        ps_ = slice(half * 64, half * 64 + 64)
        fw.dma("sp", lre[ps_, :], lamre_d, writes=[Bp], add=True)
        fw.dma("sp", lim[ps_, :], lamim_d, writes=[Bp], add=True)
        fw.dma("sp", Bin[ps_, :], bre_d, writes=[Bp], add=True)
        fw.dma("sp", Bin_im[ps_, :], bim_d, writes=[Bp], add=True)
        fw.dma("sp", Cre[ps_, :], cre_d, writes=[Bp], add=True)
        fw.dma("sp", Cim[ps_, :], cim_d, writes=[Bp], add=True)
    fw.dma("sp", dtt[:], bc_rows(logdt_d, 32), writes=[Bp], add=True)

    def d_tt(out, a, b, op):
        return TT("dve", out, a, b, op, [Bp], [Bp])

    def d_ts(out, a, s1, s2=None, op0=ALU.mult, op1=ALU.add):
        return TS("dve", out, a, s1, s2, op0, op1, [Bp], [Bp])

    def cmul(ore, oim, are_, aim_, bre_, bim_, ta, tb):
        d_tt(ta, are_, bre_, ALU.mult)
        d_tt(tb, aim_, bim_, ALU.mult)
        d_tt(ore, ta, tb, ALU.subtract)
        d_tt(ta, are_, bim_, ALU.mult)
        d_tt(tb, aim_, bre_, ALU.mult)
        d_tt(oim, ta, tb, ALU.add)

    ACT(dtt[:], dtt[:], AF.Exp, [Bp], [Bp])
    d_tt(ar[:], lre[:], dtt[:], ALU.mult)
    d_tt(ai[:], lim[:], dtt[:], ALU.mult)
    MEMSET("dve", mm_[:], 1.0, [Bp], [Bp])
    for k in range(10, 0, -1):
        d_tt(mm_[:], mm_[:], ar[:], ALU.mult)
        d_ts(mm_[:], mm_[:], 1.0 / k, 1.0)
    RECIP(minv[:], mm_[:], [Bp], [Bp])
    d_ts(kk[:], ai[:], 1.0 / (2.0 * math.pi), None)
    d_ts(kk[:], kk[:], MAGIC, MAGIC, ALU.add, ALU.subtract)
    STT(rr[:], kk[:], -6.28125, ai[:], ALU.mult, ALU.add, [Bp], [Bp])
    STT(rr[:], kk[:], -(2.0 * math.pi - 6.28125), rr[:], ALU.mult, ALU.add, [Bp], [Bp])
    d_ts(x8[:], rr[:], 0.125, None)
    d_tt(x2[:], x8[:], x8[:], ALU.mult)
    sc_ = [1.0, -1.0 / 6, 1.0 / 120, -1.0 / 5040, 1.0 / 362880, -1.0 / 39916800]
    cc_ = [1.0, -0.5, 1.0 / 24, -1.0 / 720, 1.0 / 40320, -1.0 / 3628800, 1.0 / 479001600]
    for (dst, co) in ((ss_, sc_), (cc, cc_)):
        MEMSET("dve", dst[:], co[-1], [Bp], [Bp])
        for c in co[-2::-1]:
            d_tt(dst[:], dst[:], x2[:], ALU.mult)
            d_ts(dst[:], dst[:], c, None, ALU.add)
    d_tt(ss_[:], ss_[:], x8[:], ALU.mult)
    for _ in range(3):
        d_tt(t1[:], cc[:], cc[:], ALU.mult)
        d_tt(t2[:], ss_[:], ss_[:], ALU.mult)
        STT(t3[:], ss_[:], 2.0, cc[:], ALU.mult, ALU.mult, [Bp], [Bp])
        d_tt(cc[:], t1[:], t2[:], ALU.subtract)
        CP("dve", ss_[:], t3[:], [Bp], [Bp])
    def LPv(t, j):
        return V(t, j, [[9, 32]])

    def LIv(t, j):
        return V(t, j, [[8, 32]])
    MEMSET("dve", LPv(LPre, 0), 1.0, [Bp], [Bp])
    MEMSET("dve", LPv(LPim, 0), 0.0, [Bp], [Bp])
    d_tt(LPv(LPre, 1), mm_[:], cc[:], ALU.mult)
    d_tt(LPv(LPim, 1), mm_[:], ss_[:], ALU.mult)
    for j in range(2, 9):
        cmul(LPv(LPre, j), LPv(LPim, j), LPv(LPre, j - 1), LPv(LPim, j - 1), LPv(LPre, 1), LPv(LPim, 1), t1[:], t2[:])
    MEMSET("dve", LIv(LIre, 0), 1.0, [Bp], [Bp])
    MEMSET("dve", LIv(LIim, 0), 0.0, [Bp], [Bp])
    d_tt(LIv(LIre, 1), minv[:], cc[:], ALU.mult)
    d_tt(t3[:], minv[:], ss_[:], ALU.mult)
    d_ts(LIv(LIim, 1), t3[:], -1.0, None)
    for j in range(2, 8):
        cmul(LIv(LIre, j), LIv(LIim, j), LIv(LIre, j - 1), LIv(LIim, j - 1), LIv(LIre, 1), LIv(LIim, 1), t1[:], t2[:])
    CP("dve", LPv(Are, 0), LPv(LPre, 8), [Bp], [Bp])
    CP("dve", LPv(Aim, 0), LPv(LPim, 8), [Bp], [Bp])
    for k in range(1, 9):
        d_tt(t1[:], LPv(Are, k - 1), LPv(Are, k - 1), ALU.mult)
        d_tt(t2[:], LPv(Aim, k - 1), LPv(Aim, k - 1), ALU.mult)
        d_tt(LPv(Are, k), t1[:], t2[:], ALU.subtract)
        STT(LPv(Aim, k), LPv(Are, k - 1), 2.0, LPv(Aim, k - 1), ALU.mult, ALU.mult, [Bp], [Bp])
    for gl in range(2):
        for (src, dst) in ((Are, ks_are), (Aim, ks_aim)):
            fw.op("dve", lambda e, src=src, dst=dst, gl=gl: e.tensor_copy(
                VP(dst, gl * 64, 64, 0, [[9, 16], [1, 9]]), VP(src, gl * 64, 64, gl * 9, [[18, 16], [1, 9]])), reads=[Bp], writes=[Bc], add=True)
    TS("dve", ks_naim[:], ks_aim[:], -1.0, None, ALU.mult, None, [Bc], [Bc])
    d_ts(t1[:], LPv(LPre, 1), -1.0, None, ALU.add)
    d_tt(den[:], lre[:], lre[:], ALU.mult)
    d_tt(t2[:], lim[:], lim[:], ALU.mult)
    d_tt(den[:], den[:], t2[:], ALU.add)
    RECIP(den[:], den[:], [Bp], [Bp])
    d_tt(ire[:], lre[:], den[:], ALU.mult)
    d_tt(iim[:], lim[:], den[:], ALU.mult)
    d_ts(iim[:], iim[:], -1.0, None)
    cmul(fre[:], fim[:], t1[:], LPv(LPim, 1), ire[:], iim[:], t3[:], t4[:])
    def bc16(t):
        return V(t, 0, [[1, 32], [0, 16]])

    def v3(t):
        return V(t, 0, [[16, 32], [1, 16]])
    w1a = V(W1, 0, [[16, 32], [1, 16]]); w1b = V(W1, 512, [[16, 32], [1, 16]])
    cmul(v3(Bbre), v3(Bbim), bc16(fre), bc16(fim), v3(Bin), v3(Bin_im), w1a, w1b)
    def g4(t):
        return V(t, 0, [[128, 32], [16, 8], [1, 16]])

    def li4(t):
        return V(t, 0, [[8, 32], [1, 8], [0, 16]])

    def bb4(t):
        return V(t, 0, [[16, 32], [0, 8], [1, 16]])
    cmul(g4(Gre), g4(Gim), li4(LIre), li4(LIim), bb4(Bbre), bb4(Bbim), g4(W1), g4(W2))
    def h4(t):
        return V(t, 0, [[144, 32], [16, 9], [1, 16]])

    def lp4(t):
        return V(t, 0, [[9, 32], [1, 9], [0, 16]])

    def c4(t):
        return V(t, 0, [[16, 32], [0, 9], [1, 16]])
    cmul(h4(HHre), h4(HHim), lp4(LPre), lp4(LPim), c4(Cre), c4(Cim), h4(W1), h4(W2))
    def hs4(t, j0):
        return V(t, j0 * 16, [[144, 32], [1, 128]])
    CP("dve", V(Wre_b, 0, [[128, 32], [1, 128]]), hs4(HHre, 1), [Bp], [Bs5w], add=True)
    TS("dve", V(Wimn_b, 0, [[128, 32], [1, 128]]), hs4(HHim, 1), -1.0, None, ALU.mult, None, [Bp], [Bs5w], add=True)
    def l7(t):
        return V(t, 7, [[9, 32], [0, 128]])

    def g3(t):
        return V(t, 0, [[128, 32], [1, 128]])
    Bvv = Buf("vv")
    cmul(g3(VVre), g3(VVim), l7(LPre), l7(LPim), g3(Gre), g3(Gim), g3(GS), g3(HS))
    CP("dve", VP(GS, 0, 64, 0, [[1, 4096]]), VP(Gre, 0, 64, 0, [[1, 4096]]), [Bp], [Bp])
    fw.op("dve", lambda e: e.tensor_scalar(VP(GS, 64, 64, 0, [[1, 4096]]), VP(Gim, 64, 64, 0, [[1, 4096]]), -1.0, None, ALU.mult), reads=[Bp], writes=[Bp])
    CP("dve", VP(HS, 0, 64, 0, [[128, 32], [1, 128]]), VP(HHre, 0, 64, 0, [[144, 32], [1, 128]]), [Bp], [Bp])
    CP("dve", VP(HS, 64, 64, 0, [[128, 32], [1, 128]]), VP(HHim, 64, 64, 0, [[144, 32], [1, 128]]), [Bp], [Bp])
    mask4 = A("mask4", [128, 512], F32, M0 + 178 * KB)
    MEMSET("pool", mask4[:], 1.0, [Bp], [Bp])
    fw.op("pool", lambda e: e.affine_select(mask4[:], mask4[:], pattern=[[0, 4], [16, 8], [0, 16]], compare_op=ALU.is_ge, fill=0.0, base=15, channel_multiplier=-1), reads=[Bp], writes=[Bp])
    Tm = A("Tm", [128, 512], F32, M0 + 180 * KB)
    for q4 in range(8):
        bk = q4 % 2
        for gi in range(4):
            g = q4 * 4 + gi
            MM(bank(bk, gi * 128, gi * 128 + 128), GS[:, g * 128:(g + 1) * 128], HS[:, g * 128:(g + 1) * 128], True, True, [Bp], [PB[bk]], sig=(gi == 3), add=(gi > 0))
        TT("dve", Tm[:], bank(bk), mask4[:], ALU.mult, [PB[bk], Bp], [Bp])
        for gi in range(4):
            g = q4 * 4 + gi
            STT(T_b[:, g * 128:(g + 1) * 128], ident_f[:], dvec[:, g:g + 1], Tm[:, gi * 128:(gi + 1) * 128], ALU.mult, ALU.add, [Bp, Bc], [Bs5w], add=True)
    for (src, dst) in ((VVre, VTre_b), (VVim, VTim_b)):
        for q4 in range(8):
            bk = 2 + q4 % 2
            for gi in range(4):
                g = q4 * 4 + gi
                TR(bank(bk, gi * 128, gi * 128 + 128), src[:, g * 128:(g + 1) * 128], ident_f[:], [Bp, Bc], [PB[bk]], sig=(gi == 3), add=(gi > 0))
            CP("act", V(dst, q4 * 256, [[64, 4], [1, 64]]), bass.AP(pp, bk * 512, [[4096, 128], [128, 4], [1, 64]]), [PB[bk]], [Bs5w], add=True)
    if debug:
        dtmp = A("dtmp", [128, 4096], F32, M0 + 0)
        fw.barrier()
        CP("dve", dtmp[:], T_b[:], [Bs5w], [Bp])
        fw.dma("sp", dbg["dbg_T"], dtmp[:], reads=[Bp], is_output=True)
        dks = A("dks", [128, 288], F32, M0 + 16 * KB)
        CP("dve", dks[:, 0:144], ks_are[:], [Bc], [Bp])
        CP("dve", dks[:, 144:288], ks_aim[:], [Bc], [Bp])
        fw.dma("sp", dbg["dbg_ks"], dks[:], reads=[Bp], is_output=True)
    fw.barrier()

    if stop == 'p0b':
        fw.finish()
        return nc
    def rms_tile(xt, hbt, jk, st, Bx, Bh, Bst, rows, gt=gmix_t, out_dt_bf=True):
        ACT(jk, xt, AF.Square, [Bx], [Bst, Bh], accum_out=st[:, 0:1])
        ACT(st[:, 1:2], st[:, 0:1], AF.Sqrt, [Bst], [Bst], scale=1.0 / D, bias=EPS)
        RECIP(st[:, 2:3], st[:, 1:2], [Bst], [Bst])
        STT(hbt, xt, st[:, 2:3], gt[:], ALU.mult, ALU.mult, [Bx, Bst, Bc], [Bh])

    gyT = A("gyT", [128, 4 * S], BF16, R_G)
    Wu_b = A("Wu_b", [128, 8 * 512], BF16, R_O)
    hT_sb = A("hT_sb", [128, 8 * 1024], BF16, R_O + 8 * KB)
    U_tok = A("U_tok", [128, 4 * 4096], BF16, R_K)
    Ug = A("Ug", [128, 32 * 512], BF16, R_V)
    xa = [A("xa%d" % i, [128, D], F32, R_T + i * 4 * KB) for i in range(2)]
    hba = [A("hba%d" % i, [128, D], BF16, R_T + 8 * KB + i * 2 * KB) for i in range(2)]
    jka = A("jka", [128, D], BF16, R_T + 12 * KB)
    jkaA = A("jkaA", [128, D], BF16, R_G)
    ksb = [A("ksb%d" % i, [128, 2 * 512], F32, R_T + 12 * KB + i * 4 * KB) for i in range(2)]
    Bxa = [Buf("xa0"), Buf("xa1")]; Bhba = [Buf("hba0"), Buf("hba1")]; Bsta = [Buf("sta0"), Buf("sta1")]
    BWu = Buf("Wu"); BhT = Buf("hTsb"); BUt = [Buf("Ut%d" % i) for i in range(4)]; BUg = [Buf("Ug%d" % i) for i in range(32)]
    Bjk = Buf("jk")
    fw.dma("pool", V(Wu_b, 0, [[512, 8], [1, 512]]), w_in_d[:, 1536:2048].rearrange("(k p) n -> p k n", p=128), writes=[BWu])
    for sb in range(4):
        for i in range(8):
            n = sb * 8 + i
            bi = n % 2
            fw.dma("sp", xa[bi][:], x_d[n * 128:(n + 1) * 128, :], writes=[Bxa[bi]])
            rms_tile(xa[bi][:], hba[bi][:], jkaA[:], V(stat, bi * 4, [[1, 4]]), Bxa[bi], Bhba[bi], Bsta[bi], None)
            for k in range(8):
                TR(bankb(0, k * 128, k * 128 + 128), hba[bi][:, k * 128:(k + 1) * 128], ident_b[:], [Bhba[bi], Bc], [PB[0]], sig=(k == 7), add=(k > 0))
            CP("act", V(hT_sb, i * 128, [[1024, 8], [1, 128]]), bass.AP(ppb, 0, [[8192, 128], [128, 8], [1, 128]]), [PB[0]], [BhT], add=(i > 0))
        for tau in range(8):
            bk = 1 + tau % 2
            for k in range(8):
                MM(bank(bk), V(hT_sb, k * 1024 + tau, [[8, 128]]), Wu_b[:, k * 512:(k + 1) * 512], k == 0, k == 7, [BhT, BWu], [PB[bk]], sig=(k == 7), add=(k > 0))
            eng = "act" if tau % 2 == 0 else "dve"
            CP(eng, V(U_tok, sb * 4096 + tau * 16, [[128, 32], [1, 16]]), bass.AP(pp, bk * 512, [[4096, 128], [16, 32], [1, 16]]), [PB[bk]], [BUt[sb]], add=(tau > 0))
        for g8 in range(4):
            bk = 3 + g8 % 2
            for gi in range(8):
                g = g8 * 8 + gi
                TR(bankb(bk, gi * 128, gi * 128 + 128), U_tok[:, sb * 4096 + g * 128: sb * 4096 + (g + 1) * 128], ident_b[:], [BUt[sb], Bc], [PB[bk]], sig=(gi == 7), add=(gi > 0))
            eng = "act" if g8 % 2 == 0 else "dve"
            CP(eng, V(Ug, g8 * 8 * 512 + sb * 128, [[512, 8], [1, 128]]), bass.AP(ppb, bk * 1024, [[8192, 128], [128, 8], [1, 128]]), [PB[bk]], [BUg[g8 * 8 + gi] for gi in range(8)], add=True)
    if debug:
        fw.barrier()
        dtmp2 = A("dtmp2", [128, 16384], F32, R_G)
        CP("dve", dtmp2[:], Ug[:], BUg, [Bp])
        fw.dma("sp", dbg["dbg_ug"], dtmp2[:], reads=[Bp], is_output=True)
        fw.barrier()
    if stop == 'pA1':
        fw.finish()
        return nc
    Ygel = U_tok
    BY = [Buf("Ygel%d" % i) for i in range(32)]
    Xb = [A("Xb%d" % i, [128, 2 * 512], BF16, R_O + 24 * KB + i * 2 * KB) for i in range(2)]
    BXb = [Buf("Xb0"), Buf("Xb1")]
    Bks = [Buf("ks0"), Buf("ks1")]
    gel = [A("gel%d" % i, [128, 512], F32, R_O + 28 * KB + i * 2 * KB) for i in range(2)]
    Bgel = [Buf("gel0"), Buf("gel1")]
    for gp in range(16):
        for (ri, VT) in ((0, VTre_b), (1, VTim_b)):
            bk = 5 + ri
            for gl in range(2):
                g = 2 * gp + gl
                MM(pp[gl * 64:(gl + 1) * 64, bk * 512:(bk + 1) * 512], VT[:, g * 64:(g + 1) * 64], Ug[:, g * 512:(g + 1) * 512], True, True, [Bs5w, BUg[g]], [PB[bk]], sig=(gl == 1), add=(gl > 0))
        cur, nxt = 0, 1
        CP("act", ksb[cur][:, 0:512], bank(5), [PB[5]], [Bks[cur]])
        CP("act", ksb[cur][:, 512:1024], bank(6), [PB[6]], [Bks[cur]], add=True)
        for k in range(9):
            s = 1 << k
            n = 512 - s
            a_k = ks_are[:, gp * 9 + k: gp * 9 + k + 1]
            b_k = ks_aim[:, gp * 9 + k: gp * 9 + k + 1]
            nb_k = ks_naim[:, gp * 9 + k: gp * 9 + k + 1]
            c_, n_ = ksb[cur], ksb[nxt]
            CP("act", V(n_, 0, [[512, 2], [1, s]]), V(c_, 0, [[512, 2], [1, s]]), [Bks[cur]], [Bks[nxt]])
            STT(n_[:, s:512], c_[:, 0:n], a_k, c_[:, s:512], ALU.mult, ALU.add, [Bks[cur], Bc], [Bks[nxt]], add=True)
            STT(n_[:, s:512], c_[:, 512:512 + n], nb_k, n_[:, s:512], ALU.mult, ALU.add, [Bks[cur], Bks[nxt], Bc], [Bks[nxt]], add=True)
            STT(n_[:, 512 + s:1024], c_[:, 0:n], b_k, c_[:, 512 + s:1024], ALU.mult, ALU.add, [Bks[cur], Bc], [Bks[nxt]], add=True)
            STT(n_[:, 512 + s:1024], c_[:, 512:512 + n], a_k, n_[:, 512 + s:1024], ALU.mult, ALU.add, [Bks[cur], Bks[nxt], Bc], [Bks[nxt]], add=True)
            cur, nxt = nxt, cur
        xb = Xb[gp % 2]
        CP("act", xb[:], ksb[cur][:], [Bks[cur]], [BXb[gp % 2]])
        for gl in range(2):
            g = 2 * gp + gl
            bk = 1 + g % 2
            MM(bank(bk), T_b[:, g * 128:(g + 1) * 128], Ug[:, g * 512:(g + 1) * 512], True, False, [Bs5w, BUg[g]], [PB[bk]], sig=False)
            MM(bank(bk, 1, 512), VP(Wre_b, gl * 64, 64, g * 128, [[1, 128]]), VP(xb, gl * 64, 64, 0, [[1, 511]]), False, False, [Bs5w, BXb[gp % 2]], [PB[bk]], sig=False, add=True)
            MM(bank(bk, 1, 512), VP(Wimn_b, gl * 64, 64, g * 128, [[1, 128]]), VP(xb, gl * 64, 64, 512, [[1, 511]]), False, True, [Bs5w, BXb[gp % 2]], [PB[bk]], sig=True, add=True)
            ge = gel[g % 2]
            Bg = Bgel[g % 2]
            ACT(ge[:], bank(bk), AF.Square, [PB[bk]], [Bg])
            TS("dve", ge[:], ge[:], 0.044715, 1.0, ALU.mult, ALU.add, [Bg], [Bg])
            TT("dve", ge[:], ge[:], bank(bk), ALU.mult, [Bg, PB[bk]], [Bg])
            ACT(ge[:], ge[:], AF.Sigmoid, [Bg], [Bg], scale=1.5957691216057308)
            TT("dve", Ygel[:, g * 512:(g + 1) * 512], ge[:], bank(bk), ALU.mult, [Bg, PB[bk]], [BY[g]] + BUt, add=True)
    if stop == 'pA2':
        fw.finish()
        return nc
    fw.barrier()
    Ytok = Ug
    BYt = [Buf("Ytok%d" % i) for i in range(4)]
    Bgy = [Buf("gyT%d" % i) for i in range(8)]
    gy32 = [A("gy32_%d" % i, [128, 4 * 1024], F32, R_Q + i * 16 * KB) for i in range(2)]
    Bg32 = [Buf("gy32_0"), Buf("gy32_1")]
    for sb in range(4):
        for g8 in range(4):
            bk = 3 + g8 % 2
            for gi in range(8):
                g = g8 * 8 + gi
                TR(bankb(bk, gi * 128, gi * 128 + 128), Ygel[:, g * 512 + sb * 128: g * 512 + (sb + 1) * 128], ident_b[:], [BY[g], Bc], [PB[bk]], sig=(gi == 7), add=(gi > 0))
            eng = "act" if g8 % 2 == 0 else "dve"
            CP(eng, V(Ytok, sb * 4096 + g8 * 128, [[16, 8], [512, 8], [1, 16]]), bass.AP(ppb, bk * 1024, [[8192, 128], [128, 8], [16, 8], [1, 16]]), [PB[bk]], BUg + [BYt[sb]], add=True)
        if stop == 'pA3':
            fw.finish()
            return nc
        for j in range(8):
            bk = 5 + j % 2
            for q4 in range(4):
                TR(bankb(bk, q4 * 128, q4 * 128 + 128), Ytok[:, sb * 4096 + j * 512 + q4 * 128: sb * 4096 + j * 512 + (q4 + 1) * 128], ident_b[:], [BYt[sb], Bc], [PB[bk]], sig=(q4 == 3), add=(q4 > 0))
            eng = "act" if j % 2 == 0 else "dve"
            CP(eng, V(gy32[sb % 2], j, [[1024, 4], [8, 128]]), bass.AP(ppb, bk * 1024, [[8192, 128], [128, 4], [1, 128]]), [PB[bk]], [Bg32[sb % 2]], add=(j > 0))
        if stop == 'pA4':
            fw.finish()
            return nc
        CP("pool", V(gyT, sb * 1024, [[S, 4], [1, 1024]]), V(gy32[sb % 2], 0, [[1024, 4], [1, 1024]]), [Bg32[sb % 2]], [Bgy[2 * sb], Bgy[2 * sb + 1]], add=True)
    if stop == 'pA5':
        fw.finish()
        return nc
    if debug:
        fw.barrier()
        for q_ in range(4):
            dst_ = A("stg_dbg_gy_%d" % q_, [128, S], F32, R_K)
            CP("dve", dst_[:], gyT[:, q_ * S:(q_ + 1) * S], Bgy, [Bp])
            fw.dma("sp", dbg["dbg_gy" + str(q_)], dst_[:], reads=[Bp], is_output=True)
    fw.barrier()

    if stop == 'pA':
        fw.finish()
        return nc
    qT = A("qT", [128, 4 * S], BF16, R_Q)
    kT = A("kT", [128, 4 * S], BF16, R_K)
    v_aug = A("v_aug", [128, NT * 4 * 130], BF16, R_V)
    Wqkv = A("Wqkv", [128, 8 * 1536], BF16, R_O)
    hTt = [A("hTt%d" % i, [128, 8 * 128], BF16, R_O + 24 * KB + i * 2 * KB) for i in range(2)]
    BhTt = [Buf("hTt0"), Buf("hTt1")]
    sqs = A("sqs", [128, 512], F32, R_O + 28 * KB)
    qn = [A("qn%d" % i, [128, 512], F32, R_T + 14 * KB + i * 2 * KB) for i in range(2)]
    qb_ = [A("qb%d" % i, [128, 512], BF16, R_T + 18 * KB + i * 1 * KB) for i in range(2)]
    rtmp = A("rtmp", [128, 4 * 64], F32, R_T + 20 * KB)
    Bsq = Buf("sqs"); Bqn = [Buf("qn0"), Buf("qn1")]; Bqb = [Buf("qb0"), Buf("qb1")]; Brt = Buf("rtmp")
    BW = Buf("Wqkv"); BqT = [Buf("qT%d" % i) for i in range(8)]; BkT = [Buf("kT%d" % i) for i in range(NT)]; Bv = [Buf("v%d" % i) for i in range(NT)]
    fw.dma("pool", V(Wqkv, 0, [[1536, 8], [1, 1536]]), w_in_d[:, 0:1536].rearrange("(k p) n -> p k n", p=128), writes=[BW])
    MEMSET("pool", V(v_aug, 128, [[130, NT * 4], [1, 2]]), 1.0, [], Bv)
    for n in range(NT):
        bi = n % 2
        fw.dma("sp", xa[bi][:], x_d[n * 128:(n + 1) * 128, :], writes=[Bxa[bi]])
        rms_tile(xa[bi][:], hba[bi][:], jka[:], V(stat, bi * 4, [[1, 4]]), Bxa[bi], Bhba[bi], Bsta[bi], None)
        for k in range(8):
            TR(bankb(0, k * 128, k * 128 + 128), hba[bi][:, k * 128:(k + 1) * 128], ident_b[:], [Bhba[bi], Bc], [PB[0]], sig=(k == 7), add=(k > 0))
        CP("act", hTt[bi][:], bankb(0), [PB[0]], [BhTt[bi]])
        for cb in range(3):
            bk = 1 + cb
            for k in range(8):
                MM(bank(bk), hTt[bi][:, k * 128:(k + 1) * 128], Wqkv[:, k * 1536 + cb * 512: k * 1536 + (cb + 1) * 512], k == 0, k == 7, [BhTt[bi], BW], [PB[bk]], sig=(k == 7), add=(k > 0))
            if cb == 2:
                CP("act", V(v_aug, n * 520, [[130, 4], [1, 128]]), bass.AP(pp, bk * 512, [[4096, 128], [128, 4], [1, 128]]), [PB[bk]], [Bv[n]], add=True)
                continue
            st = V(stat, 8 + cb * 24, [[1, 24]])
            Bs = Bsta[bi]
            ACT(sqs[:], bank(bk), AF.Square, [PB[bk]], [Bsq])
            RSUM(stat[:, 8 + cb * 24: 16 + cb * 24], V(sqs, 0, [[64, 8], [1, 64]]), [Bsq], [Bs])
            ACT(stat[:, 16 + cb * 24: 24 + cb * 24], stat[:, 8 + cb * 24: 16 + cb * 24], AF.Sqrt, [Bs], [Bs], scale=1.0 / 64, bias=EPS)
            RECIP(stat[:, 24 + cb * 24: 32 + cb * 24], stat[:, 16 + cb * 24: 24 + cb * 24], [Bs], [Bs])
            q_ = qn[cb]
            Bq = Bqn[cb]
            TT("dve", V(q_, 0, [[64, 8], [1, 64]]), bass.AP(pp, bk * 512, [[4096, 128], [64, 8], [1, 64]]), V(stat, 24 + cb * 24, [[1, 8], [0, 64]]), ALU.mult, [PB[bk], Bs], [Bq])
            TT("pool", q_[:], q_[:], (gq_t if cb == 0 else gk_t)[:], ALU.mult, [Bq, Bc], [Bq])
            r1 = V(q_, 0, [[64, 8], [1, 8]]); r2 = V(q_, 8, [[64, 8], [1, 8]])
            cs = V(cos_t, n * 8, [[0, 8], [1, 8]]); sn = V(sin_t, n * 8, [[0, 8], [1, 8]])
            ta = V(rtmp, 0, [[8, 8], [1, 8]]); tb = V(rtmp, 64, [[8, 8], [1, 8]]); tc = V(rtmp, 128, [[8, 8], [1, 8]]); td = V(rtmp, 192, [[8, 8], [1, 8]])
            TT("pool", ta, r1, cs, ALU.mult, [Bq, Bc], [Brt])
            TT("pool", tb, r2, sn, ALU.mult, [Bq, Bc], [Brt], add=True)
            TT("pool", tc, r2, cs, ALU.mult, [Bq, Bc], [Brt], add=True)
            TT("pool", td, r1, sn, ALU.mult, [Bq, Bc], [Brt], add=True)
            TT("pool", r1, ta, tb, ALU.subtract, [Brt, Bq], [Bq])
            TT("pool", r2, tc, td, ALU.add, [Brt, Bq], [Bq])
            CP("act", qb_[cb][:], q_[:], [Bq], [Bqb[cb]])
            bkt = 4 + cb
            for h in range(4):
                TR(bankb(bkt, h * 128, h * 128 + 128), qb_[cb][:, h * 128:(h + 1) * 128], ident_b[:], [Bqb[cb], Bc], [PB[bkt]], sig=(h == 3), add=(h > 0))
            dstT = qT if cb == 0 else kT
            dB = BqT[n // 4] if cb == 0 else BkT[n]
            CP("dve", V(dstT, n * 128, [[S, 4], [1, 128]]), bass.AP(ppb, bkt * 1024, [[8192, 128], [128, 4], [1, 128]]), [PB[bkt]], [dB], add=True)
    if debug:
        fw.barrier()
        for q_ in range(4):
            dst_ = A("stg_dbg_q_%d" % q_, [128, S], F32, R_O)
            CP("dve", dst_[:], qT[:, q_ * S:(q_ + 1) * S], BqT, [Bp])
            fw.dma("sp", dbg["dbg_q" + str(q_)], dst_[:], reads=[Bp], is_output=True)
        for q_ in range(4):
            dst_ = A("stg_dbg_k_%d" % q_, [128, S], F32, R_O)
            CP("dve", dst_[:], kT[:, q_ * S:(q_ + 1) * S], BkT, [Bp])
            fw.dma("sp", dbg["dbg_k" + str(q_)], dst_[:], reads=[Bp], is_output=True)
    fw.barrier()
    fw.barrier()

    if stop == 'pB':
        fw.finish()
        return nc
    oT = A("oT", [128, 4 * S], BF16, R_O)
    pT = [[A("pT%d%d" % (c, i), [128, 512], BF16, R_T + (c * 2 + i) * KB) for i in range(2)] for c in range(2)]
    BpT = [[Buf("pT%d%d" % (c, i)) for i in range(2)] for c in range(2)]
    of_ = [A("of%d" % i, [128, 128], F32, R_T + 4 * KB + i * 512) for i in range(2)]
    ob_ = [A("ob%d" % i, [128, 128], BF16, R_T + 5 * KB + i * 256) for i in range(2)]
    ajk = A("ajk", [128, 128], BF16, R_T + 6 * KB)
    Bof = [Buf("of0"), Buf("of1")]; Bob = [Buf("ob0"), Buf("ob1")]; Bast = [Buf("ast0"), Buf("ast1")]
    BoT = [Buf("oT%d" % i) for i in range(8)]
    Bajk = Buf("ajk")
    def accv(qs, c, a, b):
        idx = qs * 2 + c
        bk = 4 + idx // 3
        off = bk * 512 + (idx % 3) * 130
        return pp[:, off + a: off + b], PB[bk]
    fcount = 0
    for h in range(4):
        for qblk in range(8):
            nkt = 4 * qblk + 4
            for kt in range(nkt):
                q0 = max(0, kt - 4 * qblk)
                col0 = q0 * 128
                diag = kt >= 4 * qblk
                for c in range(2):
                    sb_ = c * 2 + kt % 2
                    MM(bank(sb_, col0, 512), VP(kT, c * 64, 64, h * S + kt * 128, [[1, 128]]), VP(qT, c * 64, 64, h * S + qblk * 512 + col0, [[1, 512 - col0]]),
                       True, not diag, [BkT[kt], BqT[qblk]], [PB[sb_]], sig=(not diag))
                    if diag:
                        MM(bank(sb_, col0, col0 + 128), ident_b[:], maskneg_b[:], False, True, [Bc], [PB[sb_]], sig=True, add=True)
                    p_ = pT[c][kt % 2]
                    ACT(p_[:, col0:512], bank(sb_, col0, 512), AF.Exp, [PB[sb_]], [BpT[c][kt % 2]], scale=0.125)
                    for qs in range(q0, 4):
                        av, ab = accv(qs, c, 0, 129)
                        last = (kt == 4 * qblk + qs)
                        first_in_bank = (kt == 0) and ((qs * 2 + c) in (0, 4, 6))
                        MM(av, p_[:, qs * 128:(qs + 1) * 128], V(v_aug, kt * 520 + h * 130, [[1, 129]]), first_in_bank, last, [BpT[c][kt % 2], Bv[kt]], [ab], sig=last, add=(kt > 0 or not first_in_bank), skip=True)
            for qs in range(4):
                fi = fcount % 2
                fcount += 1
                st = V(stat, 32 + fi * 8, [[1, 8]])
                so_ = 32 + fi * 8
                a0, ab0 = accv(qs, 0, 0, 128); d0, _ = accv(qs, 0, 128, 129)
                a1, ab1 = accv(qs, 1, 0, 128); d1, _ = accv(qs, 1, 128, 129)
                Bs = Bast[fi]
                RECIP(stat[:, so_:so_ + 1], d0, [ab0], [Bs])
                RECIP(stat[:, so_ + 1:so_ + 2], d1, [ab1], [Bs], add=True)
                TT("dve", stat[:, so_ + 2:so_ + 3], stat[:, so_ + 1:so_ + 2], lamv[:, 3:4], ALU.mult, [Bs, Bc], [Bs])
                TS("dve", of_[fi][:], a0, stat[:, so_:so_ + 1], None, ALU.mult, None, [ab0, Bs], [Bof[fi]])
                STT(of_[fi][:], a1, stat[:, so_ + 2:so_ + 3], of_[fi][:], ALU.mult, ALU.add, [ab1, Bs, Bof[fi]], [Bof[fi]])
                ACT(ajk[:], of_[fi][:], AF.Square, [Bof[fi]], [Bs, Bajk], accum_out=stat[:, so_ + 3:so_ + 4])
                ACT(stat[:, so_ + 4:so_ + 5], stat[:, so_ + 3:so_ + 4], AF.Sqrt, [Bs], [Bs], scale=1.0 / 128, bias=EPS)
                RECIP(stat[:, so_ + 5:so_ + 6], stat[:, so_ + 4:so_ + 5], [Bs], [Bs])
                STT(ob_[fi][:], of_[fi][:], stat[:, so_ + 5:so_ + 6], sg08_t[:], ALU.mult, ALU.mult, [Bof[fi], Bs, Bc], [Bob[fi]])
                TR(bankb(7, fi * 128, fi * 128 + 128), ob_[fi][:], ident_b[:], [Bob[fi], Bc], [PB[7]], sig=True, add=True)
                tok0 = qblk * 512 + qs * 128
                CP("act", oT[:, h * S + tok0: h * S + tok0 + 128], bankb(7, fi * 128, fi * 128 + 128), [PB[7]], [BoT[qblk]], add=True)
    if debug:
        fw.barrier()
        for q_ in range(4):
            dst_ = A("stg_dbg_o_%d" % q_, [128, S], F32, R_Q)
            CP("dve", dst_[:], oT[:, q_ * S:(q_ + 1) * S], BoT, [Bp])
            fw.dma("sp", dbg["dbg_o" + str(q_)], dst_[:], reads=[Bp], is_output=True)
    fw.barrier()

    if stop == 'att':
        fw.finish()
        return nc
    Wg_b = A("Wg_b", [128, 8 * 2048], BF16, R_Q)
    wglu_b = A("wglu_b", [128, 4 * 2048], BF16, R_K)
    wout_b = A("wout_b", [128, 8 * 1024], BF16, R_K + 16 * KB)
    wo_b = A("wo_b", [128, 4 * 1024], BF16, R_V)
    xc = [A("xc%d" % i, [128, D], F32, R_V + 8 * KB + i * 4 * KB) for i in range(4)]
    hT_blk = A("hT_blk", [128, 8 * 512], BF16, R_V + 24 * KB)
    mT = A("mT", [128, 8 * 512], BF16, R_T)
    sgA = A("sgA", [128, 512], F32, R_T + 8 * KB); sgB = A("sgB", [128, 512], F32, R_T + 10 * KB); sgE = A("sgE", [128, 512], F32, R_T + 12 * KB)
    tt1 = A("tt1", [128, 512], F32, R_T + 14 * KB); tt2 = A("tt2", [128, 512], F32, R_T + 16 * KB)
    hbc = A("hbc", [128, D], BF16, R_T + 18 * KB)
    cjk = hbc
    tT32 = A("tT32", [128, 8 * 128], F32, R_T + 8 * KB)
    BWc = Buf("Wc"); Bxc = [Buf("xc%d" % i) for i in range(4)]; BhTb = Buf("hTblk"); BmT = Buf("mT")
    BsA = Buf("sgA"); BsB = Buf("sgB"); BsE = Buf("sgE"); Bt1 = Buf("tt1"); Bt2 = Buf("tt2"); Bhbc = Buf("hbc"); Bcst = Buf("cst"); BtT32 = BsA
    Bcomb = Buf("comb")
    Bout = [Buf("out_h0"), Buf("out_h1")]
    fw.dma("pool", V(Wg_b, 0, [[2048, 8], [1, 2048]]), w_in_d[:, 2048:4096].rearrange("(k p) n -> p k n", p=128), writes=[BWc], add=True)
    fw.dma("pool", V(wglu_b, 0, [[2048, 4], [1, 2048]]), wglu_d.rearrange("(k p) n -> p k n", p=128), writes=[BWc], add=True)
    fw.dma("pool", V(wout_b, 0, [[1024, 8], [1, 1024]]), wout_d.rearrange("(k p) n -> p k n", p=128), writes=[BWc], add=True)
    fw.dma("pool", V(wo_b, 0, [[1024, 4], [1, 1024]]), wo_d.rearrange("(k p) n -> p k n", p=128), writes=[BWc], add=True)

    def tT_ap(k, tok0, n):
        base = gyT if k < 4 else oT
        return base[:, (k % 4) * S + tok0: (k % 4) * S + tok0 + n]

    for blk in range(8):
        t0 = blk * 512
        for i in range(4):
            n = blk * 4 + i
            fw.dma("sp", xc[i][:], x_d[n * 128:(n + 1) * 128, :], writes=[Bxc[i]])
            rms_tile(xc[i][:], hbc[:], cjk[:], V(stat, 48, [[1, 4]]), Bxc[i], Bhbc, Bcst, None)
            for k in range(8):
                TR(bankb(0, k * 128, k * 128 + 128), hbc[:, k * 128:(k + 1) * 128], ident_b[:], [Bhbc, Bc], [PB[0]], sig=(k == 7), add=(k > 0))
            CP("act", V(hT_blk, i * 128, [[512, 8], [1, 128]]), bass.AP(ppb, 0, [[8192, 128], [128, 8], [1, 128]]), [PB[0]], [BhTb], add=(i > 0))
        for nch in range(8):
            for (bk, col) in ((1, nch), (2, 8 + nch)):
                for k in range(8):
                    MM(bank(bk), Wg_b[:, k * 2048 + col * 128: k * 2048 + (col + 1) * 128], hT_blk[:, k * 512:(k + 1) * 512], k == 0, k == 7, [BWc, BhTb], [PB[bk]], sig=(k == 7), add=(k > 0))
            for f in range(4):
                MM(bank(3), wo_b[:, f * 1024 + nch * 128: f * 1024 + (nch + 1) * 128], oT[:, f * S + t0: f * S + t0 + 512], f == 0, f == 3, [BWc, BoT[blk]], [PB[3]], sig=(f == 3), add=(f > 0))
            for (bk, col) in ((4, nch), (5, 8 + nch)):
                for f in range(4):
                    MM(bank(bk), wglu_b[:, f * 2048 + col * 128: f * 2048 + (col + 1) * 128], gyT[:, f * S + t0: f * S + t0 + 512], f == 0, f == 3, [BWc, Bgy[blk]], [PB[bk]], sig=(f == 3), add=(f > 0))
            ACT(sgA[:], bank(1), AF.Sigmoid, [PB[1]], [BsA])
            ACT(sgB[:], bank(2), AF.Sigmoid, [PB[2]], [BsB])
            ACT(sgE[:], bank(5), AF.Sigmoid, [PB[5]], [BsE])
            TT("dve", tt1[:], bank(3), sgA[:], ALU.mult, [PB[3], BsA], [Bt1])
            TT("dve", tt2[:], bank(4), sgE[:], ALU.mult, [PB[4], BsE], [Bt2])
            TT("pool", tt2[:], tt2[:], sgB[:], ALU.mult, [Bt2, BsB], [Bt2])
            TT("pool", mT[:, nch * 512:(nch + 1) * 512], tt1[:], tt2[:], ALU.add, [Bt1, Bt2], [BmT], add=(nch > 0))
        for i in range(4):
            n = blk * 4 + i
            for half in range(2):
                bk = 6 + half
                for f in range(8):
                    MM(bank(bk), mT[:, f * 512 + i * 128: f * 512 + (i + 1) * 128], wout_b[:, f * 1024 + half * 512: f * 1024 + (half + 1) * 512], f == 0, f == 7, [BmT, BWc], [PB[bk]], sig=(f == 7), add=(f > 0))
                TT("dve", xc[i][:, half * 512:(half + 1) * 512], bank(bk), xc[i][:, half * 512:(half + 1) * 512], ALU.add, [PB[bk], Bxc[i]], [Bxc[i]], add=(half > 0))
            fw.dma("sp", out_d[n * 128:(n + 1) * 128, :], xc[i][:], reads=[Bxc[i]], writes=[Bout[n // 16]], add=True)
            if debug:
                fw.dma("sp", dbg["dbg_x1"][n * 128:(n + 1) * 128, :], xc[i][:], reads=[Bxc[i]], is_output=True)
            ACT(cjk[:], xc[i][:], AF.Square, [Bxc[i]], [Bcst, Bhbc], accum_out=stat[:, 52:53])
            ACT(stat[:, 53:54], stat[:, 52:53], AF.Sqrt, [Bcst], [Bcst], scale=1.0 / D, bias=EPS)
            RECIP(stat[:, 54:55], stat[:, 53:54], [Bcst], [Bcst])
            STT(xc[i][:], xc[i][:], stat[:, 54:55], gffn_t[:], ALU.mult, ALU.mult, [Bxc[i], Bcst, Bc], [Bxc[i]])
            for k in range(8):
                bk = k // 4
                TR(bank(bk, (k % 4) * 128, (k % 4) * 128 + 128), xc[i][:, k * 128:(k + 1) * 128], ident_f[:], [Bxc[i], Bc], [PB[bk]], sig=(k % 4 == 3), add=(k % 4 > 0))
            CP("act", tT32[:, 0:512], bank(0), [PB[0], BsA, BsB], [BsA, BsB])
            CP("act", tT32[:, 512:1024], bank(1), [PB[1]], [BsA, BsB], add=True)
            for k in range(8):
                CP("pool", tT_ap(k, n * 128, 128), tT32[:, k * 128:(k + 1) * 128], [BsA], [Bgy[blk], BoT[blk]], add=True)
            for k in range(8):
                MM(bank(2, 0, 36), tT32[:, k * 128:(k + 1) * 128], wr32[:, k * 36:(k + 1) * 36], k == 0, k == 7, [BsA, Bc], [PB[2]], sig=(k == 7), add=(k > 0))
            rt = tt1
            Br = Bt1
            lgt = rt[:, 0:36]
            TT("dve", lgt, bank(2, 0, 36), br_t[:], ALU.add, [PB[2], Bc], [Br])
            gmax = rt[:, 40:41]
            RMAX(gmax, rt[:, 0:4], [Br], [Br], add=True)
            oh = rt[:, 44:48]
            TS("dve", oh, rt[:, 0:4], gmax, None, ALU.is_equal, None, [Br], [Br], add=True)
            TS("dve", rt[:, 48:52], rt[:, 0:4], gmax, None, ALU.subtract, None, [Br], [Br], add=True)
            ACT(rt[:, 48:52], rt[:, 48:52], AF.Exp, [Br], [Br])
            RSUM(rt[:, 52:53], rt[:, 48:52], [Br], [Br], add=True)
            RECIP(rt[:, 53:54], rt[:, 52:53], [Br], [Br])
            TS("dve", rt[:, 56:64], rt[:, 4:12], rt[:, 44:45], None, ALU.mult, None, [Br], [Br], add=True)
            for g in range(1, 4):
                STT(rt[:, 56:64], rt[:, 4 + g * 8: 12 + g * 8], rt[:, 44 + g: 45 + g], rt[:, 56:64], ALU.mult, ALU.add, [Br], [Br])
            m1 = rt[:, 64:65]
            RMAX(m1, rt[:, 56:64], [Br], [Br], add=True)
            mk1 = rt[:, 72:80]
            TS("dve", mk1, rt[:, 56:64], m1, None, ALU.is_equal, None, [Br], [Br], add=True)
            es2 = rt[:, 80:88]
            STT(es2, mk1, -1e30, rt[:, 56:64], ALU.mult, ALU.add, [Br], [Br], add=True)
            m2 = rt[:, 65:66]
            RMAX(m2, es2, [Br], [Br], add=True)
            mk2 = rt[:, 88:96]
            TS("dve", mk2, es2, m2, None, ALU.is_equal, None, [Br], [Br], add=True)
            TT("dve", rt[:, 66:67], m2, m1, ALU.subtract, [Br], [Br], add=True)
            ACT(rt[:, 66:67], rt[:, 66:67], AF.Exp, [Br], [Br])
            TS("dve", rt[:, 67:68], rt[:, 66:67], 1.0, None, ALU.add, None, [Br], [Br], add=True)
            RECIP(rt[:, 68:69], rt[:, 67:68], [Br], [Br])
            TS("dve", rt[:, 69:70], rt[:, 68:69], -1.0, 1.0, ALU.mult, ALU.add, [Br], [Br], add=True)
            TT("dve", rt[:, 68:69], rt[:, 68:69], rt[:, 53:54], ALU.mult, [Br], [Br])
            TT("dve", rt[:, 69:70], rt[:, 69:70], rt[:, 53:54], ALU.mult, [Br], [Br])
            ew = rt[:, 96:104]
            TS("dve", ew, mk1, rt[:, 68:69], None, ALU.mult, None, [Br], [Br], add=True)
            STT(ew, mk2, rt[:, 69:70], ew, ALU.mult, ALU.add, [Br], [Br])
            for g in range(4):
                TS("dve", comb_all[:, n * 32 + g * 8: n * 32 + (g + 1) * 8], ew, rt[:, 44 + g:45 + g], None, ALU.mult, None, [Br], [Bcomb], add=True)
    if debug:
        fw.dma("sp", dbg["dbg_comb"], comb_all[:], reads=[Bcomb], is_output=True)
    fw.barrier()

    if stop == 'pC':
        fw.finish()
        return nc
    acc = A("acc", [128, 16 * D], F32, R_Q)
    NWB = 3
    wgu = [A("wgu%d" % i, [128, 8 * 512], BF16, R_V + i * 12 * KB) for i in range(NWB)]
    wd_ = [A("wd%d" % i, [128, 2 * 1024], BF16, R_V + i * 12 * KB + 8 * KB) for i in range(NWB)]
    Bw = [Buf("w%d" % i) for i in range(NWB)]
    sgm = [A("sgm%d" % i, [128, 256], F32, R_V + 36 * KB + i * KB) for i in range(2)]
    hid = [A("hid%d" % i, [128, 256], BF16, R_V + 38 * KB + i * 512) for i in range(2)]
    hidT = [A("hidT%d" % i, [128, 256], BF16, R_V + 39 * KB + i * 512) for i in range(2)]
    Bsg = [Buf("sgm0"), Buf("sgm1")]; Bhid = [Buf("hid0"), Buf("hid1")]; BhidT = [Buf("hidT0"), Buf("hidT1")]
    Bacc = [Buf("acc%d" % i) for i in range(16)]
    BtT = Bgy + BoT
    it = 0
    for hf in range(2):
        for i in range(16):
            n = hf * 16 + i
            fw.dma("sp", acc[:, i * D:(i + 1) * D], out_d[n * 128:(n + 1) * 128, :], reads=[Bout[hf]], writes=[Bacc[i]])
        for e in range(32):
            wi = (hf * 32 + e) % NWB
            fw.dma("pool", V(wgu[wi], 0, [[512, 8], [1, 256]]), weg_d[e].rearrange("(k p) f -> p k f", p=128), writes=[Bw[wi]])
            fw.dma("pool", V(wgu[wi], 256, [[512, 8], [1, 256]]), weu_d[e].rearrange("(k p) f -> p k f", p=128), writes=[Bw[wi]], add=True)
            fw.dma("pool", V(wd_[wi], 0, [[1024, 2], [1, 1024]]), wed_d[e].rearrange("(k p) n -> p k n", p=128), writes=[Bw[wi]], add=True)
            for i in range(16):
                n = hf * 16 + i
                bi = it % 2
                it += 1
                bk = bi
                for k in range(8):
                    MM(bank(bk), tT_ap(k, n * 128, 128), wgu[wi][:, k * 512:(k + 1) * 512], k == 0, k == 7, [BtT[n // 4], BtT[8 + n // 4], Bw[wi]], [PB[bk]], sig=(k == 7), add=(k > 0))
                ACT(sgm[bi][:], bank(bk, 0, 256), AF.Silu, [PB[bk]], [Bsg[bi]])
                STT(hid[bi][:], bank(bk, 256, 512), comb_all[:, n * 32 + e: n * 32 + e + 1], sgm[bi][:], ALU.mult, ALU.mult, [PB[bk], Bcomb, Bsg[bi]], [Bhid[bi]])
                for f in range(2):
                    TR(bankb(2 + bi, f * 128, f * 128 + 128), hid[bi][:, f * 128:(f + 1) * 128], ident_b[:], [Bhid[bi], Bc], [PB[2 + bi]], sig=(f == 1), add=(f > 0))
                CP("act", hidT[bi][:], bankb(2 + bi, 0, 256), [PB[2 + bi]], [BhidT[bi]])
                for half in range(2):
                    bkd = 4 + bi * 2 + half
                    for f in range(2):
                        MM(bank(bkd), hidT[bi][:, f * 128:(f + 1) * 128], wd_[wi][:, f * 1024 + half * 512: f * 1024 + (half + 1) * 512], f == 0, f == 1, [BhidT[bi], Bw[wi]], [PB[bkd]], sig=(f == 1), add=(f > 0))
                    TT("dve", acc[:, i * D + half * 512: i * D + (half + 1) * 512], bank(bkd), acc[:, i * D + half * 512: i * D + (half + 1) * 512], ALU.add, [PB[bkd], Bacc[i]], [Bacc[i]], add=(half > 0))
        for i in range(16):
            n = hf * 16 + i
            fw.dma("sp", out_d[n * 128:(n + 1) * 128, :], acc[:, i * D:(i + 1) * D], reads=[Bacc[i]], writes=[Bout[hf]], add=True, is_output=True)
    fw.finish()
    return nc


_NC_CACHE = {}


def _prep_inputs(inputs, b):
    f = lambda a: np.ascontiguousarray(a, dtype=np.float32)
    m = {
        "x": f(inputs["x"][b]),
        "pos": np.ascontiguousarray(inputs["positions"][b].reshape(NT, 128).T.astype(np.int32)),
        "norm_mix_g": f(inputs["norm_mix_g"][0]),
        "w_in": f(inputs["w_in"][0]),
        "q_norm_g": f(inputs["q_norm_g"][0]), "k_norm_g": f(inputs["k_norm_g"][0]),
        "lambda_q1": f(inputs["lambda_q1"][0]), "lambda_k1": f(inputs["lambda_k1"][0]),
        "lambda_q2": f(inputs["lambda_q2"][0]), "lambda_k2": f(inputs["lambda_k2"][0]),
        "subln_g": f(inputs["subln_g"][0]),
        "w_o_attn": f(inputs["w_o_attn"][0]),
        "lamre_t": f(inputs["ssm_lambda_re"][0].T), "lamim_t": f(inputs["ssm_lambda_im"][0].T),
        "ssm_log_dt": f(inputs["ssm_log_dt"][0]),
        "bre_t": f(inputs["ssm_b_re"][0].transpose(1, 0, 2).reshape(64, 512)),
        "bim_t": f(inputs["ssm_b_im"][0].transpose(1, 0, 2).reshape(64, 512)),
        "cre_t": f(inputs["ssm_c_re"][0].transpose(2, 0, 1).reshape(64, 512)),
        "cim_t": f(inputs["ssm_c_im"][0].transpose(2, 0, 1).reshape(64, 512)),
        "d_t": f(inputs["ssm_d"][0].reshape(32, 16).T),
        "w_glu": f(inputs["w_glu"][0]),
        "w_out": f(inputs["w_out"][0]),
        "norm_ffn_g": f(inputs["norm_ffn_g"][0]),
        "w_router": f(np.concatenate([inputs["w_router_group"][0], inputs["w_router_expert"][0].reshape(D, 32)], axis=1)),
        "b_router": f(np.concatenate([inputs["b_router_group"][0], inputs["b_router_expert"][0].reshape(32)])),
        "w_expert_gate": f(inputs["w_expert_gate"][0].reshape(32, D, 256)),
        "w_expert_up": f(inputs["w_expert_up"][0].reshape(32, D, 256)),
        "w_expert_down": f(inputs["w_expert_down"][0].reshape(32, 256, D)),
    }
    return m


def kernel(**inputs):
    inputs = {k: np.asarray(v) for k, v in inputs.items()}
    nb = inputs["x"].shape[0]
    nc = build_program(debug=False)
    shared = _prep_inputs(inputs, 0)
    in_maps = []
    for b in range(nb):
        m = dict(shared)
        m["x"] = np.ascontiguousarray(inputs["x"][b], dtype=np.float32)
        m["pos"] = np.ascontiguousarray(inputs["positions"][b].reshape(NT, 128).T.astype(np.int32))
        in_maps.append(m)
    res = run_bass_kernel_spmd(nc, in_maps, core_ids=list(range(nb)))
    out = np.stack([np.asarray(r["out"]).reshape(S, D) for r in res.results], axis=0)
    return out.astype(np.float32)
```

```python
import contextlib
import math
import os
import numpy as np
import concourse.bass as bass
import concourse.mybir as mybir
from concourse.bass_utils import run_bass_kernel_spmd

F32 = mybir.dt.float32
BF16 = mybir.dt.bfloat16
I32 = mybir.dt.int32
AF = mybir.ActivationFunctionType
ALU = mybir.AluOpType
AX = mybir.AxisListType

SEM_LIMIT = 30000
S = 4096
D = 1024
NT = 32
EPS = 1e-6
SB_BASE = 17408
LAM_INIT = 0.8 - 0.6 * math.exp(-0.3 * 0)


class Buf:
    __slots__ = ("name", "w", "r", "pr")

    def __init__(self, name=""):
        self.name = name
        self.w = {}
        self.r = {}
        self.pr = {}


class Eng:
    def __init__(self, name):
        self.name = name
        self.ops = []
        self.cnt = 0
        self.semidx = 0
        self.waited = {}
        self.pending = []
        self.last = None

    @property
    def semkey(self):
        return "%s_%d" % (self.name, self.semidx)


class FW:
    def __init__(self, nc, n_dma_sems=32):
        self.nc = nc
        self.stack = contextlib.ExitStack()
        self.engs = {n: Eng(n) for n in ("pe", "act", "dve", "pool", "sp")}
        self.sems = {}
        names = ["dma%d" % i for i in range(n_dma_sems)]
        self.dma_sem_val = {n: 0 for n in names}
        self.dma_rr = {"sp": 0, "pool": 0}
        k = n_dma_sems // 2
        self.dma_pool_of = {"sp": names[:k], "pool": names[k:]}
        self.out_tokens = []

    def sem(self, key):
        if key not in self.sems:
            self.sems[key] = self.stack.enter_context(self.nc.semaphore(key))
        return self.sems[key]

    def psum(self, name, shape, dt):
        return self.stack.enter_context(self.nc.psum_tensor(name, list(shape), dt))

    def _wait(self, eng, tok):
        key, val = tok[0], tok[1]
        assert val is not None, "wait on unsignalled token"
        if eng.waited.get(key, 0) >= val:
            return
        eng.waited[key] = val
        self.sem(key)
        eng.ops.append(("wait", key, val))

    def _deps(self, eng, reads, writes, add):
        toks = []
        for b in reads:
            toks.extend(b.w.values())
        for b in writes:
            if add:
                toks.extend(b.pr.values())
            else:
                toks.extend(b.w.values())
            toks.extend(b.r.values())
        for t in toks:
            if eng.name == "pe" and t[0].startswith("pe_"):
                continue
            self._wait(eng, t)

    def _update(self, key, tok, reads, writes, add):
        for b in reads:
            b.r[key] = tok
        for b in writes:
            if add:
                for k_, v_ in b.r.items():
                    b.pr["r:" + k_] = v_
                b.w[key] = tok
            else:
                npr = {}
                for k_, v_ in b.w.items():
                    npr["w:" + k_] = v_
                for k_, v_ in b.r.items():
                    npr["r:" + k_] = v_
                b.pr = npr
                b.w = {key: tok}
            b.r = {}

    def op(self, engname, fn, reads=(), writes=(), sig=True, add=False):
        eng = self.engs[engname]
        self._deps(eng, reads, writes, add)
        if sig:
            if eng.cnt >= SEM_LIMIT:
                eng.semidx += 1
                eng.cnt = 0
            eng.cnt += 1
            tok = [eng.semkey, eng.cnt]
            self.sem(tok[0])
            for p in eng.pending:
                p[0], p[1] = tok[0], tok[1]
            eng.pending = []
            eng.last = tok
            key = tok[0]
        else:
            tok = ["pe_pending", None]
            eng.pending.append(tok)
            key = "pe_pend"
        eng.ops.append(("op", fn, tok if sig else None))
        self._update(key, tok, reads, writes, add)
        return tok

    def dma(self, qname, out, in_, reads=(), writes=(), add=False, is_output=False, **kw):
        eng = self.engs[qname]
        self._deps(eng, reads, writes, add)
        pool = self.dma_pool_of[qname]
        name = pool[self.dma_rr[qname] % len(pool)]
        self.dma_rr[qname] += 1
        prev = self.dma_sem_val[name]
        if prev > 0:
            self._wait(eng, [name, prev])
        val = prev + 16
        self.dma_sem_val[name] = val
        tok = [name, val]
        self.sem(name)

        def fn(e, out=out, in_=in_, kw=kw):
            return e.dma_start(out=out, in_=in_, **kw)
        eng.ops.append(("op", fn, tok))
        self._update(name, tok, reads, writes, add)
        if is_output:
            self.out_tokens.append(tok)
        return tok

    def barrier(self):
        toks = [e.last for e in self.engs.values() if e.last is not None]
        for e in self.engs.values():
            assert not e.pending
        toks += [[n, v] for n, v in self.dma_sem_val.items() if v > 0]
        for e in self.engs.values():
            for t in toks:
                if e.name == "pe" and t[0].startswith("pe_"):
                    continue
                self._wait(e, t)

    def finish(self):
        sp = self.engs["sp"]
        for t in self.out_tokens:
            self._wait(sp, t)
        nc = self.nc
        sems = self.sems
        engs = self.engs

        def replay(e, eng):
            for o in eng.ops:
                if o[0] == "wait":
                    e.wait_ge(sems[o[1]], o[2])
                else:
                    inst = o[1](e)
                    if o[2] is not None:
                        key = o[2][0]
                        inst.then_inc(sems[key], 16 if key.startswith("dma") else 1)

        with nc.Block() as block:
            @block.tensor
            def _(e):
                replay(e, engs["pe"])

            @block.scalar
            def _(e):
                replay(e, engs["act"])

            @block.vector
            def _(e):
                replay(e, engs["dve"])

            @block.gpsimd
            def _(e):
                replay(e, engs["pool"])

            @block.sync
            def _(e):
                replay(e, engs["sp"])
        self.stack.close()


def build_program(debug=False, stop=None):
    nc = bass.Bass("TRN2", target_bir_lowering=False)
    fw = FW(nc)

    def din(name, shape, dt=F32):
        return nc.dram_tensor(name, list(shape), dt, kind="ExternalInput")

    x_d = din("x", [S, D]).ap()
    pos_d = din("pos", [128, NT], I32).ap()
    gmix_d = din("norm_mix_g", [D])
    w_in_d = din("w_in", [D, 4096]).ap()
    qg_d = din("q_norm_g", [64])
    kg_d = din("k_norm_g", [64])
    lq1_d = din("lambda_q1", [64]); lk1_d = din("lambda_k1", [64])
    lq2_d = din("lambda_q2", [64]); lk2_d = din("lambda_k2", [64])
    subg_d = din("subln_g", [128])
    wo_d = din("w_o_attn", [512, D]).ap()
    lamre_d = din("lamre_t", [64, 32]).ap(); lamim_d = din("lamim_t", [64, 32]).ap()
    logdt_d = din("ssm_log_dt", [32])
    bre_d = din("bre_t", [64, 512]).ap(); bim_d = din("bim_t", [64, 512]).ap()
    cre_d = din("cre_t", [64, 512]).ap(); cim_d = din("cim_t", [64, 512]).ap()
    dsk_d = din("d_t", [16, 32]).ap()
    wglu_d = din("w_glu", [512, 2048]).ap()
    wout_d = din("w_out", [D, D]).ap()
    gffn_d = din("norm_ffn_g", [D])
    wr_d = din("w_router", [D, 36]).ap()
    br_d = din("b_router", [36])
    weg_d = din("w_expert_gate", [32, D, 256]).ap()
    weu_d = din("w_expert_up", [32, D, 256]).ap()
    wed_d = din("w_expert_down", [32, 256, D]).ap()
    out_d = nc.dram_tensor("out", [S, D], F32, kind="ExternalOutput").ap()
    dbg = {}
    if debug:
        lst = [("dbg_x1", [S, D]), ("dbg_comb", [128, NT * 32]), ("dbg_T", [128, 4096]), ("dbg_ks", [128, 16 * 18]), ("dbg_ug", [128, 32 * 512])]
        for nm_ in ("dbg_gy", "dbg_o", "dbg_q", "dbg_k"):
            lst += [(nm_ + str(q_), [128, S]) for q_ in range(4)]
        for nm, shp in lst:
            dbg[nm] = nc.dram_tensor(nm, shp, F32, kind="ExternalOutput").ap()

    def bc_rows(t, n, reps=1, parts=128):
        if reps == 1:
            return bass.AP(t, 0, [[0, parts], [1, n]])
        return bass.AP(t, 0, [[0, parts], [0, reps], [1, n]])

    KB = 1024

    def A(name, shape, dt, off):
        nbytes = int(np.prod(shape[1:])) * (2 if dt == BF16 else 4)
        assert SB_BASE + off + nbytes <= 229376 - 32, (name, off, nbytes)
        return nc.alloc_sbuf_tensor_at(name, list(shape), dt, offset=SB_BASE + off)

    c_off = [0]

    def CA(name, shape, dt):
        n = int(np.prod(shape[1:])) * (2 if dt == BF16 else 4)
        t = A(name, shape, dt, c_off[0])
        c_off[0] += (n + 31) // 32 * 32
        return t

    ident_f = CA("ident_f", [128, 128], F32)
    ident_b = CA("ident_b", [128, 128], BF16)
    maskf = CA("maskf", [128, 128], F32)
    maskneg_b = CA("maskneg_b", [128, 128], BF16)
    gq_t = CA("gq_t", [128, 512], F32)
    gk_t = CA("gk_t", [128, 512], F32)
    sg08_t = CA("sg08_t", [128, 128], F32)
    gmix_t = CA("gmix_t", [128, D], F32)
    gffn_t = CA("gffn_t", [128, D], F32)
    cos_t = CA("cos_t", [128, NT * 8], F32)
    sin_t = CA("sin_t", [128, NT * 8], F32)
    lamv = CA("lamv", [128, 8], F32)
    comb_all = CA("comb_all", [128, NT * 32], F32)
    wr32 = CA("wr32", [128, 8 * 36], F32)
    br_t = CA("br_t", [128, 36], F32)
    ks_are = CA("ks_are", [128, 16 * 9], F32)
    ks_aim = CA("ks_aim", [128, 16 * 9], F32)
    ks_naim = CA("ks_naim", [128, 16 * 9], F32)
    dvec = CA("dvec", [128, 32], F32)
    stat = CA("stat", [128, 64], F32)
    assert c_off[0] <= 24 * KB, c_off[0]
    M0 = 24 * KB
    R_G, R_O, R_Q, R_K, R_V, R_T = M0, M0 + 32 * KB, M0 + 64 * KB, M0 + 96 * KB, M0 + 128 * KB, M0 + 161 * KB
    R_END = 229376 - SB_BASE - 64

    pp = fw.psum("pp", [128, 8 * 512], F32)
    ppb = pp.bitcast(BF16)
    PB = [Buf("psum%d" % i) for i in range(8)]

    def bank(i, a=0, b=512):
        return pp[:, i * 512 + a:i * 512 + b]

    def bankb(i, a=0, b=1024):
        return ppb[:, i * 1024 + a:i * 1024 + b]

    def MM(out, lhsT, rhs, start, stop, r, w, sig=True, add=False, skip=False):
        if skip:
            return fw.op("pe", lambda e: e.matmul(out, lhsT, rhs, start=start, stop=stop, skip_group_check=True), reads=r, writes=w, sig=sig, add=add)
        return fw.op("pe", lambda e: e.matmul(out, lhsT, rhs, start=start, stop=stop), reads=r, writes=w, sig=sig, add=add)

    def TR(out, in_, ident, r, w, sig=True, add=False):
        return fw.op("pe", lambda e: e.transpose(out, in_, ident), reads=r, writes=w, sig=sig, add=add)

    def ACT(out, in_, func, r, w, add=False, **kw):
        return fw.op("act", lambda e: e.activation(out, in_, func, **kw), reads=r, writes=w, add=add)

    def TT(eng, out, in0, in1, op, r, w, add=False):
        return fw.op(eng, lambda e: e.tensor_tensor(out, in0, in1, op), reads=r, writes=w, add=add)

    def TS(eng, out, in0, s1, s2, op0, op1, r, w, add=False):
        if s2 is None:
            return fw.op(eng, lambda e: e.tensor_scalar(out, in0, s1, None, op0), reads=r, writes=w, add=add)
        return fw.op(eng, lambda e: e.tensor_scalar(out, in0, s1, s2, op0, op1), reads=r, writes=w, add=add)

    def STT(out, in0, sc, in1, op0, op1, r, w, add=False):
        return fw.op("dve", lambda e: e.scalar_tensor_tensor(out, in0, sc, in1, op0, op1), reads=r, writes=w, add=add)

    def CP(eng, out, in_, r, w, add=False):
        if eng == "act":
            return ACT(out, in_, AF.Copy, r, w, add=add)
        return fw.op(eng, lambda e: e.tensor_copy(out, in_), reads=r, writes=w, add=add)

    def RECIP(out, in_, r, w, add=False):
        return fw.op("dve", lambda e: e.reciprocal(out, in_), reads=r, writes=w, add=add)

    def RSUM(out, in_, r, w, add=False):
        return fw.op("dve", lambda e: e.reduce_sum(out, in_, axis=AX.X), reads=r, writes=w, add=add)

    def RMAX(out, in_, r, w, add=False):
        return fw.op("dve", lambda e: e.reduce_max(out, in_, axis=AX.X), reads=r, writes=w, add=add)

    def MEMSET(eng, out, val, r, w, add=False):
        return fw.op(eng, lambda e: e.memset(out, val), reads=r, writes=w, add=add)

    def V(t, off, dims):
        pstride = int(np.prod(t.shape[1:]))
        return bass.AP(t, off, [[pstride, t.shape[0]]] + [list(d) for d in dims])

    def VP(t, p0, pn, off, dims):
        pstride = int(np.prod(t.shape[1:]))
        return bass.AP(t, p0 * pstride + off, [[pstride, pn]] + [list(d) for d in dims])

    Bc = Buf("consts")
    MEMSET("pool", ident_f[:], 1.0, [], [Bc])
    fw.op("pool", lambda e: e.affine_select(ident_f[:], ident_f[:], pattern=[[-1, 128]], compare_op=ALU.is_equal, fill=0.0, base=0, channel_multiplier=1), reads=[Bc], writes=[Bc])
    CP("pool", ident_b[:], ident_f[:], [Bc], [Bc])
    MEMSET("pool", maskf[:], 0.0, [Bc], [Bc])
    fw.op("pool", lambda e: e.affine_select(maskf[:], maskf[:], pattern=[[1, 128]], compare_op=ALU.is_ge, fill=-30000.0, base=0, channel_multiplier=-1), reads=[Bc], writes=[Bc])
    CP("pool", maskneg_b[:], maskf[:], [Bc], [Bc])
    fw.dma("sp", gq_t[:], bc_rows(qg_d, 64, 8), writes=[Bc], add=True)
    fw.dma("sp", gk_t[:], bc_rows(kg_d, 64, 8), writes=[Bc], add=True)
    fw.dma("sp", sg08_t[:], bc_rows(subg_d, 128), writes=[Bc], add=True)
    fw.dma("sp", gmix_t[:], bc_rows(gmix_d, D), writes=[Bc], add=True)
    fw.dma("sp", gffn_t[:], bc_rows(gffn_d, D), writes=[Bc], add=True)
    fw.dma("sp", br_t[:], bc_rows(br_d, 36), writes=[Bc], add=True)
    fw.dma("sp", wr32[:], wr_d.rearrange("(k p) n -> p k n", p=128), writes=[Bc], add=True)
    for hh in range(8):
        fw.dma("sp", dvec[hh * 16:(hh + 1) * 16, :], dsk_d, writes=[Bc], add=True)
    tmp0 = A("c_tmp0", [128, 4 * 64], F32, R_T)
    posi = A("c_posi", [128, NT], I32, R_T + 1 * KB)
    posf = A("c_posf", [128, NT], F32, R_T + 1 * KB + 128)
    ang = A("c_ang", [128, NT * 8], F32, R_T + 2 * KB)
    ang2 = A("c_ang2", [128, NT * 8], F32, R_T + 3 * KB)
    ang3 = A("c_ang3", [128, NT * 8], F32, R_T + 4 * KB)
    Bt = Buf("ctmp")
    for i, dd in enumerate((lq1_d, lk1_d, lq2_d, lk2_d)):
        fw.dma("sp", tmp0[:, i * 64:(i + 1) * 64], bc_rows(dd, 64), writes=[Bt], add=True)
    fw.dma("sp", posi[:], pos_d, writes=[Bt], add=True)
    TS("dve", sg08_t[:], sg08_t[:], 1.0 - LAM_INIT, None, ALU.mult, None, [Bc], [Bc])
    TT("dve", tmp0[:, 0:64], tmp0[:, 0:64], tmp0[:, 64:128], ALU.mult, [Bt], [Bt])
    TT("dve", tmp0[:, 128:192], tmp0[:, 128:192], tmp0[:, 192:256], ALU.mult, [Bt], [Bt])
    RSUM(lamv[:, 0:1], tmp0[:, 0:64], [Bt], [Bc])
    RSUM(lamv[:, 1:2], tmp0[:, 128:192], [Bt], [Bc])
    ACT(lamv[:, 0:2], lamv[:, 0:2], AF.Exp, [Bc], [Bc])
    TT("dve", lamv[:, 2:3], lamv[:, 1:2], lamv[:, 0:1], ALU.subtract, [Bc], [Bc])
    TS("dve", lamv[:, 3:4], lamv[:, 2:3], -LAM_INIT, None, ALU.add, None, [Bc], [Bc])
    CP("dve", posf[:], posi[:], [Bt], [Bt])
    for i in range(8):
        inv = (500000.0 ** (-i / 8.0)) / (2.0 * math.pi)
        TS("dve", V(ang, i, [[8, NT]]), posf[:], inv, None, ALU.mult, None, [Bt], [Bt], add=True)
    MAGIC = 12582912.0
    for (dst, shift) in ((sin_t, 0.0), (cos_t, 0.25)):
        TS("dve", ang2[:], ang[:], shift, None, ALU.add, None, [Bt], [Bt])
        TS("dve", ang3[:], ang2[:], MAGIC, MAGIC, ALU.add, ALU.subtract, [Bt], [Bt])
        TT("dve", ang2[:], ang2[:], ang3[:], ALU.subtract, [Bt], [Bt])
        ACT(dst[:], ang2[:], AF.Sin, [Bt], [Bc], scale=6.283185)

    if stop == 'p0a':
        fw.finish()
        return nc
    T_b = A("T_b", [128, 32 * 128], BF16, R_Q)
    VTre_b = A("VTre_b", [128, 32 * 64], BF16, R_Q + 8 * KB)
    VTim_b = A("VTim_b", [128, 32 * 64], BF16, R_Q + 12 * KB)
    Wre_b = A("Wre_b", [128, 32 * 128], BF16, R_Q + 16 * KB)
    Wimn_b = A("Wimn_b", [128, 32 * 128], BF16, R_Q + 24 * KB)
    Bs5w = Buf("s5w")
    Gre = A("Gre_", [128, 4096], F32, M0 + 0)
    Gim = A("Gim_", [128, 4096], F32, M0 + 16 * KB)
    HHre = A("HHre_", [128, 32 * 144], F32, M0 + 32 * KB)
    HHim = A("HHim_", [128, 32 * 144], F32, M0 + 96 * KB)
    VVre = A("VVre_", [128, 4096], F32, M0 + 114 * KB)
    VVim = A("VVim_", [128, 4096], F32, M0 + 130 * KB)
    GS = A("GS_", [128, 4096], F32, M0 + 146 * KB)
    HS = A("HS_", [128, 4096], F32, M0 + 162 * KB)
    so = [M0 + 50 * KB]

    def SA(name, n):
        t = A(name, [128, n], F32, so[0])
        so[0] += n * 4
        return t
    lre = SA("lre", 32); lim = SA("lim", 32); dtt = SA("dtt", 32); ar = SA("ar", 32); ai = SA("ai", 32)
    mm_ = SA("mm_", 32); minv = SA("minv", 32); kk = SA("kk", 32); rr = SA("rr", 32); x8 = SA("x8", 32); x2 = SA("x2", 32)
    pp_ = SA("pp_", 32); cc = SA("cc", 32); ss_ = SA("ss_", 32); t1 = SA("t1", 32); t2 = SA("t2", 32); t3 = SA("t3", 32); t4 = SA("t4", 32)
    LPre = SA("LPre", 32 * 9); LPim = SA("LPim", 32 * 9); LIre = SA("LIre", 32 * 8); LIim = SA("LIim", 32 * 8)
    Are = SA("Are", 32 * 9); Aim = SA("Aim", 32 * 9)
    fre = SA("fre", 32); fim = SA("fim", 32); ire = SA("ire", 32); iim = SA("iim", 32); den = SA("den", 32)
    assert so[0] <= M0 + 64 * KB
    Bin = A("Bin_re", [128, 512], F32, M0 + 178 * KB)
    Bin_im = A("Bin_im", [128, 512], F32, M0 + 180 * KB)
    Cre = A("Cre_in", [128, 512], F32, M0 + 146 * KB)
    Cim = A("Cim_in", [128, 512], F32, M0 + 148 * KB)
    Bbre = A("Bbre", [128, 512], F32, M0 + 150 * KB)
    Bbim = A("Bbim", [128, 512], F32, M0 + 152 * KB)
    W1 = A("W1", [128, 4608], F32, M0 + 114 * KB)
    W2 = A("W2_", [128, 4608], F32, M0 + 154 * KB)
    Bp = Buf("s5prep")

    for half in range(2):
        ps_ = slice(half * 64, half * 64 + 64)
        fw.dma("sp", lre[ps_, :], lamre_d, writes=[Bp], add=True)
        fw.dma("sp", lim[ps_, :], lamim_d, writes=[Bp], add=True)
        fw.dma("sp", Bin[ps_, :], bre_d, writes=[Bp], add=True)
        fw.dma("sp", Bin_im[ps_, :], bim_d, writes=[Bp], add=True)
        fw.dma("sp", Cre[ps_, :], cre_d, writes=[Bp], add=True)
        fw.dma("sp", Cim[ps_, :], cim_d, writes=[Bp], add=True)
    fw.dma("sp", dtt[:], bc_rows(logdt_d, 32), writes=[Bp], add=True)

    def d_tt(out, a, b, op):
        return TT("dve", out, a, b, op, [Bp], [Bp])

    def d_ts(out, a, s1, s2=None, op0=ALU.mult, op1=ALU.add):
        return TS("dve", out, a, s1, s2, op0, op1, [Bp], [Bp])

    def cmul(ore, oim, are_, aim_, bre_, bim_, ta, tb):
        d_tt(ta, are_, bre_, ALU.mult)
        d_tt(tb, aim_, bim_, ALU.mult)
        d_tt(ore, ta, tb, ALU.subtract)
        d_tt(ta, are_, bim_, ALU.mult)
        d_tt(tb, aim_, bre_, ALU.mult)
        d_tt(oim, ta, tb, ALU.add)

    ACT(dtt[:], dtt[:], AF.Exp, [Bp], [Bp])
    d_tt(ar[:], lre[:], dtt[:], ALU.mult)
    d_tt(ai[:], lim[:], dtt[:], ALU.mult)
    MEMSET("dve", mm_[:], 1.0, [Bp], [Bp])
    for k in range(10, 0, -1):
        d_tt(mm_[:], mm_[:], ar[:], ALU.mult)
        d_ts(mm_[:], mm_[:], 1.0 / k, 1.0)
    RECIP(minv[:], mm_[:], [Bp], [Bp])
    d_ts(kk[:], ai[:], 1.0 / (2.0 * math.pi), None)
    d_ts(kk[:], kk[:], MAGIC, MAGIC, ALU.add, ALU.subtract)
    STT(rr[:], kk[:], -6.28125, ai[:], ALU.mult, ALU.add, [Bp], [Bp])
    STT(rr[:], kk[:], -(2.0 * math.pi - 6.28125), rr[:], ALU.mult, ALU.add, [Bp], [Bp])
    d_ts(x8[:], rr[:], 0.125, None)
    d_tt(x2[:], x8[:], x8[:], ALU.mult)
    sc_ = [1.0, -1.0 / 6, 1.0 / 120, -1.0 / 5040, 1.0 / 362880, -1.0 / 39916800]
    cc_ = [1.0, -0.5, 1.0 / 24, -1.0 / 720, 1.0 / 40320, -1.0 / 3628800, 1.0 / 479001600]
    for (dst, co) in ((ss_, sc_), (cc, cc_)):
        MEMSET("dve", dst[:], co[-1], [Bp], [Bp])
        for c in co[-2::-1]:
            d_tt(dst[:], dst[:], x2[:], ALU.mult)
            d_ts(dst[:], dst[:], c, None, ALU.add)
    d_tt(ss_[:], ss_[:], x8[:], ALU.mult)
    for _ in range(3):
        d_tt(t1[:], cc[:], cc[:], ALU.mult)
        d_tt(t2[:], ss_[:], ss_[:], ALU.mult)
        STT(t3[:], ss_[:], 2.0, cc[:], ALU.mult, ALU.mult, [Bp], [Bp])
        d_tt(cc[:], t1[:], t2[:], ALU.subtract)
        CP("dve", ss_[:], t3[:], [Bp], [Bp])
    def LPv(t, j):
        return V(t, j, [[9, 32]])

    def LIv(t, j):
        return V(t, j, [[8, 32]])
    MEMSET("dve", LPv(LPre, 0), 1.0, [Bp], [Bp])
    MEMSET("dve", LPv(LPim, 0), 0.0, [Bp], [Bp])
    d_tt(LPv(LPre, 1), mm_[:], cc[:], ALU.mult)
    d_tt(LPv(LPim, 1), mm_[:], ss_[:], ALU.mult)
    for j in range(2, 9):
        cmul(LPv(LPre, j), LPv(LPim, j), LPv(LPre, j - 1), LPv(LPim, j - 1), LPv(LPre, 1), LPv(LPim, 1), t1[:], t2[:])
    MEMSET("dve", LIv(LIre, 0), 1.0, [Bp], [Bp])
    MEMSET("dve", LIv(LIim, 0), 0.0, [Bp], [Bp])
    d_tt(LIv(LIre, 1), minv[:], cc[:], ALU.mult)
    d_tt(t3[:], minv[:], ss_[:], ALU.mult)
    d_ts(LIv(LIim, 1), t3[:], -1.0, None)
    for j in range(2, 8):
        cmul(LIv(LIre, j), LIv(LIim, j), LIv(LIre, j - 1), LIv(LIim, j - 1), LIv(LIre, 1), LIv(LIim, 1), t1[:], t2[:])
    CP("dve", LPv(Are, 0), LPv(LPre, 8), [Bp], [Bp])
    CP("dve", LPv(Aim, 0), LPv(LPim, 8), [Bp], [Bp])
    for k in range(1, 9):
        d_tt(t1[:], LPv(Are, k - 1), LPv(Are, k - 1), ALU.mult)
        d_tt(t2[:], LPv(Aim, k - 1), LPv(Aim, k - 1), ALU.mult)
        d_tt(LPv(Are, k), t1[:], t2[:], ALU.subtract)
        STT(LPv(Aim, k), LPv(Are, k - 1), 2.0, LPv(Aim, k - 1), ALU.mult, ALU.mult, [Bp], [Bp])
    for gl in range(2):
        for (src, dst) in ((Are, ks_are), (Aim, ks_aim)):
            fw.op("dve", lambda e, src=src, dst=dst, gl=gl: e.tensor_copy(
                VP(dst, gl * 64, 64, 0, [[9, 16], [1, 9]]), VP(src, gl * 64, 64, gl * 9, [[18, 16], [1, 9]])), reads=[Bp], writes=[Bc], add=True)
    TS("dve", ks_naim[:], ks_aim[:], -1.0, None, ALU.mult, None, [Bc], [Bc])
    d_ts(t1[:], LPv(LPre, 1), -1.0, None, ALU.add)
    d_tt(den[:], lre[:], lre[:], ALU.mult)
    d_tt(t2[:], lim[:], lim[:], ALU.mult)
    d_tt(den[:], den[:], t2[:], ALU.add)
    RECIP(den[:], den[:], [Bp], [Bp])
    d_tt(ire[:], lre[:], den[:], ALU.mult)
    d_tt(iim[:], lim[:], den[:], ALU.mult)
    d_ts(iim[:], iim[:], -1.0, None)
    cmul(fre[:], fim[:], t1[:], LPv(LPim, 1), ire[:], iim[:], t3[:], t4[:])
    def bc16(t):
        return V(t, 0, [[1, 32], [0, 16]])

    def v3(t):
        return V(t, 0, [[16, 32], [1, 16]])
    w1a = V(W1, 0, [[16, 32], [1, 16]]); w1b = V(W1, 512, [[16, 32], [1, 16]])
    cmul(v3(Bbre), v3(Bbim), bc16(fre), bc16(fim), v3(Bin), v3(Bin_im), w1a, w1b)
    def g4(t):
        return V(t, 0, [[128, 32], [16, 8], [1, 16]])

    def li4(t):
        return V(t, 0, [[8, 32], [1, 8], [0, 16]])

    def bb4(t):
        return V(t, 0, [[16, 32], [0, 8], [1, 16]])
    cmul(g4(Gre), g4(Gim), li4(LIre), li4(LIim), bb4(Bbre), bb4(Bbim), g4(W1), g4(W2))
    def h4(t):
        return V(t, 0, [[144, 32], [16, 9], [1, 16]])

    def lp4(t):
        return V(t, 0, [[9, 32], [1, 9], [0, 16]])

    def c4(t):
        return V(t, 0, [[16, 32], [0, 9], [1, 16]])
    cmul(h4(HHre), h4(HHim), lp4(LPre), lp4(LPim), c4(Cre), c4(Cim), h4(W1), h4(W2))
    def hs4(t, j0):
        return V(t, j0 * 16, [[144, 32], [1, 128]])
    CP("dve", V(Wre_b, 0, [[128, 32], [1, 128]]), hs4(HHre, 1), [Bp], [Bs5w], add=True)
    TS("dve", V(Wimn_b, 0, [[128, 32], [1, 128]]), hs4(HHim, 1), -1.0, None, ALU.mult, None, [Bp], [Bs5w], add=True)
    def l7(t):
        return V(t, 7, [[9, 32], [0, 128]])

    def g3(t):
        return V(t, 0, [[128, 32], [1, 128]])
    Bvv = Buf("vv")
    cmul(g3(VVre), g3(VVim), l7(LPre), l7(LPim), g3(Gre), g3(Gim), g3(GS), g3(HS))
    CP("dve", VP(GS, 0, 64, 0, [[1, 4096]]), VP(Gre, 0, 64, 0, [[1, 4096]]), [Bp], [Bp])
    fw.op("dve", lambda e: e.tensor_scalar(VP(GS, 64, 64, 0, [[1, 4096]]), VP(Gim, 64, 64, 0, [[1, 4096]]), -1.0, None, ALU.mult), reads=[Bp], writes=[Bp])
    CP("dve", VP(HS, 0, 64, 0, [[128, 32], [1, 128]]), VP(HHre, 0, 64, 0, [[144, 32], [1, 128]]), [Bp], [Bp])
    CP("dve", VP(HS, 64, 64, 0, [[128, 32], [1, 128]]), VP(HHim, 64, 64, 0, [[144, 32], [1, 128]]), [Bp], [Bp])
    mask4 = A("mask4", [128, 512], F32, M0 + 178 * KB)
    MEMSET("pool", mask4[:], 1.0, [Bp], [Bp])
    fw.op("pool", lambda e: e.affine_select(mask4[:], mask4[:], pattern=[[0, 4], [16, 8], [0, 16]], compare_op=ALU.is_ge, fill=0.0, base=15, channel_multiplier=-1), reads=[Bp], writes=[Bp])
    Tm = A("Tm", [128, 512], F32, M0 + 180 * KB)
    for q4 in range(8):
        bk = q4 % 2
        for gi in range(4):
            g = q4 * 4 + gi
            MM(bank(bk, gi * 128, gi * 128 + 128), GS[:, g * 128:(g + 1) * 128], HS[:, g * 128:(g + 1) * 128], True, True, [Bp], [PB[bk]], sig=(gi == 3), add=(gi > 0))
        TT("dve", Tm[:], bank(bk), mask4[:], ALU.mult, [PB[bk], Bp], [Bp])
        for gi in range(4):
            g = q4 * 4 + gi
            STT(T_b[:, g * 128:(g + 1) * 128], ident_f[:], dvec[:, g:g + 1], Tm[:, gi * 128:(gi + 1) * 128], ALU.mult, ALU.add, [Bp, Bc], [Bs5w], add=True)
    for (src, dst) in ((VVre, VTre_b), (VVim, VTim_b)):
        for q4 in range(8):
            bk = 2 + q4 % 2
            for gi in range(4):
                g = q4 * 4 + gi
                TR(bank(bk, gi * 128, gi * 128 + 128), src[:, g * 128:(g + 1) * 128], ident_f[:], [Bp, Bc], [PB[bk]], sig=(gi == 3), add=(gi > 0))
            CP("act", V(dst, q4 * 256, [[64, 4], [1, 64]]), bass.AP(pp, bk * 512, [[4096, 128], [128, 4], [1, 64]]), [PB[bk]], [Bs5w], add=True)
    if debug:
        dtmp = A("dtmp", [128, 4096], F32, M0 + 0)
        fw.barrier()
        CP("dve", dtmp[:], T_b[:], [Bs5w], [Bp])
        fw.dma("sp", dbg["dbg_T"], dtmp[:], reads=[Bp], is_output=True)
        dks = A("dks", [128, 288], F32, M0 + 16 * KB)
        CP("dve", dks[:, 0:144], ks_are[:], [Bc], [Bp])
        CP("dve", dks[:, 144:288], ks_aim[:], [Bc], [Bp])
        fw.dma("sp", dbg["dbg_ks"], dks[:], reads=[Bp], is_output=True)
    fw.barrier()

    if stop == 'p0b':
        fw.finish()
        return nc
    def rms_tile(xt, hbt, jk, st, Bx, Bh, Bst, rows, gt=gmix_t, out_dt_bf=True):
        ACT(jk, xt, AF.Square, [Bx], [Bst, Bh], accum_out=st[:, 0:1])
        ACT(st[:, 1:2], st[:, 0:1], AF.Sqrt, [Bst], [Bst], scale=1.0 / D, bias=EPS)
        RECIP(st[:, 2:3], st[:, 1:2], [Bst], [Bst])
        STT(hbt, xt, st[:, 2:3], gt[:], ALU.mult, ALU.mult, [Bx, Bst, Bc], [Bh])

    gyT = A("gyT", [128, 4 * S], BF16, R_G)
    Wu_b = A("Wu_b", [128, 8 * 512], BF16, R_O)
    hT_sb = A("hT_sb", [128, 8 * 1024], BF16, R_O + 8 * KB)
    U_tok = A("U_tok", [128, 4 * 4096], BF16, R_K)
    Ug = A("Ug", [128, 32 * 512], BF16, R_V)
    xa = [A("xa%d" % i, [128, D], F32, R_T + i * 4 * KB) for i in range(2)]
    hba = [A("hba%d" % i, [128, D], BF16, R_T + 8 * KB + i * 2 * KB) for i in range(2)]
    jka = A("jka", [128, D], BF16, R_T + 12 * KB)
    jkaA = A("jkaA", [128, D], BF16, R_G)
    ksb = [A("ksb%d" % i, [128, 2 * 512], F32, R_T + 12 * KB + i * 4 * KB) for i in range(2)]
    Bxa = [Buf("xa0"), Buf("xa1")]; Bhba = [Buf("hba0"), Buf("hba1")]; Bsta = [Buf("sta0"), Buf("sta1")]
    BWu = Buf("Wu"); BhT = Buf("hTsb"); BUt = [Buf("Ut%d" % i) for i in range(4)]; BUg = [Buf("Ug%d" % i) for i in range(32)]
    Bjk = Buf("jk")
    fw.dma("pool", V(Wu_b, 0, [[512, 8], [1, 512]]), w_in_d[:, 1536:2048].rearrange("(k p) n -> p k n", p=128), writes=[BWu])
    for sb in range(4):
        for i in range(8):
            n = sb * 8 + i
            bi = n % 2
            fw.dma("sp", xa[bi][:], x_d[n * 128:(n + 1) * 128, :], writes=[Bxa[bi]])
            rms_tile(xa[bi][:], hba[bi][:], jkaA[:], V(stat, bi * 4, [[1, 4]]), Bxa[bi], Bhba[bi], Bsta[bi], None)
            for k in range(8):
                TR(bankb(0, k * 128, k * 128 + 128), hba[bi][:, k * 128:(k + 1) * 128], ident_b[:], [Bhba[bi], Bc], [PB[0]], sig=(k == 7), add=(k > 0))
            CP("act", V(hT_sb, i * 128, [[1024, 8], [1, 128]]), bass.AP(ppb, 0, [[8192, 128], [128, 8], [1, 128]]), [PB[0]], [BhT], add=(i > 0))
        for tau in range(8):
            bk = 1 + tau % 2
            for k in range(8):
                MM(bank(bk), V(hT_sb, k * 1024 + tau, [[8, 128]]), Wu_b[:, k * 512:(k + 1) * 512], k == 0, k == 7, [BhT, BWu], [PB[bk]], sig=(k == 7), add=(k > 0))
            eng = "act" if tau % 2 == 0 else "dve"
            CP(eng, V(U_tok, sb * 4096 + tau * 16, [[128, 32], [1, 16]]), bass.AP(pp, bk * 512, [[4096, 128], [16, 32], [1, 16]]), [PB[bk]], [BUt[sb]], add=(tau > 0))
        for g8 in range(4):
            bk = 3 + g8 % 2
            for gi in range(8):
                g = g8 * 8 + gi
                TR(bankb(bk, gi * 128, gi * 128 + 128), U_tok[:, sb * 4096 + g * 128: sb * 4096 + (g + 1) * 128], ident_b[:], [BUt[sb], Bc], [PB[bk]], sig=(gi == 7), add=(gi > 0))
            eng = "act" if g8 % 2 == 0 else "dve"
            CP(eng, V(Ug, g8 * 8 * 512 + sb * 128, [[512, 8], [1, 128]]), bass.AP(ppb, bk * 1024, [[8192, 128], [128, 8], [1, 128]]), [PB[bk]], [BUg[g8 * 8 + gi] for gi in range(8)], add=True)
    if debug:
        fw.barrier()
        dtmp2 = A("dtmp2", [128, 16384], F32, R_G)
        CP("dve", dtmp2[:], Ug[:], BUg, [Bp])
        fw.dma("sp", dbg["dbg_ug"], dtmp2[:], reads=[Bp], is_output=True)
        fw.barrier()
    if stop == 'pA1':
        fw.finish()
        return nc
    Ygel = U_tok
    BY = [Buf("Ygel%d" % i) for i in range(32)]
    Xb = [A("Xb%d" % i, [128, 2 * 512], BF16, R_O + 24 * KB + i * 2 * KB) for i in range(2)]
    BXb = [Buf("Xb0"), Buf("Xb1")]
    Bks = [Buf("ks0"), Buf("ks1")]
    gel = [A("gel%d" % i, [128, 512], F32, R_O + 28 * KB + i * 2 * KB) for i in range(2)]
    Bgel = [Buf("gel0"), Buf("gel1")]
    for gp in range(16):
        for (ri, VT) in ((0, VTre_b), (1, VTim_b)):
            bk = 5 + ri
            for gl in range(2):
                g = 2 * gp + gl
                MM(pp[gl * 64:(gl + 1) * 64, bk * 512:(bk + 1) * 512], VT[:, g * 64:(g + 1) * 64], Ug[:, g * 512:(g + 1) * 512], True, True, [Bs5w, BUg[g]], [PB[bk]], sig=(gl == 1), add=(gl > 0))
        cur, nxt = 0, 1
        CP("act", ksb[cur][:, 0:512], bank(5), [PB[5]], [Bks[cur]])
        CP("act", ksb[cur][:, 512:1024], bank(6), [PB[6]], [Bks[cur]], add=True)
        for k in range(9):
            s = 1 << k
            n = 512 - s
            a_k = ks_are[:, gp * 9 + k: gp * 9 + k + 1]
            b_k = ks_aim[:, gp * 9 + k: gp * 9 + k + 1]
            nb_k = ks_naim[:, gp * 9 + k: gp * 9 + k + 1]
            c_, n_ = ksb[cur], ksb[nxt]
            CP("act", V(n_, 0, [[512, 2], [1, s]]), V(c_, 0, [[512, 2], [1, s]]), [Bks[cur]], [Bks[nxt]])
            STT(n_[:, s:512], c_[:, 0:n], a_k, c_[:, s:512], ALU.mult, ALU.add, [Bks[cur], Bc], [Bks[nxt]], add=True)
            STT(n_[:, s:512], c_[:, 512:512 + n], nb_k, n_[:, s:512], ALU.mult, ALU.add, [Bks[cur], Bks[nxt], Bc], [Bks[nxt]], add=True)
            STT(n_[:, 512 + s:1024], c_[:, 0:n], b_k, c_[:, 512 + s:1024], ALU.mult, ALU.add, [Bks[cur], Bc], [Bks[nxt]], add=True)
            STT(n_[:, 512 + s:1024], c_[:, 512:512 + n], a_k, n_[:, 512 + s:1024], ALU.mult, ALU.add, [Bks[cur], Bks[nxt], Bc], [Bks[nxt]], add=True)
            cur, nxt = nxt, cur
        xb = Xb[gp % 2]
        CP("act", xb[:], ksb[cur][:], [Bks[cur]], [BXb[gp % 2]])
        for gl in range(2):
            g = 2 * gp + gl
            bk = 1 + g % 2
            MM(bank(bk), T_b[:, g * 128:(g + 1) * 128], Ug[:, g * 512:(g + 1) * 512], True, False, [Bs5w, BUg[g]], [PB[bk]], sig=False)
            MM(bank(bk, 1, 512), VP(Wre_b, gl * 64, 64, g * 128, [[1, 128]]), VP(xb, gl * 64, 64, 0, [[1, 511]]), False, False, [Bs5w, BXb[gp % 2]], [PB[bk]], sig=False, add=True)
            MM(bank(bk, 1, 512), VP(Wimn_b, gl * 64, 64, g * 128, [[1, 128]]), VP(xb, gl * 64, 64, 512, [[1, 511]]), False, True, [Bs5w, BXb[gp % 2]], [PB[bk]], sig=True, add=True)
            ge = gel[g % 2]
            Bg = Bgel[g % 2]
            ACT(ge[:], bank(bk), AF.Square, [PB[bk]], [Bg])
            TS("dve", ge[:], ge[:], 0.044715, 1.0, ALU.mult, ALU.add, [Bg], [Bg])
            TT("dve", ge[:], ge[:], bank(bk), ALU.mult, [Bg, PB[bk]], [Bg])
            ACT(ge[:], ge[:], AF.Sigmoid, [Bg], [Bg], scale=1.5957691216057308)
            TT("dve", Ygel[:, g * 512:(g + 1) * 512], ge[:], bank(bk), ALU.mult, [Bg, PB[bk]], [BY[g]] + BUt, add=True)
    if stop == 'pA2':
        fw.finish()
        return nc
    fw.barrier()
    Ytok = Ug
    BYt = [Buf("Ytok%d" % i) for i in range(4)]
    Bgy = [Buf("gyT%d" % i) for i in range(8)]
    gy32 = [A("gy32_%d" % i, [128, 4 * 1024], F32, R_Q + i * 16 * KB) for i in range(2)]
    Bg32 = [Buf("gy32_0"), Buf("gy32_1")]
    for sb in range(4):
        for g8 in range(4):
            bk = 3 + g8 % 2
            for gi in range(8):
                g = g8 * 8 + gi
                TR(bankb(bk, gi * 128, gi * 128 + 128), Ygel[:, g * 512 + sb * 128: g * 512 + (sb + 1) * 128], ident_b[:], [BY[g], Bc], [PB[bk]], sig=(gi == 7), add=(gi > 0))
            eng = "act" if g8 % 2 == 0 else "dve"
            CP(eng, V(Ytok, sb * 4096 + g8 * 128, [[16, 8], [512, 8], [1, 16]]), bass.AP(ppb, bk * 1024, [[8192, 128], [128, 8], [16, 8], [1, 16]]), [PB[bk]], BUg + [BYt[sb]], add=True)
        if stop == 'pA3':
            fw.finish()
            return nc
        for j in range(8):
            bk = 5 + j % 2
            for q4 in range(4):
                TR(bankb(bk, q4 * 128, q4 * 128 + 128), Ytok[:, sb * 4096 + j * 512 + q4 * 128: sb * 4096 + j * 512 + (q4 + 1) * 128], ident_b[:], [BYt[sb], Bc], [PB[bk]], sig=(q4 == 3), add=(q4 > 0))
            eng = "act" if j % 2 == 0 else "dve"
            CP(eng, V(gy32[sb % 2], j, [[1024, 4], [8, 128]]), bass.AP(ppb, bk * 1024, [[8192, 128], [128, 4], [1, 128]]), [PB[bk]], [Bg32[sb % 2]], add=(j > 0))
        if stop == 'pA4':
            fw.finish()
            return nc
        CP("pool", V(gyT, sb * 1024, [[S, 4], [1, 1024]]), V(gy32[sb % 2], 0, [[1024, 4], [1, 1024]]), [Bg32[sb % 2]], [Bgy[2 * sb], Bgy[2 * sb + 1]], add=True)
    if stop == 'pA5':
        fw.finish()
        return nc
    if debug:
        fw.barrier()
        for q_ in range(4):
            dst_ = A("stg_dbg_gy_%d" % q_, [128, S], F32, R_K)
            CP("dve", dst_[:], gyT[:, q_ * S:(q_ + 1) * S], Bgy, [Bp])
            fw.dma("sp", dbg["dbg_gy" + str(q_)], dst_[:], reads=[Bp], is_output=True)
    fw.barrier()

    if stop == 'pA':
        fw.finish()
        return nc
    qT = A("qT", [128, 4 * S], BF16, R_Q)
    kT = A("kT", [128, 4 * S], BF16, R_K)
    v_aug = A("v_aug", [128, NT * 4 * 130], BF16, R_V)
    Wqkv = A("Wqkv", [128, 8 * 1536], BF16, R_O)
    hTt = [A("hTt%d" % i, [128, 8 * 128], BF16, R_O + 24 * KB + i * 2 * KB) for i in range(2)]
    BhTt = [Buf("hTt0"), Buf("hTt1")]
    sqs = A("sqs", [128, 512], F32, R_O + 28 * KB)
    qn = [A("qn%d" % i, [128, 512], F32, R_T + 14 * KB + i * 2 * KB) for i in range(2)]
    qb_ = [A("qb%d" % i, [128, 512], BF16, R_T + 18 * KB + i * 1 * KB) for i in range(2)]
    rtmp = A("rtmp", [128, 4 * 64], F32, R_T + 20 * KB)
    Bsq = Buf("sqs"); Bqn = [Buf("qn0"), Buf("qn1")]; Bqb = [Buf("qb0"), Buf("qb1")]; Brt = Buf("rtmp")
    BW = Buf("Wqkv"); BqT = [Buf("qT%d" % i) for i in range(8)]; BkT = [Buf("kT%d" % i) for i in range(NT)]; Bv = [Buf("v%d" % i) for i in range(NT)]
    fw.dma("pool", V(Wqkv, 0, [[1536, 8], [1, 1536]]), w_in_d[:, 0:1536].rearrange("(k p) n -> p k n", p=128), writes=[BW])
    MEMSET("pool", V(v_aug, 128, [[130, NT * 4], [1, 2]]), 1.0, [], Bv)
    for n in range(NT):
        bi = n % 2
        fw.dma("sp", xa[bi][:], x_d[n * 128:(n + 1) * 128, :], writes=[Bxa[bi]])
        rms_tile(xa[bi][:], hba[bi][:], jka[:], V(stat, bi * 4, [[1, 4]]), Bxa[bi], Bhba[bi], Bsta[bi], None)
        for k in range(8):
            TR(bankb(0, k * 128, k * 128 + 128), hba[bi][:, k * 128:(k + 1) * 128], ident_b[:], [Bhba[bi], Bc], [PB[0]], sig=(k == 7), add=(k > 0))
        CP("act", hTt[bi][:], bankb(0), [PB[0]], [BhTt[bi]])
        for cb in range(3):
            bk = 1 + cb
            for k in range(8):
                MM(bank(bk), hTt[bi][:, k * 128:(k + 1) * 128], Wqkv[:, k * 1536 + cb * 512: k * 1536 + (cb + 1) * 512], k == 0, k == 7, [BhTt[bi], BW], [PB[bk]], sig=(k == 7), add=(k > 0))
            if cb == 2:
                CP("act", V(v_aug, n * 520, [[130, 4], [1, 128]]), bass.AP(pp, bk * 512, [[4096, 128], [128, 4], [1, 128]]), [PB[bk]], [Bv[n]], add=True)
                continue
            st = V(stat, 8 + cb * 24, [[1, 24]])
            Bs = Bsta[bi]
            ACT(sqs[:], bank(bk), AF.Square, [PB[bk]], [Bsq])
            RSUM(stat[:, 8 + cb * 24: 16 + cb * 24], V(sqs, 0, [[64, 8], [1, 64]]), [Bsq], [Bs])
            ACT(stat[:, 16 + cb * 24: 24 + cb * 24], stat[:, 8 + cb * 24: 16 + cb * 24], AF.Sqrt, [Bs], [Bs], scale=1.0 / 64, bias=EPS)
            RECIP(stat[:, 24 + cb * 24: 32 + cb * 24], stat[:, 16 + cb * 24: 24 + cb * 24], [Bs], [Bs])
            q_ = qn[cb]
            Bq = Bqn[cb]
            TT("dve", V(q_, 0, [[64, 8], [1, 64]]), bass.AP(pp, bk * 512, [[4096, 128], [64, 8], [1, 64]]), V(stat, 24 + cb * 24, [[1, 8], [0, 64]]), ALU.mult, [PB[bk], Bs], [Bq])
            TT("pool", q_[:], q_[:], (gq_t if cb == 0 else gk_t)[:], ALU.mult, [Bq, Bc], [Bq])
            r1 = V(q_, 0, [[64, 8], [1, 8]]); r2 = V(q_, 8, [[64, 8], [1, 8]])
            cs = V(cos_t, n * 8, [[0, 8], [1, 8]]); sn = V(sin_t, n * 8, [[0, 8], [1, 8]])
            ta = V(rtmp, 0, [[8, 8], [1, 8]]); tb = V(rtmp, 64, [[8, 8], [1, 8]]); tc = V(rtmp, 128, [[8, 8], [1, 8]]); td = V(rtmp, 192, [[8, 8], [1, 8]])
            TT("pool", ta, r1, cs, ALU.mult, [Bq, Bc], [Brt])
            TT("pool", tb, r2, sn, ALU.mult, [Bq, Bc], [Brt], add=True)
            TT("pool", tc, r2, cs, ALU.mult, [Bq, Bc], [Brt], add=True)
            TT("pool", td, r1, sn, ALU.mult, [Bq, Bc], [Brt], add=True)
            TT("pool", r1, ta, tb, ALU.subtract, [Brt, Bq], [Bq])
            TT("pool", r2, tc, td, ALU.add, [Brt, Bq], [Bq])
            CP("act", qb_[cb][:], q_[:], [Bq], [Bqb[cb]])
            bkt = 4 + cb
            for h in range(4):
                TR(bankb(bkt, h * 128, h * 128 + 128), qb_[cb][:, h * 128:(h + 1) * 128], ident_b[:], [Bqb[cb], Bc], [PB[bkt]], sig=(h == 3), add=(h > 0))
            dstT = qT if cb == 0 else kT
            dB = BqT[n // 4] if cb == 0 else BkT[n]
            CP("dve", V(dstT, n * 128, [[S, 4], [1, 128]]), bass.AP(ppb, bkt * 1024, [[8192, 128], [128, 4], [1, 128]]), [PB[bkt]], [dB], add=True)
    if debug:
        fw.barrier()
        for q_ in range(4):
            dst_ = A("stg_dbg_q_%d" % q_, [128, S], F32, R_O)
            CP("dve", dst_[:], qT[:, q_ * S:(q_ + 1) * S], BqT, [Bp])
            fw.dma("sp", dbg["dbg_q" + str(q_)], dst_[:], reads=[Bp], is_output=True)
        for q_ in range(4):
            dst_ = A("stg_dbg_k_%d" % q_, [128, S], F32, R_O)
            CP("dve", dst_[:], kT[:, q_ * S:(q_ + 1) * S], BkT, [Bp])
            fw.dma("sp", dbg["dbg_k" + str(q_)], dst_[:], reads=[Bp], is_output=True)
    fw.barrier()
    fw.barrier()

    if stop == 'pB':
        fw.finish()
        return nc
    oT = A("oT", [128, 4 * S], BF16, R_O)
    pT = [[A("pT%d%d" % (c, i), [128, 512], BF16, R_T + (c * 2 + i) * KB) for i in range(2)] for c in range(2)]
    BpT = [[Buf("pT%d%d" % (c, i)) for i in range(2)] for c in range(2)]
    of_ = [A("of%d" % i, [128, 128], F32, R_T + 4 * KB + i * 512) for i in range(2)]
    ob_ = [A("ob%d" % i, [128, 128], BF16, R_T + 5 * KB + i * 256) for i in range(2)]
    ajk = A("ajk", [128, 128], BF16, R_T + 6 * KB)
    Bof = [Buf("of0"), Buf("of1")]; Bob = [Buf("ob0"), Buf("ob1")]; Bast = [Buf("ast0"), Buf("ast1")]
    BoT = [Buf("oT%d" % i) for i in range(8)]
    Bajk = Buf("ajk")
    def accv(qs, c, a, b):
        idx = qs * 2 + c
        bk = 4 + idx // 3
        off = bk * 512 + (idx % 3) * 130
        return pp[:, off + a: off + b], PB[bk]
    fcount = [0]
    pT4 = [A("pT4_%d" % i, [128, 512], BF16, R_T + i * KB) for i in range(4)]
    BpT4 = [Buf("pT4_%d" % i) for i in range(4)]
    accs = [A("accs%d" % i, [128, 3 * 390], F32, R_T + 8 * KB + i * 5 * KB) for i in range(2)]
    Baccs = [Buf("accs0"), Buf("accs1")]
    att_items = [(h, qblk, kt, c) for h in range(4) for qblk in range(8) for kt in range(4 * qblk + 4) for c in range(2)]
    NA = len(att_items)

    def stage_S(k):
        h, qblk, kt, c = att_items[k]
        q0 = max(0, kt - 4 * qblk)
        col0 = q0 * 128
        diag = kt >= 4 * qblk
        sb_ = k % 4
        MM(bank(sb_, col0, 512), VP(kT, c * 64, 64, h * S + kt * 128, [[1, 128]]), VP(qT, c * 64, 64, h * S + qblk * 512 + col0, [[1, 512 - col0]]),
           True, not diag, [BkT[kt], BqT[qblk]], [PB[sb_]], sig=(not diag))
        if diag:
            MM(bank(sb_, col0, col0 + 128), ident_b[:], maskneg_b[:], False, True, [Bc], [PB[sb_]], sig=True, add=True)
        ACT(pT4[sb_][:, col0:512], bank(sb_, col0, 512), AF.Exp, [PB[sb_]], [BpT4[sb_]], scale=0.125)

    def finalize(h, qblk, rnd):
        ai = rnd % 2
        CP("dve", V(accs[ai], 0, [[390, 3], [1, 390]]), bass.AP(pp, 4 * 512, [[4096, 128], [512, 3], [1, 390]]), [PB[4], PB[5], PB[6]], [Baccs[ai]])
        for qs in range(4):
            fi = fcount[0] % 2
            fcount[0] += 1
            so_ = 32 + fi * 8

            def av_(c, a, b):
                idx = qs * 2 + c
                off = (idx // 3) * 390 + (idx % 3) * 130
                return accs[ai][:, off + a: off + b]
            Bs = Bast[fi]
            Ba = Baccs[ai]
            RECIP(stat[:, so_:so_ + 1], av_(0, 128, 129), [Ba], [Bs])
            RECIP(stat[:, so_ + 1:so_ + 2], av_(1, 128, 129), [Ba], [Bs], add=True)
            TT("dve", stat[:, so_ + 2:so_ + 3], stat[:, so_ + 1:so_ + 2], lamv[:, 3:4], ALU.mult, [Bs, Bc], [Bs])
            TS("dve", of_[fi][:], av_(0, 0, 128), stat[:, so_:so_ + 1], None, ALU.mult, None, [Ba, Bs], [Bof[fi]])
            STT(of_[fi][:], av_(1, 0, 128), stat[:, so_ + 2:so_ + 3], of_[fi][:], ALU.mult, ALU.add, [Ba, Bs, Bof[fi]], [Bof[fi]])
            ACT(ajk[:], of_[fi][:], AF.Square, [Bof[fi]], [Bs, Bajk], accum_out=stat[:, so_ + 3:so_ + 4])
            ACT(stat[:, so_ + 4:so_ + 5], stat[:, so_ + 3:so_ + 4], AF.Sqrt, [Bs], [Bs], scale=1.0 / 128, bias=EPS)
            RECIP(stat[:, so_ + 5:so_ + 6], stat[:, so_ + 4:so_ + 5], [Bs], [Bs])
            STT(ob_[fi][:], of_[fi][:], stat[:, so_ + 5:so_ + 6], sg08_t[:], ALU.mult, ALU.mult, [Bof[fi], Bs, Bc], [Bob[fi]])
            TR(bankb(7, fi * 128, fi * 128 + 128), ob_[fi][:], ident_b[:], [Bob[fi], Bc], [PB[7]], sig=True, add=True)
            tok0 = qblk * 512 + qs * 128
            CP("act", oT[:, h * S + tok0: h * S + tok0 + 128], bankb(7, fi * 128, fi * 128 + 128), [PB[7]], [BoT[qblk]], add=True)

    def stage_PV(k):
        h, qblk, kt, c = att_items[k]
        q0 = max(0, kt - 4 * qblk)
        p_ = pT4[k % 4]
        for qs in range(q0, 4):
            av, ab = accv(qs, c, 0, 129)
            last = (kt == 4 * qblk + qs)
            first_in_bank = (kt == 0) and ((qs * 2 + c) in (0, 4, 6))
            MM(av, p_[:, qs * 128:(qs + 1) * 128], V(v_aug, kt * 520 + h * 130, [[1, 129]]), first_in_bank, last, [BpT4[k % 4], Bv[kt]], [ab], sig=last, add=(kt > 0 or not first_in_bank), skip=True)
        if kt == 4 * qblk + 3 and c == 1:
            finalize(h, qblk, h * 8 + qblk)

    LOOK = 2
    for k in range(-LOOK, NA):
        if 0 <= k + LOOK < NA:
            stage_S(k + LOOK)
        if k >= 0:
            stage_PV(k)
    if debug:
        fw.barrier()
        for q_ in range(4):
            dst_ = A("stg_dbg_o_%d" % q_, [128, S], F32, R_Q)
            CP("dve", dst_[:], oT[:, q_ * S:(q_ + 1) * S], BoT, [Bp])
            fw.dma("sp", dbg["dbg_o" + str(q_)], dst_[:], reads=[Bp], is_output=True)
    fw.barrier()

    if stop == 'att':
        fw.finish()
        return nc
    Wg_b = A("Wg_b", [128, 8 * 2048], BF16, R_Q)
    wglu_b = A("wglu_b", [128, 4 * 2048], BF16, R_K)
    wout_b = A("wout_b", [128, 8 * 1024], BF16, R_K + 16 * KB)
    wo_b = A("wo_b", [128, 4 * 1024], BF16, R_V)
    xc = [A("xc%d" % i, [128, D], F32, R_V + 8 * KB + i * 4 * KB) for i in range(4)]
    hT_blk = A("hT_blk", [128, 8 * 512], BF16, R_V + 24 * KB)
    mT = A("mT", [128, 8 * 512], BF16, R_T)
    sgA = A("sgA", [128, 512], F32, R_T + 8 * KB); sgB = A("sgB", [128, 512], F32, R_T + 10 * KB); sgE = A("sgE", [128, 512], F32, R_T + 12 * KB)
    tt1 = A("tt1", [128, 512], F32, R_T + 14 * KB); tt2 = A("tt2", [128, 512], F32, R_T + 16 * KB)
    hbc = A("hbc", [128, D], BF16, R_T + 18 * KB)
    cjk = hbc
    tT32 = A("tT32", [128, 8 * 128], F32, R_T + 8 * KB)
    BWc = Buf("Wc"); Bxc = [Buf("xc%d" % i) for i in range(4)]; BhTb = Buf("hTblk"); BmT = Buf("mT")
    BsA = Buf("sgA"); BsB = Buf("sgB"); BsE = Buf("sgE"); Bt1 = Buf("tt1"); Bt2 = Buf("tt2"); Bhbc = Buf("hbc"); Bcst = Buf("cst"); BtT32 = BsA
    Bcomb = Buf("comb")
    Bout = [Buf("out_h0"), Buf("out_h1")]
    fw.dma("pool", V(Wg_b, 0, [[2048, 8], [1, 2048]]), w_in_d[:, 2048:4096].rearrange("(k p) n -> p k n", p=128), writes=[BWc], add=True)
    fw.dma("pool", V(wglu_b, 0, [[2048, 4], [1, 2048]]), wglu_d.rearrange("(k p) n -> p k n", p=128), writes=[BWc], add=True)
    fw.dma("pool", V(wout_b, 0, [[1024, 8], [1, 1024]]), wout_d.rearrange("(k p) n -> p k n", p=128), writes=[BWc], add=True)
    fw.dma("pool", V(wo_b, 0, [[1024, 4], [1, 1024]]), wo_d.rearrange("(k p) n -> p k n", p=128), writes=[BWc], add=True)

    def tT_ap(k, tok0, n):
        base = gyT if k < 4 else oT
        return base[:, (k % 4) * S + tok0: (k % 4) * S + tok0 + n]

    for blk in range(8):
        t0 = blk * 512
        for i in range(4):
            n = blk * 4 + i
            fw.dma("sp", xc[i][:], x_d[n * 128:(n + 1) * 128, :], writes=[Bxc[i]])
            rms_tile(xc[i][:], hbc[:], cjk[:], V(stat, 48, [[1, 4]]), Bxc[i], Bhbc, Bcst, None)
            for k in range(8):
                TR(bankb(0, k * 128, k * 128 + 128), hbc[:, k * 128:(k + 1) * 128], ident_b[:], [Bhbc, Bc], [PB[0]], sig=(k == 7), add=(k > 0))
            CP("act", V(hT_blk, i * 128, [[512, 8], [1, 128]]), bass.AP(ppb, 0, [[8192, 128], [128, 8], [1, 128]]), [PB[0]], [BhTb], add=(i > 0))
        for nch in range(8):
            for (bk, col) in ((1, nch), (2, 8 + nch)):
                for k in range(8):
                    MM(bank(bk), Wg_b[:, k * 2048 + col * 128: k * 2048 + (col + 1) * 128], hT_blk[:, k * 512:(k + 1) * 512], k == 0, k == 7, [BWc, BhTb], [PB[bk]], sig=(k == 7), add=(k > 0))
            for f in range(4):
                MM(bank(3), wo_b[:, f * 1024 + nch * 128: f * 1024 + (nch + 1) * 128], oT[:, f * S + t0: f * S + t0 + 512], f == 0, f == 3, [BWc, BoT[blk]], [PB[3]], sig=(f == 3), add=(f > 0))
            for (bk, col) in ((4, nch), (5, 8 + nch)):
                for f in range(4):
                    MM(bank(bk), wglu_b[:, f * 2048 + col * 128: f * 2048 + (col + 1) * 128], gyT[:, f * S + t0: f * S + t0 + 512], f == 0, f == 3, [BWc, Bgy[blk]], [PB[bk]], sig=(f == 3), add=(f > 0))
            ACT(sgA[:], bank(1), AF.Sigmoid, [PB[1]], [BsA])
            ACT(sgB[:], bank(2), AF.Sigmoid, [PB[2]], [BsB])
            ACT(sgE[:], bank(5), AF.Sigmoid, [PB[5]], [BsE])
            TT("dve", tt1[:], bank(3), sgA[:], ALU.mult, [PB[3], BsA], [Bt1])
            TT("dve", tt2[:], bank(4), sgE[:], ALU.mult, [PB[4], BsE], [Bt2])
            TT("pool", tt2[:], tt2[:], sgB[:], ALU.mult, [Bt2, BsB], [Bt2])
            TT("pool", mT[:, nch * 512:(nch + 1) * 512], tt1[:], tt2[:], ALU.add, [Bt1, Bt2], [BmT], add=(nch > 0))
        for i in range(4):
            n = blk * 4 + i
            for half in range(2):
                bk = 6 + half
                for f in range(8):
                    MM(bank(bk), mT[:, f * 512 + i * 128: f * 512 + (i + 1) * 128], wout_b[:, f * 1024 + half * 512: f * 1024 + (half + 1) * 512], f == 0, f == 7, [BmT, BWc], [PB[bk]], sig=(f == 7), add=(f > 0))
                TT("dve", xc[i][:, half * 512:(half + 1) * 512], bank(bk), xc[i][:, half * 512:(half + 1) * 512], ALU.add, [PB[bk], Bxc[i]], [Bxc[i]], add=(half > 0))
            fw.dma("sp", out_d[n * 128:(n + 1) * 128, :], xc[i][:], reads=[Bxc[i]], writes=[Bout[n // 16]], add=True)
            if debug:
                fw.dma("sp", dbg["dbg_x1"][n * 128:(n + 1) * 128, :], xc[i][:], reads=[Bxc[i]], is_output=True)
            ACT(cjk[:], xc[i][:], AF.Square, [Bxc[i]], [Bcst, Bhbc], accum_out=stat[:, 52:53])
            ACT(stat[:, 53:54], stat[:, 52:53], AF.Sqrt, [Bcst], [Bcst], scale=1.0 / D, bias=EPS)
            RECIP(stat[:, 54:55], stat[:, 53:54], [Bcst], [Bcst])
            STT(xc[i][:], xc[i][:], stat[:, 54:55], gffn_t[:], ALU.mult, ALU.mult, [Bxc[i], Bcst, Bc], [Bxc[i]])
            for k in range(8):
                bk = k // 4
                TR(bank(bk, (k % 4) * 128, (k % 4) * 128 + 128), xc[i][:, k * 128:(k + 1) * 128], ident_f[:], [Bxc[i], Bc], [PB[bk]], sig=(k % 4 == 3), add=(k % 4 > 0))
            CP("act", tT32[:, 0:512], bank(0), [PB[0], BsA, BsB], [BsA, BsB])
            CP("act", tT32[:, 512:1024], bank(1), [PB[1]], [BsA, BsB], add=True)
            for k in range(8):
                CP("pool", tT_ap(k, n * 128, 128), tT32[:, k * 128:(k + 1) * 128], [BsA], [Bgy[blk], BoT[blk]], add=True)
            for k in range(8):
                MM(bank(2, 0, 36), tT32[:, k * 128:(k + 1) * 128], wr32[:, k * 36:(k + 1) * 36], k == 0, k == 7, [BsA, Bc], [PB[2]], sig=(k == 7), add=(k > 0))
            rt = tt1
            Br = Bt1
            lgt = rt[:, 0:36]
            TT("dve", lgt, bank(2, 0, 36), br_t[:], ALU.add, [PB[2], Bc], [Br])
            gmax = rt[:, 40:41]
            RMAX(gmax, rt[:, 0:4], [Br], [Br], add=True)
            oh = rt[:, 44:48]
            TS("dve", oh, rt[:, 0:4], gmax, None, ALU.is_equal, None, [Br], [Br], add=True)
            TS("dve", rt[:, 48:52], rt[:, 0:4], gmax, None, ALU.subtract, None, [Br], [Br], add=True)
            ACT(rt[:, 48:52], rt[:, 48:52], AF.Exp, [Br], [Br])
            RSUM(rt[:, 52:53], rt[:, 48:52], [Br], [Br], add=True)
            RECIP(rt[:, 53:54], rt[:, 52:53], [Br], [Br])
            TS("dve", rt[:, 56:64], rt[:, 4:12], rt[:, 44:45], None, ALU.mult, None, [Br], [Br], add=True)
            for g in range(1, 4):
                STT(rt[:, 56:64], rt[:, 4 + g * 8: 12 + g * 8], rt[:, 44 + g: 45 + g], rt[:, 56:64], ALU.mult, ALU.add, [Br], [Br])
            m1 = rt[:, 64:65]
            RMAX(m1, rt[:, 56:64], [Br], [Br], add=True)
            mk1 = rt[:, 72:80]
            TS("dve", mk1, rt[:, 56:64], m1, None, ALU.is_equal, None, [Br], [Br], add=True)
            es2 = rt[:, 80:88]
            STT(es2, mk1, -1e30, rt[:, 56:64], ALU.mult, ALU.add, [Br], [Br], add=True)
            m2 = rt[:, 65:66]
            RMAX(m2, es2, [Br], [Br], add=True)
            mk2 = rt[:, 88:96]
            TS("dve", mk2, es2, m2, None, ALU.is_equal, None, [Br], [Br], add=True)
            TT("dve", rt[:, 66:67], m2, m1, ALU.subtract, [Br], [Br], add=True)
            ACT(rt[:, 66:67], rt[:, 66:67], AF.Exp, [Br], [Br])
            TS("dve", rt[:, 67:68], rt[:, 66:67], 1.0, None, ALU.add, None, [Br], [Br], add=True)
            RECIP(rt[:, 68:69], rt[:, 67:68], [Br], [Br])
            TS("dve", rt[:, 69:70], rt[:, 68:69], -1.0, 1.0, ALU.mult, ALU.add, [Br], [Br], add=True)
            TT("dve", rt[:, 68:69], rt[:, 68:69], rt[:, 53:54], ALU.mult, [Br], [Br])
            TT("dve", rt[:, 69:70], rt[:, 69:70], rt[:, 53:54], ALU.mult, [Br], [Br])
            ew = rt[:, 96:104]
            TS("dve", ew, mk1, rt[:, 68:69], None, ALU.mult, None, [Br], [Br], add=True)
            STT(ew, mk2, rt[:, 69:70], ew, ALU.mult, ALU.add, [Br], [Br])
            for g in range(4):
                TS("dve", comb_all[:, n * 32 + g * 8: n * 32 + (g + 1) * 8], ew, rt[:, 44 + g:45 + g], None, ALU.mult, None, [Br], [Bcomb], add=True)
    if debug:
        fw.dma("sp", dbg["dbg_comb"], comb_all[:], reads=[Bcomb], is_output=True)
    fw.barrier()

    if stop == 'pC':
        fw.finish()
        return nc
    acc = A("acc", [128, 16 * D], F32, R_Q)
    NWB = 3
    wgu = [A("wgu%d" % i, [128, 8 * 512], BF16, R_V + i * 12 * KB) for i in range(NWB)]
    wd_ = [A("wd%d" % i, [128, 2 * 1024], BF16, R_V + i * 12 * KB + 8 * KB) for i in range(NWB)]
    Bw = [Buf("w%d" % i) for i in range(NWB)]
    sgm = [A("sgm%d" % i, [128, 256], F32, R_V + 36 * KB + i * KB) for i in range(2)]
    hid = [A("hid%d" % i, [128, 256], BF16, R_V + 38 * KB + i * 512) for i in range(2)]
    hidT = [A("hidT%d" % i, [128, 256], BF16, R_V + 39 * KB + i * 512) for i in range(2)]
    Bsg = [Buf("sgm0"), Buf("sgm1")]; Bhid = [Buf("hid0"), Buf("hid1")]; BhidT = [Buf("hidT0"), Buf("hidT1")]
    Bacc = [Buf("acc%d" % i) for i in range(16)]
    BtT = Bgy + BoT
    items = [(hf, e, i) for hf in range(2) for e in range(32) for i in range(16)]
    NI = len(items)

    def wbuf(hf, e):
        return (hf * 32 + e) % NWB

    def stage_G(k):
        hf, e, i = items[k]
        n = hf * 16 + i
        bi = k % 2
        wi = wbuf(hf, e)
        if e == 0 and i == 0:
            for ii in range(16):
                nn = hf * 16 + ii
                fw.dma("sp", acc[:, ii * D:(ii + 1) * D], out_d[nn * 128:(nn + 1) * 128, :], reads=[Bout[hf]], writes=[Bacc[ii]])
        if i == 0:
            fw.dma("pool", V(wgu[wi], 0, [[512, 8], [1, 256]]), weg_d[e].rearrange("(k p) f -> p k f", p=128), writes=[Bw[wi]])
            fw.dma("pool", V(wgu[wi], 256, [[512, 8], [1, 256]]), weu_d[e].rearrange("(k p) f -> p k f", p=128), writes=[Bw[wi]], add=True)
            fw.dma("pool", V(wd_[wi], 0, [[1024, 2], [1, 1024]]), wed_d[e].rearrange("(k p) n -> p k n", p=128), writes=[Bw[wi]], add=True)
        for kk_ in range(8):
            MM(bank(bi), tT_ap(kk_, n * 128, 128), wgu[wi][:, kk_ * 512:(kk_ + 1) * 512], kk_ == 0, kk_ == 7, [BtT[n // 4], BtT[8 + n // 4], Bw[wi]], [PB[bi]], sig=(kk_ == 7), add=(kk_ > 0))
        ACT(sgm[bi][:], bank(bi, 0, 256), AF.Silu, [PB[bi]], [Bsg[bi]])
        STT(hid[bi][:], bank(bi, 256, 512), comb_all[:, n * 32 + e: n * 32 + e + 1], sgm[bi][:], ALU.mult, ALU.mult, [PB[bi], Bcomb, Bsg[bi]], [Bhid[bi]])

    def stage_T(k):
        bi = k % 2
        for f in range(2):
            TR(bankb(2 + bi, f * 128, f * 128 + 128), hid[bi][:, f * 128:(f + 1) * 128], ident_b[:], [Bhid[bi], Bc], [PB[2 + bi]], sig=(f == 1), add=(f > 0))
        CP("act", hidT[bi][:], bankb(2 + bi, 0, 256), [PB[2 + bi]], [BhidT[bi]])

    def stage_D(k):
        hf, e, i = items[k]
        bi = k % 2
        wi = wbuf(hf, e)
        for half in range(2):
            bkd = 4 + bi * 2 + half
            for f in range(2):
                MM(bank(bkd), hidT[bi][:, f * 128:(f + 1) * 128], wd_[wi][:, f * 1024 + half * 512: f * 1024 + (half + 1) * 512], f == 0, f == 1, [BhidT[bi], Bw[wi]], [PB[bkd]], sig=(f == 1), add=(f > 0))
            TT("dve", acc[:, i * D + half * 512: i * D + (half + 1) * 512], bank(bkd), acc[:, i * D + half * 512: i * D + (half + 1) * 512], ALU.add, [PB[bkd], Bacc[i]], [Bacc[i]], add=(half > 0))

    def flush_half(hf):
        for ii in range(16):
            nn = hf * 16 + ii
            fw.dma("sp", out_d[nn * 128:(nn + 1) * 128, :], acc[:, ii * D:(ii + 1) * D], reads=[Bacc[ii]], writes=[Bout[hf]], add=True, is_output=True)

    done_T = set()
    done_D = set()

    def do_T(k):
        if 0 <= k < NI and k not in done_T:
            done_T.add(k)
            stage_T(k)

    def do_D(k):
        if 0 <= k < NI and k not in done_D:
            done_D.add(k)
            stage_D(k)

    for k in range(-1, NI + 1):
        if 0 <= k + 1 < NI:
            if items[k + 1] == (1, 0, 0):
                do_T(k)
                do_D(k - 1)
                do_D(k)
                flush_half(0)
            stage_G(k + 1)
        do_T(k)
        do_D(k - 1)
    flush_half(1)
    fw.finish()
    return nc


_NC_CACHE = {}


def _prep_inputs(inputs, b):
    f = lambda a: np.ascontiguousarray(a, dtype=np.float32)
    m = {
        "x": f(inputs["x"][b]),
        "pos": np.ascontiguousarray(inputs["positions"][b].reshape(NT, 128).T.astype(np.int32)),
        "norm_mix_g": f(inputs["norm_mix_g"][0]),
        "w_in": f(inputs["w_in"][0]),
        "q_norm_g": f(inputs["q_norm_g"][0]), "k_norm_g": f(inputs["k_norm_g"][0]),
        "lambda_q1": f(inputs["lambda_q1"][0]), "lambda_k1": f(inputs["lambda_k1"][0]),
        "lambda_q2": f(inputs["lambda_q2"][0]), "lambda_k2": f(inputs["lambda_k2"][0]),
        "subln_g": f(inputs["subln_g"][0]),
        "w_o_attn": f(inputs["w_o_attn"][0]),
        "lamre_t": f(inputs["ssm_lambda_re"][0].T), "lamim_t": f(inputs["ssm_lambda_im"][0].T),
        "ssm_log_dt": f(inputs["ssm_log_dt"][0]),
        "bre_t": f(inputs["ssm_b_re"][0].transpose(1, 0, 2).reshape(64, 512)),
        "bim_t": f(inputs["ssm_b_im"][0].transpose(1, 0, 2).reshape(64, 512)),
        "cre_t": f(inputs["ssm_c_re"][0].transpose(2, 0, 1).reshape(64, 512)),
        "cim_t": f(inputs["ssm_c_im"][0].transpose(2, 0, 1).reshape(64, 512)),
        "d_t": f(inputs["ssm_d"][0].reshape(32, 16).T),
        "w_glu": f(inputs["w_glu"][0]),
        "w_out": f(inputs["w_out"][0]),
        "norm_ffn_g": f(inputs["norm_ffn_g"][0]),
        "w_router": f(np.concatenate([inputs["w_router_group"][0], inputs["w_router_expert"][0].reshape(D, 32)], axis=1)),
        "b_router": f(np.concatenate([inputs["b_router_group"][0], inputs["b_router_expert"][0].reshape(32)])),
        "w_expert_gate": f(inputs["w_expert_gate"][0].reshape(32, D, 256)),
        "w_expert_up": f(inputs["w_expert_up"][0].reshape(32, D, 256)),
        "w_expert_down": f(inputs["w_expert_down"][0].reshape(32, 256, D)),
    }
    return m


def kernel(**inputs):
    inputs = {k: np.asarray(v) for k, v in inputs.items()}
    nb = inputs["x"].shape[0]
    nc = build_program(debug=False)
    shared = _prep_inputs(inputs, 0)
    in_maps = []
    for b in range(nb):
        m = dict(shared)
        m["x"] = np.ascontiguousarray(inputs["x"][b], dtype=np.float32)
        m["pos"] = np.ascontiguousarray(inputs["positions"][b].reshape(NT, 128).T.astype(np.int32))
        in_maps.append(m)
    res = run_bass_kernel_spmd(nc, in_maps, core_ids=list(range(nb)))
    out = np.stack([np.asarray(r["out"]).reshape(S, D) for r in res.results], axis=0)
    return out.astype(np.float32)
```

```python
import contextlib
import math
import os
import numpy as np
import concourse.bass as bass
import concourse.mybir as mybir
from concourse.bass_utils import run_bass_kernel_spmd

F32 = mybir.dt.float32
BF16 = mybir.dt.bfloat16
I32 = mybir.dt.int32
AF = mybir.ActivationFunctionType
ALU = mybir.AluOpType
AX = mybir.AxisListType

SEM_LIMIT = 30000
S = 4096
D = 1024
NT = 32
EPS = 1e-6
SB_BASE = 17408
LAM_INIT = 0.8 - 0.6 * math.exp(-0.3 * 0)


class Buf:
    __slots__ = ("name", "w", "r", "pr")

    def __init__(self, name=""):
        self.name = name
        self.w = {}
        self.r = {}
        self.pr = {}


class Eng:
    def __init__(self, name):
        self.name = name
        self.ops = []
        self.cnt = 0
        self.semidx = 0
        self.waited = {}
        self.pending = []
        self.last = None

    @property
    def semkey(self):
        return "%s_%d" % (self.name, self.semidx)


class FW:
    def __init__(self, nc, n_dma_sems=32):
        self.nc = nc
        self.stack = contextlib.ExitStack()
        self.engs = {n: Eng(n) for n in ("pe", "act", "dve", "pool", "sp")}
        self.sems = {}
        names = ["dma%d" % i for i in range(n_dma_sems)]
        self.dma_sem_val = {n: 0 for n in names}
        self.dma_rr = {"sp": 0, "pool": 0}
        k = n_dma_sems // 2
        self.dma_pool_of = {"sp": names[:k], "pool": names[k:]}
        self.out_tokens = []

    def sem(self, key):
        if key not in self.sems:
            self.sems[key] = self.stack.enter_context(self.nc.semaphore(key))
        return self.sems[key]

    def psum(self, name, shape, dt):
        return self.stack.enter_context(self.nc.psum_tensor(name, list(shape), dt))

    def _wait(self, eng, tok):
        key, val = tok[0], tok[1]
        assert val is not None, "wait on unsignalled token"
        if eng.waited.get(key, 0) >= val:
            return
        eng.waited[key] = val
        self.sem(key)
        eng.ops.append(("wait", key, val))

    def _deps(self, eng, reads, writes, add):
        toks = []
        for b in reads:
            toks.extend(b.w.values())
        for b in writes:
            if add:
                toks.extend(b.pr.values())
            else:
                toks.extend(b.w.values())
            toks.extend(b.r.values())
        for t in toks:
            if eng.name == "pe" and t[0].startswith("pe_"):
                continue
            self._wait(eng, t)

    def _update(self, key, tok, reads, writes, add):
        for b in reads:
            b.r[key] = tok
        for b in writes:
            if add:
                for k_, v_ in b.r.items():
                    b.pr["r:" + k_] = v_
                b.w[key] = tok
            else:
                npr = {}
                for k_, v_ in b.w.items():
                    npr["w:" + k_] = v_
                for k_, v_ in b.r.items():
                    npr["r:" + k_] = v_
                b.pr = npr
                b.w = {key: tok}
            b.r = {}

    def op(self, engname, fn, reads=(), writes=(), sig=True, add=False):
        eng = self.engs[engname]
        self._deps(eng, reads, writes, add)
        if sig:
            if eng.cnt >= SEM_LIMIT:
                eng.semidx += 1
                eng.cnt = 0
            eng.cnt += 1
            tok = [eng.semkey, eng.cnt]
            self.sem(tok[0])
            for p in eng.pending:
                p[0], p[1] = tok[0], tok[1]
            eng.pending = []
            eng.last = tok
            key = tok[0]
        else:
            tok = ["pe_pending", None]
            eng.pending.append(tok)
            key = "pe_pend"
        eng.ops.append(("op", fn, tok if sig else None))
        self._update(key, tok, reads, writes, add)
        return tok

    def dma(self, qname, out, in_, reads=(), writes=(), add=False, is_output=False, **kw):
        eng = self.engs[qname]
        self._deps(eng, reads, writes, add)
        pool = self.dma_pool_of[qname]
        name = pool[self.dma_rr[qname] % len(pool)]
        self.dma_rr[qname] += 1
        prev = self.dma_sem_val[name]
        if prev > 0:
            self._wait(eng, [name, prev])
        val = prev + 16
        self.dma_sem_val[name] = val
        tok = [name, val]
        self.sem(name)

        def fn(e, out=out, in_=in_, kw=kw):
            return e.dma_start(out=out, in_=in_, **kw)
        eng.ops.append(("op", fn, tok))
        self._update(name, tok, reads, writes, add)
        if is_output:
            self.out_tokens.append(tok)
        return tok

    def barrier(self):
        toks = [e.last for e in self.engs.values() if e.last is not None]
        for e in self.engs.values():
            assert not e.pending
        toks += [[n, v] for n, v in self.dma_sem_val.items() if v > 0]
        for e in self.engs.values():
            for t in toks:
                if e.name == "pe" and t[0].startswith("pe_"):
                    continue
                self._wait(e, t)

    def finish(self):
        sp = self.engs["sp"]
        for t in self.out_tokens:
            self._wait(sp, t)
        nc = self.nc
        sems = self.sems
        engs = self.engs

        def replay(e, eng):
            for o in eng.ops:
                if o[0] == "wait":
                    e.wait_ge(sems[o[1]], o[2])
                else:
                    inst = o[1](e)
                    if o[2] is not None:
                        key = o[2][0]
                        inst.then_inc(sems[key], 16 if key.startswith("dma") else 1)

        with nc.Block() as block:
            @block.tensor
            def _(e):
                replay(e, engs["pe"])

            @block.scalar
            def _(e):
                replay(e, engs["act"])

            @block.vector
            def _(e):
                replay(e, engs["dve"])

            @block.gpsimd
            def _(e):
                replay(e, engs["pool"])

            @block.sync
            def _(e):
                replay(e, engs["sp"])
        self.stack.close()


def build_program(debug=False, stop=None):
    nc = bass.Bass("TRN2", target_bir_lowering=False)
    fw = FW(nc)

    def din(name, shape, dt=F32):
        return nc.dram_tensor(name, list(shape), dt, kind="ExternalInput")

    x_d = din("x", [S, D]).ap()
    pos_d = din("pos", [128, NT], I32).ap()
    gmix_d = din("norm_mix_g", [D])
    w_in_d = din("w_in", [D, 4096]).ap()
    qg_d = din("q_norm_g", [64])
    kg_d = din("k_norm_g", [64])
    lq1_d = din("lambda_q1", [64]); lk1_d = din("lambda_k1", [64])
    lq2_d = din("lambda_q2", [64]); lk2_d = din("lambda_k2", [64])
    subg_d = din("subln_g", [128])
    wo_d = din("w_o_attn", [512, D]).ap()
    lamre_d = din("lamre_t", [64, 32]).ap(); lamim_d = din("lamim_t", [64, 32]).ap()
    logdt_d = din("ssm_log_dt", [32])
    bre_d = din("bre_t", [64, 512]).ap(); bim_d = din("bim_t", [64, 512]).ap()
    cre_d = din("cre_t", [64, 512]).ap(); cim_d = din("cim_t", [64, 512]).ap()
    dsk_d = din("d_t", [16, 32]).ap()
    wglu_d = din("w_glu", [512, 2048]).ap()
    wout_d = din("w_out", [D, D]).ap()
    gffn_d = din("norm_ffn_g", [D])
    wr_d = din("w_router", [D, 36]).ap()
    br_d = din("b_router", [36])
    weg_d = din("w_expert_gate", [32, D, 256]).ap()
    weu_d = din("w_expert_up", [32, D, 256]).ap()
    wed_d = din("w_expert_down", [32, 256, D]).ap()
    out_d = nc.dram_tensor("out", [S, D], F32, kind="ExternalOutput").ap()
    dbg = {}
    if debug:
        lst = [("dbg_x1", [S, D]), ("dbg_comb", [128, NT * 32]), ("dbg_T", [128, 4096]), ("dbg_ks", [128, 16 * 18]), ("dbg_ug", [128, 32 * 512])]
        for nm_ in ("dbg_gy", "dbg_o", "dbg_q", "dbg_k"):
            lst += [(nm_ + str(q_), [128, S]) for q_ in range(4)]
        for nm, shp in lst:
            dbg[nm] = nc.dram_tensor(nm, shp, F32, kind="ExternalOutput").ap()

    def bc_rows(t, n, reps=1, parts=128):
        if reps == 1:
            return bass.AP(t, 0, [[0, parts], [1, n]])
        return bass.AP(t, 0, [[0, parts], [0, reps], [1, n]])

    KB = 1024

    def A(name, shape, dt, off):
        nbytes = int(np.prod(shape[1:])) * (2 if dt == BF16 else 4)
        assert SB_BASE + off + nbytes <= 229376 - 32, (name, off, nbytes)
        return nc.alloc_sbuf_tensor_at(name, list(shape), dt, offset=SB_BASE + off)

    c_off = [0]

    def CA(name, shape, dt):
        n = int(np.prod(shape[1:])) * (2 if dt == BF16 else 4)
        t = A(name, shape, dt, c_off[0])
        c_off[0] += (n + 31) // 32 * 32
        return t

    ident_f = CA("ident_f", [128, 128], F32)
    ident_b = CA("ident_b", [128, 128], BF16)
    maskf = CA("maskf", [128, 128], F32)
    maskneg_b = CA("maskneg_b", [128, 128], BF16)
    gq_t = CA("gq_t", [128, 512], F32)
    gk_t = CA("gk_t", [128, 512], F32)
    sg08_t = CA("sg08_t", [128, 128], F32)
    gmix_t = CA("gmix_t", [128, D], F32)
    gffn_t = CA("gffn_t", [128, D], F32)
    cos_t = CA("cos_t", [128, NT * 8], F32)
    sin_t = CA("sin_t", [128, NT * 8], F32)
    lamv = CA("lamv", [128, 8], F32)
    comb_all = CA("comb_all", [128, NT * 32], F32)
    wr32 = CA("wr32", [128, 8 * 36], F32)
    br_t = CA("br_t", [128, 36], F32)
    ks_are = CA("ks_are", [128, 16 * 9], F32)
    ks_aim = CA("ks_aim", [128, 16 * 9], F32)
    ks_naim = CA("ks_naim", [128, 16 * 9], F32)
    dvec = CA("dvec", [128, 32], F32)
    stat = CA("stat", [128, 64], F32)
    ones_f = CA("ones_f", [128, 128], F32)
    assert c_off[0] <= 24 * KB, c_off[0]
    M0 = 24 * KB
    R_G, R_O, R_Q, R_K, R_V, R_T = M0, M0 + 32 * KB, M0 + 64 * KB, M0 + 96 * KB, M0 + 128 * KB, M0 + 161 * KB
    R_END = 229376 - SB_BASE - 64

    pp = fw.psum("pp", [128, 8 * 512], F32)
    ppb = pp.bitcast(BF16)
    PB = [Buf("psum%d" % i) for i in range(8)]

    def bank(i, a=0, b=512):
        return pp[:, i * 512 + a:i * 512 + b]

    def bankb(i, a=0, b=1024):
        return ppb[:, i * 1024 + a:i * 1024 + b]

    def MM(out, lhsT, rhs, start, stop, r, w, sig=True, add=False, skip=False):
        if skip:
            return fw.op("pe", lambda e: e.matmul(out, lhsT, rhs, start=start, stop=stop, skip_group_check=True), reads=r, writes=w, sig=sig, add=add)
        return fw.op("pe", lambda e: e.matmul(out, lhsT, rhs, start=start, stop=stop), reads=r, writes=w, sig=sig, add=add)

    def TR(out, in_, ident, r, w, sig=True, add=False):
        return fw.op("pe", lambda e: e.transpose(out, in_, ident), reads=r, writes=w, sig=sig, add=add)

    def ACT(out, in_, func, r, w, add=False, **kw):
        return fw.op("act", lambda e: e.activation(out, in_, func, **kw), reads=r, writes=w, add=add)

    def TT(eng, out, in0, in1, op, r, w, add=False):
        return fw.op(eng, lambda e: e.tensor_tensor(out, in0, in1, op), reads=r, writes=w, add=add)

    def TS(eng, out, in0, s1, s2, op0, op1, r, w, add=False):
        if s2 is None:
            return fw.op(eng, lambda e: e.tensor_scalar(out, in0, s1, None, op0), reads=r, writes=w, add=add)
        return fw.op(eng, lambda e: e.tensor_scalar(out, in0, s1, s2, op0, op1), reads=r, writes=w, add=add)

    def STT(out, in0, sc, in1, op0, op1, r, w, add=False):
        return fw.op("dve", lambda e: e.scalar_tensor_tensor(out, in0, sc, in1, op0, op1), reads=r, writes=w, add=add)

    def CP(eng, out, in_, r, w, add=False):
        if eng == "act":
            return ACT(out, in_, AF.Copy, r, w, add=add)
        return fw.op(eng, lambda e: e.tensor_copy(out, in_), reads=r, writes=w, add=add)

    def RECIP(out, in_, r, w, add=False):
        return fw.op("dve", lambda e: e.reciprocal(out, in_), reads=r, writes=w, add=add)

    def RSUM(out, in_, r, w, add=False):
        return fw.op("dve", lambda e: e.reduce_sum(out, in_, axis=AX.X), reads=r, writes=w, add=add)

    def RMAX(out, in_, r, w, add=False):
        return fw.op("dve", lambda e: e.reduce_max(out, in_, axis=AX.X), reads=r, writes=w, add=add)

    def MEMSET(eng, out, val, r, w, add=False):
        return fw.op(eng, lambda e: e.memset(out, val), reads=r, writes=w, add=add)

    def V(t, off, dims):
        pstride = int(np.prod(t.shape[1:]))
        return bass.AP(t, off, [[pstride, t.shape[0]]] + [list(d) for d in dims])

    def VP(t, p0, pn, off, dims):
        pstride = int(np.prod(t.shape[1:]))
        return bass.AP(t, p0 * pstride + off, [[pstride, pn]] + [list(d) for d in dims])

    Bc = Buf("consts")
    MEMSET("pool", ident_f[:], 1.0, [], [Bc])
    fw.op("pool", lambda e: e.affine_select(ident_f[:], ident_f[:], pattern=[[-1, 128]], compare_op=ALU.is_equal, fill=0.0, base=0, channel_multiplier=1), reads=[Bc], writes=[Bc])
    CP("pool", ident_b[:], ident_f[:], [Bc], [Bc])
    MEMSET("pool", maskf[:], 0.0, [Bc], [Bc])
    fw.op("pool", lambda e: e.affine_select(maskf[:], maskf[:], pattern=[[1, 128]], compare_op=ALU.is_ge, fill=-30000.0, base=0, channel_multiplier=-1), reads=[Bc], writes=[Bc])
    CP("pool", maskneg_b[:], maskf[:], [Bc], [Bc])
    fw.dma("sp", gq_t[:], bc_rows(qg_d, 64, 8), writes=[Bc], add=True)
    fw.dma("sp", gk_t[:], bc_rows(kg_d, 64, 8), writes=[Bc], add=True)
    fw.dma("sp", sg08_t[:], bc_rows(subg_d, 128), writes=[Bc], add=True)
    fw.dma("sp", lamv[:, 4:5], bass.AP(subg_d, 0, [[1, 128], [1, 1]]), writes=[Bc], add=True)
    MEMSET("pool", ones_f[:], 1.0, [], [Bc], add=True)
    fw.dma("sp", gmix_t[:], bc_rows(gmix_d, D), writes=[Bc], add=True)
    fw.dma("sp", gffn_t[:], bc_rows(gffn_d, D), writes=[Bc], add=True)
    fw.dma("sp", br_t[:], bc_rows(br_d, 36), writes=[Bc], add=True)
    fw.dma("sp", wr32[:], wr_d.rearrange("(k p) n -> p k n", p=128), writes=[Bc], add=True)
    for hh in range(8):
        fw.dma("sp", dvec[hh * 16:(hh + 1) * 16, :], dsk_d, writes=[Bc], add=True)
    tmp0 = A("c_tmp0", [128, 4 * 64], F32, R_T)
    posi = A("c_posi", [128, NT], I32, R_T + 1 * KB)
    posf = A("c_posf", [128, NT], F32, R_T + 1 * KB + 128)
    ang = A("c_ang", [128, NT * 8], F32, R_T + 2 * KB)
    ang2 = A("c_ang2", [128, NT * 8], F32, R_T + 3 * KB)
    ang3 = A("c_ang3", [128, NT * 8], F32, R_T + 4 * KB)
    Bt = Buf("ctmp")
    for i, dd in enumerate((lq1_d, lk1_d, lq2_d, lk2_d)):
        fw.dma("sp", tmp0[:, i * 64:(i + 1) * 64], bc_rows(dd, 64), writes=[Bt], add=True)
    fw.dma("sp", posi[:], pos_d, writes=[Bt], add=True)
    TS("dve", sg08_t[:], sg08_t[:], 1.0 - LAM_INIT, None, ALU.mult, None, [Bc], [Bc])
    TS("dve", lamv[:, 4:5], lamv[:, 4:5], 1.0 - LAM_INIT, None, ALU.mult, None, [Bc], [Bc])
    TT("dve", tmp0[:, 0:64], tmp0[:, 0:64], tmp0[:, 64:128], ALU.mult, [Bt], [Bt])
    TT("dve", tmp0[:, 128:192], tmp0[:, 128:192], tmp0[:, 192:256], ALU.mult, [Bt], [Bt])
    RSUM(lamv[:, 0:1], tmp0[:, 0:64], [Bt], [Bc])
    RSUM(lamv[:, 1:2], tmp0[:, 128:192], [Bt], [Bc])
    ACT(lamv[:, 0:2], lamv[:, 0:2], AF.Exp, [Bc], [Bc])
    TT("dve", lamv[:, 2:3], lamv[:, 1:2], lamv[:, 0:1], ALU.subtract, [Bc], [Bc])
    TS("dve", lamv[:, 3:4], lamv[:, 2:3], -LAM_INIT, None, ALU.add, None, [Bc], [Bc])
    CP("dve", posf[:], posi[:], [Bt], [Bt])
    for i in range(8):
        inv = (500000.0 ** (-i / 8.0)) / (2.0 * math.pi)
        TS("dve", V(ang, i, [[8, NT]]), posf[:], inv, None, ALU.mult, None, [Bt], [Bt], add=True)
    MAGIC = 12582912.0
    for (dst, shift) in ((sin_t, 0.0), (cos_t, 0.25)):
        TS("dve", ang2[:], ang[:], shift, None, ALU.add, None, [Bt], [Bt])
        TS("dve", ang3[:], ang2[:], MAGIC, MAGIC, ALU.add, ALU.subtract, [Bt], [Bt])
        TT("dve", ang2[:], ang2[:], ang3[:], ALU.subtract, [Bt], [Bt])
        ACT(dst[:], ang2[:], AF.Sin, [Bt], [Bc], scale=6.283185)

    if stop == 'p0a':
        fw.finish()
        return nc
    T_b = A("T_b", [128, 32 * 128], BF16, R_Q)
    VTre_b = A("VTre_b", [128, 32 * 64], BF16, R_Q + 8 * KB)
    VTim_b = A("VTim_b", [128, 32 * 64], BF16, R_Q + 12 * KB)
    Wre_b = A("Wre_b", [128, 32 * 128], BF16, R_Q + 16 * KB)
    Wimn_b = A("Wimn_b", [128, 32 * 128], BF16, R_Q + 24 * KB)
    Bs5w = Buf("s5w")
    Gre = A("Gre_", [128, 4096], F32, M0 + 0)
    Gim = A("Gim_", [128, 4096], F32, M0 + 16 * KB)
    HHre = A("HHre_", [128, 32 * 144], F32, M0 + 32 * KB)
    HHim = A("HHim_", [128, 32 * 144], F32, M0 + 96 * KB)
    VVre = A("VVre_", [128, 4096], F32, M0 + 114 * KB)
    VVim = A("VVim_", [128, 4096], F32, M0 + 130 * KB)
    GS = A("GS_", [128, 4096], F32, M0 + 146 * KB)
    HS = A("HS_", [128, 4096], F32, M0 + 162 * KB)
    so = [M0 + 50 * KB]

    def SA(name, n):
        t = A(name, [128, n], F32, so[0])
        so[0] += n * 4
        return t
    lre = SA("lre", 32); lim = SA("lim", 32); dtt = SA("dtt", 32); ar = SA("ar", 32); ai = SA("ai", 32)
    mm_ = SA("mm_", 32); minv = SA("minv", 32); kk = SA("kk", 32); rr = SA("rr", 32); x8 = SA("x8", 32); x2 = SA("x2", 32)
    pp_ = SA("pp_", 32); cc = SA("cc", 32); ss_ = SA("ss_", 32); t1 = SA("t1", 32); t2 = SA("t2", 32); t3 = SA("t3", 32); t4 = SA("t4", 32)
    LPre = SA("LPre", 32 * 9); LPim = SA("LPim", 32 * 9); LIre = SA("LIre", 32 * 8); LIim = SA("LIim", 32 * 8)
    Are = SA("Are", 32 * 9); Aim = SA("Aim", 32 * 9)
    fre = SA("fre", 32); fim = SA("fim", 32); ire = SA("ire", 32); iim = SA("iim", 32); den = SA("den", 32)
    assert so[0] <= M0 + 64 * KB
    Bin = A("Bin_re", [128, 512], F32, M0 + 178 * KB)
    Bin_im = A("Bin_im", [128, 512], F32, M0 + 180 * KB)
    Cre = A("Cre_in", [128, 512], F32, M0 + 146 * KB)
    Cim = A("Cim_in", [128, 512], F32, M0 + 148 * KB)
    Bbre = A("Bbre", [128, 512], F32, M0 + 150 * KB)
    Bbim = A("Bbim", [128, 512], F32, M0 + 152 * KB)
    W1 = A("W1", [128, 4608], F32, M0 + 114 * KB)
    W2 = A("W2_", [128, 4608], F32, M0 + 154 * KB)
    Bp = Buf("s5prep")

    for half in range(2):
        ps_ = slice(half * 64, half * 64 + 64)
        fw.dma("sp", lre[ps_, :], lamre_d, writes=[Bp], add=True)
        fw.dma("sp", lim[ps_, :], lamim_d, writes=[Bp], add=True)
        fw.dma("sp", Bin[ps_, :], bre_d, writes=[Bp], add=True)
        fw.dma("sp", Bin_im[ps_, :], bim_d, writes=[Bp], add=True)
        fw.dma("sp", Cre[ps_, :], cre_d, writes=[Bp], add=True)
        fw.dma("sp", Cim[ps_, :], cim_d, writes=[Bp], add=True)
    fw.dma("sp", dtt[:], bc_rows(logdt_d, 32), writes=[Bp], add=True)

    def d_tt(out, a, b, op):
        return TT("dve", out, a, b, op, [Bp], [Bp])

    def d_ts(out, a, s1, s2=None, op0=ALU.mult, op1=ALU.add):
        return TS("dve", out, a, s1, s2, op0, op1, [Bp], [Bp])

    def cmul(ore, oim, are_, aim_, bre_, bim_, ta, tb):
        d_tt(ta, are_, bre_, ALU.mult)
        d_tt(tb, aim_, bim_, ALU.mult)
        d_tt(ore, ta, tb, ALU.subtract)
        d_tt(ta, are_, bim_, ALU.mult)
        d_tt(tb, aim_, bre_, ALU.mult)
        d_tt(oim, ta, tb, ALU.add)

    ACT(dtt[:], dtt[:], AF.Exp, [Bp], [Bp])
    d_tt(ar[:], lre[:], dtt[:], ALU.mult)
    d_tt(ai[:], lim[:], dtt[:], ALU.mult)
    MEMSET("dve", mm_[:], 1.0, [Bp], [Bp])
    for k in range(10, 0, -1):
        d_tt(mm_[:], mm_[:], ar[:], ALU.mult)
        d_ts(mm_[:], mm_[:], 1.0 / k, 1.0)
    RECIP(minv[:], mm_[:], [Bp], [Bp])
    d_ts(kk[:], ai[:], 1.0 / (2.0 * math.pi), None)
    d_ts(kk[:], kk[:], MAGIC, MAGIC, ALU.add, ALU.subtract)
    STT(rr[:], kk[:], -6.28125, ai[:], ALU.mult, ALU.add, [Bp], [Bp])
    STT(rr[:], kk[:], -(2.0 * math.pi - 6.28125), rr[:], ALU.mult, ALU.add, [Bp], [Bp])
    d_ts(x8[:], rr[:], 0.125, None)
    d_tt(x2[:], x8[:], x8[:], ALU.mult)
    sc_ = [1.0, -1.0 / 6, 1.0 / 120, -1.0 / 5040, 1.0 / 362880, -1.0 / 39916800]
    cc_ = [1.0, -0.5, 1.0 / 24, -1.0 / 720, 1.0 / 40320, -1.0 / 3628800, 1.0 / 479001600]
    for (dst, co) in ((ss_, sc_), (cc, cc_)):
        MEMSET("dve", dst[:], co[-1], [Bp], [Bp])
        for c in co[-2::-1]:
            d_tt(dst[:], dst[:], x2[:], ALU.mult)
            d_ts(dst[:], dst[:], c, None, ALU.add)
    d_tt(ss_[:], ss_[:], x8[:], ALU.mult)
    for _ in range(3):
        d_tt(t1[:], cc[:], cc[:], ALU.mult)
        d_tt(t2[:], ss_[:], ss_[:], ALU.mult)
        STT(t3[:], ss_[:], 2.0, cc[:], ALU.mult, ALU.mult, [Bp], [Bp])
        d_tt(cc[:], t1[:], t2[:], ALU.subtract)
        CP("dve", ss_[:], t3[:], [Bp], [Bp])
    def LPv(t, j):
        return V(t, j, [[9, 32]])

    def LIv(t, j):
        return V(t, j, [[8, 32]])
    MEMSET("dve", LPv(LPre, 0), 1.0, [Bp], [Bp])
    MEMSET("dve", LPv(LPim, 0), 0.0, [Bp], [Bp])
    d_tt(LPv(LPre, 1), mm_[:], cc[:], ALU.mult)
    d_tt(LPv(LPim, 1), mm_[:], ss_[:], ALU.mult)
    for j in range(2, 9):
        cmul(LPv(LPre, j), LPv(LPim, j), LPv(LPre, j - 1), LPv(LPim, j - 1), LPv(LPre, 1), LPv(LPim, 1), t1[:], t2[:])
    MEMSET("dve", LIv(LIre, 0), 1.0, [Bp], [Bp])
    MEMSET("dve", LIv(LIim, 0), 0.0, [Bp], [Bp])
    d_tt(LIv(LIre, 1), minv[:], cc[:], ALU.mult)
    d_tt(t3[:], minv[:], ss_[:], ALU.mult)
    d_ts(LIv(LIim, 1), t3[:], -1.0, None)
    for j in range(2, 8):
        cmul(LIv(LIre, j), LIv(LIim, j), LIv(LIre, j - 1), LIv(LIim, j - 1), LIv(LIre, 1), LIv(LIim, 1), t1[:], t2[:])
    CP("dve", LPv(Are, 0), LPv(LPre, 8), [Bp], [Bp])
    CP("dve", LPv(Aim, 0), LPv(LPim, 8), [Bp], [Bp])
    for k in range(1, 9):
        d_tt(t1[:], LPv(Are, k - 1), LPv(Are, k - 1), ALU.mult)
        d_tt(t2[:], LPv(Aim, k - 1), LPv(Aim, k - 1), ALU.mult)
        d_tt(LPv(Are, k), t1[:], t2[:], ALU.subtract)
        STT(LPv(Aim, k), LPv(Are, k - 1), 2.0, LPv(Aim, k - 1), ALU.mult, ALU.mult, [Bp], [Bp])
    for gl in range(2):
        for (src, dst) in ((Are, ks_are), (Aim, ks_aim)):
            fw.op("dve", lambda e, src=src, dst=dst, gl=gl: e.tensor_copy(
                VP(dst, gl * 64, 64, 0, [[9, 16], [1, 9]]), VP(src, gl * 64, 64, gl * 9, [[18, 16], [1, 9]])), reads=[Bp], writes=[Bc], add=True)
    TS("dve", ks_naim[:], ks_aim[:], -1.0, None, ALU.mult, None, [Bc], [Bc])
    d_ts(t1[:], LPv(LPre, 1), -1.0, None, ALU.add)
    d_tt(den[:], lre[:], lre[:], ALU.mult)
    d_tt(t2[:], lim[:], lim[:], ALU.mult)
    d_tt(den[:], den[:], t2[:], ALU.add)
    RECIP(den[:], den[:], [Bp], [Bp])
    d_tt(ire[:], lre[:], den[:], ALU.mult)
    d_tt(iim[:], lim[:], den[:], ALU.mult)
    d_ts(iim[:], iim[:], -1.0, None)
    cmul(fre[:], fim[:], t1[:], LPv(LPim, 1), ire[:], iim[:], t3[:], t4[:])
    def bc16(t):
        return V(t, 0, [[1, 32], [0, 16]])

    def v3(t):
        return V(t, 0, [[16, 32], [1, 16]])
    w1a = V(W1, 0, [[16, 32], [1, 16]]); w1b = V(W1, 512, [[16, 32], [1, 16]])
    cmul(v3(Bbre), v3(Bbim), bc16(fre), bc16(fim), v3(Bin), v3(Bin_im), w1a, w1b)
    def g4(t):
        return V(t, 0, [[128, 32], [16, 8], [1, 16]])

    def li4(t):
        return V(t, 0, [[8, 32], [1, 8], [0, 16]])

    def bb4(t):
        return V(t, 0, [[16, 32], [0, 8], [1, 16]])
    cmul(g4(Gre), g4(Gim), li4(LIre), li4(LIim), bb4(Bbre), bb4(Bbim), g4(W1), g4(W2))
    def h4(t):
        return V(t, 0, [[144, 32], [16, 9], [1, 16]])

    def lp4(t):
        return V(t, 0, [[9, 32], [1, 9], [0, 16]])

    def c4(t):
        return V(t, 0, [[16, 32], [0, 9], [1, 16]])
    cmul(h4(HHre), h4(HHim), lp4(LPre), lp4(LPim), c4(Cre), c4(Cim), h4(W1), h4(W2))
    def hs4(t, j0):
        return V(t, j0 * 16, [[144, 32], [1, 128]])
    CP("dve", V(Wre_b, 0, [[128, 32], [1, 128]]), hs4(HHre, 1), [Bp], [Bs5w], add=True)
    TS("dve", V(Wimn_b, 0, [[128, 32], [1, 128]]), hs4(HHim, 1), -1.0, None, ALU.mult, None, [Bp], [Bs5w], add=True)
    def l7(t):
        return V(t, 7, [[9, 32], [0, 128]])

    def g3(t):
        return V(t, 0, [[128, 32], [1, 128]])
    Bvv = Buf("vv")
    cmul(g3(VVre), g3(VVim), l7(LPre), l7(LPim), g3(Gre), g3(Gim), g3(GS), g3(HS))
    CP("dve", VP(GS, 0, 64, 0, [[1, 4096]]), VP(Gre, 0, 64, 0, [[1, 4096]]), [Bp], [Bp])
    fw.op("dve", lambda e: e.tensor_scalar(VP(GS, 64, 64, 0, [[1, 4096]]), VP(Gim, 64, 64, 0, [[1, 4096]]), -1.0, None, ALU.mult), reads=[Bp], writes=[Bp])
    CP("dve", VP(HS, 0, 64, 0, [[128, 32], [1, 128]]), VP(HHre, 0, 64, 0, [[144, 32], [1, 128]]), [Bp], [Bp])
    CP("dve", VP(HS, 64, 64, 0, [[128, 32], [1, 128]]), VP(HHim, 64, 64, 0, [[144, 32], [1, 128]]), [Bp], [Bp])
    mask4 = A("mask4", [128, 512], F32, M0 + 178 * KB)
    MEMSET("pool", mask4[:], 1.0, [Bp], [Bp])
    fw.op("pool", lambda e: e.affine_select(mask4[:], mask4[:], pattern=[[0, 4], [16, 8], [0, 16]], compare_op=ALU.is_ge, fill=0.0, base=15, channel_multiplier=-1), reads=[Bp], writes=[Bp])
    Tm = A("Tm", [128, 512], F32, M0 + 180 * KB)
    for q4 in range(8):
        bk = q4 % 2
        for gi in range(4):
            g = q4 * 4 + gi
            MM(bank(bk, gi * 128, gi * 128 + 128), GS[:, g * 128:(g + 1) * 128], HS[:, g * 128:(g + 1) * 128], True, True, [Bp], [PB[bk]], sig=(gi == 3), add=(gi > 0))
        TT("dve", Tm[:], bank(bk), mask4[:], ALU.mult, [PB[bk], Bp], [Bp])
        for gi in range(4):
            g = q4 * 4 + gi
            STT(T_b[:, g * 128:(g + 1) * 128], ident_f[:], dvec[:, g:g + 1], Tm[:, gi * 128:(gi + 1) * 128], ALU.mult, ALU.add, [Bp, Bc], [Bs5w], add=True)
    for (src, dst) in ((VVre, VTre_b), (VVim, VTim_b)):
        for q4 in range(8):
            bk = 2 + q4 % 2
            for gi in range(4):
                g = q4 * 4 + gi
                TR(bank(bk, gi * 128, gi * 128 + 128), src[:, g * 128:(g + 1) * 128], ident_f[:], [Bp, Bc], [PB[bk]], sig=(gi == 3), add=(gi > 0))
            CP("act", V(dst, q4 * 256, [[64, 4], [1, 64]]), bass.AP(pp, bk * 512, [[4096, 128], [128, 4], [1, 64]]), [PB[bk]], [Bs5w], add=True)
    if debug:
        dtmp = A("dtmp", [128, 4096], F32, M0 + 0)
        fw.barrier()
        CP("dve", dtmp[:], T_b[:], [Bs5w], [Bp])
        fw.dma("sp", dbg["dbg_T"], dtmp[:], reads=[Bp], is_output=True)
        dks = A("dks", [128, 288], F32, M0 + 16 * KB)
        CP("dve", dks[:, 0:144], ks_are[:], [Bc], [Bp])
        CP("dve", dks[:, 144:288], ks_aim[:], [Bc], [Bp])
        fw.dma("sp", dbg["dbg_ks"], dks[:], reads=[Bp], is_output=True)
    fw.barrier()

    if stop == 'p0b':
        fw.finish()
        return nc
    def rms_tile(xt, hbt, jk, st, Bx, Bh, Bst, rows, gt=gmix_t, out_dt_bf=True):
        ACT(jk, xt, AF.Square, [Bx], [Bst, Bh], accum_out=st[:, 0:1])
        ACT(st[:, 1:2], st[:, 0:1], AF.Sqrt, [Bst], [Bst], scale=1.0 / D, bias=EPS)
        RECIP(st[:, 2:3], st[:, 1:2], [Bst], [Bst])
        STT(hbt, xt, st[:, 2:3], gt[:], ALU.mult, ALU.mult, [Bx, Bst, Bc], [Bh])

    gyT = A("gyT", [128, 4 * S], BF16, R_G)
    Wu_b = A("Wu_b", [128, 8 * 512], BF16, R_O)
    hT_sb = A("hT_sb", [128, 8 * 1024], BF16, R_O + 8 * KB)
    U_tok = A("U_tok", [128, 4 * 4096], BF16, R_K)
    Ug = A("Ug", [128, 32 * 512], BF16, R_V)
    xa = [A("xa%d" % i, [128, D], F32, R_T + i * 4 * KB) for i in range(2)]
    hba = [A("hba%d" % i, [128, D], BF16, R_T + 8 * KB + i * 2 * KB) for i in range(2)]
    jka = A("jka", [128, D], BF16, R_T + 12 * KB)
    jkaA = A("jkaA", [128, D], BF16, R_G)
    ksb = [A("ksb%d" % i, [128, 2 * 512], F32, R_T + 12 * KB + i * 4 * KB) for i in range(2)]
    Bxa = [Buf("xa0"), Buf("xa1")]; Bhba = [Buf("hba0"), Buf("hba1")]; Bsta = [Buf("sta0"), Buf("sta1")]
    BWu = Buf("Wu"); BhT = Buf("hTsb"); BUt = [Buf("Ut%d" % i) for i in range(4)]; BUg = [Buf("Ug%d" % i) for i in range(32)]
    Bjk = Buf("jk")
    fw.dma("pool", V(Wu_b, 0, [[512, 8], [1, 512]]), w_in_d[:, 1536:2048].rearrange("(k p) n -> p k n", p=128), writes=[BWu])
    for sb in range(4):
        for i in range(8):
            n = sb * 8 + i
            bi = n % 2
            fw.dma("sp", xa[bi][:], x_d[n * 128:(n + 1) * 128, :], writes=[Bxa[bi]])
            rms_tile(xa[bi][:], hba[bi][:], jkaA[:], V(stat, bi * 4, [[1, 4]]), Bxa[bi], Bhba[bi], Bsta[bi], None)
            for k in range(8):
                TR(bankb(0, k * 128, k * 128 + 128), hba[bi][:, k * 128:(k + 1) * 128], ident_b[:], [Bhba[bi], Bc], [PB[0]], sig=(k == 7), add=(k > 0))
            CP("act", V(hT_sb, i * 128, [[1024, 8], [1, 128]]), bass.AP(ppb, 0, [[8192, 128], [128, 8], [1, 128]]), [PB[0]], [BhT], add=(i > 0))
        for tau in range(8):
            bk = 1 + tau % 2
            for k in range(8):
                MM(bank(bk), V(hT_sb, k * 1024 + tau, [[8, 128]]), Wu_b[:, k * 512:(k + 1) * 512], k == 0, k == 7, [BhT, BWu], [PB[bk]], sig=(k == 7), add=(k > 0))
            eng = "act" if tau % 2 == 0 else "dve"
            CP(eng, V(U_tok, sb * 4096 + tau * 16, [[128, 32], [1, 16]]), bass.AP(pp, bk * 512, [[4096, 128], [16, 32], [1, 16]]), [PB[bk]], [BUt[sb]], add=(tau > 0))
        for g8 in range(4):
            bk = 3 + g8 % 2
            for gi in range(8):
                g = g8 * 8 + gi
                TR(bankb(bk, gi * 128, gi * 128 + 128), U_tok[:, sb * 4096 + g * 128: sb * 4096 + (g + 1) * 128], ident_b[:], [BUt[sb], Bc], [PB[bk]], sig=(gi == 7), add=(gi > 0))
            eng = "act" if g8 % 2 == 0 else "dve"
            CP(eng, V(Ug, g8 * 8 * 512 + sb * 128, [[512, 8], [1, 128]]), bass.AP(ppb, bk * 1024, [[8192, 128], [128, 8], [1, 128]]), [PB[bk]], [BUg[g8 * 8 + gi] for gi in range(8)], add=True)
    if debug:
        fw.barrier()
        dtmp2 = A("dtmp2", [128, 16384], F32, R_G)
        CP("dve", dtmp2[:], Ug[:], BUg, [Bp])
        fw.dma("sp", dbg["dbg_ug"], dtmp2[:], reads=[Bp], is_output=True)
        fw.barrier()
    if stop == 'pA1':
        fw.finish()
        return nc
    Ygel = U_tok
    BY = [Buf("Ygel%d" % i) for i in range(32)]
    Xb = [A("Xb%d" % i, [128, 2 * 512], BF16, R_O + 24 * KB + i * 2 * KB) for i in range(2)]
    BXb = [Buf("Xb0"), Buf("Xb1")]
    Bks = [Buf("ks0"), Buf("ks1")]
    gel = [A("gel%d" % i, [128, 512], F32, R_O + 28 * KB + i * 2 * KB) for i in range(2)]
    Bgel = [Buf("gel0"), Buf("gel1")]
    for gp in range(16):
        for (ri, VT) in ((0, VTre_b), (1, VTim_b)):
            bk = 5 + ri
            for gl in range(2):
                g = 2 * gp + gl
                MM(pp[gl * 64:(gl + 1) * 64, bk * 512:(bk + 1) * 512], VT[:, g * 64:(g + 1) * 64], Ug[:, g * 512:(g + 1) * 512], True, True, [Bs5w, BUg[g]], [PB[bk]], sig=(gl == 1), add=(gl > 0))
        cur, nxt = 0, 1
        CP("act", ksb[cur][:, 0:512], bank(5), [PB[5]], [Bks[cur]])
        CP("act", ksb[cur][:, 512:1024], bank(6), [PB[6]], [Bks[cur]], add=True)
        for k in range(9):
            s = 1 << k
            n = 512 - s
            a_k = ks_are[:, gp * 9 + k: gp * 9 + k + 1]
            b_k = ks_aim[:, gp * 9 + k: gp * 9 + k + 1]
            nb_k = ks_naim[:, gp * 9 + k: gp * 9 + k + 1]
            c_, n_ = ksb[cur], ksb[nxt]
            CP("act", V(n_, 0, [[512, 2], [1, s]]), V(c_, 0, [[512, 2], [1, s]]), [Bks[cur]], [Bks[nxt]])
            STT(n_[:, s:512], c_[:, 0:n], a_k, c_[:, s:512], ALU.mult, ALU.add, [Bks[cur], Bc], [Bks[nxt]], add=True)
            STT(n_[:, s:512], c_[:, 512:512 + n], nb_k, n_[:, s:512], ALU.mult, ALU.add, [Bks[cur], Bks[nxt], Bc], [Bks[nxt]], add=True)
            STT(n_[:, 512 + s:1024], c_[:, 0:n], b_k, c_[:, 512 + s:1024], ALU.mult, ALU.add, [Bks[cur], Bc], [Bks[nxt]], add=True)
            STT(n_[:, 512 + s:1024], c_[:, 512:512 + n], a_k, n_[:, 512 + s:1024], ALU.mult, ALU.add, [Bks[cur], Bks[nxt], Bc], [Bks[nxt]], add=True)
            cur, nxt = nxt, cur
        xb = Xb[gp % 2]
        CP("act", xb[:], ksb[cur][:], [Bks[cur]], [BXb[gp % 2]])
        for gl in range(2):
            g = 2 * gp + gl
            bk = 1 + g % 2
            MM(bank(bk), T_b[:, g * 128:(g + 1) * 128], Ug[:, g * 512:(g + 1) * 512], True, False, [Bs5w, BUg[g]], [PB[bk]], sig=False)
            MM(bank(bk, 1, 512), VP(Wre_b, gl * 64, 64, g * 128, [[1, 128]]), VP(xb, gl * 64, 64, 0, [[1, 511]]), False, False, [Bs5w, BXb[gp % 2]], [PB[bk]], sig=False, add=True)
            MM(bank(bk, 1, 512), VP(Wimn_b, gl * 64, 64, g * 128, [[1, 128]]), VP(xb, gl * 64, 64, 512, [[1, 511]]), False, True, [Bs5w, BXb[gp % 2]], [PB[bk]], sig=True, add=True)
            ge = gel[g % 2]
            Bg = Bgel[g % 2]
            ACT(ge[:], bank(bk), AF.Square, [PB[bk]], [Bg])
            TS("dve", ge[:], ge[:], 0.044715, 1.0, ALU.mult, ALU.add, [Bg], [Bg])
            TT("dve", ge[:], ge[:], bank(bk), ALU.mult, [Bg, PB[bk]], [Bg])
            ACT(ge[:], ge[:], AF.Sigmoid, [Bg], [Bg], scale=1.5957691216057308)
            TT("dve", Ygel[:, g * 512:(g + 1) * 512], ge[:], bank(bk), ALU.mult, [Bg, PB[bk]], [BY[g]] + BUt, add=True)
    if stop == 'pA2':
        fw.finish()
        return nc
    fw.barrier()
    Ytok = Ug
    BYt = [Buf("Ytok%d" % i) for i in range(4)]
    Bgy = [Buf("gyT%d" % i) for i in range(8)]
    gy32 = [A("gy32_%d" % i, [128, 4 * 1024], F32, R_Q + i * 16 * KB) for i in range(2)]
    Bg32 = [Buf("gy32_0"), Buf("gy32_1")]
    for sb in range(4):
        for g8 in range(4):
            bk = 3 + g8 % 2
            for gi in range(8):
                g = g8 * 8 + gi
                TR(bankb(bk, gi * 128, gi * 128 + 128), Ygel[:, g * 512 + sb * 128: g * 512 + (sb + 1) * 128], ident_b[:], [BY[g], Bc], [PB[bk]], sig=(gi == 7), add=(gi > 0))
            eng = "act" if g8 % 2 == 0 else "dve"
            CP(eng, V(Ytok, sb * 4096 + g8 * 128, [[16, 8], [512, 8], [1, 16]]), bass.AP(ppb, bk * 1024, [[8192, 128], [128, 8], [16, 8], [1, 16]]), [PB[bk]], BUg + [BYt[sb]], add=True)
        if stop == 'pA3':
            fw.finish()
            return nc
        for j in range(8):
            bk = 5 + j % 2
            for q4 in range(4):
                TR(bankb(bk, q4 * 128, q4 * 128 + 128), Ytok[:, sb * 4096 + j * 512 + q4 * 128: sb * 4096 + j * 512 + (q4 + 1) * 128], ident_b[:], [BYt[sb], Bc], [PB[bk]], sig=(q4 == 3), add=(q4 > 0))
            eng = "act" if j % 2 == 0 else "dve"
            CP(eng, V(gy32[sb % 2], j, [[1024, 4], [8, 128]]), bass.AP(ppb, bk * 1024, [[8192, 128], [128, 4], [1, 128]]), [PB[bk]], [Bg32[sb % 2]], add=(j > 0))
        if stop == 'pA4':
            fw.finish()
            return nc
        CP("pool", V(gyT, sb * 1024, [[S, 4], [1, 1024]]), V(gy32[sb % 2], 0, [[1024, 4], [1, 1024]]), [Bg32[sb % 2]], [Bgy[2 * sb], Bgy[2 * sb + 1]], add=True)
    if stop == 'pA5':
        fw.finish()
        return nc
    if debug:
        fw.barrier()
        for q_ in range(4):
            dst_ = A("stg_dbg_gy_%d" % q_, [128, S], F32, R_K)
            CP("dve", dst_[:], gyT[:, q_ * S:(q_ + 1) * S], Bgy, [Bp])
            fw.dma("sp", dbg["dbg_gy" + str(q_)], dst_[:], reads=[Bp], is_output=True)
    fw.barrier()

    if stop == 'pA':
        fw.finish()
        return nc
    qT = A("qT", [128, 4 * S], BF16, R_Q)
    kT = A("kT", [128, 4 * S], BF16, R_K)
    v_aug = A("v_aug", [128, NT * 4 * 130], BF16, R_V)
    Wqkv = A("Wqkv", [128, 8 * 1536], BF16, R_O)
    hTt = [A("hTt%d" % i, [128, 8 * 128], BF16, R_O + 24 * KB + i * 2 * KB) for i in range(2)]
    BhTt = [Buf("hTt0"), Buf("hTt1")]
    sqs = A("sqs", [128, 1024], BF16, R_O + 28 * KB)
    qkb = A("qkb", [128, 1024], BF16, R_O + 30 * KB)
    qkn = [A("qkn%d" % i, [128, 1024], F32, R_T + 12 * KB + i * 4 * KB) for i in range(2)]
    rtmp = A("rtmp_", [128, 3 * 128], F32, R_T + 20 * KB)
    Bsq = Buf("sqs"); Bqkn = [Buf("qkn0"), Buf("qkn1")]; Bqkb = Buf("qkb"); Brt = Buf("rtmp"); Bqst = Buf("qst")
    BW = Buf("Wqkv"); BqT = [Buf("qT%d" % i) for i in range(8)]; BkT = [Buf("kT%d" % i) for i in range(NT)]; Bv = [Buf("v%d" % i) for i in range(NT)]
    fw.dma("pool", V(Wqkv, 0, [[1536, 8], [1, 1536]]), w_in_d[:, 0:1536].rearrange("(k p) n -> p k n", p=128), writes=[BW])
    MEMSET("pool", V(v_aug, 128, [[130, NT * 4], [1, 2]]), 1.0, [], Bv)

    def qkbank(n):
        return 1 if n % 2 == 0 else 6

    def pb_M1(n):
        bi = n % 2
        fw.dma("sp", xa[bi][:], x_d[n * 128:(n + 1) * 128, :], writes=[Bxa[bi]])
        rms_tile(xa[bi][:], hba[bi][:], hba[bi][:], V(stat, bi * 4, [[1, 4]]), Bxa[bi], Bhba[bi], Bsta[bi], None)
        for k in range(8):
            TR(bankb(0, k * 128, k * 128 + 128), hba[bi][:, k * 128:(k + 1) * 128], ident_b[:], [Bhba[bi], Bc], [PB[0]], sig=(k == 7), add=(k > 0))
        CP("act", hTt[bi][:], bankb(0), [PB[0]], [BhTt[bi]])

    def pb_M2(n):
        bi = n % 2
        b0 = qkbank(n)
        for cb in range(3):
            bk = (b0 + cb) if cb < 2 else 3
            for k in range(8):
                MM(bank(bk), hTt[bi][:, k * 128:(k + 1) * 128], Wqkv[:, k * 1536 + cb * 512: k * 1536 + (cb + 1) * 512], k == 0, k == 7, [BhTt[bi], BW], [PB[bk]], sig=(k == 7), add=(k > 0))
        CP("act", V(v_aug, n * 520, [[130, 4], [1, 128]]), bass.AP(pp, 3 * 512, [[4096, 128], [128, 4], [1, 128]]), [PB[3]], [Bv[n]], add=True)
        psqk = pp[:, b0 * 512:(b0 + 2) * 512]
        Pq = [PB[b0], PB[b0 + 1]]
        ACT(sqs[:], psqk, AF.Square, Pq, [Bsq])
        RSUM(stat[:, 8:24], V(sqs, 0, [[64, 16], [1, 64]]), [Bsq], [Bqst])
        ACT(stat[:, 24:40], stat[:, 8:24], AF.Sqrt, [Bqst], [Bqst], scale=1.0 / 64, bias=EPS)
        RECIP(stat[:, 40:56], stat[:, 24:40], [Bqst], [Bqst])
        q_ = qkn[n % 2]
        Bq = Bqkn[n % 2]
        TT("dve", V(q_, 0, [[64, 16], [1, 64]]), bass.AP(pp, b0 * 512, [[4096, 128], [64, 16], [1, 64]]), V(stat, 40, [[1, 16], [0, 64]]), ALU.mult, Pq + [Bqst], [Bq])
        TT("dve", q_[:, 0:512], q_[:, 0:512], gq_t[:], ALU.mult, [Bq, Bc], [Bq])
        TT("dve", q_[:, 512:1024], q_[:, 512:1024], gk_t[:], ALU.mult, [Bq, Bc], [Bq])

    def pb_M3(n):
        q_ = qkn[n % 2]
        Bq = Bqkn[n % 2]
        r1 = V(q_, 0, [[64, 16], [1, 8]]); r2 = V(q_, 8, [[64, 16], [1, 8]])
        cs = V(cos_t, n * 8, [[0, 16], [1, 8]]); sn = V(sin_t, n * 8, [[0, 16], [1, 8]])
        ta = V(rtmp, 0, [[8, 16], [1, 8]]); tb = V(rtmp, 128, [[8, 16], [1, 8]]); tc = V(rtmp, 256, [[8, 16], [1, 8]])
        TT("pool", ta, r1, cs, ALU.mult, [Bq, Bc], [Brt])
        TT("pool", tb, r2, sn, ALU.mult, [Bq, Bc], [Brt], add=True)
        TT("pool", tc, r1, sn, ALU.mult, [Bq, Bc], [Brt], add=True)
        TT("pool", r1, ta, tb, ALU.subtract, [Brt, Bq], [Bq])
        TT("pool", r2, r2, cs, ALU.mult, [Bq, Bc], [Bq])
        TT("pool", r2, r2, tc, ALU.add, [Brt, Bq], [Bq])
        CP("act", qkb[:], q_[:], [Bq], [Bqkb])
        for cb in range(2):
            bkt = 4 + cb
            for h in range(4):
                TR(bankb(bkt, h * 128, h * 128 + 128), qkb[:, cb * 512 + h * 128: cb * 512 + (h + 1) * 128], ident_b[:], [Bqkb, Bc], [PB[bkt]], sig=(h == 3), add=(h > 0))
            dstT = qT if cb == 0 else kT
            dB = BqT[n // 4] if cb == 0 else BkT[n]
            CP("dve", V(dstT, n * 128, [[S, 4], [1, 128]]), bass.AP(ppb, bkt * 1024, [[8192, 128], [128, 4], [1, 128]]), [PB[bkt]], [dB], add=True)

    for t in range(-2, NT):
        if 0 <= t + 1 < NT:
            pb_M2(t + 1)
        if 0 <= t + 2 < NT:
            pb_M1(t + 2)
        if 0 <= t < NT:
            pb_M3(t)
    if debug:
        fw.barrier()
        for q_ in range(4):
            dst_ = A("stg_dbg_q_%d" % q_, [128, S], F32, R_O)
            CP("dve", dst_[:], qT[:, q_ * S:(q_ + 1) * S], BqT, [Bp])
            fw.dma("sp", dbg["dbg_q" + str(q_)], dst_[:], reads=[Bp], is_output=True)
        for q_ in range(4):
            dst_ = A("stg_dbg_k_%d" % q_, [128, S], F32, R_O)
            CP("dve", dst_[:], kT[:, q_ * S:(q_ + 1) * S], BkT, [Bp])
            fw.dma("sp", dbg["dbg_k" + str(q_)], dst_[:], reads=[Bp], is_output=True)
    fw.barrier()
    fw.barrier()

    if stop == 'pB':
        fw.finish()
        return nc
    oT = A("oT", [128, 4 * S], BF16, R_O)
    pT = [[A("pT%d%d" % (c, i), [128, 512], BF16, R_T + (c * 2 + i) * KB) for i in range(2)] for c in range(2)]
    BpT = [[Buf("pT%d%d" % (c, i)) for i in range(2)] for c in range(2)]
    of_ = [A("of%d" % i, [128, 128], F32, R_T + 4 * KB + i * 512) for i in range(2)]
    ob_ = [A("ob%d" % i, [128, 128], BF16, R_T + 5 * KB + i * 256) for i in range(2)]
    ajk = A("ajk", [128, 128], BF16, R_T + 6 * KB)
    Bof = [Buf("of0"), Buf("of1")]; Bob = [Buf("ob0"), Buf("ob1")]; Bast = [Buf("ast0"), Buf("ast1")]
    BoT = [Buf("oT%d" % i) for i in range(8)]
    Bajk = Buf("ajk")
    def accv(qs, c, a, b):
        idx = qs * 2 + c
        bk = 4 + idx // 3
        off = bk * 512 + (idx % 3) * 130
        return pp[:, off + a: off + b], PB[bk]
    pT4 = [A("pT4_%d" % i, [128, 512], BF16, R_T + i * KB) for i in range(4)]
    BpT4 = [Buf("pT4_%d" % i) for i in range(4)]
    dacc = [[A("dacc%d%d" % (r, c), [128, 512], F32, R_T + 4 * KB + (r * 2 + c) * 2 * KB) for c in range(2)] for r in range(2)]
    Bdacc = [[Buf("dacc%d%d" % (r, c)) for c in range(2)] for r in range(2)]
    rd = [A("rd%d" % i, [128, 512], F32, R_T + 12 * KB + i * 2 * KB) for i in range(2)]
    Brd = [Buf("rd0"), Buf("rd1")]
    att_items = [(h, qblk, kt, c) for h in range(4) for qblk in range(8) for kt in range(4 * qblk + 4) for c in range(2)]
    NA = len(att_items)

    def stage_S(k):
        h, qblk, kt, c = att_items[k]
        q0 = max(0, kt - 4 * qblk)
        col0 = q0 * 128
        diag = kt >= 4 * qblk
        sb_ = k % 3
        MM(bank(sb_, col0, 512), VP(kT, c * 64, 64, h * S + kt * 128, [[1, 128]]), VP(qT, c * 64, 64, h * S + qblk * 512 + col0, [[1, 512 - col0]]),
           True, not diag, [BkT[kt], BqT[qblk]], [PB[sb_]], sig=(not diag))
        if diag:
            MM(bank(sb_, col0, col0 + 128), ident_b[:], maskneg_b[:], False, True, [Bc], [PB[sb_]], sig=True, add=True)
        ACT(pT4[k % 4][:, col0:512], bank(sb_, col0, 512), AF.Exp, [PB[sb_]], [BpT4[k % 4]], scale=0.125)

    def finalize_a1(h, qblk, rnd):
        r = rnd % 2
        MM(bank(3), ones_f[:], dacc[r][0][:], True, True, [Bc, Bdacc[r][0]], [PB[3]])
        RECIP(rd[0][:], bank(3), [PB[3]], [Brd[0]])

    def finalize_a2(h, qblk, rnd):
        r = rnd % 2
        ab = 4 + 2 * r
        MM(bank(3), ones_f[:], dacc[r][1][:], True, True, [Bc, Bdacc[r][1]], [PB[3]])
        RECIP(rd[1][:], bank(3), [PB[3]], [Brd[1]])
        TS("dve", rd[1][:], rd[1][:], lamv[:, 3:4], None, ALU.mult, None, [Brd[1], Bc], [Brd[1]])
        TT("dve", rd[0][:], bank(ab), rd[0][:], ALU.mult, [PB[ab], Brd[0]], [Brd[0]])
        TT("dve", rd[1][:], bank(ab + 1), rd[1][:], ALU.mult, [PB[ab + 1], Brd[1]], [Brd[1]])
        TT("dve", rd[1][:], rd[1][:], rd[0][:], ALU.add, [Brd[0], Brd[1]], [Brd[1]])
        ACT(rd[0][:], rd[1][:], AF.Square, [Brd[1]], [Brd[0]])

    def finalize_b(h, qblk):
        t0 = qblk * 512
        MM(bank(3), ones_f[:], rd[0][:], True, True, [Bc, Brd[0]], [PB[3]])
        ACT(rd[0][:], bank(3), AF.Sqrt, [PB[3]], [Brd[0]], scale=1.0 / 128, bias=EPS)
        RECIP(rd[0][:], rd[0][:], [Brd[0]], [Brd[0]])
        STT(oT[:, h * S + t0: h * S + t0 + 512], rd[1][:], lamv[:, 4:5], rd[0][:], ALU.mult, ALU.mult, [Brd[0], Brd[1], Bc], [BoT[qblk]], add=True)

    deferred = {}

    def stage_PV(k):
        h, qblk, kt, c = att_items[k]
        rnd = h * 8 + qblk
        r = rnd % 2
        q0 = max(0, kt - 4 * qblk)
        col0 = q0 * 128
        p_ = pT4[k % 4]
        ab = 4 + 2 * r + c
        last = (kt == 4 * qblk + 3)
        MM(bank(ab, col0, 512), V(v_aug, kt * 520 + h * 130, [[1, 128]]), p_[:, col0:512], kt == 0, last, [BpT4[k % 4], Bv[kt]], [PB[ab]], sig=True, add=(kt > 0))
        eng = "dve" if c == 0 else "pool"
        if kt == 0:
            CP(eng, dacc[r][c][:], p_[:], [BpT4[k % 4]], [Bdacc[r][c]])
        else:
            TT(eng, dacc[r][c][:, col0:512], dacc[r][c][:, col0:512], p_[:, col0:512], ALU.add, [BpT4[k % 4], Bdacc[r][c]], [Bdacc[r][c]])
        for fn_, args_ in deferred.pop(k, []):
            fn_(*args_)
        if last and c == 1:
            deferred.setdefault(k + 3, []).append((finalize_a1, (h, qblk, rnd)))
            deferred.setdefault(k + 6, []).append((finalize_a2, (h, qblk, rnd)))
            deferred.setdefault(k + 10, []).append((finalize_b, (h, qblk)))

    LOOK = 2
    for k in range(LOOK):
        stage_S(k)
    for i in range(48):
        MM(bank(7), ident_b[:], qT[:, 0:512], True, True, [Bc] + BqT[0:1], [PB[7]], sig=(i == 47), add=(i > 0))
    for k in range(0, NA):
        if k + LOOK < NA:
            stage_S(k + LOOK)
        stage_PV(k)
    for kk_ in sorted(deferred):
        for fn_, args_ in deferred[kk_]:
            fn_(*args_)
    deferred.clear()
    assert not deferred
    if debug:
        fw.barrier()
        for q_ in range(4):
            dst_ = A("stg_dbg_o_%d" % q_, [128, S], F32, R_Q)
            CP("dve", dst_[:], oT[:, q_ * S:(q_ + 1) * S], BoT, [Bp])
            fw.dma("sp", dbg["dbg_o" + str(q_)], dst_[:], reads=[Bp], is_output=True)
    fw.barrier()

    if stop == 'att':
        fw.finish()
        return nc
    Wg_b = A("Wg_b", [128, 8 * 2048], BF16, R_Q)
    wglu_b = A("wglu_b", [128, 4 * 2048], BF16, R_K)
    wout_b = A("wout_b", [128, 8 * 1024], BF16, R_K + 16 * KB)
    wo_b = A("wo_b", [128, 4 * 1024], BF16, R_V)
    xc = [A("xc%d" % i, [128, D], F32, R_V + 8 * KB + i * 4 * KB) for i in range(4)]
    hT_blk = A("hT_blk", [128, 8 * 512], BF16, R_V + 24 * KB)
    mT = A("mT", [128, 8 * 512], BF16, R_T)
    sgA = A("sgA", [128, 512], F32, R_T + 8 * KB); sgB = A("sgB", [128, 512], F32, R_T + 10 * KB); sgE = A("sgE", [128, 512], F32, R_T + 12 * KB)
    tt1 = A("tt1", [128, 512], F32, R_T + 14 * KB); tt2 = A("tt2", [128, 512], F32, R_T + 16 * KB)
    hbc = A("hbc", [128, D], BF16, R_T + 18 * KB)
    cjk = hbc
    tT32 = A("tT32", [128, 8 * 128], F32, R_T + 8 * KB)
    BWc = Buf("Wc"); Bxc = [Buf("xc%d" % i) for i in range(4)]; BhTb = Buf("hTblk"); BmT = Buf("mT")
    BsA = Buf("sgA"); BsB = Buf("sgB"); BsE = Buf("sgE"); Bt1 = Buf("tt1"); Bt2 = Buf("tt2"); Bhbc = Buf("hbc"); Bcst = Buf("cst"); BtT32 = BsA
    Bcomb = Buf("comb")
    Bout = [Buf("out_h0"), Buf("out_h1")]
    fw.dma("pool", V(Wg_b, 0, [[2048, 8], [1, 2048]]), w_in_d[:, 2048:4096].rearrange("(k p) n -> p k n", p=128), writes=[BWc], add=True)
    fw.dma("pool", V(wglu_b, 0, [[2048, 4], [1, 2048]]), wglu_d.rearrange("(k p) n -> p k n", p=128), writes=[BWc], add=True)
    fw.dma("pool", V(wout_b, 0, [[1024, 8], [1, 1024]]), wout_d.rearrange("(k p) n -> p k n", p=128), writes=[BWc], add=True)
    fw.dma("pool", V(wo_b, 0, [[1024, 4], [1, 1024]]), wo_d.rearrange("(k p) n -> p k n", p=128), writes=[BWc], add=True)

    def tT_ap(k, tok0, n):
        base = gyT if k < 4 else oT
        return base[:, (k % 4) * S + tok0: (k % 4) * S + tok0 + n]

    for blk in range(8):
        t0 = blk * 512
        for i in range(4):
            n = blk * 4 + i
            fw.dma("sp", xc[i][:], x_d[n * 128:(n + 1) * 128, :], writes=[Bxc[i]])
            rms_tile(xc[i][:], hbc[:], cjk[:], V(stat, 48, [[1, 4]]), Bxc[i], Bhbc, Bcst, None)
            for k in range(8):
                TR(bankb(0, k * 128, k * 128 + 128), hbc[:, k * 128:(k + 1) * 128], ident_b[:], [Bhbc, Bc], [PB[0]], sig=(k == 7), add=(k > 0))
            CP("act", V(hT_blk, i * 128, [[512, 8], [1, 128]]), bass.AP(ppb, 0, [[8192, 128], [128, 8], [1, 128]]), [PB[0]], [BhTb], add=(i > 0))
        for nch in range(8):
            for (bk, col) in ((1, nch), (2, 8 + nch)):
                for k in range(8):
                    MM(bank(bk), Wg_b[:, k * 2048 + col * 128: k * 2048 + (col + 1) * 128], hT_blk[:, k * 512:(k + 1) * 512], k == 0, k == 7, [BWc, BhTb], [PB[bk]], sig=(k == 7), add=(k > 0))
            for f in range(4):
                MM(bank(3), wo_b[:, f * 1024 + nch * 128: f * 1024 + (nch + 1) * 128], oT[:, f * S + t0: f * S + t0 + 512], f == 0, f == 3, [BWc, BoT[blk]], [PB[3]], sig=(f == 3), add=(f > 0))
            for (bk, col) in ((4, nch), (5, 8 + nch)):
                for f in range(4):
                    MM(bank(bk), wglu_b[:, f * 2048 + col * 128: f * 2048 + (col + 1) * 128], gyT[:, f * S + t0: f * S + t0 + 512], f == 0, f == 3, [BWc, Bgy[blk]], [PB[bk]], sig=(f == 3), add=(f > 0))
            ACT(sgA[:], bank(1), AF.Sigmoid, [PB[1]], [BsA])
            ACT(sgB[:], bank(2), AF.Sigmoid, [PB[2]], [BsB])
            ACT(sgE[:], bank(5), AF.Sigmoid, [PB[5]], [BsE])
            TT("dve", tt1[:], bank(3), sgA[:], ALU.mult, [PB[3], BsA], [Bt1])
            TT("dve", tt2[:], bank(4), sgE[:], ALU.mult, [PB[4], BsE], [Bt2])
            TT("pool", tt2[:], tt2[:], sgB[:], ALU.mult, [Bt2, BsB], [Bt2])
            TT("pool", mT[:, nch * 512:(nch + 1) * 512], tt1[:], tt2[:], ALU.add, [Bt1, Bt2], [BmT], add=(nch > 0))
        def back_A(i, blk=blk, t0=t0):
            n = blk * 4 + i
            for half in range(2):
                bk = 6 + half
                for f in range(8):
                    MM(bank(bk), mT[:, f * 512 + i * 128: f * 512 + (i + 1) * 128], wout_b[:, f * 1024 + half * 512: f * 1024 + (half + 1) * 512], f == 0, f == 7, [BmT, BWc], [PB[bk]], sig=(f == 7), add=(f > 0))
                TT("dve", xc[i][:, half * 512:(half + 1) * 512], bank(bk), xc[i][:, half * 512:(half + 1) * 512], ALU.add, [PB[bk], Bxc[i]], [Bxc[i]], add=(half > 0))
            fw.dma("sp", out_d[n * 128:(n + 1) * 128, :], xc[i][:], reads=[Bxc[i]], writes=[Bout[n // 16]], add=True)
            if debug:
                fw.dma("sp", dbg["dbg_x1"][n * 128:(n + 1) * 128, :], xc[i][:], reads=[Bxc[i]], is_output=True)
            ACT(cjk[:], xc[i][:], AF.Square, [Bxc[i]], [Bcst, Bhbc], accum_out=stat[:, 52:53])
            ACT(stat[:, 53:54], stat[:, 52:53], AF.Sqrt, [Bcst], [Bcst], scale=1.0 / D, bias=EPS)
            RECIP(stat[:, 54:55], stat[:, 53:54], [Bcst], [Bcst])
            STT(xc[i][:], xc[i][:], stat[:, 54:55], gffn_t[:], ALU.mult, ALU.mult, [Bxc[i], Bcst, Bc], [Bxc[i]])
        def back_B(i, blk=blk, t0=t0):
            n = blk * 4 + i
            for k in range(8):
                bk = k // 4
                TR(bank(bk, (k % 4) * 128, (k % 4) * 128 + 128), xc[i][:, k * 128:(k + 1) * 128], ident_f[:], [Bxc[i], Bc], [PB[bk]], sig=(k % 4 == 3), add=(k % 4 > 0))
            CP("act", tT32[:, 0:512], bank(0), [PB[0], BsA, BsB], [BsA, BsB])
            CP("act", tT32[:, 512:1024], bank(1), [PB[1]], [BsA, BsB], add=True)
            for k in range(8):
                CP("pool", tT_ap(k, n * 128, 128), tT32[:, k * 128:(k + 1) * 128], [BsA], [Bgy[blk], BoT[blk]], add=True)
            for k in range(8):
                MM(bank(2, 0, 36), tT32[:, k * 128:(k + 1) * 128], wr32[:, k * 36:(k + 1) * 36], k == 0, k == 7, [BsA, Bc], [PB[2]], sig=(k == 7), add=(k > 0))
            rt = tt1
            Br = Bt1
            lgt = rt[:, 0:36]
            TT("dve", lgt, bank(2, 0, 36), br_t[:], ALU.add, [PB[2], Bc], [Br])
            gmax = rt[:, 40:41]
            RMAX(gmax, rt[:, 0:4], [Br], [Br], add=True)
            oh = rt[:, 44:48]
            TS("dve", oh, rt[:, 0:4], gmax, None, ALU.is_equal, None, [Br], [Br], add=True)
            TS("dve", rt[:, 48:52], rt[:, 0:4], gmax, None, ALU.subtract, None, [Br], [Br], add=True)
            ACT(rt[:, 48:52], rt[:, 48:52], AF.Exp, [Br], [Br])
            RSUM(rt[:, 52:53], rt[:, 48:52], [Br], [Br], add=True)
            RECIP(rt[:, 53:54], rt[:, 52:53], [Br], [Br])
            TS("dve", rt[:, 56:64], rt[:, 4:12], rt[:, 44:45], None, ALU.mult, None, [Br], [Br], add=True)
            for g in range(1, 4):
                STT(rt[:, 56:64], rt[:, 4 + g * 8: 12 + g * 8], rt[:, 44 + g: 45 + g], rt[:, 56:64], ALU.mult, ALU.add, [Br], [Br])
            m1 = rt[:, 64:65]
            RMAX(m1, rt[:, 56:64], [Br], [Br], add=True)
            mk1 = rt[:, 72:80]
            TS("dve", mk1, rt[:, 56:64], m1, None, ALU.is_equal, None, [Br], [Br], add=True)
            es2 = rt[:, 80:88]
            STT(es2, mk1, -1e30, rt[:, 56:64], ALU.mult, ALU.add, [Br], [Br], add=True)
            m2 = rt[:, 65:66]
            RMAX(m2, es2, [Br], [Br], add=True)
            mk2 = rt[:, 88:96]
            TS("dve", mk2, es2, m2, None, ALU.is_equal, None, [Br], [Br], add=True)
            TT("dve", rt[:, 66:67], m2, m1, ALU.subtract, [Br], [Br], add=True)
            ACT(rt[:, 66:67], rt[:, 66:67], AF.Exp, [Br], [Br])
            TS("dve", rt[:, 67:68], rt[:, 66:67], 1.0, None, ALU.add, None, [Br], [Br], add=True)
            RECIP(rt[:, 68:69], rt[:, 67:68], [Br], [Br])
            TS("dve", rt[:, 69:70], rt[:, 68:69], -1.0, 1.0, ALU.mult, ALU.add, [Br], [Br], add=True)
            TT("dve", rt[:, 68:69], rt[:, 68:69], rt[:, 53:54], ALU.mult, [Br], [Br])
            TT("dve", rt[:, 69:70], rt[:, 69:70], rt[:, 53:54], ALU.mult, [Br], [Br])
            ew = rt[:, 96:104]
            TS("dve", ew, mk1, rt[:, 68:69], None, ALU.mult, None, [Br], [Br], add=True)
            STT(ew, mk2, rt[:, 69:70], ew, ALU.mult, ALU.add, [Br], [Br])
            for g in range(4):
                TS("dve", comb_all[:, n * 32 + g * 8: n * 32 + (g + 1) * 8], ew, rt[:, 44 + g:45 + g], None, ALU.mult, None, [Br], [Bcomb], add=True)
        back_A(0)
        for i in range(4):
            if i + 1 < 4:
                back_A(i + 1)
            back_B(i)
    if debug:
        fw.dma("sp", dbg["dbg_comb"], comb_all[:], reads=[Bcomb], is_output=True)
    fw.barrier()

    if stop == 'pC':
        fw.finish()
        return nc
    acc = A("acc", [128, 16 * D], F32, R_Q)
    NWB = 3
    wgu = [A("wgu%d" % i, [128, 8 * 512], BF16, R_V + i * 12 * KB) for i in range(NWB)]
    wd_ = [A("wd%d" % i, [128, 2 * 1024], BF16, R_V + i * 12 * KB + 8 * KB) for i in range(NWB)]
    Bw = [Buf("w%d" % i) for i in range(NWB)]
    sgm = [A("sgm%d" % i, [128, 256], F32, R_V + 36 * KB + i * KB) for i in range(2)]
    hid = [A("hid%d" % i, [128, 256], BF16, R_V + 38 * KB + i * 512) for i in range(2)]
    hidT = [A("hidT%d" % i, [128, 256], BF16, R_V + 39 * KB + i * 512) for i in range(2)]
    Bsg = [Buf("sgm0"), Buf("sgm1")]; Bhid = [Buf("hid0"), Buf("hid1")]; BhidT = [Buf("hidT0"), Buf("hidT1")]
    Bacc = [Buf("acc%d" % i) for i in range(16)]
    BtT = Bgy + BoT
    items = [(hf, e, i) for hf in range(2) for e in range(32) for i in range(16)]
    NI = len(items)

    def wbuf(hf, e):
        return (hf * 32 + e) % NWB

    def stage_G(k):
        hf, e, i = items[k]
        n = hf * 16 + i
        bi = k % 2
        wi = wbuf(hf, e)
        if e == 0 and i == 0:
            for ii in range(16):
                nn = hf * 16 + ii
                fw.dma("sp", acc[:, ii * D:(ii + 1) * D], out_d[nn * 128:(nn + 1) * 128, :], reads=[Bout[hf]], writes=[Bacc[ii]])
        if i == 0:
            fw.dma("pool", V(wgu[wi], 0, [[512, 8], [1, 256]]), weg_d[e].rearrange("(k p) f -> p k f", p=128), writes=[Bw[wi]])
            fw.dma("pool", V(wgu[wi], 256, [[512, 8], [1, 256]]), weu_d[e].rearrange("(k p) f -> p k f", p=128), writes=[Bw[wi]], add=True)
            fw.dma("pool", V(wd_[wi], 0, [[1024, 2], [1, 1024]]), wed_d[e].rearrange("(k p) n -> p k n", p=128), writes=[Bw[wi]], add=True)
        for kk_ in range(8):
            MM(bank(bi), tT_ap(kk_, n * 128, 128), wgu[wi][:, kk_ * 512:(kk_ + 1) * 512], kk_ == 0, kk_ == 7, [BtT[n // 4], BtT[8 + n // 4], Bw[wi]], [PB[bi]], sig=(kk_ == 7), add=(kk_ > 0))
        ACT(sgm[bi][:], bank(bi, 0, 256), AF.Silu, [PB[bi]], [Bsg[bi]])
        STT(hid[bi][:], bank(bi, 256, 512), comb_all[:, n * 32 + e: n * 32 + e + 1], sgm[bi][:], ALU.mult, ALU.mult, [PB[bi], Bcomb, Bsg[bi]], [Bhid[bi]])

    def stage_T(k):
        bi = k % 2
        for f in range(2):
            TR(bankb(2 + bi, f * 128, f * 128 + 128), hid[bi][:, f * 128:(f + 1) * 128], ident_b[:], [Bhid[bi], Bc], [PB[2 + bi]], sig=(f == 1), add=(f > 0))
        CP("act", hidT[bi][:], bankb(2 + bi, 0, 256), [PB[2 + bi]], [BhidT[bi]])

    def stage_D(k):
        hf, e, i = items[k]
        bi = k % 2
        wi = wbuf(hf, e)
        for half in range(2):
            bkd = 4 + bi * 2 + half
            for f in range(2):
                MM(bank(bkd), hidT[bi][:, f * 128:(f + 1) * 128], wd_[wi][:, f * 1024 + half * 512: f * 1024 + (half + 1) * 512], f == 0, f == 1, [BhidT[bi], Bw[wi]], [PB[bkd]], sig=(f == 1), add=(f > 0))
            TT("dve", acc[:, i * D + half * 512: i * D + (half + 1) * 512], bank(bkd), acc[:, i * D + half * 512: i * D + (half + 1) * 512], ALU.add, [PB[bkd], Bacc[i]], [Bacc[i]], add=(half > 0))

    def flush_half(hf):
        for ii in range(16):
            nn = hf * 16 + ii
            fw.dma("sp", out_d[nn * 128:(nn + 1) * 128, :], acc[:, ii * D:(ii + 1) * D], reads=[Bacc[ii]], writes=[Bout[hf]], add=True, is_output=True)

    done_T = set()
    done_D = set()

    def do_T(k):
        if 0 <= k < NI and k not in done_T:
            done_T.add(k)
            stage_T(k)

    def do_D(k):
        if 0 <= k < NI and k not in done_D:
            done_D.add(k)
            stage_D(k)

    for k in range(-1, NI + 1):
        if 0 <= k + 1 < NI:
            if items[k + 1] == (1, 0, 0):
                do_T(k)
                do_D(k - 1)
                do_D(k)
                flush_half(0)
            stage_G(k + 1)
        do_T(k)
        do_D(k - 1)
    flush_half(1)
    fw.finish()
    return nc


_NC_CACHE = {}


def _prep_inputs(inputs, b):
    f = lambda a: np.ascontiguousarray(a, dtype=np.float32)
    m = {
        "x": f(inputs["x"][b]),
        "pos": np.ascontiguousarray(inputs["positions"][b].reshape(NT, 128).T.astype(np.int32)),
        "norm_mix_g": f(inputs["norm_mix_g"][0]),
        "w_in": f(inputs["w_in"][0]),
        "q_norm_g": f(inputs["q_norm_g"][0]), "k_norm_g": f(inputs["k_norm_g"][0]),
        "lambda_q1": f(inputs["lambda_q1"][0]), "lambda_k1": f(inputs["lambda_k1"][0]),
        "lambda_q2": f(inputs["lambda_q2"][0]), "lambda_k2": f(inputs["lambda_k2"][0]),
        "subln_g": f(inputs["subln_g"][0]),
        "w_o_attn": f(inputs["w_o_attn"][0]),
        "lamre_t": f(inputs["ssm_lambda_re"][0].T), "lamim_t": f(inputs["ssm_lambda_im"][0].T),
        "ssm_log_dt": f(inputs["ssm_log_dt"][0]),
        "bre_t": f(inputs["ssm_b_re"][0].transpose(1, 0, 2).reshape(64, 512)),
        "bim_t": f(inputs["ssm_b_im"][0].transpose(1, 0, 2).reshape(64, 512)),
        "cre_t": f(inputs["ssm_c_re"][0].transpose(2, 0, 1).reshape(64, 512)),
        "cim_t": f(inputs["ssm_c_im"][0].transpose(2, 0, 1).reshape(64, 512)),
        "d_t": f(inputs["ssm_d"][0].reshape(32, 16).T),
        "w_glu": f(inputs["w_glu"][0]),
        "w_out": f(inputs["w_out"][0]),
        "norm_ffn_g": f(inputs["norm_ffn_g"][0]),
        "w_router": f(np.concatenate([inputs["w_router_group"][0], inputs["w_router_expert"][0].reshape(D, 32)], axis=1)),
        "b_router": f(np.concatenate([inputs["b_router_group"][0], inputs["b_router_expert"][0].reshape(32)])),
        "w_expert_gate": f(inputs["w_expert_gate"][0].reshape(32, D, 256)),
        "w_expert_up": f(inputs["w_expert_up"][0].reshape(32, D, 256)),
        "w_expert_down": f(inputs["w_expert_down"][0].reshape(32, 256, D)),
    }
    return m


def kernel(**inputs):
    inputs = {k: np.asarray(v) for k, v in inputs.items()}
    nb = inputs["x"].shape[0]
    nc = build_program(debug=False)
    shared = _prep_inputs(inputs, 0)
    in_maps = []
    for b in range(nb):
        m = dict(shared)
        m["x"] = np.ascontiguousarray(inputs["x"][b], dtype=np.float32)
        m["pos"] = np.ascontiguousarray(inputs["positions"][b].reshape(NT, 128).T.astype(np.int32))
        in_maps.append(m)
    res = run_bass_kernel_spmd(nc, in_maps, core_ids=list(range(nb)))
    out = np.stack([np.asarray(r["out"]).reshape(S, D) for r in res.results], axis=0)
    return out.astype(np.float32)
```

```python
import contextlib
import math
import os
import numpy as np
import concourse.bass as bass
import concourse.mybir as mybir
from concourse.bass_utils import run_bass_kernel_spmd

F32 = mybir.dt.float32
BF16 = mybir.dt.bfloat16
I32 = mybir.dt.int32
AF = mybir.ActivationFunctionType
ALU = mybir.AluOpType
AX = mybir.AxisListType

SEM_LIMIT = 30000
S = 4096
D = 1024
NT = 32
EPS = 1e-6
SB_BASE = 17408
LAM_INIT = 0.8 - 0.6 * math.exp(-0.3 * 0)


class Buf:
    __slots__ = ("name", "w", "r", "pr")

    def __init__(self, name=""):
        self.name = name
        self.w = {}
        self.r = {}
        self.pr = {}


class Eng:
    def __init__(self, name):
        self.name = name
        self.ops = []
        self.cnt = 0
        self.semidx = 0
        self.waited = {}
        self.pending = []
        self.last = None

    @property
    def semkey(self):
        return "%s_%d" % (self.name, self.semidx)


class FW:
    def __init__(self, nc, n_dma_sems=32):
        self.nc = nc
        self.stack = contextlib.ExitStack()
        self.engs = {n: Eng(n) for n in ("pe", "act", "dve", "pool", "sp")}
        self.sems = {}
        names = ["dma%d" % i for i in range(n_dma_sems)]
        self.dma_sem_val = {n: 0 for n in names}
        self.dma_rr = {"sp": 0, "pool": 0}
        k = n_dma_sems // 2
        self.dma_pool_of = {"sp": names[:k], "pool": names[k:]}
        self.out_tokens = []

    def sem(self, key):
        if key not in self.sems:
            self.sems[key] = self.stack.enter_context(self.nc.semaphore(key))
        return self.sems[key]

    def psum(self, name, shape, dt):
        return self.stack.enter_context(self.nc.psum_tensor(name, list(shape), dt))

    def _wait(self, eng, tok):
        key, val = tok[0], tok[1]
        assert val is not None, "wait on unsignalled token"
        if eng.waited.get(key, 0) >= val:
            return
        eng.waited[key] = val
        self.sem(key)
        eng.ops.append(("wait", key, val))

    def _deps(self, eng, reads, writes, add):
        toks = []
        for b in reads:
            toks.extend(b.w.values())
        for b in writes:
            if add:
                toks.extend(b.pr.values())
            else:
                toks.extend(b.w.values())
            toks.extend(b.r.values())
        for t in toks:
            if eng.name == "pe" and t[0].startswith("pe_"):
                continue
            self._wait(eng, t)

    def _update(self, key, tok, reads, writes, add):
        for b in reads:
            b.r[key] = tok
        for b in writes:
            if add:
                for k_, v_ in b.r.items():
                    b.pr["r:" + k_] = v_
                b.w[key] = tok
            else:
                npr = {}
                for k_, v_ in b.w.items():
                    npr["w:" + k_] = v_
                for k_, v_ in b.r.items():
                    npr["r:" + k_] = v_
                b.pr = npr
                b.w = {key: tok}
            b.r = {}

    def op(self, engname, fn, reads=(), writes=(), sig=True, add=False):
        eng = self.engs[engname]
        self._deps(eng, reads, writes, add)
        if sig:
            if eng.cnt >= SEM_LIMIT:
                eng.semidx += 1
                eng.cnt = 0
            eng.cnt += 1
            tok = [eng.semkey, eng.cnt]
            self.sem(tok[0])
            for p in eng.pending:
                p[0], p[1] = tok[0], tok[1]
            eng.pending = []
            eng.last = tok
            key = tok[0]
        else:
            tok = ["pe_pending", None]
            eng.pending.append(tok)
            key = "pe_pend"
        eng.ops.append(("op", fn, tok if sig else None))
        self._update(key, tok, reads, writes, add)
        return tok

    def dma(self, qname, out, in_, reads=(), writes=(), add=False, is_output=False, **kw):
        eng = self.engs[qname]
        self._deps(eng, reads, writes, add)
        pool = self.dma_pool_of[qname]
        name = pool[self.dma_rr[qname] % len(pool)]
        self.dma_rr[qname] += 1
        prev = self.dma_sem_val[name]
        if prev > 0:
            self._wait(eng, [name, prev])
        val = prev + 16
        self.dma_sem_val[name] = val
        tok = [name, val]
        self.sem(name)

        def fn(e, out=out, in_=in_, kw=kw):
            return e.dma_start(out=out, in_=in_, **kw)
        eng.ops.append(("op", fn, tok))
        self._update(name, tok, reads, writes, add)
        if is_output:
            self.out_tokens.append(tok)
        return tok

    def barrier(self):
        toks = [e.last for e in self.engs.values() if e.last is not None]
        for e in self.engs.values():
            assert not e.pending
        toks += [[n, v] for n, v in self.dma_sem_val.items() if v > 0]
        for e in self.engs.values():
            for t in toks:
                if e.name == "pe" and t[0].startswith("pe_"):
                    continue
                self._wait(e, t)

    def finish(self):
        sp = self.engs["sp"]
        for t in self.out_tokens:
            self._wait(sp, t)
        nc = self.nc
        sems = self.sems
        engs = self.engs

        def replay(e, eng):
            for o in eng.ops:
                if o[0] == "wait":
                    e.wait_ge(sems[o[1]], o[2])
                else:
                    inst = o[1](e)
                    if o[2] is not None:
                        key = o[2][0]
                        inst.then_inc(sems[key], 16 if key.startswith("dma") else 1)

        with nc.Block() as block:
            @block.tensor
            def _(e):
                replay(e, engs["pe"])

            @block.scalar
            def _(e):
                replay(e, engs["act"])

            @block.vector
            def _(e):
                replay(e, engs["dve"])

            @block.gpsimd
            def _(e):
                replay(e, engs["pool"])

            @block.sync
            def _(e):
                replay(e, engs["sp"])
        self.stack.close()


def build_program(debug=False, stop=None):
    nc = bass.Bass("TRN2", target_bir_lowering=False)
    fw = FW(nc)

    def din(name, shape, dt=F32):
        return nc.dram_tensor(name, list(shape), dt, kind="ExternalInput")

    x_d = din("x", [S, D]).ap()
    pos_d = din("pos", [128, NT], I32).ap()
    gmix_d = din("norm_mix_g", [D])
    w_in_d = din("w_in", [D, 4096]).ap()
    qg_d = din("q_norm_g", [64])
    kg_d = din("k_norm_g", [64])
    lq1_d = din("lambda_q1", [64]); lk1_d = din("lambda_k1", [64])
    lq2_d = din("lambda_q2", [64]); lk2_d = din("lambda_k2", [64])
    subg_d = din("subln_g", [128])
    wo_d = din("w_o_attn", [512, D]).ap()
    lamre_d = din("lamre_t", [64, 32]).ap(); lamim_d = din("lamim_t", [64, 32]).ap()
    logdt_d = din("ssm_log_dt", [32])
    bre_d = din("bre_t", [64, 512]).ap(); bim_d = din("bim_t", [64, 512]).ap()
    cre_d = din("cre_t", [64, 512]).ap(); cim_d = din("cim_t", [64, 512]).ap()
    dsk_d = din("d_t", [16, 32]).ap()
    wglu_d = din("w_glu", [512, 2048]).ap()
    wout_d = din("w_out", [D, D]).ap()
    gffn_d = din("norm_ffn_g", [D])
    wr_d = din("w_router", [D, 36]).ap()
    br_d = din("b_router", [36])
    weg_d = din("w_expert_gate", [32, D, 256]).ap()
    weu_d = din("w_expert_up", [32, D, 256]).ap()
    wed_d = din("w_expert_down", [32, 256, D]).ap()
    out_d = nc.dram_tensor("out", [S, D], F32, kind="ExternalOutput").ap()
    dbg = {}
    if debug:
        lst = [("dbg_x1", [S, D]), ("dbg_comb", [128, NT * 32]), ("dbg_T", [128, 4096]), ("dbg_ks", [128, 16 * 18]), ("dbg_ug", [128, 32 * 512])]
        for nm_ in ("dbg_gy", "dbg_o", "dbg_q", "dbg_k"):
            lst += [(nm_ + str(q_), [128, S]) for q_ in range(4)]
        for nm, shp in lst:
            dbg[nm] = nc.dram_tensor(nm, shp, F32, kind="ExternalOutput").ap()

    def bc_rows(t, n, reps=1, parts=128):
        if reps == 1:
            return bass.AP(t, 0, [[0, parts], [1, n]])
        return bass.AP(t, 0, [[0, parts], [0, reps], [1, n]])

    KB = 1024

    def A(name, shape, dt, off):
        nbytes = int(np.prod(shape[1:])) * (2 if dt == BF16 else 4)
        assert SB_BASE + off + nbytes <= 229376 - 32, (name, off, nbytes)
        return nc.alloc_sbuf_tensor_at(name, list(shape), dt, offset=SB_BASE + off)

    c_off = [0]

    def CA(name, shape, dt):
        n = int(np.prod(shape[1:])) * (2 if dt == BF16 else 4)
        t = A(name, shape, dt, c_off[0])
        c_off[0] += (n + 31) // 32 * 32
        return t

    ident_f = CA("ident_f", [128, 128], F32)
    ident_b = CA("ident_b", [128, 128], BF16)
    maskf = CA("maskf", [128, 128], F32)
    maskneg_b = CA("maskneg_b", [128, 128], BF16)
    gq_t = CA("gq_t", [128, 512], F32)
    gk_t = CA("gk_t", [128, 512], F32)
    sg08_t = CA("sg08_t", [128, 128], F32)
    gmix_t = CA("gmix_t", [128, D], F32)
    gffn_t = CA("gffn_t", [128, D], F32)
    cos_t = CA("cos_t", [128, NT * 8], F32)
    sin_t = CA("sin_t", [128, NT * 8], F32)
    lamv = CA("lamv", [128, 8], F32)
    comb_all = CA("comb_all", [128, NT * 32], F32)
    wr32 = CA("wr32", [128, 8 * 36], F32)
    br_t = CA("br_t", [128, 36], F32)
    ks_are = CA("ks_are", [128, 16 * 9], F32)
    ks_aim = CA("ks_aim", [128, 16 * 9], F32)
    ks_naim = CA("ks_naim", [128, 16 * 9], F32)
    dvec = CA("dvec", [128, 32], F32)
    stat = CA("stat", [128, 64], F32)
    ones_f = CA("ones_f", [128, 128], F32)
    assert c_off[0] <= 24 * KB, c_off[0]
    M0 = 24 * KB
    R_G, R_O, R_Q, R_K, R_V, R_T = M0, M0 + 32 * KB, M0 + 64 * KB, M0 + 96 * KB, M0 + 128 * KB, M0 + 161 * KB
    R_END = 229376 - SB_BASE - 64

    pp = fw.psum("pp", [128, 8 * 512], F32)
    ppb = pp.bitcast(BF16)
    PB = [Buf("psum%d" % i) for i in range(8)]

    def bank(i, a=0, b=512):
        return pp[:, i * 512 + a:i * 512 + b]

    def bankb(i, a=0, b=1024):
        return ppb[:, i * 1024 + a:i * 1024 + b]

    def MM(out, lhsT, rhs, start, stop, r, w, sig=True, add=False, skip=False):
        if skip:
            return fw.op("pe", lambda e: e.matmul(out, lhsT, rhs, start=start, stop=stop, skip_group_check=True), reads=r, writes=w, sig=sig, add=add)
        return fw.op("pe", lambda e: e.matmul(out, lhsT, rhs, start=start, stop=stop), reads=r, writes=w, sig=sig, add=add)

    def TR(out, in_, ident, r, w, sig=True, add=False):
        return fw.op("pe", lambda e: e.transpose(out, in_, ident), reads=r, writes=w, sig=sig, add=add)

    def ACT(out, in_, func, r, w, add=False, **kw):
        return fw.op("act", lambda e: e.activation(out, in_, func, **kw), reads=r, writes=w, add=add)

    def TT(eng, out, in0, in1, op, r, w, add=False):
        return fw.op(eng, lambda e: e.tensor_tensor(out, in0, in1, op), reads=r, writes=w, add=add)

    def TS(eng, out, in0, s1, s2, op0, op1, r, w, add=False):
        if s2 is None:
            return fw.op(eng, lambda e: e.tensor_scalar(out, in0, s1, None, op0), reads=r, writes=w, add=add)
        return fw.op(eng, lambda e: e.tensor_scalar(out, in0, s1, s2, op0, op1), reads=r, writes=w, add=add)

    def STT(out, in0, sc, in1, op0, op1, r, w, add=False):
        return fw.op("dve", lambda e: e.scalar_tensor_tensor(out, in0, sc, in1, op0, op1), reads=r, writes=w, add=add)

    def CP(eng, out, in_, r, w, add=False):
        if eng == "act":
            return ACT(out, in_, AF.Copy, r, w, add=add)
        return fw.op(eng, lambda e: e.tensor_copy(out, in_), reads=r, writes=w, add=add)

    def RECIP(out, in_, r, w, add=False):
        return fw.op("dve", lambda e: e.reciprocal(out, in_), reads=r, writes=w, add=add)

    def RSUM(out, in_, r, w, add=False):
        return fw.op("dve", lambda e: e.reduce_sum(out, in_, axis=AX.X), reads=r, writes=w, add=add)

    def RMAX(out, in_, r, w, add=False):
        return fw.op("dve", lambda e: e.reduce_max(out, in_, axis=AX.X), reads=r, writes=w, add=add)

    def MEMSET(eng, out, val, r, w, add=False):
        return fw.op(eng, lambda e: e.memset(out, val), reads=r, writes=w, add=add)

    def V(t, off, dims):
        pstride = int(np.prod(t.shape[1:]))
        return bass.AP(t, off, [[pstride, t.shape[0]]] + [list(d) for d in dims])

    def VP(t, p0, pn, off, dims):
        pstride = int(np.prod(t.shape[1:]))
        return bass.AP(t, p0 * pstride + off, [[pstride, pn]] + [list(d) for d in dims])

    Bc = Buf("consts")
    MEMSET("pool", ident_f[:], 1.0, [], [Bc])
    fw.op("pool", lambda e: e.affine_select(ident_f[:], ident_f[:], pattern=[[-1, 128]], compare_op=ALU.is_equal, fill=0.0, base=0, channel_multiplier=1), reads=[Bc], writes=[Bc])
    CP("pool", ident_b[:], ident_f[:], [Bc], [Bc])
    MEMSET("pool", maskf[:], 0.0, [Bc], [Bc])
    fw.op("pool", lambda e: e.affine_select(maskf[:], maskf[:], pattern=[[1, 128]], compare_op=ALU.is_ge, fill=-30000.0, base=0, channel_multiplier=-1), reads=[Bc], writes=[Bc])
    CP("pool", maskneg_b[:], maskf[:], [Bc], [Bc])
    fw.dma("sp", gq_t[:], bc_rows(qg_d, 64, 8), writes=[Bc], add=True)
    fw.dma("sp", gk_t[:], bc_rows(kg_d, 64, 8), writes=[Bc], add=True)
    fw.dma("sp", sg08_t[:], bc_rows(subg_d, 128), writes=[Bc], add=True)
    fw.dma("sp", lamv[:, 4:5], bass.AP(subg_d, 0, [[1, 128], [1, 1]]), writes=[Bc], add=True)
    MEMSET("pool", ones_f[:], 1.0, [], [Bc], add=True)
    fw.dma("sp", gmix_t[:], bc_rows(gmix_d, D), writes=[Bc], add=True)
    fw.dma("sp", gffn_t[:], bc_rows(gffn_d, D), writes=[Bc], add=True)
    fw.dma("sp", br_t[:], bc_rows(br_d, 36), writes=[Bc], add=True)
    fw.dma("sp", wr32[:], wr_d.rearrange("(k p) n -> p k n", p=128), writes=[Bc], add=True)
    for hh in range(8):
        fw.dma("sp", dvec[hh * 16:(hh + 1) * 16, :], dsk_d, writes=[Bc], add=True)
    tmp0 = A("c_tmp0", [128, 4 * 64], F32, R_T)
    posi = A("c_posi", [128, NT], I32, R_T + 1 * KB)
    posf = A("c_posf", [128, NT], F32, R_T + 1 * KB + 128)
    ang = A("c_ang", [128, NT * 8], F32, R_T + 2 * KB)
    ang2 = A("c_ang2", [128, NT * 8], F32, R_T + 3 * KB)
    ang3 = A("c_ang3", [128, NT * 8], F32, R_T + 4 * KB)
    Bt = Buf("ctmp")
    for i, dd in enumerate((lq1_d, lk1_d, lq2_d, lk2_d)):
        fw.dma("sp", tmp0[:, i * 64:(i + 1) * 64], bc_rows(dd, 64), writes=[Bt], add=True)
    fw.dma("sp", posi[:], pos_d, writes=[Bt], add=True)
    TS("dve", sg08_t[:], sg08_t[:], 1.0 - LAM_INIT, None, ALU.mult, None, [Bc], [Bc])
    TS("dve", lamv[:, 4:5], lamv[:, 4:5], 1.0 - LAM_INIT, None, ALU.mult, None, [Bc], [Bc])
    TT("dve", tmp0[:, 0:64], tmp0[:, 0:64], tmp0[:, 64:128], ALU.mult, [Bt], [Bt])
    TT("dve", tmp0[:, 128:192], tmp0[:, 128:192], tmp0[:, 192:256], ALU.mult, [Bt], [Bt])
    RSUM(lamv[:, 0:1], tmp0[:, 0:64], [Bt], [Bc])
    RSUM(lamv[:, 1:2], tmp0[:, 128:192], [Bt], [Bc])
    ACT(lamv[:, 0:2], lamv[:, 0:2], AF.Exp, [Bc], [Bc])
    TT("dve", lamv[:, 2:3], lamv[:, 1:2], lamv[:, 0:1], ALU.subtract, [Bc], [Bc])
    TS("dve", lamv[:, 3:4], lamv[:, 2:3], -LAM_INIT, None, ALU.add, None, [Bc], [Bc])
    CP("dve", posf[:], posi[:], [Bt], [Bt])
    for i in range(8):
        inv = (500000.0 ** (-i / 8.0)) / (2.0 * math.pi)
        TS("dve", V(ang, i, [[8, NT]]), posf[:], inv, None, ALU.mult, None, [Bt], [Bt], add=True)
    MAGIC = 12582912.0
    for (dst, shift) in ((sin_t, 0.0), (cos_t, 0.25)):
        TS("dve", ang2[:], ang[:], shift, None, ALU.add, None, [Bt], [Bt])
        TS("dve", ang3[:], ang2[:], MAGIC, MAGIC, ALU.add, ALU.subtract, [Bt], [Bt])
        TT("dve", ang2[:], ang2[:], ang3[:], ALU.subtract, [Bt], [Bt])
        ACT(dst[:], ang2[:], AF.Sin, [Bt], [Bc], scale=6.283185)

    if stop == 'p0a':
        fw.finish()
        return nc
    T_b = A("T_b", [128, 32 * 128], BF16, R_Q)
    VTre_b = A("VTre_b", [128, 32 * 64], BF16, R_Q + 8 * KB)
    VTim_b = A("VTim_b", [128, 32 * 64], BF16, R_Q + 12 * KB)
    Wre_b = A("Wre_b", [128, 32 * 128], BF16, R_Q + 16 * KB)
    Wimn_b = A("Wimn_b", [128, 32 * 128], BF16, R_Q + 24 * KB)
    Bs5w = Buf("s5w")
    Gre = A("Gre_", [128, 4096], F32, M0 + 0)
    Gim = A("Gim_", [128, 4096], F32, M0 + 16 * KB)
    HHre = A("HHre_", [128, 32 * 144], F32, M0 + 32 * KB)
    HHim = A("HHim_", [128, 32 * 144], F32, M0 + 96 * KB)
    VVre = A("VVre_", [128, 4096], F32, M0 + 114 * KB)
    VVim = A("VVim_", [128, 4096], F32, M0 + 130 * KB)
    GS = A("GS_", [128, 4096], F32, M0 + 146 * KB)
    HS = A("HS_", [128, 4096], F32, M0 + 162 * KB)
    so = [M0 + 50 * KB]

    def SA(name, n):
        t = A(name, [128, n], F32, so[0])
        so[0] += n * 4
        return t
    lre = SA("lre", 32); lim = SA("lim", 32); dtt = SA("dtt", 32); ar = SA("ar", 32); ai = SA("ai", 32)
    mm_ = SA("mm_", 32); minv = SA("minv", 32); kk = SA("kk", 32); rr = SA("rr", 32); x8 = SA("x8", 32); x2 = SA("x2", 32)
    pp_ = SA("pp_", 32); cc = SA("cc", 32); ss_ = SA("ss_", 32); t1 = SA("t1", 32); t2 = SA("t2", 32); t3 = SA("t3", 32); t4 = SA("t4", 32)
    LPre = SA("LPre", 32 * 9); LPim = SA("LPim", 32 * 9); LIre = SA("LIre", 32 * 8); LIim = SA("LIim", 32 * 8)
    Are = SA("Are", 32 * 9); Aim = SA("Aim", 32 * 9)
    fre = SA("fre", 32); fim = SA("fim", 32); ire = SA("ire", 32); iim = SA("iim", 32); den = SA("den", 32)
    assert so[0] <= M0 + 64 * KB
    Bin = A("Bin_re", [128, 512], F32, M0 + 178 * KB)
    Bin_im = A("Bin_im", [128, 512], F32, M0 + 180 * KB)
    Cre = A("Cre_in", [128, 512], F32, M0 + 146 * KB)
    Cim = A("Cim_in", [128, 512], F32, M0 + 148 * KB)
    Bbre = A("Bbre", [128, 512], F32, M0 + 150 * KB)
    Bbim = A("Bbim", [128, 512], F32, M0 + 152 * KB)
    W1 = A("W1", [128, 4608], F32, M0 + 114 * KB)
    W2 = A("W2_", [128, 4608], F32, M0 + 154 * KB)
    Bp = Buf("s5prep")

    for half in range(2):
        ps_ = slice(half * 64, half * 64 + 64)
        fw.dma("sp", lre[ps_, :], lamre_d, writes=[Bp], add=True)
        fw.dma("sp", lim[ps_, :], lamim_d, writes=[Bp], add=True)
        fw.dma("sp", Bin[ps_, :], bre_d, writes=[Bp], add=True)
        fw.dma("sp", Bin_im[ps_, :], bim_d, writes=[Bp], add=True)
        fw.dma("sp", Cre[ps_, :], cre_d, writes=[Bp], add=True)
        fw.dma("sp", Cim[ps_, :], cim_d, writes=[Bp], add=True)
    fw.dma("sp", dtt[:], bc_rows(logdt_d, 32), writes=[Bp], add=True)

    def d_tt(out, a, b, op):
        return TT("dve", out, a, b, op, [Bp], [Bp])

    def d_ts(out, a, s1, s2=None, op0=ALU.mult, op1=ALU.add):
        return TS("dve", out, a, s1, s2, op0, op1, [Bp], [Bp])

    def cmul(ore, oim, are_, aim_, bre_, bim_, ta, tb):
        d_tt(ta, are_, bre_, ALU.mult)
        d_tt(tb, aim_, bim_, ALU.mult)
        d_tt(ore, ta, tb, ALU.subtract)
        d_tt(ta, are_, bim_, ALU.mult)
        d_tt(tb, aim_, bre_, ALU.mult)
        d_tt(oim, ta, tb, ALU.add)

    ACT(dtt[:], dtt[:], AF.Exp, [Bp], [Bp])
    d_tt(ar[:], lre[:], dtt[:], ALU.mult)
    d_tt(ai[:], lim[:], dtt[:], ALU.mult)
    MEMSET("dve", mm_[:], 1.0, [Bp], [Bp])
    for k in range(10, 0, -1):
        d_tt(mm_[:], mm_[:], ar[:], ALU.mult)
        d_ts(mm_[:], mm_[:], 1.0 / k, 1.0)
    RECIP(minv[:], mm_[:], [Bp], [Bp])
    d_ts(kk[:], ai[:], 1.0 / (2.0 * math.pi), None)
    d_ts(kk[:], kk[:], MAGIC, MAGIC, ALU.add, ALU.subtract)
    STT(rr[:], kk[:], -6.28125, ai[:], ALU.mult, ALU.add, [Bp], [Bp])
    STT(rr[:], kk[:], -(2.0 * math.pi - 6.28125), rr[:], ALU.mult, ALU.add, [Bp], [Bp])
    d_ts(x8[:], rr[:], 0.125, None)
    d_tt(x2[:], x8[:], x8[:], ALU.mult)
    sc_ = [1.0, -1.0 / 6, 1.0 / 120, -1.0 / 5040, 1.0 / 362880, -1.0 / 39916800]
    cc_ = [1.0, -0.5, 1.0 / 24, -1.0 / 720, 1.0 / 40320, -1.0 / 3628800, 1.0 / 479001600]
    for (dst, co) in ((ss_, sc_), (cc, cc_)):
        MEMSET("dve", dst[:], co[-1], [Bp], [Bp])
        for c in co[-2::-1]:
            d_tt(dst[:], dst[:], x2[:], ALU.mult)
            d_ts(dst[:], dst[:], c, None, ALU.add)
    d_tt(ss_[:], ss_[:], x8[:], ALU.mult)
    for _ in range(3):
        d_tt(t1[:], cc[:], cc[:], ALU.mult)
        d_tt(t2[:], ss_[:], ss_[:], ALU.mult)
        STT(t3[:], ss_[:], 2.0, cc[:], ALU.mult, ALU.mult, [Bp], [Bp])
        d_tt(cc[:], t1[:], t2[:], ALU.subtract)
        CP("dve", ss_[:], t3[:], [Bp], [Bp])
    def LPv(t, j):
        return V(t, j, [[9, 32]])

    def LIv(t, j):
        return V(t, j, [[8, 32]])
    MEMSET("dve", LPv(LPre, 0), 1.0, [Bp], [Bp])
    MEMSET("dve", LPv(LPim, 0), 0.0, [Bp], [Bp])
    d_tt(LPv(LPre, 1), mm_[:], cc[:], ALU.mult)
    d_tt(LPv(LPim, 1), mm_[:], ss_[:], ALU.mult)
    for j in range(2, 9):
        cmul(LPv(LPre, j), LPv(LPim, j), LPv(LPre, j - 1), LPv(LPim, j - 1), LPv(LPre, 1), LPv(LPim, 1), t1[:], t2[:])
    MEMSET("dve", LIv(LIre, 0), 1.0, [Bp], [Bp])
    MEMSET("dve", LIv(LIim, 0), 0.0, [Bp], [Bp])
    d_tt(LIv(LIre, 1), minv[:], cc[:], ALU.mult)
    d_tt(t3[:], minv[:], ss_[:], ALU.mult)
    d_ts(LIv(LIim, 1), t3[:], -1.0, None)
    for j in range(2, 8):
        cmul(LIv(LIre, j), LIv(LIim, j), LIv(LIre, j - 1), LIv(LIim, j - 1), LIv(LIre, 1), LIv(LIim, 1), t1[:], t2[:])
    CP("dve", LPv(Are, 0), LPv(LPre, 8), [Bp], [Bp])
    CP("dve", LPv(Aim, 0), LPv(LPim, 8), [Bp], [Bp])
    for k in range(1, 9):
        d_tt(t1[:], LPv(Are, k - 1), LPv(Are, k - 1), ALU.mult)
        d_tt(t2[:], LPv(Aim, k - 1), LPv(Aim, k - 1), ALU.mult)
        d_tt(LPv(Are, k), t1[:], t2[:], ALU.subtract)
        STT(LPv(Aim, k), LPv(Are, k - 1), 2.0, LPv(Aim, k - 1), ALU.mult, ALU.mult, [Bp], [Bp])
    for gl in range(2):
        for (src, dst) in ((Are, ks_are), (Aim, ks_aim)):
            fw.op("dve", lambda e, src=src, dst=dst, gl=gl: e.tensor_copy(
                VP(dst, gl * 64, 64, 0, [[9, 16], [1, 9]]), VP(src, gl * 64, 64, gl * 9, [[18, 16], [1, 9]])), reads=[Bp], writes=[Bc], add=True)
    TS("dve", ks_naim[:], ks_aim[:], -1.0, None, ALU.mult, None, [Bc], [Bc])
    d_ts(t1[:], LPv(LPre, 1), -1.0, None, ALU.add)
    d_tt(den[:], lre[:], lre[:], ALU.mult)
    d_tt(t2[:], lim[:], lim[:], ALU.mult)
    d_tt(den[:], den[:], t2[:], ALU.add)
    RECIP(den[:], den[:], [Bp], [Bp])
    d_tt(ire[:], lre[:], den[:], ALU.mult)
    d_tt(iim[:], lim[:], den[:], ALU.mult)
    d_ts(iim[:], iim[:], -1.0, None)
    cmul(fre[:], fim[:], t1[:], LPv(LPim, 1), ire[:], iim[:], t3[:], t4[:])
    def bc16(t):
        return V(t, 0, [[1, 32], [0, 16]])

    def v3(t):
        return V(t, 0, [[16, 32], [1, 16]])
    w1a = V(W1, 0, [[16, 32], [1, 16]]); w1b = V(W1, 512, [[16, 32], [1, 16]])
    cmul(v3(Bbre), v3(Bbim), bc16(fre), bc16(fim), v3(Bin), v3(Bin_im), w1a, w1b)
    def g4(t):
        return V(t, 0, [[128, 32], [16, 8], [1, 16]])

    def li4(t):
        return V(t, 0, [[8, 32], [1, 8], [0, 16]])

    def bb4(t):
        return V(t, 0, [[16, 32], [0, 8], [1, 16]])
    cmul(g4(Gre), g4(Gim), li4(LIre), li4(LIim), bb4(Bbre), bb4(Bbim), g4(W1), g4(W2))
    def h4(t):
        return V(t, 0, [[144, 32], [16, 9], [1, 16]])

    def lp4(t):
        return V(t, 0, [[9, 32], [1, 9], [0, 16]])

    def c4(t):
        return V(t, 0, [[16, 32], [0, 9], [1, 16]])
    cmul(h4(HHre), h4(HHim), lp4(LPre), lp4(LPim), c4(Cre), c4(Cim), h4(W1), h4(W2))
    def hs4(t, j0):
        return V(t, j0 * 16, [[144, 32], [1, 128]])
    CP("dve", V(Wre_b, 0, [[128, 32], [1, 128]]), hs4(HHre, 1), [Bp], [Bs5w], add=True)
    TS("dve", V(Wimn_b, 0, [[128, 32], [1, 128]]), hs4(HHim, 1), -1.0, None, ALU.mult, None, [Bp], [Bs5w], add=True)
    def l7(t):
        return V(t, 7, [[9, 32], [0, 128]])

    def g3(t):
        return V(t, 0, [[128, 32], [1, 128]])
    Bvv = Buf("vv")
    cmul(g3(VVre), g3(VVim), l7(LPre), l7(LPim), g3(Gre), g3(Gim), g3(GS), g3(HS))
    CP("dve", VP(GS, 0, 64, 0, [[1, 4096]]), VP(Gre, 0, 64, 0, [[1, 4096]]), [Bp], [Bp])
    fw.op("dve", lambda e: e.tensor_scalar(VP(GS, 64, 64, 0, [[1, 4096]]), VP(Gim, 64, 64, 0, [[1, 4096]]), -1.0, None, ALU.mult), reads=[Bp], writes=[Bp])
    CP("dve", VP(HS, 0, 64, 0, [[128, 32], [1, 128]]), VP(HHre, 0, 64, 0, [[144, 32], [1, 128]]), [Bp], [Bp])
    CP("dve", VP(HS, 64, 64, 0, [[128, 32], [1, 128]]), VP(HHim, 64, 64, 0, [[144, 32], [1, 128]]), [Bp], [Bp])
    mask4 = A("mask4", [128, 512], F32, M0 + 178 * KB)
    MEMSET("pool", mask4[:], 1.0, [Bp], [Bp])
    fw.op("pool", lambda e: e.affine_select(mask4[:], mask4[:], pattern=[[0, 4], [16, 8], [0, 16]], compare_op=ALU.is_ge, fill=0.0, base=15, channel_multiplier=-1), reads=[Bp], writes=[Bp])
    Tm = A("Tm", [128, 512], F32, M0 + 180 * KB)
    for q4 in range(8):
        bk = q4 % 2
        for gi in range(4):
            g = q4 * 4 + gi
            MM(bank(bk, gi * 128, gi * 128 + 128), GS[:, g * 128:(g + 1) * 128], HS[:, g * 128:(g + 1) * 128], True, True, [Bp], [PB[bk]], sig=(gi == 3), add=(gi > 0))
        TT("dve", Tm[:], bank(bk), mask4[:], ALU.mult, [PB[bk], Bp], [Bp])
        for gi in range(4):
            g = q4 * 4 + gi
            STT(T_b[:, g * 128:(g + 1) * 128], ident_f[:], dvec[:, g:g + 1], Tm[:, gi * 128:(gi + 1) * 128], ALU.mult, ALU.add, [Bp, Bc], [Bs5w], add=True)
    for (src, dst) in ((VVre, VTre_b), (VVim, VTim_b)):
        for q4 in range(8):
            bk = 2 + q4 % 2
            for gi in range(4):
                g = q4 * 4 + gi
                TR(bank(bk, gi * 128, gi * 128 + 128), src[:, g * 128:(g + 1) * 128], ident_f[:], [Bp, Bc], [PB[bk]], sig=(gi == 3), add=(gi > 0))
            CP("act", V(dst, q4 * 256, [[64, 4], [1, 64]]), bass.AP(pp, bk * 512, [[4096, 128], [128, 4], [1, 64]]), [PB[bk]], [Bs5w], add=True)
    if debug:
        dtmp = A("dtmp", [128, 4096], F32, M0 + 0)
        fw.barrier()
        CP("dve", dtmp[:], T_b[:], [Bs5w], [Bp])
        fw.dma("sp", dbg["dbg_T"], dtmp[:], reads=[Bp], is_output=True)
        dks = A("dks", [128, 288], F32, M0 + 16 * KB)
        CP("dve", dks[:, 0:144], ks_are[:], [Bc], [Bp])
        CP("dve", dks[:, 144:288], ks_aim[:], [Bc], [Bp])
        fw.dma("sp", dbg["dbg_ks"], dks[:], reads=[Bp], is_output=True)
    fw.barrier()

    if stop == 'p0b':
        fw.finish()
        return nc
    def rms_tile(xt, hbt, jk, st, Bx, Bh, Bst, rows, gt=gmix_t, out_dt_bf=True):
        ACT(jk, xt, AF.Square, [Bx], [Bst, Bh], accum_out=st[:, 0:1])
        ACT(st[:, 1:2], st[:, 0:1], AF.Sqrt, [Bst], [Bst], scale=1.0 / D, bias=EPS)
        RECIP(st[:, 2:3], st[:, 1:2], [Bst], [Bst])
        STT(hbt, xt, st[:, 2:3], gt[:], ALU.mult, ALU.mult, [Bx, Bst, Bc], [Bh])

    gyT = A("gyT", [128, 4 * S], BF16, R_G)
    Wu_b = A("Wu_b", [128, 8 * 512], BF16, R_O)
    hT_sb = A("hT_sb", [128, 8 * 1024], BF16, R_O + 8 * KB)
    U_tok = A("U_tok", [128, 4 * 4096], BF16, R_K)
    Ug = A("Ug", [128, 32 * 512], BF16, R_V)
    xa = [A("xa%d" % i, [128, D], F32, R_T + i * 4 * KB) for i in range(2)]
    hba = [A("hba%d" % i, [128, D], BF16, R_T + 8 * KB + i * 2 * KB) for i in range(2)]
    jka = A("jka", [128, D], BF16, R_T + 12 * KB)
    jkaA = A("jkaA", [128, D], BF16, R_G)
    ksb = [A("ksb%d" % i, [128, 2 * 512], F32, R_T + 12 * KB + i * 4 * KB) for i in range(2)]
    Bxa = [Buf("xa0"), Buf("xa1")]; Bhba = [Buf("hba0"), Buf("hba1")]; Bsta = [Buf("sta0"), Buf("sta1")]
    BWu = Buf("Wu"); BhT = Buf("hTsb"); BUt = [Buf("Ut%d" % i) for i in range(4)]; BUg = [Buf("Ug%d" % i) for i in range(32)]
    Bjk = Buf("jk")
    fw.dma("pool", V(Wu_b, 0, [[512, 8], [1, 512]]), w_in_d[:, 1536:2048].rearrange("(k p) n -> p k n", p=128), writes=[BWu])
    for sb in range(4):
        for i in range(8):
            n = sb * 8 + i
            bi = n % 2
            fw.dma("sp", xa[bi][:], x_d[n * 128:(n + 1) * 128, :], writes=[Bxa[bi]])
            rms_tile(xa[bi][:], hba[bi][:], jkaA[:], V(stat, bi * 4, [[1, 4]]), Bxa[bi], Bhba[bi], Bsta[bi], None)
            for k in range(8):
                TR(bankb(0, k * 128, k * 128 + 128), hba[bi][:, k * 128:(k + 1) * 128], ident_b[:], [Bhba[bi], Bc], [PB[0]], sig=(k == 7), add=(k > 0))
            CP("act", V(hT_sb, i * 128, [[1024, 8], [1, 128]]), bass.AP(ppb, 0, [[8192, 128], [128, 8], [1, 128]]), [PB[0]], [BhT], add=(i > 0))
        for tau in range(8):
            bk = 1 + tau % 2
            for k in range(8):
                MM(bank(bk), V(hT_sb, k * 1024 + tau, [[8, 128]]), Wu_b[:, k * 512:(k + 1) * 512], k == 0, k == 7, [BhT, BWu], [PB[bk]], sig=(k == 7), add=(k > 0))
            eng = "act" if tau % 2 == 0 else "dve"
            CP(eng, V(U_tok, sb * 4096 + tau * 16, [[128, 32], [1, 16]]), bass.AP(pp, bk * 512, [[4096, 128], [16, 32], [1, 16]]), [PB[bk]], [BUt[sb]], add=(tau > 0))
        for g8 in range(4):
            bk = 3 + g8 % 2
            for gi in range(8):
                g = g8 * 8 + gi
                TR(bankb(bk, gi * 128, gi * 128 + 128), U_tok[:, sb * 4096 + g * 128: sb * 4096 + (g + 1) * 128], ident_b[:], [BUt[sb], Bc], [PB[bk]], sig=(gi == 7), add=(gi > 0))
            eng = "act" if g8 % 2 == 0 else "dve"
            CP(eng, V(Ug, g8 * 8 * 512 + sb * 128, [[512, 8], [1, 128]]), bass.AP(ppb, bk * 1024, [[8192, 128], [128, 8], [1, 128]]), [PB[bk]], [BUg[g8 * 8 + gi] for gi in range(8)], add=True)
    if debug:
        fw.barrier()
        dtmp2 = A("dtmp2", [128, 16384], F32, R_G)
        CP("dve", dtmp2[:], Ug[:], BUg, [Bp])
        fw.dma("sp", dbg["dbg_ug"], dtmp2[:], reads=[Bp], is_output=True)
        fw.barrier()
    if stop == 'pA1':
        fw.finish()
        return nc
    Ygel = U_tok
    BY = [Buf("Ygel%d" % i) for i in range(32)]
    Xb = [A("Xb%d" % i, [128, 2 * 512], BF16, R_O + 24 * KB + i * 2 * KB) for i in range(2)]
    BXb = [Buf("Xb0"), Buf("Xb1")]
    Bks = [Buf("ks0"), Buf("ks1")]
    gel = [A("gel%d" % i, [128, 512], F32, R_O + 28 * KB + i * 2 * KB) for i in range(2)]
    Bgel = [Buf("gel0"), Buf("gel1")]
    for gp in range(16):
        for (ri, VT) in ((0, VTre_b), (1, VTim_b)):
            bk = 5 + ri
            for gl in range(2):
                g = 2 * gp + gl
                MM(pp[gl * 64:(gl + 1) * 64, bk * 512:(bk + 1) * 512], VT[:, g * 64:(g + 1) * 64], Ug[:, g * 512:(g + 1) * 512], True, True, [Bs5w, BUg[g]], [PB[bk]], sig=(gl == 1), add=(gl > 0))
        cur, nxt = 0, 1
        CP("act", ksb[cur][:, 0:512], bank(5), [PB[5]], [Bks[cur]])
        CP("act", ksb[cur][:, 512:1024], bank(6), [PB[6]], [Bks[cur]], add=True)
        for k in range(9):
            s = 1 << k
            n = 512 - s
            a_k = ks_are[:, gp * 9 + k: gp * 9 + k + 1]
            b_k = ks_aim[:, gp * 9 + k: gp * 9 + k + 1]
            nb_k = ks_naim[:, gp * 9 + k: gp * 9 + k + 1]
            c_, n_ = ksb[cur], ksb[nxt]
            CP("act", V(n_, 0, [[512, 2], [1, s]]), V(c_, 0, [[512, 2], [1, s]]), [Bks[cur]], [Bks[nxt]])
            STT(n_[:, s:512], c_[:, 0:n], a_k, c_[:, s:512], ALU.mult, ALU.add, [Bks[cur], Bc], [Bks[nxt]], add=True)
            STT(n_[:, s:512], c_[:, 512:512 + n], nb_k, n_[:, s:512], ALU.mult, ALU.add, [Bks[cur], Bks[nxt], Bc], [Bks[nxt]], add=True)
            STT(n_[:, 512 + s:1024], c_[:, 0:n], b_k, c_[:, 512 + s:1024], ALU.mult, ALU.add, [Bks[cur], Bc], [Bks[nxt]], add=True)
            STT(n_[:, 512 + s:1024], c_[:, 512:512 + n], a_k, n_[:, 512 + s:1024], ALU.mult, ALU.add, [Bks[cur], Bks[nxt], Bc], [Bks[nxt]], add=True)
            cur, nxt = nxt, cur
        xb = Xb[gp % 2]
        CP("act", xb[:], ksb[cur][:], [Bks[cur]], [BXb[gp % 2]])
        for gl in range(2):
            g = 2 * gp + gl
            bk = 1 + g % 2
            MM(bank(bk), T_b[:, g * 128:(g + 1) * 128], Ug[:, g * 512:(g + 1) * 512], True, False, [Bs5w, BUg[g]], [PB[bk]], sig=False)
            MM(bank(bk, 1, 512), VP(Wre_b, gl * 64, 64, g * 128, [[1, 128]]), VP(xb, gl * 64, 64, 0, [[1, 511]]), False, False, [Bs5w, BXb[gp % 2]], [PB[bk]], sig=False, add=True)
            MM(bank(bk, 1, 512), VP(Wimn_b, gl * 64, 64, g * 128, [[1, 128]]), VP(xb, gl * 64, 64, 512, [[1, 511]]), False, True, [Bs5w, BXb[gp % 2]], [PB[bk]], sig=True, add=True)
            ge = gel[g % 2]
            Bg = Bgel[g % 2]
            ACT(ge[:], bank(bk), AF.Square, [PB[bk]], [Bg])
            TS("dve", ge[:], ge[:], 0.044715, 1.0, ALU.mult, ALU.add, [Bg], [Bg])
            TT("dve", ge[:], ge[:], bank(bk), ALU.mult, [Bg, PB[bk]], [Bg])
            ACT(ge[:], ge[:], AF.Sigmoid, [Bg], [Bg], scale=1.5957691216057308)
            TT("dve", Ygel[:, g * 512:(g + 1) * 512], ge[:], bank(bk), ALU.mult, [Bg, PB[bk]], [BY[g]] + BUt, add=True)
    if stop == 'pA2':
        fw.finish()
        return nc
    fw.barrier()
    Ytok = Ug
    BYt = [Buf("Ytok%d" % i) for i in range(4)]
    Bgy = [Buf("gyT%d" % i) for i in range(8)]
    gy32 = [A("gy32_%d" % i, [128, 4 * 1024], F32, R_Q + i * 16 * KB) for i in range(2)]
    Bg32 = [Buf("gy32_0"), Buf("gy32_1")]
    for sb in range(4):
        for g8 in range(4):
            bk = 3 + g8 % 2
            for gi in range(8):
                g = g8 * 8 + gi
                TR(bankb(bk, gi * 128, gi * 128 + 128), Ygel[:, g * 512 + sb * 128: g * 512 + (sb + 1) * 128], ident_b[:], [BY[g], Bc], [PB[bk]], sig=(gi == 7), add=(gi > 0))
            eng = "act" if g8 % 2 == 0 else "dve"
            CP(eng, V(Ytok, sb * 4096 + g8 * 128, [[16, 8], [512, 8], [1, 16]]), bass.AP(ppb, bk * 1024, [[8192, 128], [128, 8], [16, 8], [1, 16]]), [PB[bk]], BUg + [BYt[sb]], add=True)
        if stop == 'pA3':
            fw.finish()
            return nc
        for j in range(8):
            bk = 5 + j % 2
            for q4 in range(4):
                TR(bankb(bk, q4 * 128, q4 * 128 + 128), Ytok[:, sb * 4096 + j * 512 + q4 * 128: sb * 4096 + j * 512 + (q4 + 1) * 128], ident_b[:], [BYt[sb], Bc], [PB[bk]], sig=(q4 == 3), add=(q4 > 0))
            eng = "act" if j % 2 == 0 else "dve"
            CP(eng, V(gy32[sb % 2], j, [[1024, 4], [8, 128]]), bass.AP(ppb, bk * 1024, [[8192, 128], [128, 4], [1, 128]]), [PB[bk]], [Bg32[sb % 2]], add=(j > 0))
        if stop == 'pA4':
            fw.finish()
            return nc
        CP("pool", V(gyT, sb * 1024, [[S, 4], [1, 1024]]), V(gy32[sb % 2], 0, [[1024, 4], [1, 1024]]), [Bg32[sb % 2]], [Bgy[2 * sb], Bgy[2 * sb + 1]], add=True)
    if stop == 'pA5':
        fw.finish()
        return nc
    if debug:
        fw.barrier()
        for q_ in range(4):
            dst_ = A("stg_dbg_gy_%d" % q_, [128, S], F32, R_K)
            CP("dve", dst_[:], gyT[:, q_ * S:(q_ + 1) * S], Bgy, [Bp])
            fw.dma("sp", dbg["dbg_gy" + str(q_)], dst_[:], reads=[Bp], is_output=True)
    fw.barrier()

    if stop == 'pA':
        fw.finish()
        return nc
    qT = A("qT", [128, 4 * S], BF16, R_Q)
    kT = A("kT", [128, 4 * S], BF16, R_K)
    v_aug = A("v_aug", [128, NT * 4 * 130], BF16, R_V)
    Wqkv = A("Wqkv", [128, 8 * 1536], BF16, R_O)
    hTt = [A("hTt%d" % i, [128, 8 * 128], BF16, R_O + 24 * KB + i * 2 * KB) for i in range(2)]
    BhTt = [Buf("hTt0"), Buf("hTt1")]
    sqs = A("sqs", [128, 1024], BF16, R_O + 28 * KB)
    qkb = A("qkb", [128, 1024], BF16, R_O + 30 * KB)
    qkn = [A("qkn%d" % i, [128, 1024], F32, R_T + 12 * KB + i * 4 * KB) for i in range(2)]
    rtmp = A("rtmp_", [128, 3 * 128], F32, R_T + 20 * KB)
    Bsq = Buf("sqs"); Bqkn = [Buf("qkn0"), Buf("qkn1")]; Bqkb = Buf("qkb"); Brt = Buf("rtmp"); Bqst = Buf("qst")
    BW = Buf("Wqkv"); BqT = [Buf("qT%d" % i) for i in range(8)]; BkT = [Buf("kT%d" % i) for i in range(NT)]; Bv = [Buf("v%d" % i) for i in range(NT)]
    fw.dma("pool", V(Wqkv, 0, [[1536, 8], [1, 1536]]), w_in_d[:, 0:1536].rearrange("(k p) n -> p k n", p=128), writes=[BW])
    MEMSET("pool", V(v_aug, 128, [[130, NT * 4], [1, 2]]), 1.0, [], Bv)

    def qkbank(n):
        return 1 if n % 2 == 0 else 6

    def pb_M1(n):
        bi = n % 2
        fw.dma("sp", xa[bi][:], x_d[n * 128:(n + 1) * 128, :], writes=[Bxa[bi]])
        rms_tile(xa[bi][:], hba[bi][:], hba[bi][:], V(stat, bi * 4, [[1, 4]]), Bxa[bi], Bhba[bi], Bsta[bi], None)
        for k in range(8):
            TR(bankb(0, k * 128, k * 128 + 128), hba[bi][:, k * 128:(k + 1) * 128], ident_b[:], [Bhba[bi], Bc], [PB[0]], sig=(k == 7), add=(k > 0))
        CP("act", hTt[bi][:], bankb(0), [PB[0]], [BhTt[bi]])

    def pb_M2(n):
        bi = n % 2
        b0 = qkbank(n)
        for cb in range(3):
            bk = (b0 + cb) if cb < 2 else 3
            for k in range(8):
                MM(bank(bk), hTt[bi][:, k * 128:(k + 1) * 128], Wqkv[:, k * 1536 + cb * 512: k * 1536 + (cb + 1) * 512], k == 0, k == 7, [BhTt[bi], BW], [PB[bk]], sig=(k == 7), add=(k > 0))
        CP("act", V(v_aug, n * 520, [[130, 4], [1, 128]]), bass.AP(pp, 3 * 512, [[4096, 128], [128, 4], [1, 128]]), [PB[3]], [Bv[n]], add=True)
        psqk = pp[:, b0 * 512:(b0 + 2) * 512]
        Pq = [PB[b0], PB[b0 + 1]]
        ACT(sqs[:], psqk, AF.Square, Pq, [Bsq])
        RSUM(stat[:, 8:24], V(sqs, 0, [[64, 16], [1, 64]]), [Bsq], [Bqst])
        ACT(stat[:, 24:40], stat[:, 8:24], AF.Sqrt, [Bqst], [Bqst], scale=1.0 / 64, bias=EPS)
        RECIP(stat[:, 40:56], stat[:, 24:40], [Bqst], [Bqst])
        q_ = qkn[n % 2]
        Bq = Bqkn[n % 2]
        TT("dve", V(q_, 0, [[64, 16], [1, 64]]), bass.AP(pp, b0 * 512, [[4096, 128], [64, 16], [1, 64]]), V(stat, 40, [[1, 16], [0, 64]]), ALU.mult, Pq + [Bqst], [Bq])
        TT("dve", q_[:, 0:512], q_[:, 0:512], gq_t[:], ALU.mult, [Bq, Bc], [Bq])
        TT("dve", q_[:, 512:1024], q_[:, 512:1024], gk_t[:], ALU.mult, [Bq, Bc], [Bq])

    def pb_M3(n):
        q_ = qkn[n % 2]
        Bq = Bqkn[n % 2]
        r1 = V(q_, 0, [[64, 16], [1, 8]]); r2 = V(q_, 8, [[64, 16], [1, 8]])
        cs = V(cos_t, n * 8, [[0, 16], [1, 8]]); sn = V(sin_t, n * 8, [[0, 16], [1, 8]])
        ta = V(rtmp, 0, [[8, 16], [1, 8]]); tb = V(rtmp, 128, [[8, 16], [1, 8]]); tc = V(rtmp, 256, [[8, 16], [1, 8]])
        TT("pool", ta, r1, cs, ALU.mult, [Bq, Bc], [Brt])
        TT("pool", tb, r2, sn, ALU.mult, [Bq, Bc], [Brt], add=True)
        TT("pool", tc, r1, sn, ALU.mult, [Bq, Bc], [Brt], add=True)
        TT("pool", r1, ta, tb, ALU.subtract, [Brt, Bq], [Bq])
        TT("pool", r2, r2, cs, ALU.mult, [Bq, Bc], [Bq])
        TT("pool", r2, r2, tc, ALU.add, [Brt, Bq], [Bq])
        CP("act", qkb[:], q_[:], [Bq], [Bqkb])
        for cb in range(2):
            bkt = 4 + cb
            for h in range(4):
                TR(bankb(bkt, h * 128, h * 128 + 128), qkb[:, cb * 512 + h * 128: cb * 512 + (h + 1) * 128], ident_b[:], [Bqkb, Bc], [PB[bkt]], sig=(h == 3), add=(h > 0))
            dstT = qT if cb == 0 else kT
            dB = BqT[n // 4] if cb == 0 else BkT[n]
            CP("dve", V(dstT, n * 128, [[S, 4], [1, 128]]), bass.AP(ppb, bkt * 1024, [[8192, 128], [128, 4], [1, 128]]), [PB[bkt]], [dB], add=True)

    for t in range(-2, NT):
        if 0 <= t + 1 < NT:
            pb_M2(t + 1)
        if 0 <= t + 2 < NT:
            pb_M1(t + 2)
        if 0 <= t < NT:
            pb_M3(t)
    if debug:
        fw.barrier()
        for q_ in range(4):
            dst_ = A("stg_dbg_q_%d" % q_, [128, S], F32, R_O)
            CP("dve", dst_[:], qT[:, q_ * S:(q_ + 1) * S], BqT, [Bp])
            fw.dma("sp", dbg["dbg_q" + str(q_)], dst_[:], reads=[Bp], is_output=True)
        for q_ in range(4):
            dst_ = A("stg_dbg_k_%d" % q_, [128, S], F32, R_O)
            CP("dve", dst_[:], kT[:, q_ * S:(q_ + 1) * S], BkT, [Bp])
            fw.dma("sp", dbg["dbg_k" + str(q_)], dst_[:], reads=[Bp], is_output=True)
    fw.barrier()
    fw.barrier()

    if stop == 'pB':
        fw.finish()
        return nc
    oT = A("oT", [128, 4 * S], BF16, R_O)
    pT = [[A("pT%d%d" % (c, i), [128, 512], BF16, R_T + (c * 2 + i) * KB) for i in range(2)] for c in range(2)]
    BpT = [[Buf("pT%d%d" % (c, i)) for i in range(2)] for c in range(2)]
    of_ = [A("of%d" % i, [128, 128], F32, R_T + 4 * KB + i * 512) for i in range(2)]
    ob_ = [A("ob%d" % i, [128, 128], BF16, R_T + 5 * KB + i * 256) for i in range(2)]
    ajk = A("ajk", [128, 128], BF16, R_T + 6 * KB)
    Bof = [Buf("of0"), Buf("of1")]; Bob = [Buf("ob0"), Buf("ob1")]; Bast = [Buf("ast0"), Buf("ast1")]
    BoT = [Buf("oT%d" % i) for i in range(8)]
    Bajk = Buf("ajk")
    def accv(qs, c, a, b):
        idx = qs * 2 + c
        bk = 4 + idx // 3
        off = bk * 512 + (idx % 3) * 130
        return pp[:, off + a: off + b], PB[bk]
    pT4 = [A("pT4_%d" % i, [128, 512], BF16, R_T + i * KB) for i in range(4)]
    BpT4 = [Buf("pT4_%d" % i) for i in range(4)]
    dacc = [[A("dacc%d%d" % (r, c), [128, 512], F32, R_T + 4 * KB + (r * 2 + c) * 2 * KB) for c in range(2)] for r in range(2)]
    Bdacc = [[Buf("dacc%d%d" % (r, c)) for c in range(2)] for r in range(2)]
    rd = [A("rd%d" % i, [128, 512], F32, R_T + 12 * KB + i * 2 * KB) for i in range(2)]
    Brd = [Buf("rd0"), Buf("rd1")]
    att_items = [(h, qblk, kt, c) for h in range(4) for qblk in range(8) for kt in range(4 * qblk + 4) for c in range(2)]
    NA = len(att_items)
    qz = [[A("qz%d%d" % (r, c), [128, 512], BF16, R_T + 16 * KB + (r * 2 + c) * KB) for c in range(2)] for r in range(2)]
    Bqz = [[Buf("qz%d%d" % (r, c)) for c in range(2)] for r in range(2)]
    for r in range(2):
        for c in range(2):
            MEMSET("pool", qz[r][c][:], 0.0, [], [Bqz[r][c]])

    def stage_S(k):
        h, qblk, kt, c = att_items[k]
        q0 = max(0, kt - 4 * qblk)
        col0 = q0 * 128
        diag = kt >= 4 * qblk
        sb_ = k % 3
        r = (h * 8 + qblk) % 2
        if kt == 0:
            CP("dve", VP(qz[r][c], c * 64, 64, 0, [[1, 512]]), VP(qT, c * 64, 64, h * S + qblk * 512, [[1, 512]]), [BqT[qblk]], [Bqz[r][c]])
        MM(bank(sb_, col0, 512), kT[:, h * S + kt * 128: h * S + (kt + 1) * 128], qz[r][c][:, col0:512],
           True, not diag, [BkT[kt], Bqz[r][c]], [PB[sb_]], sig=(not diag))
        if diag:
            MM(bank(sb_, col0, col0 + 128), ident_b[:], maskneg_b[:], False, True, [Bc], [PB[sb_]], sig=True, add=True)
        ACT(pT4[k % 4][:, col0:512], bank(sb_, col0, 512), AF.Exp, [PB[sb_]], [BpT4[k % 4]], scale=0.125)

    def finalize_a1(h, qblk, rnd):
        r = rnd % 2
        MM(bank(3), ones_f[:], dacc[r][0][:], True, True, [Bc, Bdacc[r][0]], [PB[3]])
        RECIP(rd[0][:], bank(3), [PB[3]], [Brd[0]])

    def finalize_a2(h, qblk, rnd):
        r = rnd % 2
        ab = 4 + 2 * r
        MM(bank(3), ones_f[:], dacc[r][1][:], True, True, [Bc, Bdacc[r][1]], [PB[3]])
        RECIP(rd[1][:], bank(3), [PB[3]], [Brd[1]])
        TS("dve", rd[1][:], rd[1][:], lamv[:, 3:4], None, ALU.mult, None, [Brd[1], Bc], [Brd[1]])
        TT("dve", rd[0][:], bank(ab), rd[0][:], ALU.mult, [PB[ab], Brd[0]], [Brd[0]])
        TT("dve", rd[1][:], bank(ab + 1), rd[1][:], ALU.mult, [PB[ab + 1], Brd[1]], [Brd[1]])
        TT("dve", rd[1][:], rd[1][:], rd[0][:], ALU.add, [Brd[0], Brd[1]], [Brd[1]])
        ACT(rd[0][:], rd[1][:], AF.Square, [Brd[1]], [Brd[0]])

    def finalize_b(h, qblk):
        t0 = qblk * 512
        MM(bank(3), ones_f[:], rd[0][:], True, True, [Bc, Brd[0]], [PB[3]])
        ACT(rd[0][:], bank(3), AF.Sqrt, [PB[3]], [Brd[0]], scale=1.0 / 128, bias=EPS)
        RECIP(rd[0][:], rd[0][:], [Brd[0]], [Brd[0]])
        STT(oT[:, h * S + t0: h * S + t0 + 512], rd[1][:], lamv[:, 4:5], rd[0][:], ALU.mult, ALU.mult, [Brd[0], Brd[1], Bc], [BoT[qblk]], add=True)

    deferred = {}

    def stage_PV(k):
        h, qblk, kt, c = att_items[k]
        rnd = h * 8 + qblk
        r = rnd % 2
        q0 = max(0, kt - 4 * qblk)
        col0 = q0 * 128
        p_ = pT4[k % 4]
        ab = 4 + 2 * r + c
        last = (kt == 4 * qblk + 3)
        MM(bank(ab, col0, 512), V(v_aug, kt * 520 + h * 130, [[1, 128]]), p_[:, col0:512], kt == 0, last, [BpT4[k % 4], Bv[kt]], [PB[ab]], sig=True, add=(kt > 0))
        eng = "dve" if c == 0 else "pool"
        if kt == 0:
            CP(eng, dacc[r][c][:], p_[:], [BpT4[k % 4]], [Bdacc[r][c]])
        else:
            TT(eng, dacc[r][c][:, col0:512], dacc[r][c][:, col0:512], p_[:, col0:512], ALU.add, [BpT4[k % 4], Bdacc[r][c]], [Bdacc[r][c]])
        for fn_, args_ in deferred.pop(k, []):
            fn_(*args_)
        if last and c == 1:
            deferred.setdefault(k + 3, []).append((finalize_a1, (h, qblk, rnd)))
            deferred.setdefault(k + 6, []).append((finalize_a2, (h, qblk, rnd)))
            deferred.setdefault(k + 10, []).append((finalize_b, (h, qblk)))

    LOOK = 2
    for k in range(LOOK):
        stage_S(k)
    for i in range(48):
        MM(bank(7), ident_b[:], qT[:, 0:512], True, True, [Bc] + BqT[0:1], [PB[7]], sig=(i == 47), add=(i > 0))
    for k in range(0, NA):
        if k + LOOK < NA:
            stage_S(k + LOOK)
        stage_PV(k)
    for kk_ in sorted(deferred):
        for fn_, args_ in deferred[kk_]:
            fn_(*args_)
    deferred.clear()
    assert not deferred
    if debug:
        fw.barrier()
        for q_ in range(4):
            dst_ = A("stg_dbg_o_%d" % q_, [128, S], F32, R_Q)
            CP("dve", dst_[:], oT[:, q_ * S:(q_ + 1) * S], BoT, [Bp])
            fw.dma("sp", dbg["dbg_o" + str(q_)], dst_[:], reads=[Bp], is_output=True)
    fw.barrier()

    if stop == 'att':
        fw.finish()
        return nc
    Wg_b = A("Wg_b", [128, 8 * 2048], BF16, R_Q)
    wglu_b = A("wglu_b", [128, 4 * 2048], BF16, R_K)
    wout_b = A("wout_b", [128, 8 * 1024], BF16, R_K + 16 * KB)
    wo_b = A("wo_b", [128, 4 * 1024], BF16, R_V)
    xc = [A("xc%d" % i, [128, D], F32, R_V + 8 * KB + i * 4 * KB) for i in range(4)]
    hT_blk = A("hT_blk", [128, 8 * 512], BF16, R_V + 24 * KB)
    mT = A("mT", [128, 8 * 512], BF16, R_T)
    sgA = A("sgA", [128, 512], F32, R_T + 8 * KB); sgB = A("sgB", [128, 512], F32, R_T + 10 * KB); sgE = A("sgE", [128, 512], F32, R_T + 12 * KB)
    tt1 = A("tt1", [128, 512], F32, R_T + 14 * KB); tt2 = A("tt2", [128, 512], F32, R_T + 16 * KB)
    hbc = A("hbc", [128, D], BF16, R_T + 18 * KB)
    cjk = hbc
    tT32 = A("tT32", [128, 8 * 128], F32, R_T + 8 * KB)
    BWc = Buf("Wc"); Bxc = [Buf("xc%d" % i) for i in range(4)]; BhTb = Buf("hTblk"); BmT = Buf("mT")
    BsA = Buf("sgA"); BsB = Buf("sgB"); BsE = Buf("sgE"); Bt1 = Buf("tt1"); Bt2 = Buf("tt2"); Bhbc = Buf("hbc"); Bcst = Buf("cst"); BtT32 = BsA
    Bcomb = Buf("comb")
    Bout = [Buf("out_h0"), Buf("out_h1")]
    fw.dma("pool", V(Wg_b, 0, [[2048, 8], [1, 2048]]), w_in_d[:, 2048:4096].rearrange("(k p) n -> p k n", p=128), writes=[BWc], add=True)
    fw.dma("pool", V(wglu_b, 0, [[2048, 4], [1, 2048]]), wglu_d.rearrange("(k p) n -> p k n", p=128), writes=[BWc], add=True)
    fw.dma("pool", V(wout_b, 0, [[1024, 8], [1, 1024]]), wout_d.rearrange("(k p) n -> p k n", p=128), writes=[BWc], add=True)
    fw.dma("pool", V(wo_b, 0, [[1024, 4], [1, 1024]]), wo_d.rearrange("(k p) n -> p k n", p=128), writes=[BWc], add=True)

    def tT_ap(k, tok0, n):
        base = gyT if k < 4 else oT
        return base[:, (k % 4) * S + tok0: (k % 4) * S + tok0 + n]

    for blk in range(8):
        t0 = blk * 512
        for i in range(4):
            n = blk * 4 + i
            fw.dma("sp", xc[i][:], x_d[n * 128:(n + 1) * 128, :], writes=[Bxc[i]])
            rms_tile(xc[i][:], hbc[:], cjk[:], V(stat, 48, [[1, 4]]), Bxc[i], Bhbc, Bcst, None)
            for k in range(8):
                TR(bankb(0, k * 128, k * 128 + 128), hbc[:, k * 128:(k + 1) * 128], ident_b[:], [Bhbc, Bc], [PB[0]], sig=(k == 7), add=(k > 0))
            CP("act", V(hT_blk, i * 128, [[512, 8], [1, 128]]), bass.AP(ppb, 0, [[8192, 128], [128, 8], [1, 128]]), [PB[0]], [BhTb], add=(i > 0))
        for nch in range(8):
            for (bk, col) in ((1, nch), (2, 8 + nch)):
                for k in range(8):
                    MM(bank(bk), Wg_b[:, k * 2048 + col * 128: k * 2048 + (col + 1) * 128], hT_blk[:, k * 512:(k + 1) * 512], k == 0, k == 7, [BWc, BhTb], [PB[bk]], sig=(k == 7), add=(k > 0))
            for f in range(4):
                MM(bank(3), wo_b[:, f * 1024 + nch * 128: f * 1024 + (nch + 1) * 128], oT[:, f * S + t0: f * S + t0 + 512], f == 0, f == 3, [BWc, BoT[blk]], [PB[3]], sig=(f == 3), add=(f > 0))
            for (bk, col) in ((4, nch), (5, 8 + nch)):
                for f in range(4):
                    MM(bank(bk), wglu_b[:, f * 2048 + col * 128: f * 2048 + (col + 1) * 128], gyT[:, f * S + t0: f * S + t0 + 512], f == 0, f == 3, [BWc, Bgy[blk]], [PB[bk]], sig=(f == 3), add=(f > 0))
            ACT(sgA[:], bank(1), AF.Sigmoid, [PB[1]], [BsA])
            ACT(sgB[:], bank(2), AF.Sigmoid, [PB[2]], [BsB])
            ACT(sgE[:], bank(5), AF.Sigmoid, [PB[5]], [BsE])
            TT("dve", tt1[:], bank(3), sgA[:], ALU.mult, [PB[3], BsA], [Bt1])
            TT("dve", tt2[:], bank(4), sgE[:], ALU.mult, [PB[4], BsE], [Bt2])
            TT("pool", tt2[:], tt2[:], sgB[:], ALU.mult, [Bt2, BsB], [Bt2])
            TT("pool", mT[:, nch * 512:(nch + 1) * 512], tt1[:], tt2[:], ALU.add, [Bt1, Bt2], [BmT], add=(nch > 0))
        def back_A(i, blk=blk, t0=t0):
            n = blk * 4 + i
            for half in range(2):
                bk = 6 + half
                for f in range(8):
                    MM(bank(bk), mT[:, f * 512 + i * 128: f * 512 + (i + 1) * 128], wout_b[:, f * 1024 + half * 512: f * 1024 + (half + 1) * 512], f == 0, f == 7, [BmT, BWc], [PB[bk]], sig=(f == 7), add=(f > 0))
                TT("dve", xc[i][:, half * 512:(half + 1) * 512], bank(bk), xc[i][:, half * 512:(half + 1) * 512], ALU.add, [PB[bk], Bxc[i]], [Bxc[i]], add=(half > 0))
            fw.dma("sp", out_d[n * 128:(n + 1) * 128, :], xc[i][:], reads=[Bxc[i]], writes=[Bout[n // 16]], add=True)
            if debug:
                fw.dma("sp", dbg["dbg_x1"][n * 128:(n + 1) * 128, :], xc[i][:], reads=[Bxc[i]], is_output=True)
            ACT(cjk[:], xc[i][:], AF.Square, [Bxc[i]], [Bcst, Bhbc], accum_out=stat[:, 52:53])
            ACT(stat[:, 53:54], stat[:, 52:53], AF.Sqrt, [Bcst], [Bcst], scale=1.0 / D, bias=EPS)
            RECIP(stat[:, 54:55], stat[:, 53:54], [Bcst], [Bcst])
            STT(xc[i][:], xc[i][:], stat[:, 54:55], gffn_t[:], ALU.mult, ALU.mult, [Bxc[i], Bcst, Bc], [Bxc[i]])
        def back_B(i, blk=blk, t0=t0):
            n = blk * 4 + i
            for k in range(8):
                bk = k // 4
                TR(bank(bk, (k % 4) * 128, (k % 4) * 128 + 128), xc[i][:, k * 128:(k + 1) * 128], ident_f[:], [Bxc[i], Bc], [PB[bk]], sig=(k % 4 == 3), add=(k % 4 > 0))
            CP("act", tT32[:, 0:512], bank(0), [PB[0], BsA, BsB], [BsA, BsB])
            CP("act", tT32[:, 512:1024], bank(1), [PB[1]], [BsA, BsB], add=True)
            for k in range(8):
                CP("pool", tT_ap(k, n * 128, 128), tT32[:, k * 128:(k + 1) * 128], [BsA], [Bgy[blk], BoT[blk]], add=True)
            for k in range(8):
                MM(bank(2, 0, 36), tT32[:, k * 128:(k + 1) * 128], wr32[:, k * 36:(k + 1) * 36], k == 0, k == 7, [BsA, Bc], [PB[2]], sig=(k == 7), add=(k > 0))
            rt = tt1
            Br = Bt1
            lgt = rt[:, 0:36]
            TT("dve", lgt, bank(2, 0, 36), br_t[:], ALU.add, [PB[2], Bc], [Br])
            gmax = rt[:, 40:41]
            RMAX(gmax, rt[:, 0:4], [Br], [Br], add=True)
            oh = rt[:, 44:48]
            TS("dve", oh, rt[:, 0:4], gmax, None, ALU.is_equal, None, [Br], [Br], add=True)
            TS("dve", rt[:, 48:52], rt[:, 0:4], gmax, None, ALU.subtract, None, [Br], [Br], add=True)
            ACT(rt[:, 48:52], rt[:, 48:52], AF.Exp, [Br], [Br])
            RSUM(rt[:, 52:53], rt[:, 48:52], [Br], [Br], add=True)
            RECIP(rt[:, 53:54], rt[:, 52:53], [Br], [Br])
            TS("dve", rt[:, 56:64], rt[:, 4:12], rt[:, 44:45], None, ALU.mult, None, [Br], [Br], add=True)
            for g in range(1, 4):
                STT(rt[:, 56:64], rt[:, 4 + g * 8: 12 + g * 8], rt[:, 44 + g: 45 + g], rt[:, 56:64], ALU.mult, ALU.add, [Br], [Br])
            m1 = rt[:, 64:65]
            RMAX(m1, rt[:, 56:64], [Br], [Br], add=True)
            mk1 = rt[:, 72:80]
            TS("dve", mk1, rt[:, 56:64], m1, None, ALU.is_equal, None, [Br], [Br], add=True)
            es2 = rt[:, 80:88]
            STT(es2, mk1, -1e30, rt[:, 56:64], ALU.mult, ALU.add, [Br], [Br], add=True)
            m2 = rt[:, 65:66]
            RMAX(m2, es2, [Br], [Br], add=True)
            mk2 = rt[:, 88:96]
            TS("dve", mk2, es2, m2, None, ALU.is_equal, None, [Br], [Br], add=True)
            TT("dve", rt[:, 66:67], m2, m1, ALU.subtract, [Br], [Br], add=True)
            ACT(rt[:, 66:67], rt[:, 66:67], AF.Exp, [Br], [Br])
            TS("dve", rt[:, 67:68], rt[:, 66:67], 1.0, None, ALU.add, None, [Br], [Br], add=True)
            RECIP(rt[:, 68:69], rt[:, 67:68], [Br], [Br])
            TS("dve", rt[:, 69:70], rt[:, 68:69], -1.0, 1.0, ALU.mult, ALU.add, [Br], [Br], add=True)
            TT("dve", rt[:, 68:69], rt[:, 68:69], rt[:, 53:54], ALU.mult, [Br], [Br])
            TT("dve", rt[:, 69:70], rt[:, 69:70], rt[:, 53:54], ALU.mult, [Br], [Br])
            ew = rt[:, 96:104]
            TS("dve", ew, mk1, rt[:, 68:69], None, ALU.mult, None, [Br], [Br], add=True)
            STT(ew, mk2, rt[:, 69:70], ew, ALU.mult, ALU.add, [Br], [Br])
            for g in range(4):
                TS("dve", comb_all[:, n * 32 + g * 8: n * 32 + (g + 1) * 8], ew, rt[:, 44 + g:45 + g], None, ALU.mult, None, [Br], [Bcomb], add=True)
        back_A(0)
        for i in range(4):
            if i + 1 < 4:
                back_A(i + 1)
            back_B(i)
    if debug:
        fw.dma("sp", dbg["dbg_comb"], comb_all[:], reads=[Bcomb], is_output=True)
    fw.barrier()

    if stop == 'pC':
        fw.finish()
        return nc
    acc = A("acc", [128, 16 * D], F32, R_Q)
    NWB = 3
    wgu = [A("wgu%d" % i, [128, 8 * 512], BF16, R_V + i * 12 * KB) for i in range(NWB)]
    wd_ = [A("wd%d" % i, [128, 2 * 1024], BF16, R_V + i * 12 * KB + 8 * KB) for i in range(NWB)]
    Bw = [Buf("w%d" % i) for i in range(NWB)]
    sgm = [A("sgm%d" % i, [128, 256], F32, R_V + 36 * KB + i * KB) for i in range(2)]
    hid = [A("hid%d" % i, [128, 256], BF16, R_V + 38 * KB + i * 512) for i in range(2)]
    hidT = [A("hidT%d" % i, [128, 256], BF16, R_V + 39 * KB + i * 512) for i in range(2)]
    Bsg = [Buf("sgm0"), Buf("sgm1")]; Bhid = [Buf("hid0"), Buf("hid1")]; BhidT = [Buf("hidT0"), Buf("hidT1")]
    Bacc = [Buf("acc%d" % i) for i in range(16)]
    BtT = Bgy + BoT
    items = [(hf, e, i) for hf in range(2) for e in range(32) for i in range(16)]
    NI = len(items)

    def wbuf(hf, e):
        return (hf * 32 + e) % NWB

    def stage_G(k):
        hf, e, i = items[k]
        n = hf * 16 + i
        bi = k % 2
        wi = wbuf(hf, e)
        if e == 0 and i == 0:
            for ii in range(16):
                nn = hf * 16 + ii
                fw.dma("sp", acc[:, ii * D:(ii + 1) * D], out_d[nn * 128:(nn + 1) * 128, :], reads=[Bout[hf]], writes=[Bacc[ii]])
        if i == 0:
            fw.dma("pool", V(wgu[wi], 0, [[512, 8], [1, 256]]), weg_d[e].rearrange("(k p) f -> p k f", p=128), writes=[Bw[wi]])
            fw.dma("pool", V(wgu[wi], 256, [[512, 8], [1, 256]]), weu_d[e].rearrange("(k p) f -> p k f", p=128), writes=[Bw[wi]], add=True)
            fw.dma("pool", V(wd_[wi], 0, [[1024, 2], [1, 1024]]), wed_d[e].rearrange("(k p) n -> p k n", p=128), writes=[Bw[wi]], add=True)
        for kk_ in range(8):
            MM(bank(bi), tT_ap(kk_, n * 128, 128), wgu[wi][:, kk_ * 512:(kk_ + 1) * 512], kk_ == 0, kk_ == 7, [BtT[n // 4], BtT[8 + n // 4], Bw[wi]], [PB[bi]], sig=(kk_ == 7), add=(kk_ > 0))
        ACT(sgm[bi][:], bank(bi, 0, 256), AF.Silu, [PB[bi]], [Bsg[bi]])
        STT(hid[bi][:], bank(bi, 256, 512), comb_all[:, n * 32 + e: n * 32 + e + 1], sgm[bi][:], ALU.mult, ALU.mult, [PB[bi], Bcomb, Bsg[bi]], [Bhid[bi]])

    def stage_T(k):
        bi = k % 2
        for f in range(2):
            TR(bankb(2 + bi, f * 128, f * 128 + 128), hid[bi][:, f * 128:(f + 1) * 128], ident_b[:], [Bhid[bi], Bc], [PB[2 + bi]], sig=(f == 1), add=(f > 0))
        CP("act", hidT[bi][:], bankb(2 + bi, 0, 256), [PB[2 + bi]], [BhidT[bi]])

    def stage_D(k):
        hf, e, i = items[k]
        bi = k % 2
        wi = wbuf(hf, e)
        for half in range(2):
            bkd = 4 + bi * 2 + half
            for f in range(2):
                MM(bank(bkd), hidT[bi][:, f * 128:(f + 1) * 128], wd_[wi][:, f * 1024 + half * 512: f * 1024 + (half + 1) * 512], f == 0, f == 1, [BhidT[bi], Bw[wi]], [PB[bkd]], sig=(f == 1), add=(f > 0))
            TT("dve", acc[:, i * D + half * 512: i * D + (half + 1) * 512], bank(bkd), acc[:, i * D + half * 512: i * D + (half + 1) * 512], ALU.add, [PB[bkd], Bacc[i]], [Bacc[i]], add=(half > 0))

    def flush_half(hf):
        for ii in range(16):
            nn = hf * 16 + ii
            fw.dma("sp", out_d[nn * 128:(nn + 1) * 128, :], acc[:, ii * D:(ii + 1) * D], reads=[Bacc[ii]], writes=[Bout[hf]], add=True, is_output=True)

    done_T = set()
    done_D = set()

    def do_T(k):
        if 0 <= k < NI and k not in done_T:
            done_T.add(k)
            stage_T(k)

    def do_D(k):
        if 0 <= k < NI and k not in done_D:
            done_D.add(k)
            stage_D(k)

    for k in range(-1, NI + 1):
        if 0 <= k + 1 < NI:
            if items[k + 1] == (1, 0, 0):
                do_T(k)
                do_D(k - 1)
                do_D(k)
                flush_half(0)
            stage_G(k + 1)
        do_T(k)
        do_D(k - 1)
    flush_half(1)
    fw.finish()
    return nc


_NC_CACHE = {}


def _prep_inputs(inputs, b):
    f = lambda a: np.ascontiguousarray(a, dtype=np.float32)
    m = {
        "x": f(inputs["x"][b]),
        "pos": np.ascontiguousarray(inputs["positions"][b].reshape(NT, 128).T.astype(np.int32)),
        "norm_mix_g": f(inputs["norm_mix_g"][0]),
        "w_in": f(inputs["w_in"][0]),
        "q_norm_g": f(inputs["q_norm_g"][0]), "k_norm_g": f(inputs["k_norm_g"][0]),
        "lambda_q1": f(inputs["lambda_q1"][0]), "lambda_k1": f(inputs["lambda_k1"][0]),
        "lambda_q2": f(inputs["lambda_q2"][0]), "lambda_k2": f(inputs["lambda_k2"][0]),
        "subln_g": f(inputs["subln_g"][0]),
        "w_o_attn": f(inputs["w_o_attn"][0]),
        "lamre_t": f(inputs["ssm_lambda_re"][0].T), "lamim_t": f(inputs["ssm_lambda_im"][0].T),
        "ssm_log_dt": f(inputs["ssm_log_dt"][0]),
        "bre_t": f(inputs["ssm_b_re"][0].transpose(1, 0, 2).reshape(64, 512)),
        "bim_t": f(inputs["ssm_b_im"][0].transpose(1, 0, 2).reshape(64, 512)),
        "cre_t": f(inputs["ssm_c_re"][0].transpose(2, 0, 1).reshape(64, 512)),
        "cim_t": f(inputs["ssm_c_im"][0].transpose(2, 0, 1).reshape(64, 512)),
        "d_t": f(inputs["ssm_d"][0].reshape(32, 16).T),
        "w_glu": f(inputs["w_glu"][0]),
        "w_out": f(inputs["w_out"][0]),
        "norm_ffn_g": f(inputs["norm_ffn_g"][0]),
        "w_router": f(np.concatenate([inputs["w_router_group"][0], inputs["w_router_expert"][0].reshape(D, 32)], axis=1)),
        "b_router": f(np.concatenate([inputs["b_router_group"][0], inputs["b_router_expert"][0].reshape(32)])),
        "w_expert_gate": f(inputs["w_expert_gate"][0].reshape(32, D, 256)),
        "w_expert_up": f(inputs["w_expert_up"][0].reshape(32, D, 256)),
        "w_expert_down": f(inputs["w_expert_down"][0].reshape(32, 256, D)),
    }
    return m


def kernel(**inputs):
    inputs = {k: np.asarray(v) for k, v in inputs.items()}
    nb = inputs["x"].shape[0]
    nc = build_program(debug=False)
    shared = _prep_inputs(inputs, 0)
    in_maps = []
    for b in range(nb):
        m = dict(shared)
        m["x"] = np.ascontiguousarray(inputs["x"][b], dtype=np.float32)
        m["pos"] = np.ascontiguousarray(inputs["positions"][b].reshape(NT, 128).T.astype(np.int32))
        in_maps.append(m)
    res = run_bass_kernel_spmd(nc, in_maps, core_ids=list(range(nb)))
    out = np.stack([np.asarray(r["out"]).reshape(S, D) for r in res.results], axis=0)
    return out.astype(np.float32)
```

```python
import contextlib
import math
import os
import numpy as np
import concourse.bass as bass
import concourse.mybir as mybir
from concourse.bass_utils import run_bass_kernel_spmd

F32 = mybir.dt.float32
BF16 = mybir.dt.bfloat16
I32 = mybir.dt.int32
AF = mybir.ActivationFunctionType
ALU = mybir.AluOpType
AX = mybir.AxisListType

SEM_LIMIT = 30000
S = 4096
D = 1024
NT = 32
EPS = 1e-6
SB_BASE = 17408
LAM_INIT = 0.8 - 0.6 * math.exp(-0.3 * 0)


class Buf:
    __slots__ = ("name", "w", "r", "pr")

    def __init__(self, name=""):
        self.name = name
        self.w = {}
        self.r = {}
        self.pr = {}


class Eng:
    def __init__(self, name):
        self.name = name
        self.ops = []
        self.cnt = 0
        self.semidx = 0
        self.waited = {}
        self.pending = []
        self.last = None

    @property
    def semkey(self):
        return "%s_%d" % (self.name, self.semidx)


class FW:
    def __init__(self, nc, n_dma_sems=32):
        self.nc = nc
        self.stack = contextlib.ExitStack()
        self.engs = {n: Eng(n) for n in ("pe", "act", "dve", "pool", "sp")}
        self.sems = {}
        names = ["dma%d" % i for i in range(n_dma_sems)]
        self.dma_sem_val = {n: 0 for n in names}
        self.dma_rr = {"sp": 0, "pool": 0}
        k = n_dma_sems // 2
        self.dma_pool_of = {"sp": names[:k], "pool": names[k:]}
        self.out_tokens = []

    def sem(self, key):
        if key not in self.sems:
            self.sems[key] = self.stack.enter_context(self.nc.semaphore(key))
        return self.sems[key]

    def psum(self, name, shape, dt):
        return self.stack.enter_context(self.nc.psum_tensor(name, list(shape), dt))

    def _wait(self, eng, tok):
        key, val = tok[0], tok[1]
        assert val is not None, "wait on unsignalled token"
        if eng.waited.get(key, 0) >= val:
            return
        eng.waited[key] = val
        self.sem(key)
        eng.ops.append(("wait", key, val))

    def _deps(self, eng, reads, writes, add):
        toks = []
        for b in reads:
            toks.extend(b.w.values())
        for b in writes:
            if add:
                toks.extend(b.pr.values())
            else:
                toks.extend(b.w.values())
            toks.extend(b.r.values())
        for t in toks:
            if eng.name == "pe" and t[0].startswith("pe_"):
                continue
            self._wait(eng, t)

    def _update(self, key, tok, reads, writes, add):
        for b in reads:
            b.r[key] = tok
        for b in writes:
            if add:
                for k_, v_ in b.r.items():
                    b.pr["r:" + k_] = v_
                b.w[key] = tok
            else:
                npr = {}
                for k_, v_ in b.w.items():
                    npr["w:" + k_] = v_
                for k_, v_ in b.r.items():
                    npr["r:" + k_] = v_
                b.pr = npr
                b.w = {key: tok}
            b.r = {}

    def op(self, engname, fn, reads=(), writes=(), sig=True, add=False):
        eng = self.engs[engname]
        self._deps(eng, reads, writes, add)
        if sig:
            if eng.cnt >= SEM_LIMIT:
                eng.semidx += 1
                eng.cnt = 0
            eng.cnt += 1
            tok = [eng.semkey, eng.cnt]
            self.sem(tok[0])
            for p in eng.pending:
                p[0], p[1] = tok[0], tok[1]
            eng.pending = []
            eng.last = tok
            key = tok[0]
        else:
            tok = ["pe_pending", None]
            eng.pending.append(tok)
            key = "pe_pend"
        eng.ops.append(("op", fn, tok if sig else None))
        self._update(key, tok, reads, writes, add)
        return tok

    def dma(self, qname, out, in_, reads=(), writes=(), add=False, is_output=False, **kw):
        eng = self.engs[qname]
        self._deps(eng, reads, writes, add)
        pool = self.dma_pool_of[qname]
        name = pool[self.dma_rr[qname] % len(pool)]
        self.dma_rr[qname] += 1
        prev = self.dma_sem_val[name]
        if prev > 0:
            self._wait(eng, [name, prev])
        val = prev + 16
        self.dma_sem_val[name] = val
        tok = [name, val]
        self.sem(name)

        def fn(e, out=out, in_=in_, kw=kw):
            return e.dma_start(out=out, in_=in_, **kw)
        eng.ops.append(("op", fn, tok))
        self._update(name, tok, reads, writes, add)
        if is_output:
            self.out_tokens.append(tok)
        return tok

    def barrier(self):
        toks = [e.last for e in self.engs.values() if e.last is not None]
        for e in self.engs.values():
            assert not e.pending
        toks += [[n, v] for n, v in self.dma_sem_val.items() if v > 0]
        for e in self.engs.values():
            for t in toks:
                if e.name == "pe" and t[0].startswith("pe_"):
                    continue
                self._wait(e, t)

    def finish(self):
        sp = self.engs["sp"]
        for t in self.out_tokens:
            self._wait(sp, t)
        nc = self.nc
        sems = self.sems
        engs = self.engs

        def replay(e, eng):
            for o in eng.ops:
                if o[0] == "wait":
                    e.wait_ge(sems[o[1]], o[2])
                else:
                    inst = o[1](e)
                    if o[2] is not None:
                        key = o[2][0]
                        inst.then_inc(sems[key], 16 if key.startswith("dma") else 1)

        with nc.Block() as block:
            @block.tensor
            def _(e):
                replay(e, engs["pe"])

            @block.scalar
            def _(e):
                replay(e, engs["act"])

            @block.vector
            def _(e):
                replay(e, engs["dve"])

            @block.gpsimd
            def _(e):
                replay(e, engs["pool"])

            @block.sync
            def _(e):
                replay(e, engs["sp"])
        self.stack.close()


def build_program(debug=False, stop=None):
    nc = bass.Bass("TRN2", target_bir_lowering=False)
    fw = FW(nc)

    def din(name, shape, dt=F32):
        return nc.dram_tensor(name, list(shape), dt, kind="ExternalInput")

    x_d = din("x", [S, D]).ap()
    pos_d = din("pos", [128, NT], I32).ap()
    gmix_d = din("norm_mix_g", [D])
    w_in_d = din("w_in", [D, 4096]).ap()
    qg_d = din("q_norm_g", [64])
    kg_d = din("k_norm_g", [64])
    lq1_d = din("lambda_q1", [64]); lk1_d = din("lambda_k1", [64])
    lq2_d = din("lambda_q2", [64]); lk2_d = din("lambda_k2", [64])
    subg_d = din("subln_g", [128])
    wo_d = din("w_o_attn", [512, D]).ap()
    lamre_d = din("lamre_t", [64, 32]).ap(); lamim_d = din("lamim_t", [64, 32]).ap()
    logdt_d = din("ssm_log_dt", [32])
    bre_d = din("bre_t", [64, 512]).ap(); bim_d = din("bim_t", [64, 512]).ap()
    cre_d = din("cre_t", [64, 512]).ap(); cim_d = din("cim_t", [64, 512]).ap()
    dsk_d = din("d_t", [16, 32]).ap()
    wglu_d = din("w_glu", [512, 2048]).ap()
    wout_d = din("w_out", [D, D]).ap()
    gffn_d = din("norm_ffn_g", [D])
    wr_d = din("w_router", [D, 36]).ap()
    br_d = din("b_router", [36])
    weg_d = din("w_expert_gate", [32, D, 256]).ap()
    weu_d = din("w_expert_up", [32, D, 256]).ap()
    wed_d = din("w_expert_down", [32, 256, D]).ap()
    out_d = nc.dram_tensor("out", [S, D], F32, kind="ExternalOutput").ap()
    dbg = {}
    if debug:
        lst = [("dbg_x1", [S, D]), ("dbg_comb", [128, NT * 32]), ("dbg_T", [128, 4096]), ("dbg_ks", [128, 16 * 18]), ("dbg_ug", [128, 32 * 512])]
        for nm_ in ("dbg_gy", "dbg_o", "dbg_q", "dbg_k"):
            lst += [(nm_ + str(q_), [128, S]) for q_ in range(4)]
        for nm, shp in lst:
            dbg[nm] = nc.dram_tensor(nm, shp, F32, kind="ExternalOutput").ap()

    def bc_rows(t, n, reps=1, parts=128):
        if reps == 1:
            return bass.AP(t, 0, [[0, parts], [1, n]])
        return bass.AP(t, 0, [[0, parts], [0, reps], [1, n]])

    KB = 1024

    def A(name, shape, dt, off):
        nbytes = int(np.prod(shape[1:])) * (2 if dt == BF16 else 4)
        assert SB_BASE + off + nbytes <= 229376 - 32, (name, off, nbytes)
        return nc.alloc_sbuf_tensor_at(name, list(shape), dt, offset=SB_BASE + off)

    c_off = [0]

    def CA(name, shape, dt):
        n = int(np.prod(shape[1:])) * (2 if dt == BF16 else 4)
        t = A(name, shape, dt, c_off[0])
        c_off[0] += (n + 31) // 32 * 32
        return t

    ident_f = CA("ident_f", [128, 128], F32)
    ident_b = CA("ident_b", [128, 128], BF16)
    maskf = CA("maskf", [128, 128], F32)
    maskneg_b = CA("maskneg_b", [128, 128], BF16)
    gq_t = CA("gq_t", [128, 512], F32)
    gk_t = CA("gk_t", [128, 512], F32)
    sg08_t = CA("sg08_t", [128, 128], F32)
    gmix_t = CA("gmix_t", [128, D], F32)
    gffn_t = CA("gffn_t", [128, D], F32)
    cos_t = CA("cos_t", [128, NT * 8], F32)
    sin_t = CA("sin_t", [128, NT * 8], F32)
    lamv = CA("lamv", [128, 8], F32)
    comb_all = CA("comb_all", [128, NT * 32], F32)
    wr32 = CA("wr32", [128, 8 * 36], F32)
    br_t = CA("br_t", [128, 36], F32)
    ks_are = CA("ks_are", [128, 16 * 9], F32)
    ks_aim = CA("ks_aim", [128, 16 * 9], F32)
    ks_naim = CA("ks_naim", [128, 16 * 9], F32)
    dvec = CA("dvec", [128, 32], F32)
    stat = CA("stat", [128, 64], F32)
    ones_f = CA("ones_f", [128, 128], F32)
    assert c_off[0] <= 24 * KB, c_off[0]
    M0 = 24 * KB
    R_G, R_O, R_Q, R_K, R_V, R_T = M0, M0 + 32 * KB, M0 + 64 * KB, M0 + 96 * KB, M0 + 128 * KB, M0 + 161 * KB
    R_END = 229376 - SB_BASE - 64

    pp = fw.psum("pp", [128, 8 * 512], F32)
    ppb = pp.bitcast(BF16)
    PB = [Buf("psum%d" % i) for i in range(8)]

    def bank(i, a=0, b=512):
        return pp[:, i * 512 + a:i * 512 + b]

    def bankb(i, a=0, b=1024):
        return ppb[:, i * 1024 + a:i * 1024 + b]

    def MM(out, lhsT, rhs, start, stop, r, w, sig=True, add=False, skip=False):
        if skip:
            return fw.op("pe", lambda e: e.matmul(out, lhsT, rhs, start=start, stop=stop, skip_group_check=True), reads=r, writes=w, sig=sig, add=add)
        return fw.op("pe", lambda e: e.matmul(out, lhsT, rhs, start=start, stop=stop), reads=r, writes=w, sig=sig, add=add)

    def TR(out, in_, ident, r, w, sig=True, add=False):
        return fw.op("pe", lambda e: e.transpose(out, in_, ident), reads=r, writes=w, sig=sig, add=add)

    def ACT(out, in_, func, r, w, add=False, **kw):
        return fw.op("act", lambda e: e.activation(out, in_, func, **kw), reads=r, writes=w, add=add)

    def TT(eng, out, in0, in1, op, r, w, add=False):
        return fw.op(eng, lambda e: e.tensor_tensor(out, in0, in1, op), reads=r, writes=w, add=add)

    def TS(eng, out, in0, s1, s2, op0, op1, r, w, add=False):
        if s2 is None:
            return fw.op(eng, lambda e: e.tensor_scalar(out, in0, s1, None, op0), reads=r, writes=w, add=add)
        return fw.op(eng, lambda e: e.tensor_scalar(out, in0, s1, s2, op0, op1), reads=r, writes=w, add=add)

    def STT(out, in0, sc, in1, op0, op1, r, w, add=False):
        return fw.op("dve", lambda e: e.scalar_tensor_tensor(out, in0, sc, in1, op0, op1), reads=r, writes=w, add=add)

    def CP(eng, out, in_, r, w, add=False):
        if eng == "act":
            return ACT(out, in_, AF.Copy, r, w, add=add)
        return fw.op(eng, lambda e: e.tensor_copy(out, in_), reads=r, writes=w, add=add)

    def RECIP(out, in_, r, w, add=False):
        return fw.op("dve", lambda e: e.reciprocal(out, in_), reads=r, writes=w, add=add)

    def RSUM(out, in_, r, w, add=False):
        return fw.op("dve", lambda e: e.reduce_sum(out, in_, axis=AX.X), reads=r, writes=w, add=add)

    def RMAX(out, in_, r, w, add=False):
        return fw.op("dve", lambda e: e.reduce_max(out, in_, axis=AX.X), reads=r, writes=w, add=add)

    def MEMSET(eng, out, val, r, w, add=False):
        return fw.op(eng, lambda e: e.memset(out, val), reads=r, writes=w, add=add)

    def V(t, off, dims):
        pstride = int(np.prod(t.shape[1:]))
        return bass.AP(t, off, [[pstride, t.shape[0]]] + [list(d) for d in dims])

    def VP(t, p0, pn, off, dims):
        pstride = int(np.prod(t.shape[1:]))
        return bass.AP(t, p0 * pstride + off, [[pstride, pn]] + [list(d) for d in dims])

    Bc = Buf("consts")
    MEMSET("pool", ident_f[:], 1.0, [], [Bc])
    fw.op("pool", lambda e: e.affine_select(ident_f[:], ident_f[:], pattern=[[-1, 128]], compare_op=ALU.is_equal, fill=0.0, base=0, channel_multiplier=1), reads=[Bc], writes=[Bc])
    CP("pool", ident_b[:], ident_f[:], [Bc], [Bc])
    MEMSET("pool", maskf[:], 0.0, [Bc], [Bc])
    fw.op("pool", lambda e: e.affine_select(maskf[:], maskf[:], pattern=[[1, 128]], compare_op=ALU.is_ge, fill=-30000.0, base=0, channel_multiplier=-1), reads=[Bc], writes=[Bc])
    CP("pool", maskneg_b[:], maskf[:], [Bc], [Bc])
    fw.dma("sp", gq_t[:], bc_rows(qg_d, 64, 8), writes=[Bc], add=True)
    fw.dma("sp", gk_t[:], bc_rows(kg_d, 64, 8), writes=[Bc], add=True)
    fw.dma("sp", sg08_t[:], bc_rows(subg_d, 128), writes=[Bc], add=True)
    fw.dma("sp", lamv[:, 4:5], bass.AP(subg_d, 0, [[1, 128], [1, 1]]), writes=[Bc], add=True)
    MEMSET("pool", ones_f[:], 1.0, [], [Bc], add=True)
    fw.dma("sp", gmix_t[:], bc_rows(gmix_d, D), writes=[Bc], add=True)
    fw.dma("sp", gffn_t[:], bc_rows(gffn_d, D), writes=[Bc], add=True)
    fw.dma("sp", br_t[:], bc_rows(br_d, 36), writes=[Bc], add=True)
    fw.dma("sp", wr32[:], wr_d.rearrange("(k p) n -> p k n", p=128), writes=[Bc], add=True)
    for hh in range(8):
        fw.dma("sp", dvec[hh * 16:(hh + 1) * 16, :], dsk_d, writes=[Bc], add=True)
    tmp0 = A("c_tmp0", [128, 4 * 64], F32, R_T)
    posi = A("c_posi", [128, NT], I32, R_T + 1 * KB)
    posf = A("c_posf", [128, NT], F32, R_T + 1 * KB + 128)
    ang = A("c_ang", [128, NT * 8], F32, R_T + 2 * KB)
    ang2 = A("c_ang2", [128, NT * 8], F32, R_T + 3 * KB)
    ang3 = A("c_ang3", [128, NT * 8], F32, R_T + 4 * KB)
    Bt = Buf("ctmp")
    for i, dd in enumerate((lq1_d, lk1_d, lq2_d, lk2_d)):
        fw.dma("sp", tmp0[:, i * 64:(i + 1) * 64], bc_rows(dd, 64), writes=[Bt], add=True)
    fw.dma("sp", posi[:], pos_d, writes=[Bt], add=True)
    TS("dve", sg08_t[:], sg08_t[:], 1.0 - LAM_INIT, None, ALU.mult, None, [Bc], [Bc])
    TS("dve", lamv[:, 4:5], lamv[:, 4:5], 1.0 - LAM_INIT, None, ALU.mult, None, [Bc], [Bc])
    TT("dve", tmp0[:, 0:64], tmp0[:, 0:64], tmp0[:, 64:128], ALU.mult, [Bt], [Bt])
    TT("dve", tmp0[:, 128:192], tmp0[:, 128:192], tmp0[:, 192:256], ALU.mult, [Bt], [Bt])
    RSUM(lamv[:, 0:1], tmp0[:, 0:64], [Bt], [Bc])
    RSUM(lamv[:, 1:2], tmp0[:, 128:192], [Bt], [Bc])
    ACT(lamv[:, 0:2], lamv[:, 0:2], AF.Exp, [Bc], [Bc])
    TT("dve", lamv[:, 2:3], lamv[:, 1:2], lamv[:, 0:1], ALU.subtract, [Bc], [Bc])
    TS("dve", lamv[:, 3:4], lamv[:, 2:3], -LAM_INIT, None, ALU.add, None, [Bc], [Bc])
    CP("dve", posf[:], posi[:], [Bt], [Bt])
    for i in range(8):
        inv = (500000.0 ** (-i / 8.0)) / (2.0 * math.pi)
        TS("dve", V(ang, i, [[8, NT]]), posf[:], inv, None, ALU.mult, None, [Bt], [Bt], add=True)
    MAGIC = 12582912.0
    for (dst, shift) in ((sin_t, 0.0), (cos_t, 0.25)):
        TS("dve", ang2[:], ang[:], shift, None, ALU.add, None, [Bt], [Bt])
        TS("dve", ang3[:], ang2[:], MAGIC, MAGIC, ALU.add, ALU.subtract, [Bt], [Bt])
        TT("dve", ang2[:], ang2[:], ang3[:], ALU.subtract, [Bt], [Bt])
        ACT(dst[:], ang2[:], AF.Sin, [Bt], [Bc], scale=6.283185)

    if stop == 'p0a':
        fw.finish()
        return nc
    T_b = A("T_b", [128, 32 * 128], BF16, R_Q)
    VTre_b = A("VTre_b", [128, 32 * 64], BF16, R_Q + 8 * KB)
    VTim_b = A("VTim_b", [128, 32 * 64], BF16, R_Q + 12 * KB)
    Wre_b = A("Wre_b", [128, 32 * 128], BF16, R_Q + 16 * KB)
    Wimn_b = A("Wimn_b", [128, 32 * 128], BF16, R_Q + 24 * KB)
    Bs5w = Buf("s5w")
    Gre = A("Gre_", [128, 4096], F32, M0 + 0)
    Gim = A("Gim_", [128, 4096], F32, M0 + 16 * KB)
    HHre = A("HHre_", [128, 32 * 144], F32, M0 + 32 * KB)
    HHim = A("HHim_", [128, 32 * 144], F32, M0 + 96 * KB)
    VVre = A("VVre_", [128, 4096], F32, M0 + 114 * KB)
    VVim = A("VVim_", [128, 4096], F32, M0 + 130 * KB)
    GS = A("GS_", [128, 4096], F32, M0 + 146 * KB)
    HS = A("HS_", [128, 4096], F32, M0 + 162 * KB)
    so = [M0 + 50 * KB]

    def SA(name, n):
        t = A(name, [128, n], F32, so[0])
        so[0] += n * 4
        return t
    lre = SA("lre", 32); lim = SA("lim", 32); dtt = SA("dtt", 32); ar = SA("ar", 32); ai = SA("ai", 32)
    mm_ = SA("mm_", 32); minv = SA("minv", 32); kk = SA("kk", 32); rr = SA("rr", 32); x8 = SA("x8", 32); x2 = SA("x2", 32)
    pp_ = SA("pp_", 32); cc = SA("cc", 32); ss_ = SA("ss_", 32); t1 = SA("t1", 32); t2 = SA("t2", 32); t3 = SA("t3", 32); t4 = SA("t4", 32)
    LPre = SA("LPre", 32 * 9); LPim = SA("LPim", 32 * 9); LIre = SA("LIre", 32 * 8); LIim = SA("LIim", 32 * 8)
    Are = SA("Are", 32 * 9); Aim = SA("Aim", 32 * 9)
    fre = SA("fre", 32); fim = SA("fim", 32); ire = SA("ire", 32); iim = SA("iim", 32); den = SA("den", 32)
    assert so[0] <= M0 + 64 * KB
    Bin = A("Bin_re", [128, 512], F32, M0 + 178 * KB)
    Bin_im = A("Bin_im", [128, 512], F32, M0 + 180 * KB)
    Cre = A("Cre_in", [128, 512], F32, M0 + 146 * KB)
    Cim = A("Cim_in", [128, 512], F32, M0 + 148 * KB)
    Bbre = A("Bbre", [128, 512], F32, M0 + 150 * KB)
    Bbim = A("Bbim", [128, 512], F32, M0 + 152 * KB)
    W1 = A("W1", [128, 4608], F32, M0 + 114 * KB)
    W2 = A("W2_", [128, 4608], F32, M0 + 154 * KB)
    Bp = Buf("s5prep")

    for half in range(2):
        ps_ = slice(half * 64, half * 64 + 64)
        fw.dma("sp", lre[ps_, :], lamre_d, writes=[Bp], add=True)
        fw.dma("sp", lim[ps_, :], lamim_d, writes=[Bp], add=True)
        fw.dma("sp", Bin[ps_, :], bre_d, writes=[Bp], add=True)
        fw.dma("sp", Bin_im[ps_, :], bim_d, writes=[Bp], add=True)
        fw.dma("sp", Cre[ps_, :], cre_d, writes=[Bp], add=True)
        fw.dma("sp", Cim[ps_, :], cim_d, writes=[Bp], add=True)
    fw.dma("sp", dtt[:], bc_rows(logdt_d, 32), writes=[Bp], add=True)

    def d_tt(out, a, b, op):
        return TT("dve", out, a, b, op, [Bp], [Bp])

    def d_ts(out, a, s1, s2=None, op0=ALU.mult, op1=ALU.add):
        return TS("dve", out, a, s1, s2, op0, op1, [Bp], [Bp])

    def cmul(ore, oim, are_, aim_, bre_, bim_, ta, tb):
        d_tt(ta, are_, bre_, ALU.mult)
        d_tt(tb, aim_, bim_, ALU.mult)
        d_tt(ore, ta, tb, ALU.subtract)
        d_tt(ta, are_, bim_, ALU.mult)
        d_tt(tb, aim_, bre_, ALU.mult)
        d_tt(oim, ta, tb, ALU.add)

    ACT(dtt[:], dtt[:], AF.Exp, [Bp], [Bp])
    d_tt(ar[:], lre[:], dtt[:], ALU.mult)
    d_tt(ai[:], lim[:], dtt[:], ALU.mult)
    MEMSET("dve", mm_[:], 1.0, [Bp], [Bp])
    for k in range(10, 0, -1):
        d_tt(mm_[:], mm_[:], ar[:], ALU.mult)
        d_ts(mm_[:], mm_[:], 1.0 / k, 1.0)
    RECIP(minv[:], mm_[:], [Bp], [Bp])
    d_ts(kk[:], ai[:], 1.0 / (2.0 * math.pi), None)
    d_ts(kk[:], kk[:], MAGIC, MAGIC, ALU.add, ALU.subtract)
    STT(rr[:], kk[:], -6.28125, ai[:], ALU.mult, ALU.add, [Bp], [Bp])
    STT(rr[:], kk[:], -(2.0 * math.pi - 6.28125), rr[:], ALU.mult, ALU.add, [Bp], [Bp])
    d_ts(x8[:], rr[:], 0.125, None)
    d_tt(x2[:], x8[:], x8[:], ALU.mult)
    sc_ = [1.0, -1.0 / 6, 1.0 / 120, -1.0 / 5040, 1.0 / 362880, -1.0 / 39916800]
    cc_ = [1.0, -0.5, 1.0 / 24, -1.0 / 720, 1.0 / 40320, -1.0 / 3628800, 1.0 / 479001600]
    for (dst, co) in ((ss_, sc_), (cc, cc_)):
        MEMSET("dve", dst[:], co[-1], [Bp], [Bp])
        for c in co[-2::-1]:
            d_tt(dst[:], dst[:], x2[:], ALU.mult)
            d_ts(dst[:], dst[:], c, None, ALU.add)
    d_tt(ss_[:], ss_[:], x8[:], ALU.mult)
    for _ in range(3):
        d_tt(t1[:], cc[:], cc[:], ALU.mult)
        d_tt(t2[:], ss_[:], ss_[:], ALU.mult)
        STT(t3[:], ss_[:], 2.0, cc[:], ALU.mult, ALU.mult, [Bp], [Bp])
        d_tt(cc[:], t1[:], t2[:], ALU.subtract)
        CP("dve", ss_[:], t3[:], [Bp], [Bp])
    def LPv(t, j):
        return V(t, j, [[9, 32]])

    def LIv(t, j):
        return V(t, j, [[8, 32]])
    MEMSET("dve", LPv(LPre, 0), 1.0, [Bp], [Bp])
    MEMSET("dve", LPv(LPim, 0), 0.0, [Bp], [Bp])
    d_tt(LPv(LPre, 1), mm_[:], cc[:], ALU.mult)
    d_tt(LPv(LPim, 1), mm_[:], ss_[:], ALU.mult)
    for j in range(2, 9):
        cmul(LPv(LPre, j), LPv(LPim, j), LPv(LPre, j - 1), LPv(LPim, j - 1), LPv(LPre, 1), LPv(LPim, 1), t1[:], t2[:])
    MEMSET("dve", LIv(LIre, 0), 1.0, [Bp], [Bp])
    MEMSET("dve", LIv(LIim, 0), 0.0, [Bp], [Bp])
    d_tt(LIv(LIre, 1), minv[:], cc[:], ALU.mult)
    d_tt(t3[:], minv[:], ss_[:], ALU.mult)
    d_ts(LIv(LIim, 1), t3[:], -1.0, None)
    for j in range(2, 8):
        cmul(LIv(LIre, j), LIv(LIim, j), LIv(LIre, j - 1), LIv(LIim, j - 1), LIv(LIre, 1), LIv(LIim, 1), t1[:], t2[:])
    CP("dve", LPv(Are, 0), LPv(LPre, 8), [Bp], [Bp])
    CP("dve", LPv(Aim, 0), LPv(LPim, 8), [Bp], [Bp])
    for k in range(1, 9):
        d_tt(t1[:], LPv(Are, k - 1), LPv(Are, k - 1), ALU.mult)
        d_tt(t2[:], LPv(Aim, k - 1), LPv(Aim, k - 1), ALU.mult)
        d_tt(LPv(Are, k), t1[:], t2[:], ALU.subtract)
        STT(LPv(Aim, k), LPv(Are, k - 1), 2.0, LPv(Aim, k - 1), ALU.mult, ALU.mult, [Bp], [Bp])
    for gl in range(2):
        for (src, dst) in ((Are, ks_are), (Aim, ks_aim)):
            fw.op("dve", lambda e, src=src, dst=dst, gl=gl: e.tensor_copy(
                VP(dst, gl * 64, 64, 0, [[9, 16], [1, 9]]), VP(src, gl * 64, 64, gl * 9, [[18, 16], [1, 9]])), reads=[Bp], writes=[Bc], add=True)
    TS("dve", ks_naim[:], ks_aim[:], -1.0, None, ALU.mult, None, [Bc], [Bc])
    d_ts(t1[:], LPv(LPre, 1), -1.0, None, ALU.add)
    d_tt(den[:], lre[:], lre[:], ALU.mult)
    d_tt(t2[:], lim[:], lim[:], ALU.mult)
    d_tt(den[:], den[:], t2[:], ALU.add)
    RECIP(den[:], den[:], [Bp], [Bp])
    d_tt(ire[:], lre[:], den[:], ALU.mult)
    d_tt(iim[:], lim[:], den[:], ALU.mult)
    d_ts(iim[:], iim[:], -1.0, None)
    cmul(fre[:], fim[:], t1[:], LPv(LPim, 1), ire[:], iim[:], t3[:], t4[:])
    def bc16(t):
        return V(t, 0, [[1, 32], [0, 16]])

    def v3(t):
        return V(t, 0, [[16, 32], [1, 16]])
    w1a = V(W1, 0, [[16, 32], [1, 16]]); w1b = V(W1, 512, [[16, 32], [1, 16]])
    cmul(v3(Bbre), v3(Bbim), bc16(fre), bc16(fim), v3(Bin), v3(Bin_im), w1a, w1b)
    def g4(t):
        return V(t, 0, [[128, 32], [16, 8], [1, 16]])

    def li4(t):
        return V(t, 0, [[8, 32], [1, 8], [0, 16]])

    def bb4(t):
        return V(t, 0, [[16, 32], [0, 8], [1, 16]])
    cmul(g4(Gre), g4(Gim), li4(LIre), li4(LIim), bb4(Bbre), bb4(Bbim), g4(W1), g4(W2))
    def h4(t):
        return V(t, 0, [[144, 32], [16, 9], [1, 16]])

    def lp4(t):
        return V(t, 0, [[9, 32], [1, 9], [0, 16]])

    def c4(t):
        return V(t, 0, [[16, 32], [0, 9], [1, 16]])
    cmul(h4(HHre), h4(HHim), lp4(LPre), lp4(LPim), c4(Cre), c4(Cim), h4(W1), h4(W2))
    def hs4(t, j0):
        return V(t, j0 * 16, [[144, 32], [1, 128]])
    CP("dve", V(Wre_b, 0, [[128, 32], [1, 128]]), hs4(HHre, 1), [Bp], [Bs5w], add=True)
    TS("dve", V(Wimn_b, 0, [[128, 32], [1, 128]]), hs4(HHim, 1), -1.0, None, ALU.mult, None, [Bp], [Bs5w], add=True)
    def l7(t):
        return V(t, 7, [[9, 32], [0, 128]])

    def g3(t):
        return V(t, 0, [[128, 32], [1, 128]])
    Bvv = Buf("vv")
    cmul(g3(VVre), g3(VVim), l7(LPre), l7(LPim), g3(Gre), g3(Gim), g3(GS), g3(HS))
    CP("dve", VP(GS, 0, 64, 0, [[1, 4096]]), VP(Gre, 0, 64, 0, [[1, 4096]]), [Bp], [Bp])
    fw.op("dve", lambda e: e.tensor_scalar(VP(GS, 64, 64, 0, [[1, 4096]]), VP(Gim, 64, 64, 0, [[1, 4096]]), -1.0, None, ALU.mult), reads=[Bp], writes=[Bp])
    CP("dve", VP(HS, 0, 64, 0, [[128, 32], [1, 128]]), VP(HHre, 0, 64, 0, [[144, 32], [1, 128]]), [Bp], [Bp])
    CP("dve", VP(HS, 64, 64, 0, [[128, 32], [1, 128]]), VP(HHim, 64, 64, 0, [[144, 32], [1, 128]]), [Bp], [Bp])
    mask4 = A("mask4", [128, 512], F32, M0 + 178 * KB)
    MEMSET("pool", mask4[:], 1.0, [Bp], [Bp])
    fw.op("pool", lambda e: e.affine_select(mask4[:], mask4[:], pattern=[[0, 4], [16, 8], [0, 16]], compare_op=ALU.is_ge, fill=0.0, base=15, channel_multiplier=-1), reads=[Bp], writes=[Bp])
    Tm = A("Tm", [128, 512], F32, M0 + 180 * KB)
    for q4 in range(8):
        bk = q4 % 2
        for gi in range(4):
            g = q4 * 4 + gi
            MM(bank(bk, gi * 128, gi * 128 + 128), GS[:, g * 128:(g + 1) * 128], HS[:, g * 128:(g + 1) * 128], True, True, [Bp], [PB[bk]], sig=(gi == 3), add=(gi > 0))
        TT("dve", Tm[:], bank(bk), mask4[:], ALU.mult, [PB[bk], Bp], [Bp])
        for gi in range(4):
            g = q4 * 4 + gi
            STT(T_b[:, g * 128:(g + 1) * 128], ident_f[:], dvec[:, g:g + 1], Tm[:, gi * 128:(gi + 1) * 128], ALU.mult, ALU.add, [Bp, Bc], [Bs5w], add=True)
    for (src, dst) in ((VVre, VTre_b), (VVim, VTim_b)):
        for q4 in range(8):
            bk = 2 + q4 % 2
            for gi in range(4):
                g = q4 * 4 + gi
                TR(bank(bk, gi * 128, gi * 128 + 128), src[:, g * 128:(g + 1) * 128], ident_f[:], [Bp, Bc], [PB[bk]], sig=(gi == 3), add=(gi > 0))
            CP("act", V(dst, q4 * 256, [[64, 4], [1, 64]]), bass.AP(pp, bk * 512, [[4096, 128], [128, 4], [1, 64]]), [PB[bk]], [Bs5w], add=True)
    if debug:
        dtmp = A("dtmp", [128, 4096], F32, M0 + 0)
        fw.barrier()
        CP("dve", dtmp[:], T_b[:], [Bs5w], [Bp])
        fw.dma("sp", dbg["dbg_T"], dtmp[:], reads=[Bp], is_output=True)
        dks = A("dks", [128, 288], F32, M0 + 16 * KB)
        CP("dve", dks[:, 0:144], ks_are[:], [Bc], [Bp])
        CP("dve", dks[:, 144:288], ks_aim[:], [Bc], [Bp])
        fw.dma("sp", dbg["dbg_ks"], dks[:], reads=[Bp], is_output=True)
    fw.barrier()

    if stop == 'p0b':
        fw.finish()
        return nc
    def rms_tile(xt, hbt, jk, st, Bx, Bh, Bst, rows, gt=gmix_t, out_dt_bf=True):
        ACT(jk, xt, AF.Square, [Bx], [Bst, Bh], accum_out=st[:, 0:1])
        ACT(st[:, 1:2], st[:, 0:1], AF.Sqrt, [Bst], [Bst], scale=1.0 / D, bias=EPS)
        RECIP(st[:, 2:3], st[:, 1:2], [Bst], [Bst])
        STT(hbt, xt, st[:, 2:3], gt[:], ALU.mult, ALU.mult, [Bx, Bst, Bc], [Bh])

    gyT = A("gyT", [128, 4 * S], BF16, R_G)
    Wu_b = A("Wu_b", [128, 8 * 512], BF16, R_O)
    hT_sb = A("hT_sb", [128, 8 * 1024], BF16, R_O + 8 * KB)
    U_tok = A("U_tok", [128, 4 * 4096], BF16, R_K)
    Ug = A("Ug", [128, 32 * 512], BF16, R_V)
    xa = [A("xa%d" % i, [128, D], F32, R_T + i * 4 * KB) for i in range(2)]
    hba = [A("hba%d" % i, [128, D], BF16, R_T + 8 * KB + i * 2 * KB) for i in range(2)]
    jka = A("jka", [128, D], BF16, R_T + 12 * KB)
    jkaA = A("jkaA", [128, D], BF16, R_G)
    ksb = [A("ksb%d" % i, [128, 2 * 512], F32, R_T + 12 * KB + i * 4 * KB) for i in range(2)]
    Bxa = [Buf("xa0"), Buf("xa1")]; Bhba = [Buf("hba0"), Buf("hba1")]; Bsta = [Buf("sta0"), Buf("sta1")]
    BWu = Buf("Wu"); BhT = Buf("hTsb"); BUt = [Buf("Ut%d" % i) for i in range(4)]; BUg = [Buf("Ug%d" % i) for i in range(32)]
    Bjk = Buf("jk")
    fw.dma("pool", V(Wu_b, 0, [[512, 8], [1, 512]]), w_in_d[:, 1536:2048].rearrange("(k p) n -> p k n", p=128), writes=[BWu])
    for sb in range(4):
        def pa_rms(i, sb=sb):
            n = sb * 8 + i
            bi = n % 2
            fw.dma("sp", xa[bi][:], x_d[n * 128:(n + 1) * 128, :], writes=[Bxa[bi]])
            rms_tile(xa[bi][:], hba[bi][:], hba[bi][:], V(stat, bi * 4, [[1, 4]]), Bxa[bi], Bhba[bi], Bsta[bi], None)

        def pa_tr(i, sb=sb):
            n = sb * 8 + i
            bi = n % 2
            pb_ = n % 2 * 7
            for k in range(8):
                TR(bankb(pb_, k * 128, k * 128 + 128), hba[bi][:, k * 128:(k + 1) * 128], ident_b[:], [Bhba[bi], Bc], [PB[pb_]], sig=(k == 7), add=(k > 0))
            CP("act" if i % 2 == 0 else "dve", V(hT_sb, i * 128, [[1024, 8], [1, 128]]), bass.AP(ppb, pb_ * 1024, [[8192, 128], [128, 8], [1, 128]]), [PB[pb_]], [BhT], add=(i > 0))
        pa_rms(0)
        for i in range(8):
            if i + 1 < 8:
                pa_rms(i + 1)
            pa_tr(i)
        for tau in range(8):
            bk = 1 + tau % 2
            for k in range(8):
                MM(bank(bk), V(hT_sb, k * 1024 + tau, [[8, 128]]), Wu_b[:, k * 512:(k + 1) * 512], k == 0, k == 7, [BhT, BWu], [PB[bk]], sig=(k == 7), add=(k > 0))
            eng = "act" if tau % 2 == 0 else "dve"
            CP(eng, V(U_tok, sb * 4096 + tau * 16, [[128, 32], [1, 16]]), bass.AP(pp, bk * 512, [[4096, 128], [16, 32], [1, 16]]), [PB[bk]], [BUt[sb]], add=(tau > 0))
        for g8 in range(4):
            bk = 3 + g8 % 2
            for gi in range(8):
                g = g8 * 8 + gi
                TR(bankb(bk, gi * 128, gi * 128 + 128), U_tok[:, sb * 4096 + g * 128: sb * 4096 + (g + 1) * 128], ident_b[:], [BUt[sb], Bc], [PB[bk]], sig=(gi == 7), add=(gi > 0))
            eng = "act" if g8 % 2 == 0 else "dve"
            CP(eng, V(Ug, g8 * 8 * 512 + sb * 128, [[512, 8], [1, 128]]), bass.AP(ppb, bk * 1024, [[8192, 128], [128, 8], [1, 128]]), [PB[bk]], [BUg[g8 * 8 + gi] for gi in range(8)], add=True)
    if debug:
        fw.barrier()
        dtmp2 = A("dtmp2", [128, 16384], F32, R_G)
        CP("dve", dtmp2[:], Ug[:], BUg, [Bp])
        fw.dma("sp", dbg["dbg_ug"], dtmp2[:], reads=[Bp], is_output=True)
        fw.barrier()
    if stop == 'pA1':
        fw.finish()
        return nc
    Ygel = U_tok
    BY = [Buf("Ygel%d" % i) for i in range(32)]
    Xb = [A("Xb%d" % i, [128, 2 * 512], BF16, R_O + 24 * KB + i * 2 * KB) for i in range(2)]
    BXb = [Buf("Xb0"), Buf("Xb1")]
    Bks = [Buf("ks0"), Buf("ks1")]
    gel = [A("gel%d" % i, [128, 512], F32, R_O + 28 * KB + i * 2 * KB) for i in range(2)]
    Bgel = [Buf("gel0"), Buf("gel1")]
    for gp in range(16):
        for (ri, VT) in ((0, VTre_b), (1, VTim_b)):
            bk = 5 + ri
            for gl in range(2):
                g = 2 * gp + gl
                MM(pp[gl * 64:(gl + 1) * 64, bk * 512:(bk + 1) * 512], VT[:, g * 64:(g + 1) * 64], Ug[:, g * 512:(g + 1) * 512], True, True, [Bs5w, BUg[g]], [PB[bk]], sig=(gl == 1), add=(gl > 0))
        cur, nxt = 0, 1
        CP("act", ksb[cur][:, 0:512], bank(5), [PB[5]], [Bks[cur]])
        CP("act", ksb[cur][:, 512:1024], bank(6), [PB[6]], [Bks[cur]], add=True)
        for k in range(9):
            s = 1 << k
            n = 512 - s
            a_k = ks_are[:, gp * 9 + k: gp * 9 + k + 1]
            b_k = ks_aim[:, gp * 9 + k: gp * 9 + k + 1]
            nb_k = ks_naim[:, gp * 9 + k: gp * 9 + k + 1]
            c_, n_ = ksb[cur], ksb[nxt]
            CP("act", V(n_, 0, [[512, 2], [1, s]]), V(c_, 0, [[512, 2], [1, s]]), [Bks[cur]], [Bks[nxt]])
            STT(n_[:, s:512], c_[:, 0:n], a_k, c_[:, s:512], ALU.mult, ALU.add, [Bks[cur], Bc], [Bks[nxt]], add=True)
            STT(n_[:, s:512], c_[:, 512:512 + n], nb_k, n_[:, s:512], ALU.mult, ALU.add, [Bks[cur], Bks[nxt], Bc], [Bks[nxt]], add=True)
            STT(n_[:, 512 + s:1024], c_[:, 0:n], b_k, c_[:, 512 + s:1024], ALU.mult, ALU.add, [Bks[cur], Bc], [Bks[nxt]], add=True)
            STT(n_[:, 512 + s:1024], c_[:, 512:512 + n], a_k, n_[:, 512 + s:1024], ALU.mult, ALU.add, [Bks[cur], Bks[nxt], Bc], [Bks[nxt]], add=True)
            cur, nxt = nxt, cur
        xb = Xb[gp % 2]
        CP("act", xb[:], ksb[cur][:], [Bks[cur]], [BXb[gp % 2]])
        for gl in range(2):
            g = 2 * gp + gl
            bk = 1 + g % 2
            MM(bank(bk), T_b[:, g * 128:(g + 1) * 128], Ug[:, g * 512:(g + 1) * 512], True, False, [Bs5w, BUg[g]], [PB[bk]], sig=False)
            MM(bank(bk, 1, 512), VP(Wre_b, gl * 64, 64, g * 128, [[1, 128]]), VP(xb, gl * 64, 64, 0, [[1, 511]]), False, False, [Bs5w, BXb[gp % 2]], [PB[bk]], sig=False, add=True)
            MM(bank(bk, 1, 512), VP(Wimn_b, gl * 64, 64, g * 128, [[1, 128]]), VP(xb, gl * 64, 64, 512, [[1, 511]]), False, True, [Bs5w, BXb[gp % 2]], [PB[bk]], sig=True, add=True)
            ge = gel[g % 2]
            Bg = Bgel[g % 2]
            ACT(ge[:], bank(bk), AF.Square, [PB[bk]], [Bg])
            TS("dve", ge[:], ge[:], 0.044715, 1.0, ALU.mult, ALU.add, [Bg], [Bg])
            TT("dve", ge[:], ge[:], bank(bk), ALU.mult, [Bg, PB[bk]], [Bg])
            ACT(ge[:], ge[:], AF.Sigmoid, [Bg], [Bg], scale=1.5957691216057308)
            TT("dve", Ygel[:, g * 512:(g + 1) * 512], ge[:], bank(bk), ALU.mult, [Bg, PB[bk]], [BY[g]] + BUt, add=True)
    if stop == 'pA2':
        fw.finish()
        return nc
    fw.barrier()
    Ytok = Ug
    BYt = [Buf("Ytok%d" % i) for i in range(4)]
    Bgy = [Buf("gyT%d" % i) for i in range(8)]
    gy32 = [A("gy32_%d" % i, [128, 4 * 1024], F32, R_Q + i * 16 * KB) for i in range(2)]
    Bg32 = [Buf("gy32_0"), Buf("gy32_1")]
    for sb in range(4):
        for g8 in range(4):
            bk = 3 + g8 % 2
            for gi in range(8):
                g = g8 * 8 + gi
                TR(bankb(bk, gi * 128, gi * 128 + 128), Ygel[:, g * 512 + sb * 128: g * 512 + (sb + 1) * 128], ident_b[:], [BY[g], Bc], [PB[bk]], sig=(gi == 7), add=(gi > 0))
            eng = "act" if g8 % 2 == 0 else "dve"
            CP(eng, V(Ytok, sb * 4096 + g8 * 128, [[16, 8], [512, 8], [1, 16]]), bass.AP(ppb, bk * 1024, [[8192, 128], [128, 8], [16, 8], [1, 16]]), [PB[bk]], BUg + [BYt[sb]], add=True)
        if stop == 'pA3':
            fw.finish()
            return nc
        for j in range(8):
            bk = 5 + j % 2
            for q4 in range(4):
                TR(bankb(bk, q4 * 128, q4 * 128 + 128), Ytok[:, sb * 4096 + j * 512 + q4 * 128: sb * 4096 + j * 512 + (q4 + 1) * 128], ident_b[:], [BYt[sb], Bc], [PB[bk]], sig=(q4 == 3), add=(q4 > 0))
            eng = "act" if j % 2 == 0 else "dve"
            CP(eng, V(gy32[sb % 2], j, [[1024, 4], [8, 128]]), bass.AP(ppb, bk * 1024, [[8192, 128], [128, 4], [1, 128]]), [PB[bk]], [Bg32[sb % 2]], add=(j > 0))
        if stop == 'pA4':
            fw.finish()
            return nc
        CP("pool", V(gyT, sb * 1024, [[S, 4], [1, 1024]]), V(gy32[sb % 2], 0, [[1024, 4], [1, 1024]]), [Bg32[sb % 2]], [Bgy[2 * sb], Bgy[2 * sb + 1]], add=True)
    if stop == 'pA5':
        fw.finish()
        return nc
    if debug:
        fw.barrier()
        for q_ in range(4):
            dst_ = A("stg_dbg_gy_%d" % q_, [128, S], F32, R_K)
            CP("dve", dst_[:], gyT[:, q_ * S:(q_ + 1) * S], Bgy, [Bp])
            fw.dma("sp", dbg["dbg_gy" + str(q_)], dst_[:], reads=[Bp], is_output=True)
    fw.barrier()

    if stop == 'pA':
        fw.finish()
        return nc
    qT = A("qT", [128, 4 * S], BF16, R_Q)
    kT = A("kT", [128, 4 * S], BF16, R_K)
    v_aug = A("v_aug", [128, NT * 4 * 130], BF16, R_V)
    Wqkv = A("Wqkv", [128, 8 * 1536], BF16, R_O)
    hTt = [A("hTt%d" % i, [128, 8 * 128], BF16, R_O + 24 * KB + i * 2 * KB) for i in range(2)]
    BhTt = [Buf("hTt0"), Buf("hTt1")]
    sqs = A("sqs", [128, 1024], BF16, R_O + 28 * KB)
    qkb = A("qkb", [128, 1024], BF16, R_O + 30 * KB)
    qkn = [A("qkn%d" % i, [128, 1024], F32, R_T + 12 * KB + i * 4 * KB) for i in range(2)]
    rtmp = A("rtmp_", [128, 3 * 128], F32, R_T + 20 * KB)
    Bsq = Buf("sqs"); Bqkn = [Buf("qkn0"), Buf("qkn1")]; Bqkb = Buf("qkb"); Brt = Buf("rtmp"); Bqst = Buf("qst")
    BW = Buf("Wqkv"); BqT = [Buf("qT%d" % i) for i in range(8)]; BkT = [Buf("kT%d" % i) for i in range(NT)]; Bv = [Buf("v%d" % i) for i in range(NT)]
    fw.dma("pool", V(Wqkv, 0, [[1536, 8], [1, 1536]]), w_in_d[:, 0:1536].rearrange("(k p) n -> p k n", p=128), writes=[BW])
    MEMSET("pool", V(v_aug, 128, [[130, NT * 4], [1, 2]]), 1.0, [], Bv)

    def qkbank(n):
        return 1 if n % 2 == 0 else 6

    def pb_M1(n):
        bi = n % 2
        fw.dma("sp", xa[bi][:], x_d[n * 128:(n + 1) * 128, :], writes=[Bxa[bi]])
        rms_tile(xa[bi][:], hba[bi][:], hba[bi][:], V(stat, bi * 4, [[1, 4]]), Bxa[bi], Bhba[bi], Bsta[bi], None)
        for k in range(8):
            TR(bankb(0, k * 128, k * 128 + 128), hba[bi][:, k * 128:(k + 1) * 128], ident_b[:], [Bhba[bi], Bc], [PB[0]], sig=(k == 7), add=(k > 0))
        CP("act", hTt[bi][:], bankb(0), [PB[0]], [BhTt[bi]])

    def pb_M2(n):
        bi = n % 2
        b0 = qkbank(n)
        for cb in range(3):
            bk = (b0 + cb) if cb < 2 else 3
            for k in range(8):
                MM(bank(bk), hTt[bi][:, k * 128:(k + 1) * 128], Wqkv[:, k * 1536 + cb * 512: k * 1536 + (cb + 1) * 512], k == 0, k == 7, [BhTt[bi], BW], [PB[bk]], sig=(k == 7), add=(k > 0))
        CP("act", V(v_aug, n * 520, [[130, 4], [1, 128]]), bass.AP(pp, 3 * 512, [[4096, 128], [128, 4], [1, 128]]), [PB[3]], [Bv[n]], add=True)
        psqk = pp[:, b0 * 512:(b0 + 2) * 512]
        Pq = [PB[b0], PB[b0 + 1]]
        ACT(sqs[:], psqk, AF.Square, Pq, [Bsq])
        RSUM(stat[:, 8:24], V(sqs, 0, [[64, 16], [1, 64]]), [Bsq], [Bqst])
        ACT(stat[:, 24:40], stat[:, 8:24], AF.Sqrt, [Bqst], [Bqst], scale=1.0 / 64, bias=EPS)
        RECIP(stat[:, 40:56], stat[:, 24:40], [Bqst], [Bqst])
        q_ = qkn[n % 2]
        Bq = Bqkn[n % 2]
        TT("dve", V(q_, 0, [[64, 16], [1, 64]]), bass.AP(pp, b0 * 512, [[4096, 128], [64, 16], [1, 64]]), V(stat, 40, [[1, 16], [0, 64]]), ALU.mult, Pq + [Bqst], [Bq])
        TT("dve", q_[:, 0:512], q_[:, 0:512], gq_t[:], ALU.mult, [Bq, Bc], [Bq])
        TT("dve", q_[:, 512:1024], q_[:, 512:1024], gk_t[:], ALU.mult, [Bq, Bc], [Bq])

    def pb_M3(n):
        q_ = qkn[n % 2]
        Bq = Bqkn[n % 2]
        r1 = V(q_, 0, [[64, 16], [1, 8]]); r2 = V(q_, 8, [[64, 16], [1, 8]])
        cs = V(cos_t, n * 8, [[0, 16], [1, 8]]); sn = V(sin_t, n * 8, [[0, 16], [1, 8]])
        ta = V(rtmp, 0, [[8, 16], [1, 8]]); tb = V(rtmp, 128, [[8, 16], [1, 8]]); tc = V(rtmp, 256, [[8, 16], [1, 8]])
        TT("pool", ta, r1, cs, ALU.mult, [Bq, Bc], [Brt])
        TT("pool", tb, r2, sn, ALU.mult, [Bq, Bc], [Brt], add=True)
        TT("pool", tc, r1, sn, ALU.mult, [Bq, Bc], [Brt], add=True)
        TT("pool", r1, ta, tb, ALU.subtract, [Brt, Bq], [Bq])
        TT("pool", r2, r2, cs, ALU.mult, [Bq, Bc], [Bq])
        TT("pool", r2, r2, tc, ALU.add, [Brt, Bq], [Bq])
        CP("act", qkb[:], q_[:], [Bq], [Bqkb])
        for cb in range(2):
            bkt = 4 + cb
            for h in range(4):
                TR(bankb(bkt, h * 128, h * 128 + 128), qkb[:, cb * 512 + h * 128: cb * 512 + (h + 1) * 128], ident_b[:], [Bqkb, Bc], [PB[bkt]], sig=(h == 3), add=(h > 0))
            dstT = qT if cb == 0 else kT
            dB = BqT[n // 4] if cb == 0 else BkT[n]
            CP("dve", V(dstT, n * 128, [[S, 4], [1, 128]]), bass.AP(ppb, bkt * 1024, [[8192, 128], [128, 4], [1, 128]]), [PB[bkt]], [dB], add=True)

    for t in range(-2, NT):
        if 0 <= t + 1 < NT:
            pb_M2(t + 1)
        if 0 <= t + 2 < NT:
            pb_M1(t + 2)
        if 0 <= t < NT:
            pb_M3(t)
    if debug:
        fw.barrier()
        for q_ in range(4):
            dst_ = A("stg_dbg_q_%d" % q_, [128, S], F32, R_O)
            CP("dve", dst_[:], qT[:, q_ * S:(q_ + 1) * S], BqT, [Bp])
            fw.dma("sp", dbg["dbg_q" + str(q_)], dst_[:], reads=[Bp], is_output=True)
        for q_ in range(4):
            dst_ = A("stg_dbg_k_%d" % q_, [128, S], F32, R_O)
            CP("dve", dst_[:], kT[:, q_ * S:(q_ + 1) * S], BkT, [Bp])
            fw.dma("sp", dbg["dbg_k" + str(q_)], dst_[:], reads=[Bp], is_output=True)
    fw.barrier()
    fw.barrier()

    if stop == 'pB':
        fw.finish()
        return nc
    oT = A("oT", [128, 4 * S], BF16, R_O)
    pT = [[A("pT%d%d" % (c, i), [128, 512], BF16, R_T + (c * 2 + i) * KB) for i in range(2)] for c in range(2)]
    BpT = [[Buf("pT%d%d" % (c, i)) for i in range(2)] for c in range(2)]
    of_ = [A("of%d" % i, [128, 128], F32, R_T + 4 * KB + i * 512) for i in range(2)]
    ob_ = [A("ob%d" % i, [128, 128], BF16, R_T + 5 * KB + i * 256) for i in range(2)]
    ajk = A("ajk", [128, 128], BF16, R_T + 6 * KB)
    Bof = [Buf("of0"), Buf("of1")]; Bob = [Buf("ob0"), Buf("ob1")]; Bast = [Buf("ast0"), Buf("ast1")]
    BoT = [Buf("oT%d" % i) for i in range(8)]
    Bajk = Buf("ajk")
    def accv(qs, c, a, b):
        idx = qs * 2 + c
        bk = 4 + idx // 3
        off = bk * 512 + (idx % 3) * 130
        return pp[:, off + a: off + b], PB[bk]
    pT4 = [A("pT4_%d" % i, [128, 512], BF16, R_T + i * KB) for i in range(4)]
    BpT4 = [Buf("pT4_%d" % i) for i in range(4)]
    dacc = [[A("dacc%d%d" % (r, c), [128, 512], F32, R_T + 4 * KB + (r * 2 + c) * 2 * KB) for c in range(2)] for r in range(2)]
    Bdacc = [[Buf("dacc%d%d" % (r, c)) for c in range(2)] for r in range(2)]
    rd = [A("rd%d" % i, [128, 512], F32, R_T + 12 * KB + i * 2 * KB) for i in range(2)]
    Brd = [Buf("rd0"), Buf("rd1")]
    att_items = [(h, qblk, kt, c) for h in range(4) for qblk in range(8) for kt in range(4 * qblk + 4) for c in range(2)]
    NA = len(att_items)
    qz = [[A("qz%d%d" % (r, c), [128, 512], BF16, R_T + 16 * KB + (r * 2 + c) * KB) for c in range(2)] for r in range(2)]
    Bqz = [[Buf("qz%d%d" % (r, c)) for c in range(2)] for r in range(2)]
    for r in range(2):
        for c in range(2):
            MEMSET("pool", qz[r][c][:], 0.0, [], [Bqz[r][c]])

    def stage_S(k):
        h, qblk, kt, c = att_items[k]
        q0 = max(0, kt - 4 * qblk)
        col0 = q0 * 128
        diag = kt >= 4 * qblk
        sb_ = k % 3
        r = (h * 8 + qblk) % 2
        if kt == 0:
            CP("dve", VP(qz[r][c], c * 64, 64, 0, [[1, 512]]), VP(qT, c * 64, 64, h * S + qblk * 512, [[1, 512]]), [BqT[qblk]], [Bqz[r][c]])
        MM(bank(sb_, col0, 512), kT[:, h * S + kt * 128: h * S + (kt + 1) * 128], qz[r][c][:, col0:512],
           True, not diag, [BkT[kt], Bqz[r][c]], [PB[sb_]], sig=(not diag))
        if diag:
            MM(bank(sb_, col0, col0 + 128), ident_b[:], maskneg_b[:], False, True, [Bc], [PB[sb_]], sig=True, add=True)
        ACT(pT4[k % 4][:, col0:512], bank(sb_, col0, 512), AF.Exp, [PB[sb_]], [BpT4[k % 4]], scale=0.125)

    def finalize_a1(h, qblk, rnd):
        r = rnd % 2
        MM(bank(3), ones_f[:], dacc[r][0][:], True, True, [Bc, Bdacc[r][0]], [PB[3]])
        RECIP(rd[0][:], bank(3), [PB[3]], [Brd[0]])

    def finalize_a2(h, qblk, rnd):
        r = rnd % 2
        ab = 4 + 2 * r
        MM(bank(3), ones_f[:], dacc[r][1][:], True, True, [Bc, Bdacc[r][1]], [PB[3]])
        RECIP(rd[1][:], bank(3), [PB[3]], [Brd[1]])
        TS("dve", rd[1][:], rd[1][:], lamv[:, 3:4], None, ALU.mult, None, [Brd[1], Bc], [Brd[1]])
        TT("dve", rd[0][:], bank(ab), rd[0][:], ALU.mult, [PB[ab], Brd[0]], [Brd[0]])
        TT("dve", rd[1][:], bank(ab + 1), rd[1][:], ALU.mult, [PB[ab + 1], Brd[1]], [Brd[1]])
        TT("dve", rd[1][:], rd[1][:], rd[0][:], ALU.add, [Brd[0], Brd[1]], [Brd[1]])
        ACT(rd[0][:], rd[1][:], AF.Square, [Brd[1]], [Brd[0]])

    def finalize_b(h, qblk):
        t0 = qblk * 512
        MM(bank(3), ones_f[:], rd[0][:], True, True, [Bc, Brd[0]], [PB[3]])
        ACT(rd[0][:], bank(3), AF.Sqrt, [PB[3]], [Brd[0]], scale=1.0 / 128, bias=EPS)
        RECIP(rd[0][:], rd[0][:], [Brd[0]], [Brd[0]])
        STT(oT[:, h * S + t0: h * S + t0 + 512], rd[1][:], lamv[:, 4:5], rd[0][:], ALU.mult, ALU.mult, [Brd[0], Brd[1], Bc], [BoT[qblk]], add=True)

    deferred = {}

    def stage_PV(k):
        h, qblk, kt, c = att_items[k]
        rnd = h * 8 + qblk
        r = rnd % 2
        q0 = max(0, kt - 4 * qblk)
        col0 = q0 * 128
        p_ = pT4[k % 4]
        ab = 4 + 2 * r + c
        last = (kt == 4 * qblk + 3)
        MM(bank(ab, col0, 512), V(v_aug, kt * 520 + h * 130, [[1, 128]]), p_[:, col0:512], kt == 0, last, [BpT4[k % 4], Bv[kt]], [PB[ab]], sig=True, add=(kt > 0))
        eng = "dve" if c == 0 else "pool"
        if kt == 0:
            CP(eng, dacc[r][c][:], p_[:], [BpT4[k % 4]], [Bdacc[r][c]])
        else:
            TT(eng, dacc[r][c][:, col0:512], dacc[r][c][:, col0:512], p_[:, col0:512], ALU.add, [BpT4[k % 4], Bdacc[r][c]], [Bdacc[r][c]])
        for fn_, args_ in deferred.pop(k, []):
            fn_(*args_)
        if last and c == 1:
            deferred.setdefault(k + 3, []).append((finalize_a1, (h, qblk, rnd)))
            deferred.setdefault(k + 6, []).append((finalize_a2, (h, qblk, rnd)))
            deferred.setdefault(k + 10, []).append((finalize_b, (h, qblk)))

    LOOK = 2
    for k in range(LOOK):
        stage_S(k)
    for i in range(48):
        MM(bank(7), ident_b[:], qT[:, 0:512], True, True, [Bc] + BqT[0:1], [PB[7]], sig=(i == 47), add=(i > 0))
    for k in range(0, NA):
        if k + LOOK < NA:
            stage_S(k + LOOK)
        stage_PV(k)
    for kk_ in sorted(deferred):
        for fn_, args_ in deferred[kk_]:
            fn_(*args_)
    deferred.clear()
    assert not deferred
    if debug:
        fw.barrier()
        for q_ in range(4):
            dst_ = A("stg_dbg_o_%d" % q_, [128, S], F32, R_Q)
            CP("dve", dst_[:], oT[:, q_ * S:(q_ + 1) * S], BoT, [Bp])
            fw.dma("sp", dbg["dbg_o" + str(q_)], dst_[:], reads=[Bp], is_output=True)
    fw.barrier()

    if stop == 'att':
        fw.finish()
        return nc
    Wg_b = A("Wg_b", [128, 8 * 2048], BF16, R_Q)
    wglu_b = A("wglu_b", [128, 4 * 2048], BF16, R_K)
    wout_b = A("wout_b", [128, 8 * 1024], BF16, R_K + 16 * KB)
    wo_b = A("wo_b", [128, 4 * 1024], BF16, R_V)
    xc = [A("xc%d" % i, [128, D], F32, R_V + 8 * KB + i * 4 * KB) for i in range(4)]
    hT_blk = A("hT_blk", [128, 8 * 512], BF16, R_V + 24 * KB)
    mT = A("mT", [128, 8 * 512], BF16, R_T)
    sgA = A("sgA", [128, 512], F32, R_T + 8 * KB); sgB = A("sgB", [128, 512], F32, R_T + 10 * KB); sgE = A("sgE", [128, 512], F32, R_T + 12 * KB)
    tt1 = A("tt1", [128, 512], F32, R_T + 14 * KB); tt2 = A("tt2", [128, 512], F32, R_T + 16 * KB)
    hbc = A("hbc", [128, D], BF16, R_T + 18 * KB)
    cjk = hbc
    tT32 = A("tT32", [128, 8 * 128], F32, R_T + 8 * KB)
    BWc = Buf("Wc"); Bxc = [Buf("xc%d" % i) for i in range(4)]; BhTb = Buf("hTblk"); BmT = Buf("mT")
    BsA = Buf("sgA"); BsB = Buf("sgB"); BsE = Buf("sgE"); Bt1 = Buf("tt1"); Bt2 = Buf("tt2"); Bhbc = Buf("hbc"); Bcst = Buf("cst"); BtT32 = BsA
    Bcomb = Buf("comb")
    Bout = [Buf("out_h0"), Buf("out_h1")]
    fw.dma("pool", V(Wg_b, 0, [[2048, 8], [1, 2048]]), w_in_d[:, 2048:4096].rearrange("(k p) n -> p k n", p=128), writes=[BWc], add=True)
    fw.dma("pool", V(wglu_b, 0, [[2048, 4], [1, 2048]]), wglu_d.rearrange("(k p) n -> p k n", p=128), writes=[BWc], add=True)
    fw.dma("pool", V(wout_b, 0, [[1024, 8], [1, 1024]]), wout_d.rearrange("(k p) n -> p k n", p=128), writes=[BWc], add=True)
    fw.dma("pool", V(wo_b, 0, [[1024, 4], [1, 1024]]), wo_d.rearrange("(k p) n -> p k n", p=128), writes=[BWc], add=True)

    def tT_ap(k, tok0, n):
        base = gyT if k < 4 else oT
        return base[:, (k % 4) * S + tok0: (k % 4) * S + tok0 + n]

    for blk in range(8):
        t0 = blk * 512
        for i in range(4):
            n = blk * 4 + i
            fw.dma("sp", xc[i][:], x_d[n * 128:(n + 1) * 128, :], writes=[Bxc[i]])
            rms_tile(xc[i][:], hbc[:], cjk[:], V(stat, 48, [[1, 4]]), Bxc[i], Bhbc, Bcst, None)
            for k in range(8):
                TR(bankb(0, k * 128, k * 128 + 128), hbc[:, k * 128:(k + 1) * 128], ident_b[:], [Bhbc, Bc], [PB[0]], sig=(k == 7), add=(k > 0))
            CP("act", V(hT_blk, i * 128, [[512, 8], [1, 128]]), bass.AP(ppb, 0, [[8192, 128], [128, 8], [1, 128]]), [PB[0]], [BhTb], add=(i > 0))
        for nch in range(8):
            for (bk, col) in ((1, nch), (2, 8 + nch)):
                for k in range(8):
                    MM(bank(bk), Wg_b[:, k * 2048 + col * 128: k * 2048 + (col + 1) * 128], hT_blk[:, k * 512:(k + 1) * 512], k == 0, k == 7, [BWc, BhTb], [PB[bk]], sig=(k == 7), add=(k > 0))
            for f in range(4):
                MM(bank(3), wo_b[:, f * 1024 + nch * 128: f * 1024 + (nch + 1) * 128], oT[:, f * S + t0: f * S + t0 + 512], f == 0, f == 3, [BWc, BoT[blk]], [PB[3]], sig=(f == 3), add=(f > 0))
            for (bk, col) in ((4, nch), (5, 8 + nch)):
                for f in range(4):
                    MM(bank(bk), wglu_b[:, f * 2048 + col * 128: f * 2048 + (col + 1) * 128], gyT[:, f * S + t0: f * S + t0 + 512], f == 0, f == 3, [BWc, Bgy[blk]], [PB[bk]], sig=(f == 3), add=(f > 0))
            ACT(sgA[:], bank(1), AF.Sigmoid, [PB[1]], [BsA])
            ACT(sgB[:], bank(2), AF.Sigmoid, [PB[2]], [BsB])
            ACT(sgE[:], bank(5), AF.Sigmoid, [PB[5]], [BsE])
            TT("dve", tt1[:], bank(3), sgA[:], ALU.mult, [PB[3], BsA], [Bt1])
            TT("dve", tt2[:], bank(4), sgE[:], ALU.mult, [PB[4], BsE], [Bt2])
            TT("pool", tt2[:], tt2[:], sgB[:], ALU.mult, [Bt2, BsB], [Bt2])
            TT("pool", mT[:, nch * 512:(nch + 1) * 512], tt1[:], tt2[:], ALU.add, [Bt1, Bt2], [BmT], add=(nch > 0))
        def back_A(i, blk=blk, t0=t0):
            n = blk * 4 + i
            for half in range(2):
                bk = 6 + half
                for f in range(8):
                    MM(bank(bk), mT[:, f * 512 + i * 128: f * 512 + (i + 1) * 128], wout_b[:, f * 1024 + half * 512: f * 1024 + (half + 1) * 512], f == 0, f == 7, [BmT, BWc], [PB[bk]], sig=(f == 7), add=(f > 0))
                TT("dve", xc[i][:, half * 512:(half + 1) * 512], bank(bk), xc[i][:, half * 512:(half + 1) * 512], ALU.add, [PB[bk], Bxc[i]], [Bxc[i]], add=(half > 0))
            fw.dma("sp", out_d[n * 128:(n + 1) * 128, :], xc[i][:], reads=[Bxc[i]], writes=[Bout[n // 16]], add=True)
            if debug:
                fw.dma("sp", dbg["dbg_x1"][n * 128:(n + 1) * 128, :], xc[i][:], reads=[Bxc[i]], is_output=True)
            ACT(cjk[:], xc[i][:], AF.Square, [Bxc[i]], [Bcst, Bhbc], accum_out=stat[:, 52:53])
            ACT(stat[:, 53:54], stat[:, 52:53], AF.Sqrt, [Bcst], [Bcst], scale=1.0 / D, bias=EPS)
            RECIP(stat[:, 54:55], stat[:, 53:54], [Bcst], [Bcst])
            STT(xc[i][:], xc[i][:], stat[:, 54:55], gffn_t[:], ALU.mult, ALU.mult, [Bxc[i], Bcst, Bc], [Bxc[i]])
        def back_B(i, blk=blk, t0=t0):
            n = blk * 4 + i
            for k in range(8):
                bk = k // 4
                TR(bank(bk, (k % 4) * 128, (k % 4) * 128 + 128), xc[i][:, k * 128:(k + 1) * 128], ident_f[:], [Bxc[i], Bc], [PB[bk]], sig=(k % 4 == 3), add=(k % 4 > 0))
            CP("act", tT32[:, 0:512], bank(0), [PB[0], BsA, BsB], [BsA, BsB])
            CP("act", tT32[:, 512:1024], bank(1), [PB[1]], [BsA, BsB], add=True)
            for k in range(8):
                CP("pool", tT_ap(k, n * 128, 128), tT32[:, k * 128:(k + 1) * 128], [BsA], [Bgy[blk], BoT[blk]], add=True)
            for k in range(8):
                MM(bank(2, 0, 36), tT32[:, k * 128:(k + 1) * 128], wr32[:, k * 36:(k + 1) * 36], k == 0, k == 7, [BsA, Bc], [PB[2]], sig=(k == 7), add=(k > 0))
            rt = tt1
            Br = Bt1
            lgt = rt[:, 0:36]
            TT("dve", lgt, bank(2, 0, 36), br_t[:], ALU.add, [PB[2], Bc], [Br])
            gmax = rt[:, 40:41]
            RMAX(gmax, rt[:, 0:4], [Br], [Br], add=True)
            oh = rt[:, 44:48]
            TS("dve", oh, rt[:, 0:4], gmax, None, ALU.is_equal, None, [Br], [Br], add=True)
            TS("dve", rt[:, 48:52], rt[:, 0:4], gmax, None, ALU.subtract, None, [Br], [Br], add=True)
            ACT(rt[:, 48:52], rt[:, 48:52], AF.Exp, [Br], [Br])
            RSUM(rt[:, 52:53], rt[:, 48:52], [Br], [Br], add=True)
            RECIP(rt[:, 53:54], rt[:, 52:53], [Br], [Br])
            TS("dve", rt[:, 56:64], rt[:, 4:12], rt[:, 44:45], None, ALU.mult, None, [Br], [Br], add=True)
            for g in range(1, 4):
                STT(rt[:, 56:64], rt[:, 4 + g * 8: 12 + g * 8], rt[:, 44 + g: 45 + g], rt[:, 56:64], ALU.mult, ALU.add, [Br], [Br])
            m1 = rt[:, 64:65]
            RMAX(m1, rt[:, 56:64], [Br], [Br], add=True)
            mk1 = rt[:, 72:80]
            TS("dve", mk1, rt[:, 56:64], m1, None, ALU.is_equal, None, [Br], [Br], add=True)
            es2 = rt[:, 80:88]
            STT(es2, mk1, -1e30, rt[:, 56:64], ALU.mult, ALU.add, [Br], [Br], add=True)
            m2 = rt[:, 65:66]
            RMAX(m2, es2, [Br], [Br], add=True)
            mk2 = rt[:, 88:96]
            TS("dve", mk2, es2, m2, None, ALU.is_equal, None, [Br], [Br], add=True)
            TT("dve", rt[:, 66:67], m2, m1, ALU.subtract, [Br], [Br], add=True)
            ACT(rt[:, 66:67], rt[:, 66:67], AF.Exp, [Br], [Br])
            TS("dve", rt[:, 67:68], rt[:, 66:67], 1.0, None, ALU.add, None, [Br], [Br], add=True)
            RECIP(rt[:, 68:69], rt[:, 67:68], [Br], [Br])
            TS("dve", rt[:, 69:70], rt[:, 68:69], -1.0, 1.0, ALU.mult, ALU.add, [Br], [Br], add=True)
            TT("dve", rt[:, 68:69], rt[:, 68:69], rt[:, 53:54], ALU.mult, [Br], [Br])
            TT("dve", rt[:, 69:70], rt[:, 69:70], rt[:, 53:54], ALU.mult, [Br], [Br])
            ew = rt[:, 96:104]
            TS("dve", ew, mk1, rt[:, 68:69], None, ALU.mult, None, [Br], [Br], add=True)
            STT(ew, mk2, rt[:, 69:70], ew, ALU.mult, ALU.add, [Br], [Br])
            for g in range(4):
                TS("dve", comb_all[:, n * 32 + g * 8: n * 32 + (g + 1) * 8], ew, rt[:, 44 + g:45 + g], None, ALU.mult, None, [Br], [Bcomb], add=True)
        back_A(0)
        for i in range(4):
            if i + 1 < 4:
                back_A(i + 1)
            back_B(i)
    if debug:
        fw.dma("sp", dbg["dbg_comb"], comb_all[:], reads=[Bcomb], is_output=True)
    fw.barrier()

    if stop == 'pC':
        fw.finish()
        return nc
    acc = A("acc", [128, 16 * D], F32, R_Q)
    NWB = 3
    wgu = [A("wgu%d" % i, [128, 8 * 512], BF16, R_V + i * 12 * KB) for i in range(NWB)]
    wd_ = [A("wd%d" % i, [128, 2 * 1024], BF16, R_V + i * 12 * KB + 8 * KB) for i in range(NWB)]
    Bw = [Buf("w%d" % i) for i in range(NWB)]
    sgm = [A("sgm%d" % i, [128, 256], F32, R_V + 36 * KB + i * KB) for i in range(2)]
    hid = [A("hid%d" % i, [128, 256], BF16, R_V + 38 * KB + i * 512) for i in range(2)]
    hidT = [A("hidT%d" % i, [128, 256], BF16, R_V + 39 * KB + i * 512) for i in range(2)]
    Bsg = [Buf("sgm0"), Buf("sgm1")]; Bhid = [Buf("hid0"), Buf("hid1")]; BhidT = [Buf("hidT0"), Buf("hidT1")]
    Bacc = [Buf("acc%d" % i) for i in range(16)]
    BtT = Bgy + BoT
    items = [(hf, e, i) for hf in range(2) for e in range(32) for i in range(16)]
    NI = len(items)

    def wbuf(hf, e):
        return (hf * 32 + e) % NWB

    def stage_G(k):
        hf, e, i = items[k]
        n = hf * 16 + i
        bi = k % 2
        wi = wbuf(hf, e)
        if e == 0 and i == 0:
            for ii in range(16):
                nn = hf * 16 + ii
                fw.dma("sp", acc[:, ii * D:(ii + 1) * D], out_d[nn * 128:(nn + 1) * 128, :], reads=[Bout[hf]], writes=[Bacc[ii]])
        if i == 0:
            fw.dma("pool", V(wgu[wi], 0, [[512, 8], [1, 256]]), weg_d[e].rearrange("(k p) f -> p k f", p=128), writes=[Bw[wi]])
            fw.dma("pool", V(wgu[wi], 256, [[512, 8], [1, 256]]), weu_d[e].rearrange("(k p) f -> p k f", p=128), writes=[Bw[wi]], add=True)
            fw.dma("pool", V(wd_[wi], 0, [[1024, 2], [1, 1024]]), wed_d[e].rearrange("(k p) n -> p k n", p=128), writes=[Bw[wi]], add=True)
        for kk_ in range(8):
            MM(bank(bi), tT_ap(kk_, n * 128, 128), wgu[wi][:, kk_ * 512:(kk_ + 1) * 512], kk_ == 0, kk_ == 7, [BtT[n // 4], BtT[8 + n // 4], Bw[wi]], [PB[bi]], sig=(kk_ == 7), add=(kk_ > 0))
        ACT(sgm[bi][:], bank(bi, 0, 256), AF.Silu, [PB[bi]], [Bsg[bi]])
        STT(hid[bi][:], bank(bi, 256, 512), comb_all[:, n * 32 + e: n * 32 + e + 1], sgm[bi][:], ALU.mult, ALU.mult, [PB[bi], Bcomb, Bsg[bi]], [Bhid[bi]])

    def stage_T(k):
        bi = k % 2
        for f in range(2):
            TR(bankb(2 + bi, f * 128, f * 128 + 128), hid[bi][:, f * 128:(f + 1) * 128], ident_b[:], [Bhid[bi], Bc], [PB[2 + bi]], sig=(f == 1), add=(f > 0))
        CP("act", hidT[bi][:], bankb(2 + bi, 0, 256), [PB[2 + bi]], [BhidT[bi]])

    def stage_D(k):
        hf, e, i = items[k]
        bi = k % 2
        wi = wbuf(hf, e)
        for half in range(2):
            bkd = 4 + bi * 2 + half
            for f in range(2):
                MM(bank(bkd), hidT[bi][:, f * 128:(f + 1) * 128], wd_[wi][:, f * 1024 + half * 512: f * 1024 + (half + 1) * 512], f == 0, f == 1, [BhidT[bi], Bw[wi]], [PB[bkd]], sig=(f == 1), add=(f > 0))
            TT("dve", acc[:, i * D + half * 512: i * D + (half + 1) * 512], bank(bkd), acc[:, i * D + half * 512: i * D + (half + 1) * 512], ALU.add, [PB[bkd], Bacc[i]], [Bacc[i]], add=(half > 0))

    def flush_half(hf):
        for ii in range(16):
            nn = hf * 16 + ii
            fw.dma("sp", out_d[nn * 128:(nn + 1) * 128, :], acc[:, ii * D:(ii + 1) * D], reads=[Bacc[ii]], writes=[Bout[hf]], add=True, is_output=True)

    done_T = set()
    done_D = set()

    def do_T(k):
        if 0 <= k < NI and k not in done_T:
            done_T.add(k)
            stage_T(k)

    def do_D(k):
        if 0 <= k < NI and k not in done_D:
            done_D.add(k)
            stage_D(k)

    for k in range(-1, NI + 1):
        if 0 <= k + 1 < NI:
            if items[k + 1] == (1, 0, 0):
                do_T(k)
                do_D(k - 1)
                do_D(k)
                flush_half(0)
            stage_G(k + 1)
        do_T(k)
        do_D(k - 1)
    flush_half(1)
    fw.finish()
    return nc


_NC_CACHE = {}


def _prep_inputs(inputs, b):
    f = lambda a: np.ascontiguousarray(a, dtype=np.float32)
    m = {
        "x": f(inputs["x"][b]),
        "pos": np.ascontiguousarray(inputs["positions"][b].reshape(NT, 128).T.astype(np.int32)),
        "norm_mix_g": f(inputs["norm_mix_g"][0]),
        "w_in": f(inputs["w_in"][0]),
        "q_norm_g": f(inputs["q_norm_g"][0]), "k_norm_g": f(inputs["k_norm_g"][0]),
        "lambda_q1": f(inputs["lambda_q1"][0]), "lambda_k1": f(inputs["lambda_k1"][0]),
        "lambda_q2": f(inputs["lambda_q2"][0]), "lambda_k2": f(inputs["lambda_k2"][0]),
        "subln_g": f(inputs["subln_g"][0]),
        "w_o_attn": f(inputs["w_o_attn"][0]),
        "lamre_t": f(inputs["ssm_lambda_re"][0].T), "lamim_t": f(inputs["ssm_lambda_im"][0].T),
        "ssm_log_dt": f(inputs["ssm_log_dt"][0]),
        "bre_t": f(inputs["ssm_b_re"][0].transpose(1, 0, 2).reshape(64, 512)),
        "bim_t": f(inputs["ssm_b_im"][0].transpose(1, 0, 2).reshape(64, 512)),
        "cre_t": f(inputs["ssm_c_re"][0].transpose(2, 0, 1).reshape(64, 512)),
        "cim_t": f(inputs["ssm_c_im"][0].transpose(2, 0, 1).reshape(64, 512)),
        "d_t": f(inputs["ssm_d"][0].reshape(32, 16).T),
        "w_glu": f(inputs["w_glu"][0]),
        "w_out": f(inputs["w_out"][0]),
        "norm_ffn_g": f(inputs["norm_ffn_g"][0]),
        "w_router": f(np.concatenate([inputs["w_router_group"][0], inputs["w_router_expert"][0].reshape(D, 32)], axis=1)),
        "b_router": f(np.concatenate([inputs["b_router_group"][0], inputs["b_router_expert"][0].reshape(32)])),
        "w_expert_gate": f(inputs["w_expert_gate"][0].reshape(32, D, 256)),
        "w_expert_up": f(inputs["w_expert_up"][0].reshape(32, D, 256)),
        "w_expert_down": f(inputs["w_expert_down"][0].reshape(32, 256, D)),
    }
    return m


def kernel(**inputs):
    inputs = {k: np.asarray(v) for k, v in inputs.items()}
    nb = inputs["x"].shape[0]
    nc = build_program(debug=False)
    shared = _prep_inputs(inputs, 0)
    in_maps = []
    for b in range(nb):
        m = dict(shared)
        m["x"] = np.ascontiguousarray(inputs["x"][b], dtype=np.float32)
        m["pos"] = np.ascontiguousarray(inputs["positions"][b].reshape(NT, 128).T.astype(np.int32))
        in_maps.append(m)
    res = run_bass_kernel_spmd(nc, in_maps, core_ids=list(range(nb)))
    out = np.stack([np.asarray(r["out"]).reshape(S, D) for r in res.results], axis=0)
    return out.astype(np.float32)
```
